# Optimizing a Trainium2 kernel written in Bass

```python
import math
import jax
import jax.numpy as jnp
from jax import lax
import numpy as np

D_MODEL = 2048
BATCH = 8
SEQ = 2048
DEPTH = 2

MEM_LEN = 256
RMS_EPS = 1e-6
ML_HEADS = 4
ML_DQK = 128
ML_DV = 256
ML_CHUNK = 64
SSM_HEADS = 16
SSM_HEADDIM = 64
SSM_DINNER = SSM_HEADS * SSM_HEADDIM
SSM_GROUPS = 2
SSM_STATE = 128
SSM_CONV = 4
SSM_CHUNK = 128
SSM_CONV_CH = SSM_DINNER + 2 * SSM_GROUPS * SSM_STATE
MLA_HEADS = 8
MLA_Q_LORA = 512
MLA_KV_LORA = 256
MLA_NOPE = 128
MLA_ROPE = 64
MLA_V = 128
ROPE_THETA = 10000.0
ATTN_BLOCK = 128
SWA_HEADS = 16
SWA_KV_HEADS = 4
SWA_HEAD_DIM = 64
SWA_WINDOW = 128
N_BRANCH = 4
BRANCH_W = 1024
XA_HEADS = 4
XA_HEAD_DIM = 128
FFN_DIM = 7168
N_EXPERTS = 8
TOP_K = 2
MOE_BLOCK = 512
N_DENSE = (DEPTH + 1) // 2
N_MOE = DEPTH // 2

IN_SPLITS = (
    ML_HEADS * ML_DQK,
    ML_HEADS * ML_DQK,
    ML_HEADS * ML_DV,
    ML_HEADS * ML_DV,
    ML_HEADS,
    ML_HEADS,
    SSM_DINNER,
    SSM_CONV_CH,
    SSM_HEADS,
    MLA_Q_LORA,
    MLA_KV_LORA,
    MLA_ROPE,
    SWA_HEADS * SWA_HEAD_DIM,
    SWA_KV_HEADS * SWA_HEAD_DIM,
    SWA_KV_HEADS * SWA_HEAD_DIM,
    N_BRANCH * D_MODEL,
)
IN_TOTAL = sum(IN_SPLITS)
IN_OFFSETS = tuple(int(v) for v in np.cumsum(IN_SPLITS)[:-1])

kernel_name = 'hybrid_gated_mlstm_ssd_mla_swa_moe'


def rms_norm(x, w):
    xf = x.astype(jnp.float32)
    y = xf * lax.rsqrt(jnp.mean(xf * xf, axis=-1, keepdims=True) + RMS_EPS)
    return (y * w.astype(jnp.float32)).astype(x.dtype)


def rope_tables(seq, dim):
    inv_freq = 1.0 / (ROPE_THETA ** (jnp.arange(0, dim, 2, dtype=jnp.float32) / dim))
    ang = jnp.arange(seq, dtype=jnp.float32)[:, None] * inv_freq[None, :]
    return jnp.cos(ang), jnp.sin(ang)


def apply_rope(x, cos, sin):
    x1, x2 = jnp.split(x, 2, axis=-1)
    c = cos[None, :, None, :].astype(x.dtype)
    s = sin[None, :, None, :].astype(x.dtype)
    return jnp.concatenate([x1 * c - x2 * s, x2 * c + x1 * s], axis=-1)


def mlstm(q, k, v, i_pre, f_pre):
    f32 = jnp.float32
    bsz, seq, nh, dk = q.shape
    dv = v.shape[-1]
    L = ML_CHUNK
    nc = seq // L

    def chunks(t):
        t = t.astype(f32).reshape((bsz, nc, L, nh) + t.shape[3:])
        return jnp.moveaxis(t, 3, 1)

    qc = chunks(q)
    kc = chunks(k) * dk ** -0.5
    vc = chunks(v)
    ig = chunks(i_pre)
    b = jnp.cumsum(jax.nn.log_sigmoid(chunks(f_pre)), axis=-1)
    causal = jnp.tril(jnp.ones((L, L), dtype=bool))
    dmat = jnp.where(causal, b[..., :, None] - b[..., None, :] + ig[..., None, :], -jnp.inf)
    m_intra = jnp.max(dmat, axis=-1)
    a = b[..., -1:] - b + ig
    m_loc = jnp.max(a, axis=-1)
    wl = jnp.exp(a - m_loc[..., None])
    s_loc = jnp.einsum('bhclv,bhclk->bhcvk', vc * wl[..., None], kc)
    n_loc = jnp.einsum('bhcl,bhclk->bhck', wl, kc)
    b_tot = b[..., -1]

    def step(carry, inp):
        c_st, n_st, m_st = carry
        bt, ml, sl, nl = inp
        m_new = jnp.maximum(bt + m_st, ml)
        da = jnp.exp(bt + m_st - m_new)
        db = jnp.exp(ml - m_new)
        c_new = da[..., None, None] * c_st + db[..., None, None] * sl
        n_new = da[..., None] * n_st + db[..., None] * nl
        return (c_new, n_new, m_new), (c_st, n_st, m_st)

    init = (jnp.zeros((bsz, nh, dv, dk), f32), jnp.zeros((bsz, nh, dk), f32), jnp.zeros((bsz, nh), f32))
    xs = tuple(jnp.moveaxis(t, 2, 0) for t in (b_tot, m_loc, s_loc, n_loc))
    _, (c_prev, n_prev, m_prev) = lax.scan(step, init, xs)
    c_prev = jnp.moveaxis(c_prev, 0, 2)
    n_prev = jnp.moveaxis(n_prev, 0, 2)
    m_prev = jnp.moveaxis(m_prev, 0, 2)

    g = b + m_prev[..., None]
    m_s = jnp.maximum(g, m_intra)
    p = jnp.exp(dmat - m_s[..., None]) * jnp.einsum('bhcsk,bhctk->bhcst', qc, kc)
    inter = jnp.exp(g - m_s)
    num = jnp.einsum('bhcst,bhctv->bhcsv', p, vc) + inter[..., None] * jnp.einsum('bhcvk,bhcsk->bhcsv', c_prev, qc)
    den = jnp.sum(p, axis=-1) + inter * jnp.einsum('bhck,bhcsk->bhcs', n_prev, qc)
    h = num / jnp.maximum(jnp.abs(den), jnp.exp(-m_s))[..., None]
    h = jnp.moveaxis(h, 1, 3).reshape(bsz, seq, nh, dv)
    return h.astype(q.dtype)


def causal_conv(x, w, b):
    K = w.shape[0]
    seq = x.shape[1]
    xp = jnp.pad(x, ((0, 0), (K - 1, 0), (0, 0)))
    y = xp[:, 0:seq] * w[0]
    for j in range(1, K):
        y = y + xp[:, j:j + seq] * w[j]
    return y + b


def ssd(x, dt, a_neg, bm, cm):
    f32 = jnp.float32
    bsz, seq, nh, hp = x.shape
    ng, ns = bm.shape[2], bm.shape[3]
    R = nh // ng
    L = SSM_CHUNK
    nc = seq // L
    xs = (x.astype(f32) * dt[..., None]).reshape(bsz, nc, L, ng, R, hp)
    a = jnp.transpose((dt * a_neg).reshape(bsz, nc, L, ng, R), (0, 3, 4, 1, 2))
    a_cs = jnp.cumsum(a, axis=-1)
    bc = bm.astype(f32).reshape(bsz, nc, L, ng, ns)
    cc = cm.astype(f32).reshape(bsz, nc, L, ng, ns)
    causal = jnp.tril(jnp.ones((L, L), dtype=bool))
    decay = jnp.exp(jnp.where(causal, a_cs[..., :, None] - a_cs[..., None, :], -jnp.inf))
    cb = jnp.einsum('bcsgn,bctgn->bgcst', cc, bc)
    y_diag = jnp.einsum('bgrcst,bctgrp->bcsgrp', decay * cb[:, :, None], xs)
    decay_st = jnp.transpose(jnp.exp(a_cs[..., -1:] - a_cs), (0, 3, 4, 1, 2))
    states = jnp.einsum('bctgn,bctgrp->bcgrpn', bc, xs * decay_st[..., None])
    chunk_decay = jnp.moveaxis(jnp.exp(a_cs[..., -1]), 3, 0)

    def step(st, inp):
        dec, s_new = inp
        return dec[..., None, None] * st + s_new, st

    _, prev = lax.scan(step, jnp.zeros((bsz, ng, R, hp, ns), f32), (chunk_decay, jnp.moveaxis(states, 1, 0)))
    prev = jnp.moveaxis(prev, 0, 1)
    in_decay = jnp.transpose(jnp.exp(a_cs), (0, 3, 4, 1, 2))
    y_off = jnp.einsum('bcsgn,bcgrpn->bcsgrp', cc, prev) * in_decay[..., None]
    return (y_diag + y_off).reshape(bsz, seq, nh, hp)


def causal_block_attention(q, k, v, scale):
    seq = q.shape[1]
    blk = ATTN_BLOCK
    outs = []
    for i in range(seq // blk):
        kend = (i + 1) * blk
        s = jnp.einsum('bqhd,bkhd->bhqk', q[:, i * blk:kend], k[:, :kend]).astype(jnp.float32) * scale
        qpos = i * blk + jnp.arange(blk)
        mask = jnp.arange(kend)[None, :] <= qpos[:, None]
        p = jax.nn.softmax(jnp.where(mask, s, -jnp.inf), axis=-1)
        outs.append(jnp.einsum('bhqk,bkhd->bqhd', p.astype(v.dtype), v[:, :kend]))
    return jnp.concatenate(outs, axis=1)


def swa_sink_attention(q, k, v, sinks):
    f32 = jnp.float32
    bsz, seq, hq, d = q.shape
    hkv = k.shape[2]
    grp = hq // hkv
    W = SWA_WINDOW
    nb = seq // W
    qb = q.reshape(bsz, nb, W, hkv, grp, d)

    def with_prev(t):
        prev = jnp.concatenate([jnp.zeros_like(t[:, :1]), t[:, :-1]], axis=1)
        return jnp.concatenate([prev, t], axis=2)

    kk = with_prev(k.reshape(bsz, nb, W, hkv, d))
    vv = with_prev(v.reshape(bsz, nb, W, hkv, d))
    s = jnp.einsum('bnqkgd,bntkd->bnkgqt', qb, kk).astype(f32) * d ** -0.5
    qpos = jnp.arange(W)[:, None] + W
    tpos = jnp.arange(2 * W)[None, :]
    band = (tpos <= qpos) & (qpos - tpos < W)
    valid = (jnp.arange(nb)[:, None, None] * W + tpos[None] - W) >= 0
    mask = band[None] & valid
    s = jnp.where(mask[None, :, None, None], s, -jnp.inf)
    sink = jnp.broadcast_to(sinks.astype(f32).reshape(1, 1, hkv, grp, 1, 1), s.shape[:-1] + (1,))
    p = jax.nn.softmax(jnp.concatenate([s, sink], axis=-1), axis=-1)[..., :-1]
    o = jnp.einsum('bnkgqt,bntkd->bnqkgd', p.astype(v.dtype), vv)
    return o.reshape(bsz, seq, hq * d)


def hybrid_mixer(xn, w_in, ml_igate_bias, ml_fgate_bias, ml_norm, ssm_conv_w, ssm_conv_b, ssm_dt_bias,
                 ssm_a_log, ssm_d, ssm_norm, mla_q_norm, mla_w_uq, mla_kv_norm, mla_w_ukv, swa_sinks,
                 w_branch, w_out, cos, sin):
    bsz, seq, _ = xn.shape
    u = xn @ w_in
    (ml_q, ml_k, ml_v, ml_o, ml_i, ml_f, ssm_z, ssm_xbc, ssm_dt, mla_cq, mla_ckv, mla_kr,
     swa_q, swa_k, swa_v, gate_pre) = jnp.split(u, IN_OFFSETS, axis=-1)

    h_a = mlstm(ml_q.reshape(bsz, seq, ML_HEADS, ML_DQK), ml_k.reshape(bsz, seq, ML_HEADS, ML_DQK),
                ml_v.reshape(bsz, seq, ML_HEADS, ML_DV), ml_i + ml_igate_bias, ml_f + ml_fgate_bias)
    h_a = rms_norm(h_a, ml_norm.reshape(ML_HEADS, ML_DV))
    y_a = jax.nn.sigmoid(ml_o) * h_a.reshape(bsz, seq, ML_HEADS * ML_DV)

    xbc = jax.nn.silu(causal_conv(ssm_xbc, ssm_conv_w, ssm_conv_b))
    x_s, b_s, c_s = jnp.split(xbc, (SSM_DINNER, SSM_DINNER + SSM_GROUPS * SSM_STATE), axis=-1)
    x_s = x_s.reshape(bsz, seq, SSM_HEADS, SSM_HEADDIM)
    dt = jax.nn.softplus((ssm_dt + ssm_dt_bias).astype(jnp.float32))
    a_neg = -jnp.exp(ssm_a_log.astype(jnp.float32))
    y_s = ssd(x_s, dt, a_neg, b_s.reshape(bsz, seq, SSM_GROUPS, SSM_STATE),
              c_s.reshape(bsz, seq, SSM_GROUPS, SSM_STATE))
    y_s = y_s.astype(xn.dtype) + x_s * ssm_d[:, None]
    y_s = y_s.reshape(bsz, seq, SSM_DINNER) * jax.nn.silu(ssm_z)
    y_b = rms_norm(y_s.reshape(bsz, seq, SSM_GROUPS, SSM_DINNER // SSM_GROUPS),
                   ssm_norm.reshape(SSM_GROUPS, SSM_DINNER // SSM_GROUPS)).reshape(bsz, seq, SSM_DINNER)

    qf = (rms_norm(mla_cq, mla_q_norm) @ mla_w_uq).reshape(bsz, seq, MLA_HEADS, MLA_NOPE + MLA_ROPE)
    q_nope, q_rope = jnp.split(qf, (MLA_NOPE,), axis=-1)
    kvf = (rms_norm(mla_ckv, mla_kv_norm) @ mla_w_ukv).reshape(bsz, seq, MLA_HEADS, MLA_NOPE + MLA_V)
    k_nope, v_c = jnp.split(kvf, (MLA_NOPE,), axis=-1)
    k_rope = apply_rope(mla_kr.reshape(bsz, seq, 1, MLA_ROPE), cos, sin)
    q_c = jnp.concatenate([q_nope, apply_rope(q_rope, cos, sin)], axis=-1)
    k_c = jnp.concatenate([k_nope, jnp.broadcast_to(k_rope, (bsz, seq, MLA_HEADS, MLA_ROPE))], axis=-1)
    y_c = causal_block_attention(q_c, k_c, v_c, (MLA_NOPE + MLA_ROPE) ** -0.5).reshape(bsz, seq, MLA_HEADS * MLA_V)

    y_d = swa_sink_attention(swa_q.reshape(bsz, seq, SWA_HEADS, SWA_HEAD_DIM),
                             swa_k.reshape(bsz, seq, SWA_KV_HEADS, SWA_HEAD_DIM),
                             swa_v.reshape(bsz, seq, SWA_KV_HEADS, SWA_HEAD_DIM), swa_sinks)

    branches = jnp.stack([y_a, y_b, y_c, y_d], axis=2)
    gates = jax.nn.sigmoid(gate_pre).reshape(bsz, seq, N_BRANCH, D_MODEL)
    merged = jnp.sum(gates * jnp.einsum('bsnw,nwd->bsnd', branches, w_branch), axis=2)
    return merged @ w_out


def cross_attention(hn, memn, wq, wkv, wo):
    bsz, seq, _ = hn.shape
    m = memn.shape[1]
    q = (hn @ wq).reshape(bsz, seq, XA_HEADS, XA_HEAD_DIM)
    k, v = jnp.split(memn @ wkv, 2, axis=-1)
    k = k.reshape(bsz, m, XA_HEADS, XA_HEAD_DIM)
    v = v.reshape(bsz, m, XA_HEADS, XA_HEAD_DIM)
    s = jnp.einsum('bshd,bmhd->bhsm', q, k).astype(jnp.float32) * XA_HEAD_DIM ** -0.5
    p = jax.nn.softmax(s, axis=-1)
    o = jnp.einsum('bhsm,bmhd->bshd', p.astype(v.dtype), v).reshape(bsz, seq, XA_HEADS * XA_HEAD_DIM)
    return o @ wo


def swiglu(xn, w13, w2):
    h1, h3 = jnp.split(xn @ w13, 2, axis=-1)
    return (jax.nn.silu(h1) * h3) @ w2


def moe_swiglu(xn, router, w13, w2):
    bsz, seq, dm = xn.shape
    n = bsz * seq
    nk = n * TOP_K
    xf = xn.reshape(n, dm)
    logits = (xf @ router).astype(jnp.float32)
    top_val, top_idx = lax.top_k(logits, TOP_K)
    gate = jax.nn.softmax(top_val, axis=-1).astype(xn.dtype)
    e_flat = top_idx.reshape(-1).astype(jnp.int32)
    tok_flat = jnp.arange(nk, dtype=jnp.int32) // TOP_K
    g_flat = gate.reshape(-1)
    order = jnp.argsort(e_flat)
    e_sorted = e_flat[order]
    counts = jnp.bincount(e_flat, length=N_EXPERTS).astype(jnp.int32)
    padded = (counts + MOE_BLOCK - 1) // MOE_BLOCK * MOE_BLOCK
    start = jnp.cumsum(counts) - counts
    pend = jnp.cumsum(padded)
    pstart = pend - padded
    dest = pstart[e_sorted] + (jnp.arange(nk, dtype=jnp.int32) - start[e_sorted])
    cap = (-(-nk // MOE_BLOCK) + N_EXPERTS) * MOE_BLOCK
    n_blocks = cap // MOE_BLOCK
    row_tok = jnp.full((cap,), n, dtype=jnp.int32).at[dest].set(tok_flat[order])
    row_gate = jnp.zeros((cap,), xn.dtype).at[dest].set(g_flat[order])
    blk_expert = jnp.minimum(
        jnp.searchsorted(pend, jnp.arange(n_blocks, dtype=jnp.int32) * MOE_BLOCK, side='right'), N_EXPERTS - 1)
    x_pad = jnp.concatenate([xf, jnp.zeros((1, dm), xf.dtype)], axis=0)
    xg = x_pad[row_tok].reshape(n_blocks, MOE_BLOCK, dm)

    def expert_block(args):
        xb, e = args
        return swiglu(xb, w13[e], w2[e])

    yg = lax.map(expert_block, (xg, blk_expert)).reshape(cap, dm)
    y = jax.ops.segment_sum(yg * row_gate[:, None], row_tok, num_segments=n + 1)[:n]
    return y.reshape(bsz, seq, dm)


def setup_inputs(seed: int = 0) -> dict:
    key = jax.random.key(seed)
    ks = iter(jax.random.split(key, 40))
    f32 = jnp.float32

    def nrm(shape, fan_in):
        return jax.random.normal(next(ks), shape, f32) * fan_in ** -0.5

    def gain(shape):
        return 1.0 + 0.02 * jax.random.normal(next(ks), shape, f32)

    def small(shape, scale):
        return scale * jax.random.normal(next(ks), shape, f32)

    x = jax.random.normal(next(ks), (BATCH, SEQ, D_MODEL), f32)
    mem = jax.random.normal(next(ks), (BATCH, MEM_LEN, D_MODEL), f32)
    norm_mix = gain((DEPTH, D_MODEL))
    w_in = nrm((DEPTH, D_MODEL, IN_TOTAL), D_MODEL)
    ml_igate_bias = small((DEPTH, ML_HEADS), 0.1)
    ml_fgate_bias = jnp.linspace(3.0, 6.0, ML_HEADS, dtype=f32)[None, :] + small((DEPTH, ML_HEADS), 0.1)
    ml_norm = gain((DEPTH, ML_HEADS * ML_DV))
    ssm_conv_w = nrm((DEPTH, SSM_CONV, SSM_CONV_CH), SSM_CONV)
    ssm_conv_b = small((DEPTH, SSM_CONV_CH), 0.02)
    dt0 = jnp.exp(jax.random.uniform(next(ks), (DEPTH, SSM_HEADS), f32, math.log(1e-3), math.log(1e-1)))
    ssm_dt_bias = dt0 + jnp.log(-jnp.expm1(-dt0))
    ssm_a_log = jnp.log(jax.random.uniform(next(ks), (DEPTH, SSM_HEADS), f32, 1.0, 16.0))
    ssm_d = gain((DEPTH, SSM_HEADS))
    ssm_norm = gain((DEPTH, SSM_DINNER))
    mla_q_norm = gain((DEPTH, MLA_Q_LORA))
    mla_w_uq = nrm((DEPTH, MLA_Q_LORA, MLA_HEADS * (MLA_NOPE + MLA_ROPE)), MLA_Q_LORA)
    mla_kv_norm = gain((DEPTH, MLA_KV_LORA))
    mla_w_ukv = nrm((DEPTH, MLA_KV_LORA, MLA_HEADS * (MLA_NOPE + MLA_V)), MLA_KV_LORA)
    swa_sinks = small((DEPTH, SWA_HEADS), 0.5)
    w_branch = nrm((DEPTH, N_BRANCH, BRANCH_W, D_MODEL), BRANCH_W)
    w_out = nrm((DEPTH, D_MODEL, D_MODEL), D_MODEL)
    norm_cross = gain((DEPTH, D_MODEL))
    norm_mem = gain((DEPTH, D_MODEL))
    xa_wq = nrm((DEPTH, D_MODEL, XA_HEADS * XA_HEAD_DIM), D_MODEL)
    xa_wkv = nrm((DEPTH, D_MODEL, 2 * XA_HEADS * XA_HEAD_DIM), D_MODEL)
    xa_wo = nrm((DEPTH, XA_HEADS * XA_HEAD_DIM, D_MODEL), XA_HEADS * XA_HEAD_DIM)
    norm_ffn = gain((DEPTH, D_MODEL))
    ffn_w13 = nrm((N_DENSE, D_MODEL, 2 * FFN_DIM), D_MODEL)
    ffn_w2 = nrm((N_DENSE, FFN_DIM, D_MODEL), FFN_DIM)
    moe_router = nrm((N_MOE, D_MODEL, N_EXPERTS), D_MODEL)
    moe_w13 = nrm((N_MOE, N_EXPERTS, D_MODEL, 2 * FFN_DIM), D_MODEL)
    moe_w2 = nrm((N_MOE, N_EXPERTS, FFN_DIM, D_MODEL), FFN_DIM)
    norm_final = gain((D_MODEL,))
    return {
        'x': x, 'mem': mem, 'norm_mix': norm_mix, 'w_in': w_in,
        'ml_igate_bias': ml_igate_bias, 'ml_fgate_bias': ml_fgate_bias, 'ml_norm': ml_norm,
        'ssm_conv_w': ssm_conv_w, 'ssm_conv_b': ssm_conv_b, 'ssm_dt_bias': ssm_dt_bias,
        'ssm_a_log': ssm_a_log, 'ssm_d': ssm_d, 'ssm_norm': ssm_norm,
        'mla_q_norm': mla_q_norm, 'mla_w_uq': mla_w_uq, 'mla_kv_norm': mla_kv_norm, 'mla_w_ukv': mla_w_ukv,
        'swa_sinks': swa_sinks, 'w_branch': w_branch, 'w_out': w_out,
        'norm_cross': norm_cross, 'norm_mem': norm_mem, 'xa_wq': xa_wq, 'xa_wkv': xa_wkv, 'xa_wo': xa_wo,
        'norm_ffn': norm_ffn, 'ffn_w13': ffn_w13, 'ffn_w2': ffn_w2,
        'moe_router': moe_router, 'moe_w13': moe_w13, 'moe_w2': moe_w2, 'norm_final': norm_final,
    }


def reference(x, mem, norm_mix, w_in, ml_igate_bias, ml_fgate_bias, ml_norm, ssm_conv_w, ssm_conv_b,
              ssm_dt_bias, ssm_a_log, ssm_d, ssm_norm, mla_q_norm, mla_w_uq, mla_kv_norm, mla_w_ukv,
              swa_sinks, w_branch, w_out, norm_cross, norm_mem, xa_wq, xa_wkv, xa_wo, norm_ffn,
              ffn_w13, ffn_w2, moe_router, moe_w13, moe_w2, norm_final):
    seq = x.shape[1]
    cos, sin = rope_tables(seq, MLA_ROPE)
    h = x
    for l in range(DEPTH):
        xn = rms_norm(h, norm_mix[l])
        h = h + hybrid_mixer(xn, w_in[l], ml_igate_bias[l], ml_fgate_bias[l], ml_norm[l], ssm_conv_w[l],
                             ssm_conv_b[l], ssm_dt_bias[l], ssm_a_log[l], ssm_d[l], ssm_norm[l],
                             mla_q_norm[l], mla_w_uq[l], mla_kv_norm[l], mla_w_ukv[l], swa_sinks[l],
                             w_branch[l], w_out[l], cos, sin)
        h = h + cross_attention(rms_norm(h, norm_cross[l]), rms_norm(mem, norm_mem[l]),
                                xa_wq[l], xa_wkv[l], xa_wo[l])
        hn = rms_norm(h, norm_ffn[l])
        if l % 2 == 0:
            h = h + swiglu(hn, ffn_w13[l // 2], ffn_w2[l // 2])
        else:
            h = h + moe_swiglu(hn, moe_router[l // 2], moe_w13[l // 2], moe_w2[l // 2])
    return rms_norm(h, norm_final)
```

```python
import contextlib
import math
import numpy as np
import ml_dtypes
import concourse.bass as bass
import concourse.mybir as mybir
from concourse.bass_utils import run_bass_kernel_spmd

F32 = mybir.dt.float32
BF16 = mybir.dt.bfloat16
AF = mybir.ActivationFunctionType
ALU = mybir.AluOpType
AX = mybir.AxisListType

EPOCH = 4000
N_DMA_SEMS = 40

D = 2048
KC = D // 128
IN_TOTAL = 16216
FFN = 7168
NEXP = 8
MEM = 256
O_MLQ, O_MLK, O_MLV, O_MLO, O_MLI, O_MLF = 0, 512, 1024, 2048, 3072, 3076
O_Z, O_XBC, O_DT, O_CQ, O_CKV, O_KR = 3080, 4104, 5640, 5656, 6168, 6424
O_SWQ, O_SWK, O_SWV, O_GATE = 6488, 7512, 7768, 8024
NEG = -30000.0


class Prog:
    ENGS = ("pe", "act", "dve", "pool", "sp")

    def __init__(self, nc):
        self.nc = nc
        self.ops = []
        self.last_w = {}
        self.readers = {}
        self.stack = contextlib.ExitStack()
        self.nd = 0
        self.last_on_eng = {}
        self.last_dma_slot = {}
        self.uid = 0

    def sbuf(self, name, shape, dtype, stack=None):
        self.uid += 1
        return (stack or self.stack).enter_context(
            self.nc.sbuf_tensor(f"{name}_{self.uid}", list(shape), dtype))

    def psum(self, name, shape, dtype):
        return self.stack.enter_context(self.nc.psum_tensor(name, list(shape), dtype))

    def dram(self, name, shape, dtype, kind="Internal"):
        return self.nc.dram_tensor(name, list(shape), dtype, kind=kind).ap()

    def add(self, eng, emit, reads=(), writes=(), dma=False):
        idx = len(self.ops)
        raw, other = set(), set()
        for r in reads:
            w = self.last_w.get(r)
            if w is not None:
                raw.add(w)
        for r in writes:
            w = self.last_w.get(r)
            if w is not None:
                other.add(w)
            for rd in self.readers.get(r, ()):
                other.add(rd)
        for r in reads:
            lst = self.readers.setdefault(r, [])
            if not dma:
                lst[:] = [j for j in lst if self.ops[j]["dma"] or self.ops[j]["eng"] != eng]
            lst.append(idx)
        for r in writes:
            self.last_w[r] = idx
            self.readers[r] = []
        raw.discard(idx)
        other.discard(idx)
        other -= raw
        op = dict(eng=eng, emit=emit, raw=raw, other=other, dma=dma, need_inc=False,
                  ticket=None, explicit=None)
        if dma:
            op["slot"] = self.nd % N_DMA_SEMS
            self.nd += 1
            self.last_dma_slot[op["slot"]] = idx
        self.last_on_eng[eng] = idx
        self.ops.append(op)
        return idx

    def barrier(self):
        targets = set(self.last_on_eng.values()) | set(self.last_dma_slot.values())
        for e in self.ENGS:
            self.ops.append(dict(eng=e, emit=None, raw=set(), other=set(), dma=False,
                                 need_inc=False, ticket=None, explicit=set(targets)))
        self.last_w = {}
        self.readers = {}

    @contextlib.contextmanager
    def phase(self):
        st = contextlib.ExitStack()
        try:
            yield st
        finally:
            self.barrier()
            st.close()

    def dma(self, eng, out, in_, r=(), w=(), **kw):
        return self.add(eng, lambda e: e.dma_start(out=out, in_=in_, **kw), r, w, dma=True)

    def mm(self, out, lhsT, rhs, start, stop, r, w):
        return self.add("pe", lambda e: e.matmul(out=out, lhsT=lhsT, rhs=rhs, start=start, stop=stop), r, w)

    def tr(self, out, in_, ident, r, w):
        return self.add("pe", lambda e: e.transpose(out=out, in_=in_, identity=ident), r, w)

    def act(self, out, in_, func, r, w, **kw):
        return self.add("act", lambda e: e.activation(out=out, in_=in_, func=func, **kw), r, w)

    def ts(self, eng, out, in0, s1, s2, op0, op1, r, w):
        if op1 is None:
            return self.add(eng, lambda e: e.tensor_scalar(out=out, in0=in0, scalar1=s1, scalar2=None, op0=op0), r, w)
        return self.add(eng, lambda e: e.tensor_scalar(out=out, in0=in0, scalar1=s1, scalar2=s2, op0=op0, op1=op1), r, w)

    def tt(self, eng, out, in0, in1, op, r, w):
        return self.add(eng, lambda e: e.tensor_tensor(out=out, in0=in0, in1=in1, op=op), r, w)

    def stt(self, eng, out, in0, scalar, in1, op0, op1, r, w):
        return self.add(eng, lambda e: e.scalar_tensor_tensor(out=out, in0=in0, scalar=scalar, in1=in1,
                                                              op0=op0, op1=op1), r, w)

    def cp(self, eng, out, in_, r, w):
        if eng == "act":
            return self.add(eng, lambda e: e.copy(out=out, in_=in_), r, w)
        return self.add(eng, lambda e: e.tensor_copy(out=out, in_=in_), r, w)

    def memset(self, eng, ap, val, w):
        return self.add(eng, lambda e: e.memset(ap, val), (), w)

    def emit_all(self):
        nc, ops = self.nc, self.ops
        self.barrier()
        for i, op in enumerate(ops):
            if op["explicit"] is not None:
                deps = [j for j in op["explicit"] if j < i]
            else:
                deps = []
                for j in op["raw"]:
                    pj = ops[j]
                    if pj["eng"] == op["eng"] and not pj["dma"] and op["eng"] == "pe" and not op["dma"]:
                        continue
                    deps.append(j)
                for j in op["other"]:
                    pj = ops[j]
                    if pj["eng"] == op["eng"] and not pj["dma"] and not op["dma"]:
                        continue
                    deps.append(j)
            op["deps"] = sorted(set(deps))
            for j in op["deps"]:
                ops[j]["need_inc"] = True
        counts = {e: 0 for e in self.ENGS}
        nsem = {e: 1 for e in self.ENGS}
        dma_uses = [0] * N_DMA_SEMS
        dma_prev = [None] * N_DMA_SEMS
        for i, op in enumerate(ops):
            if op["dma"]:
                s = op["slot"]
                dma_uses[s] += 1
                op["ticket"] = ("dma", s, dma_uses[s] * 16)
                op["dma_prev"] = dma_prev[s]
                dma_prev[s] = i
            elif op["need_inc"]:
                e = op["eng"]
                ep, v = divmod(counts[e], EPOCH)
                counts[e] += 1
                op["ticket"] = (e, ep, v + 1)
                nsem[e] = max(nsem[e], ep + 1)
        sems = {}
        for e in self.ENGS:
            for ep in range(nsem[e]):
                sems[(e, ep)] = self.stack.enter_context(nc.semaphore(f"s_{e}_{ep}"))
        for s in range(N_DMA_SEMS):
            sems[("dma", s)] = self.stack.enter_context(nc.semaphore(f"s_dma_{s}"))
        print("ops", len(ops), "incs", counts, "ndma", self.nd, "nsems", len(sems), flush=True)
        by_eng = {e: [] for e in self.ENGS}
        for i, op in enumerate(ops):
            by_eng[op["eng"]].append(i)

        def run_engine(ename, eng):
            waited = {}

            def wait(t):
                key = (t[0], t[1])
                if waited.get(key, 0) >= t[2]:
                    return
                eng.wait_ge(sems[key], t[2])
                waited[key] = t[2]

            for i in by_eng[ename]:
                op = ops[i]
                for j in op["deps"]:
                    wait(ops[j]["ticket"])
                if op["emit"] is None:
                    continue
                if op["dma"] and op["dma_prev"] is not None:
                    wait(ops[op["dma_prev"]]["ticket"])
                ins = op["emit"](eng)
                t = op["ticket"]
                if t is not None:
                    ins.then_inc(sems[(t[0], t[1])], 16 if op["dma"] else 1)

        with nc.Block() as block:
            @block.tensor
            def _(e):
                run_engine("pe", e)

            @block.scalar
            def _(e):
                run_engine("act", e)

            @block.vector
            def _(e):
                run_engine("dve", e)

            @block.gpsimd
            def _(e):
                run_engine("pool", e)

            @block.sync
            def _(e):
                run_engine("sp", e)
        self.stack.close()


class Ring:
    def __init__(self, P, st, name, n, shape, dtype):
        self.n = n
        self.bufs = [P.sbuf(name, shape, dtype, st) for _ in range(n)]
        P.uid += 1
        self.keys = [f"{name}#{P.uid}#{i}" for i in range(n)]
        self.i = 0

    def next(self):
        j = self.i % self.n
        self.i += 1
        return self.bufs[j], self.keys[j]


class KeyRing:
    def __init__(self, objs, keys):
        self.objs, self.keys, self.i = objs, keys, 0

    def next(self):
        j = self.i % len(self.objs)
        self.i += 1
        return self.objs[j], self.keys[j]


class Builder:
    def __init__(self, S, depth=2, debug=False, stop_after=None):
        self.S = S
        self.NT = S // 128
        self.GW = min(512, S)
        self.NG = S // self.GW
        self.depth = depth
        self.debug = debug
        self.stop_after = stop_after
        nc = bass.Bass("TRN2", target_bir_lowering=False)
        self.nc = nc
        self.P = Prog(nc)

    def norm_T(self, st, src, gamma, dstT, dkey, R, W, eps=1e-6, dt=BF16, rows_at=None):
        P, c = self.P, self.c
        kc = W // 128
        gcol = P.sbuf("gcol", [128, kc], F32, st)
        P.dma("sp", gcol[:], gamma.rearrange("(c p) -> p c", p=128), w=["gcol"], allow_slow_non_contiguous=True)
        hr = Ring(P, st, "nt_h", 2, [128, W], F32)
        hbr = Ring(P, st, "nt_hb", 2, [128, W], dt)
        junk = P.sbuf("nt_junk", [128, W], F32, st)
        ssr = Ring(P, st, "nt_ss", 2, [128, 2], F32)
        ptr = KeyRing(self.pt if dt == BF16 else self.ps[4:6], ["pt0", "pt1"] if dt == BF16 else ["ps4", "ps5"])
        ident = c["ident"] if dt == BF16 else c["identf"]
        for t in range(R // 128):
            ht, hk = hr.next()
            hb, hbk = hbr.next()
            ss, sk = ssr.next()
            P.dma("sp", ht[:], src[t * 128:(t + 1) * 128, :], w=[hk])
            P.act(junk[:], ht[:], AF.Square, [hk], ["nt_junk", sk], accum_out=ss[:, 0:1])
            P.ts("dve", ss[:, 1:2], ss[:, 0:1], 1.0 / W, eps, ALU.mult, ALU.add, [sk], [sk])
            P.act(ss[:, 1:2], ss[:, 1:2], AF.Sqrt, [sk], [sk])
            P.add("dve", lambda e, ss=ss: e.reciprocal(out=ss[:, 1:2], in_=ss[:, 1:2]), [sk], [sk])
            P.ts("pool", hb[:], ht[:], ss[:, 1:2], None, ALU.mult, None, [hk, sk], [hbk])
            for c4 in range(0, kc, 4):
                n4 = min(4, kc - c4)
                pst, pk = ptr.next()
                pv = pst[:, 0:512].rearrange("p (a b) -> p a b", b=128)
                for j in range(n4):
                    P.tr(pv[:, j, :], hb[:, (c4 + j) * 128:(c4 + j + 1) * 128], ident[:], [hbk, "const"], [pk])
                P.tt("dve", dstT[:, c4:c4 + n4, t * 128:(t + 1) * 128], pv[:, 0:n4, :],
                     gcol[:, c4:c4 + n4].unsqueeze(2).to_broadcast([128, n4, 128]), ALU.mult,
                     [pk, "gcol"], [(dkey, t)])

    def wload(self, wring, W_ap, c0, w, kc):
        wt, wk = wring.next()
        self.P.dma("pool", wt[:, 0:kc, 0:w], W_ap[:, c0:c0 + w].rearrange("(c p) n -> p c n", p=128), w=[wk])
        return wt, wk

    def proj_fm(self, st, xT, xkey, kc, W_ap, c0, ncols, dst, rings, func=None, dtype=F32):
        P, S, GW = self.P, self.S, self.GW
        wring, psr, stg32, stg16 = rings
        stgr = stg32 if dtype == F32 else stg16
        xr = [(xkey, t) for t in range(self.NT)]
        for cb in range(0, ncols, 512):
            w = min(512, ncols - cb)
            wt, wk = self.wload(wring, W_ap, c0 + cb, w, kc)
            for sb in range(0, w, 128):
                m = min(128, w - sb)
                stg, sk = stgr.next()
                for g in range(self.NG):
                    ps, pk = psr.next()
                    for k in range(kc):
                        P.mm(ps[0:m, 0:GW], wt[:, k, sb:sb + m], xT[:, k, g * GW:(g + 1) * GW],
                             k == 0, k == kc - 1, [wk] + xr, [pk])
                    if func is not None or (g % 2 == 0):
                        P.act(stg[0:m, g * GW:(g + 1) * GW], ps[0:m, 0:GW], func or AF.Copy, [pk], [sk])
                    else:
                        P.cp("dve", stg[0:m, g * GW:(g + 1) * GW], ps[0:m, 0:GW], [pk], [sk])
                P.dma("sp", dst[cb + sb:cb + sb + m, :], stg[0:m, 0:S], r=[sk])

    def proj_tm(self, st, xT, xkey, kc, W_ap, c0, ncols, dst, rings, dtype=F32):
        P, S = self.P, self.S
        wring, psr, stg32, stg16 = rings
        stgr = stg32 if dtype == F32 else stg16
        for cb in range(0, ncols, 512):
            w = min(512, ncols - cb)
            wt, wk = self.wload(wring, W_ap, c0 + cb, w, kc)
            for t in range(self.NT):
                ps, pk = psr.next()
                stg, sk = stgr.next()
                for k in range(kc):
                    P.mm(ps[:, 0:w], xT[:, k, t * 128:(t + 1) * 128], wt[:, k, 0:w], k == 0, k == kc - 1,
                         [wk, (xkey, t)], [pk])
                if t % 2 == 0:
                    P.act(stg[:, 0:w], ps[:, 0:w], AF.Copy, [pk], [sk])
                else:
                    P.cp("dve", stg[:, 0:w], ps[:, 0:w], [pk], [sk])
                P.dma("sp", dst[t * 128:(t + 1) * 128, cb:cb + w], stg[:, 0:w], r=[sk])

    def bcast_row(self, st, name, src_row, n):
        t = self.P.sbuf(name, [128, n], F32, st)
        self.P.dma("sp", t[:], src_row.partition_broadcast(128), w=[name])
        return t

    def build(self):
        P, nc, S, NT = self.P, self.nc, self.S, self.NT
        inp = lambda n, sh, dt=F32: P.dram(n, sh, dt, kind="ExternalInput")
        A = {}
        A["x"] = inp("x", [S, D])
        A["mem"] = inp("mem", [MEM, D])
        for n, sh in (("norm_mix", [2, D]), ("w_in", [2, D, IN_TOTAL]), ("ml_igate_bias", [2, 4]),
                      ("ml_fgate_bias", [2, 4]), ("ml_norm", [2, 1024]), ("ssm_conv_w", [2, 4, 1536]),
                      ("ssm_conv_b", [2, 1536]), ("ssm_dt_bias", [2, 16]), ("ssm_a_log", [2, 16]),
                      ("ssm_d", [2, 16]), ("ssm_norm", [2, 1024]), ("mla_q_norm", [2, 512]),
                      ("mla_w_uq", [2, 512, 1536]), ("mla_kv_norm", [2, 256]), ("mla_w_ukv", [2, 256, 2048]),
                      ("swa_sinks", [2, 16]), ("w_branch", [2, 4, 1024, D]), ("w_out", [2, D, D]),
                      ("norm_cross", [2, D]), ("norm_mem", [2, D]), ("xa_wq", [2, D, 512]),
                      ("xa_wkv", [2, D, 1024]), ("xa_wo", [2, 512, D]), ("norm_ffn", [2, D]),
                      ("ffn_w13", [1, D, 2 * FFN]), ("ffn_w2", [1, FFN, D]), ("moe_router", [1, D, NEXP]),
                      ("moe_w13", [1, NEXP, D, 2 * FFN]), ("moe_w2", [1, NEXP, FFN, D]), ("norm_final", [D])):
            A[n] = inp(n, sh)
        A["c_ident"] = inp("c_ident", [128, 128], BF16)
        A["c_identf"] = inp("c_identf", [128, 128])
        A["c_tri"] = inp("c_tri", [128, 128])
        A["c_trib"] = inp("c_trib", [128, 128], BF16)
        A["c_lowb"] = inp("c_lowb", [128, 128], BF16)
        A["c_mneg"] = inp("c_mneg", [128, 128])
        A["c_ones"] = inp("c_ones", [128, 128])
        A["c_cos2"] = inp("c_cos2", [64, S])
        A["c_sin2"] = inp("c_sin2", [64, S])
        self.A = A
        out = P.dram("out", [S, D], F32, kind="ExternalOutput")
        self.out = out
        Sc = {}
        Sc["h"] = P.dram("s_h", [S, D], F32)
        Sc["ufm32"] = P.dram("s_ufm32", [IN_TOTAL + 64, S], F32)
        Sc["ufm16"] = P.dram("s_ufm16", [IN_TOTAL, S], BF16)
        Sc["utm32"] = P.dram("s_utm32", [S, IN_TOTAL], F32)
        Sc["utm16"] = P.dram("s_utm16", [S, IN_TOTAL], BF16)
        Sc["xcT"] = P.dram("s_xcT", [1536, S], F32)
        kind = "ExternalOutput" if self.debug else "Internal"
        for i, n in enumerate("abcd"):
            Sc["y" + n] = P.dram("s_y" + n, [S, 1024], BF16, kind=kind)
        if self.debug:
            Sc["dbg_h"] = [P.dram(f"dbg_h{i}", [S, D], F32, kind="ExternalOutput") for i in range(3 * self.depth)]
        self.Sc = Sc

        c = {}
        self.c = c
        for n, dt in (("ident", BF16), ("identf", F32), ("tri", F32), ("trib", BF16), ("lowb", BF16),
                      ("mneg", F32), ("ones", F32)):
            c[n] = P.sbuf("c_" + n, [128, 128], dt)
            P.dma("sp", c[n][:], A["c_" + n], w=["const"])
        self.ps = [P.psum(f"ps{i}", [128, 512], F32) for i in range(6)]
        self.pt = [P.psum(f"pt{i}", [128, 1024], BF16) for i in range(2)]
        P.barrier()

        with P.phase() as st:
            r = Ring(P, st, "cpx", 3, [128, D], F32)
            for t in range(NT):
                b, k = r.next()
                P.dma("sp", b[:], A["x"][t * 128:(t + 1) * 128, :], w=[k])
                P.dma("sp", Sc["h"][t * 128:(t + 1) * 128, :], b[:], r=[k])

        for l in range(self.depth):
            self.layer(l)
            if self.stop_after is not None and l == self.stop_after[0]:
                break
        self.final_norm()
        P.emit_all()
        return nc

    def dbg_copy_h(self, idx):
        if not self.debug:
            return
        P, Sc = self.P, self.Sc
        with P.phase() as st:
            r = Ring(P, st, "cpd", 3, [128, D], F32)
            for t in range(self.NT):
                b, k = r.next()
                P.dma("sp", b[:], Sc["h"][t * 128:(t + 1) * 128, :], w=[k])
                P.dma("sp", Sc["dbg_h"][idx][t * 128:(t + 1) * 128, :], b[:], r=[k])

    def final_norm(self):
        P, S, NT, A, Sc = self.P, self.S, self.NT, self.A, self.Sc
        with P.phase() as st:
            g = self.bcast_row(st, "fn_g", A["norm_final"], D)
            hr = Ring(P, st, "fn_h", 2, [128, D], F32)
            orr = Ring(P, st, "fn_o", 2, [128, D], F32)
            junk = P.sbuf("fn_junk", [128, D], F32, st)
            ssr = Ring(P, st, "fn_ss", 2, [128, 2], F32)
            for t in range(NT):
                ht, hk = hr.next()
                ot, ok = orr.next()
                ss, sk = ssr.next()
                P.dma("sp", ht[:], Sc["h"][t * 128:(t + 1) * 128, :], w=[hk])
                P.act(junk[:], ht[:], AF.Square, [hk], ["fn_junk", sk], accum_out=ss[:, 0:1])
                P.ts("dve", ss[:, 1:2], ss[:, 0:1], 1.0 / D, 1e-6, ALU.mult, ALU.add, [sk], [sk])
                P.act(ss[:, 1:2], ss[:, 1:2], AF.Sqrt, [sk], [sk])
                P.add("dve", lambda e, ss=ss: e.reciprocal(out=ss[:, 1:2], in_=ss[:, 1:2]), [sk], [sk])
                P.stt("dve", ot[:], ht[:], ss[:, 1:2], g[:], ALU.mult, ALU.mult, [hk, sk, "fn_g"], [ok])
                P.dma("sp", self.out[t * 128:(t + 1) * 128, :], ot[:], r=[ok])

    def layer(self, l):
        sa = self.stop_after
        self.in_proj(l)
        self.mlstm(l)
        self.ssd(l)
        self.mla(l)
        self.swa(l)
        if sa is not None and sa == (l, "branches"):
            return
        self.merge(l)
        self.dbg_copy_h(3 * l + 0)
        if sa is not None and sa == (l, "mixer"):
            return
        self.xattn(l)
        self.dbg_copy_h(3 * l + 1)
        if sa is not None and sa == (l, "xattn"):
            return
        self.ffn_layer(l)
        self.dbg_copy_h(3 * l + 2)

    def in_proj(self, l):
        P, S, NT, A, Sc = self.P, self.S, self.NT, self.A, self.Sc
        W = A["w_in"][l]
        with P.phase() as st:
            xnT = P.sbuf("xnT", [128, KC, S], BF16, st)
            with P.phase() as st2:
                self.norm_T(st2, Sc["h"], A["norm_mix"][l], xnT, "xnT", S, D)
            wring = Ring(P, st, "ip_w", 3, [128, KC, 512], BF16)
            psr = KeyRing(self.ps[0:4], ["ps0", "ps1", "ps2", "ps3"])
            stg32 = Ring(P, st, "ip_s32", 3, [128, max(S, 512)], F32)
            stg16 = Ring(P, st, "ip_s16", 3, [128, max(S, 512)], BF16)
            rings = (wring, psr, stg32, stg16)
            fm = lambda c0, n, dt, func=None, drow=None: self.proj_fm(
                st, xnT, "xnT", KC, W, c0, n,
                (Sc["ufm32"] if dt == F32 else Sc["ufm16"])[(c0 if drow is None else drow):(c0 if drow is None else drow) + n, :],
                rings, func=func, dtype=dt)
            tm = lambda c0, n, dt: self.proj_tm(
                st, xnT, "xnT", KC, W, c0, n, (Sc["utm32"] if dt == F32 else Sc["utm16"])[:, c0:c0 + n], rings, dtype=dt)
            tm(O_MLK, 512, BF16)
            tm(O_MLV, 1024, BF16)
            tm(O_MLO, 1032, F32)
            tm(O_Z, 1024, F32)
            tm(O_DT, 784, F32)
            tm(O_SWV, 256, BF16)
            fm(O_MLQ, 1024, BF16)
            fm(O_XBC, 1536, F32)
            fm(O_KR, 64, F32)
            fm(O_KR + 32, 32, F32, drow=IN_TOTAL)
            fm(O_KR, 32, F32, drow=IN_TOTAL + 32)
            fm(O_SWQ, 1280, BF16)
            fm(O_GATE, 4 * D, BF16, func=AF.Sigmoid)

    def mlstm(self, l):
        P, S, NT, A, Sc, c = self.P, self.S, self.NT, self.A, self.Sc, self.c
        ps = self.ps
        with P.phase() as st:
            gif = P.sbuf("ml_gif", [128, NT, 8], F32, st)
            P.dma("sp", gif[:], Sc["utm32"][:, O_MLI:O_MLI + 8].rearrange("(t p) c -> p t c", p=128), w=["gif"])
            bi = self.bcast_row(st, "ml_bi", A["ml_igate_bias"][l], 4)
            bf = self.bcast_row(st, "ml_bf", A["ml_fgate_bias"][l], 4)
            nw = self.bcast_row(st, "ml_nw", A["ml_norm"][l], 1024)
            ig = P.sbuf("ml_ig", [128, NT, 4], F32, st)
            lf = P.sbuf("ml_lf", [128, NT, 4], F32, st)
            P.tt("dve", ig[:], gif[:, :, 0:4], bi[:].unsqueeze(1).to_broadcast([128, NT, 4]), ALU.add, ["gif", "ml_bi"], ["ig"])
            P.tt("dve", lf[:], gif[:, :, 4:8], bf[:].unsqueeze(1).to_broadcast([128, NT, 4]), ALU.add, ["gif", "ml_bf"], ["lf"])
            P.act(lf[:], lf[:], AF.Exp, ["lf"], ["lf"], scale=-1.0)
            P.act(lf[:], lf[:], AF.Ln, ["lf"], ["lf"], bias=1.0)
            P.ts("dve", lf[:], lf[:], -1.0, None, ALU.mult, None, ["lf"], ["lf"])
            bcs = P.sbuf("ml_b", [128, NT, 4], F32, st)
            ibm = P.sbuf("ml_ibm", [128, NT, 4], F32, st)
            eb = P.sbuf("ml_eb", [128, NT, 4], F32, st)
            wst = P.sbuf("ml_wst", [128, NT, 4], F32, st)
            ebt = P.sbuf("ml_ebt", [128, NT, 4], F32, st)
            for t in range(NT):
                P.mm(ps[0][:, 0:4], c["tri"][:], lf[:, t, :], True, True, ["lf", "const"], ["ps0"])
                P.mm(ps[0][:, 4:8], c["ones"][:], lf[:, t, :], True, True, ["lf", "const"], ["ps0"])
                P.cp("dve", bcs[:, t, :], ps[0][:, 0:4], ["ps0"], ["bcs"])
                P.cp("dve", ebt[:, t, :], ps[0][:, 4:8], ["ps0"], ["ebt"])
            P.tt("dve", ibm[:], ig[:], bcs[:], ALU.subtract, ["ig", "bcs"], ["ibm"])
            P.tt("dve", wst[:], ibm[:], ebt[:], ALU.add, ["ibm", "ebt"], ["wst"])
            P.act(wst[:], wst[:], AF.Exp, ["wst"], ["wst"])
            P.ts("dve", wst[:], wst[:], 128.0 ** -0.5, None, ALU.mult, None, ["wst"], ["wst"])
            P.act(eb[:], bcs[:], AF.Exp, ["bcs"], ["eb"])
            P.act(ebt[:], ebt[:], AF.Exp, ["ebt"], ["ebt"])

            qTr = Ring(P, st, "ml_qT", 2, [128, S], BF16)
            kTr = Ring(P, st, "ml_kT", 2, [128, S], BF16)
            ktr = Ring(P, st, "ml_kt", 2, [128, NT, 128], BF16)
            var = Ring(P, st, "ml_va", 2, [128, NT, 257], BF16)
            otr = Ring(P, st, "ml_o", 2, [128, NT, 256], F32)
            ltr = Ring(P, st, "ml_lt", 2, [128, 128], F32)
            etr = Ring(P, st, "ml_et", 2, [128, 128], F32)
            ptr_ = Ring(P, st, "ml_pt", 2, [128, 128], BF16)
            kwr = Ring(P, st, "ml_kw", 2, [128, 128], BF16)
            Cst = P.sbuf("ml_C", [128, 257], F32, st)
            Cbr = Ring(P, st, "ml_Cb", 2, [128, 257], BF16)
            tmr = Ring(P, st, "ml_tmp", 2, [128, 257], F32)
            hor = Ring(P, st, "ml_ho", 2, [128, 257], F32)
            smr = Ring(P, st, "ml_sm", 2, [128, 4], F32)
            yr = Ring(P, st, "ml_y", 2, [128, 256], BF16)
            junk = P.sbuf("ml_junk", [128, 256], F32, st)
            for hd in range(4):
                qT, qk = qTr.next()
                kT, kk = kTr.next()
                kt, ktk = ktr.next()
                va, vk = var.next()
                og, ogk = otr.next()
                P.dma("sp", qT[:], Sc["ufm16"][O_MLQ + hd * 128:O_MLQ + (hd + 1) * 128, :], w=[qk])
                P.dma("sp", kT[:], Sc["ufm16"][O_MLK + hd * 128:O_MLK + (hd + 1) * 128, :], w=[kk])
                P.dma("sp", kt[:], Sc["utm16"][:, O_MLK + hd * 128:O_MLK + (hd + 1) * 128].rearrange("(t p) c -> p t c", p=128), w=[ktk])
                P.dma("sp", va[:, :, 0:256], Sc["utm16"][:, O_MLV + hd * 256:O_MLV + (hd + 1) * 256].rearrange("(t p) c -> p t c", p=128), w=[vk])
                P.memset("pool", va[:, :, 256:257], 1.0, [vk])
                P.dma("sp", og[:], Sc["utm32"][:, O_MLO + hd * 256:O_MLO + (hd + 1) * 256].rearrange("(t p) c -> p t c", p=128), w=[ogk])
                P.act(og[:], og[:], AF.Sigmoid, [ogk], [ogk])
                P.memset("dve", Cst[:], 0.0, ["C"])
                Cb, cbk = Cbr.next()
                P.memset("pool", Cb[:], 0.0, [cbk])
                for t in range(NT):
                    sl = slice(t * 128, (t + 1) * 128)
                    lt, ltk = ltr.next()
                    et, etk = etr.next()
                    pt, ptk = ptr_.next()
                    kw, kwk = kwr.next()
                    tmp, tmk = tmr.next()
                    ho, hok = hor.next()
                    sm, smk = smr.next()
                    y, yk = yr.next()
                    P.ts("pool", lt[:], c["tri"][:], lf[:, t, hd:hd + 1], None, ALU.mult, None, ["lf", "const"], [ltk])
                    P.mm(ps[1][:, 0:128], c["ones"][:], lt[:], True, False, [ltk, "const"], ["ps1"])
                    P.mm(ps[1][:, 0:128], c["identf"][:], c["mneg"][:], False, True, ["const"], ["ps1"])
                    P.act(et[:], ps[1][:, 0:128], AF.Exp, ["ps1", "ibm"], [etk], bias=ibm[:, t, hd:hd + 1])
                    P.mm(ps[2][:, 0:128], kT[:, sl], qT[:, sl], True, True, [kk, qk], ["ps2"])
                    P.stt("dve", pt[:], ps[2][:, 0:128], 128.0 ** -0.5, et[:], ALU.mult, ALU.mult, ["ps2", etk], [ptk])
                    P.mm(ps[3][:, 0:257], pt[:], va[:, t, :], True, True, [ptk, vk], ["ps3"])
                    P.mm(ps[4][:, 0:257], qT[:, sl], Cb[:], True, True, [qk, cbk], ["ps4"])
                    P.act(tmp[:], ps[4][:, 0:257], AF.Copy, ["ps4", "eb"], [tmk], scale=eb[:, t, hd:hd + 1])
                    P.tt("dve", ho[:], tmp[:], ps[3][:, 0:257], ALU.add, [tmk, "ps3"], [hok])
                    P.act(sm[:, 0:1], ho[:, 256:257], AF.Abs, [hok], [smk])
                    P.ts("dve", sm[:, 0:1], sm[:, 0:1], 1.0, None, ALU.max, None, [smk], [smk])
                    P.add("dve", lambda e, sm=sm: e.reciprocal(out=sm[:, 0:1], in_=sm[:, 0:1]), [smk], [smk])
                    P.ts("dve", ho[:, 0:256], ho[:, 0:256], sm[:, 0:1], None, ALU.mult, None, [hok, smk], [hok])
                    P.act(junk[:], ho[:, 0:256], AF.Square, [hok], ["ml_junk", smk], accum_out=sm[:, 1:2])
                    P.ts("dve", sm[:, 2:3], sm[:, 1:2], 1.0 / 256, 1e-6, ALU.mult, ALU.add, [smk], [smk])
                    P.act(sm[:, 2:3], sm[:, 2:3], AF.Sqrt, [smk], [smk])
                    P.add("dve", lambda e, sm=sm: e.reciprocal(out=sm[:, 2:3], in_=sm[:, 2:3]), [smk], [smk])
                    P.stt("dve", ho[:, 0:256], ho[:, 0:256], sm[:, 2:3], nw[:, hd * 256:(hd + 1) * 256], ALU.mult, ALU.mult,
                          [hok, smk, "ml_nw"], [hok])
                    P.tt("pool", y[:], ho[:, 0:256], og[:, t, :], ALU.mult, [hok, ogk], [yk])
                    P.dma("sp", Sc["ya"][sl, hd * 256:(hd + 1) * 256], y[:], r=[yk])
                    if t < NT - 1:
                        P.ts("pool", kw[:], kt[:, t, :], wst[:, t, hd:hd + 1], None, ALU.mult, None, [ktk, "wst"], [kwk])
                        P.mm(ps[5][:, 0:257], kw[:], va[:, t, :], True, True, [kwk, vk], ["ps5"])
                        P.stt("dve", Cst[:], Cst[:], ebt[:, t, hd:hd + 1], ps[5][:, 0:257], ALU.mult, ALU.add,
                              ["C", "ebt", "ps5"], ["C"])
                        Cb, cbk = Cbr.next()
                        P.cp("act", Cb[:], Cst[:], ["C"], [cbk])

    def ssd(self, l):
        P, S, NT, A, Sc, c = self.P, self.S, self.NT, self.A, self.Sc, self.c
        ps, pt = self.ps, self.pt
        with P.phase() as st:
            cw = P.sbuf("sd_cw", [128, 12, 4], F32, st)
            cb = P.sbuf("sd_cb", [128, 12], F32, st)
            for j in range(4):
                P.dma("sp", cw[:, :, j], A["ssm_conv_w"][l, j].rearrange("(c p) -> p c", p=128), w=["cw"], allow_slow_non_contiguous=True)
            P.dma("sp", cb[:], A["ssm_conv_b"][l].rearrange("(c p) -> p c", p=128), w=["cb"], allow_slow_non_contiguous=True)
            xr = Ring(P, st, "sd_x", 2, [128, S + 3], F32)
            ar = Ring(P, st, "sd_a", 2, [128, S], F32)
            for ch in range(12):
                x, xk = xr.next()
                a, ak = ar.next()
                P.memset("pool", x[:, 0:3], 0.0, [xk])
                P.dma("sp", x[:, 3:S + 3], Sc["ufm32"][O_XBC + ch * 128:O_XBC + (ch + 1) * 128, :], w=[xk])
                P.ts("dve", a[:], x[:, 3:S + 3], cw[:, ch, 3:4], None, ALU.mult, None, [xk, "cw"], [ak])
                for j in range(3):
                    P.stt("dve", a[:], x[:, j:S + j], cw[:, ch, j:j + 1], a[:], ALU.mult, ALU.add, [xk, "cw", ak], [ak])
                P.act(a[:], a[:], AF.Silu, [ak, "cb"], [ak], bias=cb[:, ch:ch + 1])
                P.dma("sp", Sc["xcT"][ch * 128:(ch + 1) * 128, :], a[:], r=[ak])
        with P.phase() as st:
            dtb = self.bcast_row(st, "sd_dtb", A["ssm_dt_bias"][l], 16)
            alog = self.bcast_row(st, "sd_alog", A["ssm_a_log"][l], 16)
            dsk = self.bcast_row(st, "sd_dsk", A["ssm_d"][l], 16)
            nw = self.bcast_row(st, "sd_nw", A["ssm_norm"][l], 1024)
            P.act(alog[:], alog[:], AF.Exp, ["sd_alog"], ["sd_alog"])
            dt = P.sbuf("sd_dt", [128, NT, 16], F32, st)
            P.dma("sp", dt[:], Sc["utm32"][:, O_DT:O_DT + 16].rearrange("(t p) c -> p t c", p=128), w=["dt"])
            P.tt("dve", dt[:], dt[:], dtb[:].unsqueeze(1).to_broadcast([128, NT, 16]), ALU.add, ["dt", "sd_dtb"], ["dt"])
            P.act(dt[:], dt[:], AF.Exp, ["dt"], ["dt"])
            P.act(dt[:], dt[:], AF.Ln, ["dt"], ["dt"], bias=1.0)
            av = P.sbuf("sd_av", [128, NT, 16], F32, st)
            P.stt("dve", av[:], dt[:], -1.0, alog[:].unsqueeze(1).to_broadcast([128, NT, 16]), ALU.mult, ALU.mult,
                  ["dt", "sd_alog"], ["av"])
            acs = P.sbuf("sd_acs", [128, NT, 16], F32, st)
            nacs = P.sbuf("sd_nacs", [128, NT, 16], F32, st)
            eat = P.sbuf("sd_eat", [128, NT, 16], F32, st)
            ea = P.sbuf("sd_ea", [128, NT, 16], F32, st)
            dsc = P.sbuf("sd_dsc", [128, NT, 16], F32, st)
            for t in range(NT):
                P.mm(ps[0][:, 0:16], c["tri"][:], av[:, t, :], True, True, ["av", "const"], ["ps0"])
                P.mm(ps[0][:, 16:32], c["ones"][:], av[:, t, :], True, True, ["av", "const"], ["ps0"])
                P.cp("dve", acs[:, t, :], ps[0][:, 0:16], ["ps0"], ["acs"])
                P.cp("dve", eat[:, t, :], ps[0][:, 16:32], ["ps0"], ["eat"])
            P.ts("dve", nacs[:], acs[:], -1.0, None, ALU.mult, None, ["acs"], ["nacs"])
            P.tt("dve", dsc[:], eat[:], acs[:], ALU.subtract, ["eat", "acs"], ["dsc"])
            P.act(dsc[:], dsc[:], AF.Exp, ["dsc"], ["dsc"])
            P.tt("dve", dsc[:], dsc[:], dt[:], ALU.mult, ["dsc", "dt"], ["dsc"])
            P.act(ea[:], acs[:], AF.Exp, ["acs"], ["ea"])
            P.act(eat[:], eat[:], AF.Exp, ["eat"], ["eat"])

            xcr = Ring(P, st, "sd_xc", 2, [128, 12, 128], F32)
            xcbr = Ring(P, st, "sd_xcb", 2, [128, 12, 128], BF16)
            xtr = Ring(P, st, "sd_xt", 2, [128, 1280], BF16)
            xsr = Ring(P, st, "sd_xs", 2, [128, 1024], BF16)
            xdr = Ring(P, st, "sd_xd", 2, [128, 1024], BF16)
            cbr = Ring(P, st, "sd_cbT", 2, [128, 2, 128], F32)
            atr = Ring(P, st, "sd_at", 3, [128, 128], F32)
            der = Ring(P, st, "sd_de", 3, [128, 128], F32)
            mtr = Ring(P, st, "sd_mt", 3, [128, 128], BF16)
            zr = Ring(P, st, "sd_z", 2, [128, 1024], F32)
            yr = Ring(P, st, "sd_y", 2, [128, 1024], F32)
            tr_ = Ring(P, st, "sd_t", 2, [128, 1024], F32)
            ybr = Ring(P, st, "sd_yb", 2, [128, 1024], BF16)
            smr = Ring(P, st, "sd_sm", 2, [128, 4], F32)
            junk = P.sbuf("sd_junk", [128, 512], F32, st)
            state = P.sbuf("sd_state", [128, 2, 512], F32, st)
            stbr = Ring(P, st, "sd_stb", 2, [128, 2, 512], BF16)
            P.memset("dve", state[:], 0.0, ["state"])
            stb, stbk = stbr.next()
            P.memset("pool", stb[:], 0.0, [stbk])
            for t in range(NT):
                sl = slice(t * 128, (t + 1) * 128)
                xc, xck = xcr.next()
                xcb, xcbk = xcbr.next()
                xt, xtk = xtr.next()
                xs, xsk = xsr.next()
                xd, xdk = xdr.next()
                cbT, cbTk = cbr.next()
                z, zk = zr.next()
                y, yk = yr.next()
                tm_, tmk = tr_.next()
                yb, ybk = ybr.next()
                sm, smk = smr.next()
                P.dma("sp", xc[:], Sc["xcT"][:, sl].rearrange("(c p) s -> p c s", p=128), w=[xck])
                P.dma("sp", z[:], Sc["utm32"][sl, O_Z:O_Z + 1024], w=[zk])
                P.cp("pool", xcb[:], xc[:], [xck], [xcbk])
                for q in range(3):
                    n4 = 4 if q < 2 else 2
                    pb, pbk = pt[q % 2], ("pt0", "pt1")[q % 2]
                    pv = pb[:, 0:512].rearrange("p (a b) -> p a b", b=128)
                    for j in range(n4):
                        P.tr(pv[:, j, :], xcb[:, q * 4 + j, :], c["ident"][:], [xcbk, "const"], [pbk])
                    P.cp("dve" if q % 2 == 0 else "act", xt[:, q * 512:q * 512 + n4 * 128], pb[:, 0:n4 * 128], [pbk], [xtk])
                x3 = xt[:, 0:1024].rearrange("p (h d) -> p h d", d=64)
                P.tt("dve", xs[:].rearrange("p (h d) -> p h d", d=64), x3, dt[:, t, :].unsqueeze(2).to_broadcast([128, 16, 64]),
                     ALU.mult, [xtk, "dt"], [xsk])
                P.tt("pool", xd[:].rearrange("p (h d) -> p h d", d=64), x3, dsc[:, t, :].unsqueeze(2).to_broadcast([128, 16, 64]),
                     ALU.mult, [xtk, "dsc"], [xdk])
                for g in range(2):
                    P.mm(ps[1][:, g * 128:(g + 1) * 128], xcb[:, 8 + g, :], xcb[:, 10 + g, :], True, True, [xcbk], ["ps1"])
                P.cp("act", cbT[:].rearrange("p a b -> p (a b)"), ps[1][:, 0:256], ["ps1"], [cbTk])
                for g in range(2):
                    P.mm(ps[2 + g][:, 0:512], xcb[:, 10 + g, :], stb[:, g, :], True, True, [xcbk, stbk], [f"ps{2 + g}"])
                for hd in range(16):
                    g = hd // 8
                    at, atk = atr.next()
                    de, dek = der.next()
                    mt, mtk = mtr.next()
                    P.ts("pool", at[:], c["tri"][:], av[:, t, hd:hd + 1], None, ALU.mult, None, ["av", "const"], [atk])
                    P.mm(ps[0][:, 0:128], c["ones"][:], at[:], True, False, [atk, "const"], ["ps0"])
                    P.mm(ps[0][:, 0:128], c["identf"][:], c["mneg"][:], False, True, ["const"], ["ps0"])
                    P.act(de[:], ps[0][:, 0:128], AF.Exp, ["ps0", "nacs"], [dek], bias=nacs[:, t, hd:hd + 1])
                    P.tt("dve", mt[:], de[:], cbT[:, g, :], ALU.mult, [dek, cbTk], [mtk])
                    P.mm(ps[4 + g][:, (hd % 8) * 64:(hd % 8 + 1) * 64], mt[:], xs[:, hd * 64:(hd + 1) * 64], True, True,
                         [mtk, xsk], [f"ps{4 + g}"])
                for g in range(2):
                    P.tt("dve", tm_[:, g * 512:(g + 1) * 512].rearrange("p (h d) -> p h d", d=64),
                         ps[2 + g][:, 0:512].rearrange("p (h d) -> p h d", d=64),
                         ea[:, t, g * 8:(g + 1) * 8].unsqueeze(2).to_broadcast([128, 8, 64]), ALU.mult,
                         [f"ps{2 + g}", "ea"], [tmk])
                    P.tt("dve", y[:, g * 512:(g + 1) * 512], tm_[:, g * 512:(g + 1) * 512], ps[4 + g][:, 0:512], ALU.add,
                         [tmk, f"ps{4 + g}"], [yk])
                P.tt("pool", tm_[:].rearrange("p (h d) -> p h d", d=64), x3, dsk[:].unsqueeze(2).to_broadcast([128, 16, 64]),
                     ALU.mult, [xtk, "sd_dsk", yk], [tmk])
                P.tt("pool", y[:], y[:], tm_[:], ALU.add, [tmk, yk], [yk])
                P.act(z[:], z[:], AF.Silu, [zk], [zk])
                P.tt("dve", y[:], y[:], z[:], ALU.mult, [yk, zk], [yk])
                for g in range(2):
                    P.act(junk[:], y[:, g * 512:(g + 1) * 512], AF.Square, [yk], ["sd_junk", smk], accum_out=sm[:, g:g + 1])
                P.ts("dve", sm[:, 2:4], sm[:, 0:2], 1.0 / 512, 1e-6, ALU.mult, ALU.add, [smk], [smk])
                P.act(sm[:, 2:4], sm[:, 2:4], AF.Sqrt, [smk], [smk])
                P.add("dve", lambda e, sm=sm: e.reciprocal(out=sm[:, 2:4], in_=sm[:, 2:4]), [smk], [smk])
                for g in range(2):
                    P.stt("dve", yb[:, g * 512:(g + 1) * 512], y[:, g * 512:(g + 1) * 512], sm[:, 2 + g:3 + g],
                          nw[:, g * 512:(g + 1) * 512], ALU.mult, ALU.mult, [yk, smk, "sd_nw"], [ybk])
                P.dma("sp", Sc["yb"][sl, :], yb[:], r=[ybk])
                if t < NT - 1:
                    for g in range(2):
                        P.mm(ps[2 + g][:, 0:512], xt[:, 1024 + g * 128:1024 + (g + 1) * 128], xd[:, g * 512:(g + 1) * 512],
                             True, True, [xtk, xdk], [f"ps{2 + g}"])
                        P.tt("dve", state[:, g, :].rearrange("p (h d) -> p h d", d=64),
                             state[:, g, :].rearrange("p (h d) -> p h d", d=64),
                             eat[:, t, g * 8:(g + 1) * 8].unsqueeze(2).to_broadcast([128, 8, 64]), ALU.mult,
                             ["state", "eat"], ["state"])
                        P.tt("dve", state[:, g, :], state[:, g, :], ps[2 + g][:, 0:512], ALU.add, ["state", f"ps{2 + g}"], ["state"])
                    stb, stbk = stbr.next()
                    P.cp("act", stb[:], state[:], ["state"], [stbk])

    def mla(self, l):
        P, S, NT, A, Sc, c = self.P, self.S, self.NT, self.A, self.Sc, self.c
        ps, GW, NG = self.ps, self.GW, self.NG
        scale = 192.0 ** -0.5
        with P.phase() as st:
            cqT = P.sbuf("la_cqT", [128, 4, S], BF16, st)
            ckT = P.sbuf("la_ckT", [128, 2, S], BF16, st)
            with P.phase() as st2:
                self.norm_T(st2, Sc["utm32"][:, O_CQ:O_CQ + 512], A["mla_q_norm"][l], cqT, "cqT", S, 512)
            with P.phase() as st2:
                self.norm_T(st2, Sc["utm32"][:, O_CKV:O_CKV + 256], A["mla_kv_norm"][l], ckT, "ckT", S, 256)
            cos2 = P.sbuf("la_cos", [64, S], F32, st)
            sin2 = P.sbuf("la_sin", [64, S], F32, st)
            P.dma("sp", cos2[:], A["c_cos2"], w=["cos"])
            P.dma("sp", sin2[:], A["c_sin2"], w=["sin"])
            krT = P.sbuf("la_krT", [64, S], BF16, st)
            k1 = P.sbuf("la_k1", [64, S], F32, st)
            k2 = P.sbuf("la_k2", [64, S], F32, st)
            P.dma("sp", k1[:], Sc["ufm32"][O_KR:O_KR + 64, :], w=["k1"])
            P.dma("sp", k2[:], Sc["ufm32"][IN_TOTAL:IN_TOTAL + 64, :], w=["k2"])
            P.tt("dve", k1[:], k1[:], cos2[:], ALU.mult, ["k1", "cos"], ["k1"])
            P.tt("pool", k2[:], k2[:], sin2[:], ALU.mult, ["k2", "sin"], ["k2"])
            P.tt("dve", krT[:], k1[:], k2[:], ALU.add, ["k1", "k2"], ["krT"])
            Wq, Wkv = A["mla_w_uq"][l], A["mla_w_ukv"][l]
            wqr = Ring(P, st, "la_wq", 2, [128, 4, 256], BF16)
            wkr = Ring(P, st, "la_wk", 2, [128, 2, 256], BF16)
            qnr = Ring(P, st, "la_qn", 2, [128, S], BF16)
            qrr = Ring(P, st, "la_qr", 2, [64, S], BF16)
            knr = Ring(P, st, "la_kn", 2, [128, S], BF16)
            var = Ring(P, st, "la_va", 2, [128, NT, 129], BF16)
            t1r = Ring(P, st, "la_t1", 2, [64, GW], F32)
            t2r = Ring(P, st, "la_t2", 2, [64, GW], F32)
            ptr_ = Ring(P, st, "la_pt", 3, [128, GW], BF16)
            yr = Ring(P, st, "la_y", 4, [128, 128], BF16)
            smr = Ring(P, st, "la_sm", 4, [128, 2], F32)
            ycur = {}
            for hd in range(8):
                wq, wqk = wqr.next()
                wk, wkk = wkr.next()
                qn, qnk = qnr.next()
                qr, qrk = qrr.next()
                kn, knk = knr.next()
                va, vak = var.next()
                q0 = hd * 192
                vw = lambda a, b: Wq[:, a:b].rearrange("(c p) n -> p c n", p=128)
                P.dma("pool", wq[:, :, 0:192], vw(q0, q0 + 192), w=[wqk])
                P.dma("pool", wq[:, :, 192:224], vw(q0 + 160, q0 + 192), w=[wqk])
                P.dma("pool", wq[:, :, 224:256], vw(q0 + 128, q0 + 160), w=[wqk])
                P.dma("pool", wk[:], Wkv[:, hd * 256:(hd + 1) * 256].rearrange("(c p) n -> p c n", p=128), w=[wkk])
                cqr = [("cqT", t) for t in range(NT)]
                ckr = [("ckT", t) for t in range(NT)]
                for g in range(NG):
                    gs = slice(g * GW, (g + 1) * GW)
                    for k in range(4):
                        P.mm(ps[0][:, 0:GW], wq[:, k, 0:128], cqT[:, k, gs], k == 0, k == 3, [wqk] + cqr, ["ps0"])
                    P.cp("act", qn[:, gs], ps[0][:, 0:GW], ["ps0"], [qnk])
                    for k in range(4):
                        P.mm(ps[1][0:64, 0:GW], wq[:, k, 128:192], cqT[:, k, gs], k == 0, k == 3, [wqk] + cqr, ["ps1"])
                    for k in range(4):
                        P.mm(ps[2][0:64, 0:GW], wq[:, k, 192:256], cqT[:, k, gs], k == 0, k == 3, [wqk] + cqr, ["ps2"])
                    t1, t1k = t1r.next()
                    t2, t2k = t2r.next()
                    P.tt("dve", t1[:], ps[1][0:64, 0:GW], cos2[:, gs], ALU.mult, ["ps1", "cos"], [t1k])
                    P.tt("dve", t2[:], ps[2][0:64, 0:GW], sin2[:, gs], ALU.mult, ["ps2", "sin"], [t2k])
                    P.tt("pool", qr[:, gs], t1[:], t2[:], ALU.add, [t1k, t2k], [qrk])
                    for k in range(2):
                        P.mm(ps[3][:, 0:GW], wk[:, k, 0:128], ckT[:, k, gs], k == 0, k == 1, [wkk] + ckr, ["ps3"])
                    P.cp("act", kn[:, gs], ps[3][:, 0:GW], ["ps3"], [knk])
                for t in range(NT):
                    for k in range(2):
                        P.mm(ps[4][:, 0:128], ckT[:, k, t * 128:(t + 1) * 128], wk[:, k, 128:256], k == 0, k == 1,
                             [wkk, ("ckT", t)], ["ps4"])
                    P.cp("dve", va[:, t, 0:128], ps[4][:, 0:128], ["ps4"], [vak])
                P.memset("pool", va[:, :, 128:129], 1.0, [vak])
                TPG = GW // 128
                for g in range(NG):
                    gs0 = g * GW
                    nkt = (g + 1) * TPG
                    for j in range(nkt):
                        r0 = max(0, j - g * TPG)
                        c0 = r0 * 128
                        pt, ptk = ptr_.next()
                        sb = ps[j % 2]
                        sbk = f"ps{j % 2}"
                        P.mm(sb[:, c0:GW], kn[:, j * 128:(j + 1) * 128], qn[:, gs0 + c0:gs0 + GW], True, False, [knk, qnk], [sbk])
                        P.mm(sb[:, c0:GW], krT[:, j * 128:(j + 1) * 128], qr[:, gs0 + c0:gs0 + GW], False, True, ["krT", qrk], [sbk])
                        P.act(pt[:, c0:GW], sb[:, c0:GW], AF.Exp, [sbk], [ptk], scale=scale)
                        if j >= g * TPG:
                            P.tt("pool", pt[:, c0:c0 + 128], pt[:, c0:c0 + 128], c["trib"][:], ALU.mult, [ptk, "const"], [ptk])
                        for r in range(r0, TPG):
                            qi = g * TPG + r
                            P.mm(ps[2 + r][:, 0:129], pt[:, r * 128:(r + 1) * 128], va[:, j, :], j == 0, j == qi,
                                 [ptk, vak], [f"ps{2 + r}"])
                            if j == qi:
                                y, yk = yr.next()
                                sm, smk = smr.next()
                                P.add("dve", lambda e, sm=sm, r=r: e.reciprocal(out=sm[:, 0:1], in_=ps[2 + r][:, 128:129]),
                                      [f"ps{2 + r}"], [smk])
                                P.ts("dve", y[:], ps[2 + r][:, 0:128], sm[:, 0:1], None, ALU.mult, None,
                                     [f"ps{2 + r}", smk], [yk])
                                P.dma("sp", Sc["yc"][qi * 128:(qi + 1) * 128, hd * 128:(hd + 1) * 128], y[:], r=[yk])

    def swa(self, l):
        P, S, NT, A, Sc, c = self.P, self.S, self.NT, self.A, self.Sc, self.c
        ps = self.ps
        scale = 64.0 ** -0.5
        with P.phase() as st:
            sk_ = self.bcast_row(st, "sw_sink", A["swa_sinks"][l], 16)
            P.act(sk_[:], sk_[:], AF.Exp, ["sw_sink"], ["sw_sink"])
            qr = Ring(P, st, "sw_q", 2, [64, 4, S], BF16)
            kr = Ring(P, st, "sw_k", 2, [64, S], BF16)
            var = Ring(P, st, "sw_va", 2, [128, NT, 65], BF16)
            ptr_ = Ring(P, st, "sw_pt", 4, [128, 512], BF16)
            yr = Ring(P, st, "sw_y", 3, [128, 256], BF16)
            smr = Ring(P, st, "sw_sm", 3, [128, 4], F32)
            for g in range(4):
                q, qk = qr.next()
                k, kk = kr.next()
                va, vak = var.next()
                P.dma("sp", q[:], Sc["ufm16"][O_SWQ + g * 256:O_SWQ + (g + 1) * 256, :].rearrange("(r p) s -> p r s", p=64), w=[qk])
                P.dma("sp", k[:], Sc["ufm16"][O_SWK + g * 64:O_SWK + (g + 1) * 64, :], w=[kk])
                P.dma("sp", va[:, :, 0:64], Sc["utm16"][:, O_SWV + g * 64:O_SWV + (g + 1) * 64].rearrange("(t p) c -> p t c", p=128), w=[vak])
                P.memset("pool", va[:, :, 64:65], 1.0, [vak])
                for n in range(NT):
                    sl = slice(n * 128, (n + 1) * 128)
                    pc, pck = ptr_.next()
                    P.mm(ps[0][:, 0:512].rearrange("p (r s) -> p r s", r=4), k[:, sl], q[:, :, sl], True, True, [kk, qk], ["ps0"])
                    P.act(pc[:], ps[0][:, 0:512], AF.Exp, ["ps0"], [pck], scale=scale)
                    P.tt("pool", pc[:].rearrange("p (r s) -> p r s", r=4), pc[:].rearrange("p (r s) -> p r s", r=4),
                         c["trib"][:].unsqueeze(1).to_broadcast([128, 4, 128]), ALU.mult, [pck, "const"], [pck])
                    if n > 0:
                        pp, ppk = ptr_.next()
                        psl = slice((n - 1) * 128, n * 128)
                        P.mm(ps[1][:, 0:512].rearrange("p (r s) -> p r s", r=4), k[:, psl], q[:, :, sl], True, True, [kk, qk], ["ps1"])
                        P.act(pp[:], ps[1][:, 0:512], AF.Exp, ["ps1"], [ppk], scale=scale)
                        P.tt("pool", pp[:].rearrange("p (r s) -> p r s", r=4), pp[:].rearrange("p (r s) -> p r s", r=4),
                             c["lowb"][:].unsqueeze(1).to_broadcast([128, 4, 128]), ALU.mult, [ppk, "const"], [ppk])
                    ob = ps[2 + (n % 2)]
                    obk = f"ps{2 + (n % 2)}"
                    for r in range(4):
                        if n > 0:
                            P.mm(ob[:, r * 65:(r + 1) * 65], pp[:, r * 128:(r + 1) * 128], va[:, n - 1, :], True, False, [ppk, vak], [obk])
                        P.mm(ob[:, r * 65:(r + 1) * 65], pc[:, r * 128:(r + 1) * 128], va[:, n, :], n == 0, True, [pck, vak], [obk])
                    y, yk = yr.next()
                    sm, smk = smr.next()
                    o3 = ob[:, 0:260].rearrange("p (r d) -> p r d", d=65)
                    P.tt("dve", sm[:].unsqueeze(2), o3[:, :, 64:65], sk_[:, g * 4:(g + 1) * 4].unsqueeze(2), ALU.add, [obk, "sw_sink"], [smk])
                    P.add("dve", lambda e, sm=sm: e.reciprocal(out=sm[:], in_=sm[:]), [smk], [smk])
                    P.tt("dve", y[:].rearrange("p (r d) -> p r d", d=64), o3[:, :, 0:64], sm[:].unsqueeze(2).to_broadcast([128, 4, 64]),
                         ALU.mult, [obk, smk], [yk])
                    P.dma("sp", Sc["yd"][sl, g * 256:(g + 1) * 256], y[:], r=[yk])

    def merge(self, l):
        P, S, NT, A, Sc, c = self.P, self.S, self.NT, self.A, self.Sc, self.c
        ps, pt, GW, NG = self.ps, self.pt, self.GW, self.NG
        TPG = GW // 128
        with P.phase() as st:
            yTr = [P.sbuf(f"mg_yT{n}", [128, 8, GW], BF16, st) for n in range(4)]
            ytr = Ring(P, st, "mg_yt", 3, [128, 1024], BF16)
            wbr = Ring(P, st, "mg_wb", 2, [128, 8, 512], BF16)
            wor = Ring(P, st, "mg_wo", 2, [128, 16, 512], BF16)
            gtr = Ring(P, st, "mg_g", 3, [128, GW], BF16)
            tmr = Ring(P, st, "mg_tmp", 2, [128, GW], F32)
            mT = P.sbuf("mg_mT", [128, 16, GW], F32, st)
            mTb = P.sbuf("mg_mTb", [128, 16, GW], BF16, st)
            hr = Ring(P, st, "mg_h", 3, [128, 512], F32)
            ptr = KeyRing(pt, ["pt0", "pt1"])
            for g in range(NG):
                gs = slice(g * GW, (g + 1) * GW)
                for n, nm in enumerate("abcd"):
                    for tt_ in range(TPG):
                        t = g * TPG + tt_
                        yt, ytk = ytr.next()
                        P.dma("sp", yt[:], Sc["y" + nm][t * 128:(t + 1) * 128, :], w=[ytk])
                        for q in range(2):
                            pb, pbk = ptr.next()
                            pv = pb[:, 0:512].rearrange("p (a b) -> p a b", b=128)
                            for j in range(4):
                                P.tr(pv[:, j, :], yt[:, (q * 4 + j) * 128:(q * 4 + j + 1) * 128], c["ident"][:], [ytk, "const"], [pbk])
                            P.cp("dve" if q == 0 else "act", yTr[n][:, q * 4:(q + 1) * 4, tt_ * 128:(tt_ + 1) * 128], pv[:, 0:4, :],
                                 [pbk], [("yT", n)])
                for n in range(4):
                    for dcb in range(4):
                        wb, wbk = wbr.next()
                        P.dma("pool", wb[:], A["w_branch"][l, n][:, dcb * 512:(dcb + 1) * 512].rearrange("(c p) n -> p c n", p=128), w=[wbk])
                        for dc in range(4):
                            dch = dcb * 4 + dc
                            pb, pbk = ps[dc % 4], f"ps{dc % 4}"
                            for k in range(8):
                                P.mm(pb[:, 0:GW], wb[:, k, dc * 128:(dc + 1) * 128], yTr[n][:, k, :], k == 0, k == 7,
                                     [wbk, ("yT", n)], [pbk])
                            gt, gtk = gtr.next()
                            r0 = O_GATE + n * D + dch * 128
                            P.dma("sp", gt[:], Sc["ufm16"][r0:r0 + 128, gs], w=[gtk])
                            if n == 0:
                                P.tt("dve", mT[:, dch, :], pb[:, 0:GW], gt[:], ALU.mult, [pbk, gtk], [("mT", dch)])
                            else:
                                tmp, tmk = tmr.next()
                                P.tt("dve", tmp[:], pb[:, 0:GW], gt[:], ALU.mult, [pbk, gtk], [tmk])
                                P.tt("pool", mT[:, dch, :], mT[:, dch, :], tmp[:], ALU.add, [("mT", dch), tmk], [("mT", dch)])
                for dch in range(16):
                    P.cp("act", mTb[:, dch, :], mT[:, dch, :], [("mT", dch)], [("mTb", dch)])
                mr = [("mTb", d_) for d_ in range(16)]
                for ocb in range(4):
                    wo, wok = wor.next()
                    P.dma("pool", wo[:], A["w_out"][l][:, ocb * 512:(ocb + 1) * 512].rearrange("(c p) n -> p c n", p=128), w=[wok])
                    for tt_ in range(TPG):
                        t = g * TPG + tt_
                        pb, pbk = ps[4 + tt_ % 2], f"ps{4 + tt_ % 2}"
                        ht, hk = hr.next()
                        P.dma("sp", ht[:], Sc["h"][t * 128:(t + 1) * 128, ocb * 512:(ocb + 1) * 512], w=[hk])
                        for k in range(16):
                            P.mm(pb[:, 0:512], mTb[:, k, tt_ * 128:(tt_ + 1) * 128], wo[:, k, :], k == 0, k == 15, [wok] + mr, [pbk])
                        P.tt("dve", ht[:], ht[:], pb[:, 0:512], ALU.add, [hk, pbk], [hk])
                        P.dma("sp", Sc["h"][t * 128:(t + 1) * 128, ocb * 512:(ocb + 1) * 512], ht[:], r=[hk])

    def xattn(self, l):
        P, S, NT, A, Sc, c = self.P, self.S, self.NT, self.A, self.Sc, self.c
        ps, pt, GW, NG = self.ps, self.pt, self.GW, self.NG
        scale = 128.0 ** -0.5
        with P.phase() as st:
            hnT = P.sbuf("xa_hnT", [128, KC, S], BF16, st)
            mnT = P.sbuf("xa_mnT", [128, KC, MEM], BF16, st)
            with P.phase() as st2:
                self.norm_T(st2, Sc["h"], A["norm_cross"][l], hnT, "hnT", S, D)
            with P.phase() as st2:
                self.norm_T(st2, A["mem"], A["norm_mem"][l], mnT, "mnT", MEM, D)
            wkv = P.sbuf("xa_wkv", [128, KC, 1024], BF16, st)
            wq = P.sbuf("xa_wq", [128, KC, 512], BF16, st)
            wo = P.sbuf("xa_wo", [128, 4, D], BF16, st)
            rr = lambda ap: ap.rearrange("(c p) n -> p c n", p=128)
            P.dma("pool", wkv[:, :, 0:512], rr(A["xa_wkv"][l][:, 0:512]), w=["wkv"])
            P.dma("pool", wkv[:, :, 512:1024], rr(A["xa_wkv"][l][:, 512:1024]), w=["wkv"])
            P.dma("pool", wq[:], rr(A["xa_wq"][l]), w=["wq"])
            for q in range(4):
                P.dma("pool", wo[:, :, q * 512:(q + 1) * 512], rr(A["xa_wo"][l][:, q * 512:(q + 1) * 512]), w=["wo"])
            KT = P.sbuf("xa_KT", [128, 4, MEM], BF16, st)
            VA = P.sbuf("xa_VA", [128, 2, 4, 129], BF16, st)
            QT = P.sbuf("xa_QT", [128, 4, S], BF16, st)
            mnr = [("mnT", t) for t in range(MEM // 128)]
            hnr = [("hnT", t) for t in range(NT)]
            for hd in range(4):
                for k in range(KC):
                    P.mm(ps[0][:, 0:MEM], wkv[:, k, hd * 128:(hd + 1) * 128], mnT[:, k, :], k == 0, k == KC - 1, ["wkv"] + mnr, ["ps0"])
                P.cp("act", KT[:, hd, :], ps[0][:, 0:MEM], ["ps0"], ["KT"])
            for mt in range(2):
                for k in range(KC):
                    P.mm(ps[1][:, 0:512], mnT[:, k, mt * 128:(mt + 1) * 128], wkv[:, k, 512:1024], k == 0, k == KC - 1, ["wkv"] + mnr, ["ps1"])
                P.cp("dve", VA[:, mt, :, 0:128], ps[1][:, 0:512].rearrange("p (h d) -> p h d", d=128), ["ps1"], ["VA"])
            P.memset("pool", VA[:, :, :, 128:129], 1.0, ["VA"])
            for hd in range(4):
                for g in range(NG):
                    pb, pbk = ps[2 + g % 2], f"ps{2 + g % 2}"
                    for k in range(KC):
                        P.mm(pb[:, 0:GW], wq[:, k, hd * 128:(hd + 1) * 128], hnT[:, k, g * GW:(g + 1) * GW], k == 0, k == KC - 1,
                             ["wq"] + hnr, [pbk])
                    P.cp("act" if g % 2 == 0 else "dve", QT[:, hd, g * GW:(g + 1) * GW], pb[:, 0:GW], [pbk], ["QT"])
            ptr_ = Ring(P, st, "xa_pt", 4, [128, GW], BF16)
            oTr = Ring(P, st, "xa_oT", 2, [128, 4, 128], BF16)
            smr = Ring(P, st, "xa_sm", 4, [128, 1], F32)
            hr = Ring(P, st, "xa_h", 2, [128, D], F32)
            TPG = GW // 128
            for g in range(NG):
                gs = slice(g * GW, (g + 1) * GW)
                pts = {}
                for hd in range(4):
                    for mt in range(2):
                        pb, pbk = ps[mt], f"ps{mt}"
                        p_, pk_ = ptr_.next()
                        P.mm(pb[:, 0:GW], KT[:, hd, mt * 128:(mt + 1) * 128], QT[:, hd, gs], True, True, ["KT", "QT"], [pbk])
                        P.act(p_[:], pb[:, 0:GW], AF.Exp, [pbk], [pk_], scale=scale)
                        pts[(hd % 2, mt)] = (p_, pk_)
                    for tt_ in range(TPG):
                        t = g * TPG + tt_
                        ob, obk = ps[2 + tt_ % 2], f"ps{2 + tt_ % 2}"
                        for mt in range(2):
                            p_, pk_ = pts[(hd % 2, mt)]
                            P.mm(ob[:, 0:129], p_[:, tt_ * 128:(tt_ + 1) * 128], VA[:, mt, hd, :], mt == 0, mt == 1, [pk_, "VA"], [obk])
                        sm, smk = smr.next()
                        o_, ok_ = self._xa_o(st, t)
                        P.add("dve", lambda e, sm=sm, ob=ob: e.reciprocal(out=sm[:, 0:1], in_=ob[:, 128:129]), [obk], [smk])
                        P.ts("dve", o_[:, hd * 128:(hd + 1) * 128], ob[:, 0:128], sm[:, 0:1], None, ALU.mult, None, [obk, smk], [(ok_, hd)])
                for tt_ in range(TPG):
                    t = g * TPG + tt_
                    o_, ok_ = self._xa_o(st, t)
                    oT, oTk = oTr.next()
                    pb, pbk = pt[tt_ % 2], f"pt{tt_ % 2}"
                    pv = pb[:, 0:512].rearrange("p (a b) -> p a b", b=128)
                    for j in range(4):
                        P.tr(pv[:, j, :], o_[:, j * 128:(j + 1) * 128], c["ident"][:], [(ok_, j), "const"], [pbk])
                    P.cp("act", oT[:], pv[:, 0:4, :], [pbk], [oTk])
                    ht, hk = hr.next()
                    P.dma("sp", ht[:], Sc["h"][t * 128:(t + 1) * 128, :], w=[hk])
                    for ocb in range(4):
                        ob, obk = ps[4 + ocb % 2], f"ps{4 + ocb % 2}"
                        for k in range(4):
                            P.mm(ob[:, 0:512], oT[:, k, :], wo[:, k, ocb * 512:(ocb + 1) * 512], k == 0, k == 3, [oTk, "wo"], [obk])
                        P.tt("dve", ht[:, ocb * 512:(ocb + 1) * 512], ht[:, ocb * 512:(ocb + 1) * 512], ob[:, 0:512], ALU.add, [hk, obk], [hk])
                    P.dma("sp", Sc["h"][t * 128:(t + 1) * 128, :], ht[:], r=[hk])

    def _xa_o(self, st, t):
        if not hasattr(self, "_xao") or self._xao_st is not st:
            self._xao = {}
            self._xao_st = st
        if t not in self._xao:
            self.P.uid += 1
            self._xao[t] = (self.P.sbuf("xa_ot", [128, 512], BF16, st), f"xa_ot#{self.P.uid}")
        return self._xao[t]

    def ffn_layer(self, l):
        P, S, NT, A, Sc, c = self.P, self.S, self.NT, self.A, self.Sc, self.c
        ps, pt, GW, NG = self.ps, self.pt, self.GW, self.NG
        TPG = GW // 128
        moe = (l % 2 == 1)
        FC = FFN // 128
        with P.phase() as st:
            hacc = P.sbuf("ff_hacc", [128, TPG, D], F32, st)
            hnT = P.sbuf("ff_hnT", [128, KC, GW], BF16, st)
            actT = P.sbuf("ff_actT", [128, FC, GW], BF16, st)
            w13r = Ring(P, st, "ff_w13", 2, [128, KC, 2, 128], BF16)
            w2r = Ring(P, st, "ff_w2", 2, [128, 8, 512], BF16)
            sir = Ring(P, st, "ff_si", 2, [128, GW], F32)
            gcol = P.sbuf("ff_gcol", [128, KC], F32, st)
            P.dma("sp", gcol[:], A["norm_ffn"][l].rearrange("(c p) -> p c", p=128), w=["gcol"], allow_slow_non_contiguous=True)
            hbr = Ring(P, st, "ff_hb", 2, [128, D], BF16)
            junk = P.sbuf("ff_junk", [128, D], BF16, st)
            ssr = Ring(P, st, "ff_ss", 2, [128, 2], F32)
            if moe:
                hn32 = Ring(P, st, "ff_hn32", 1, [128, D], F32)
                hnT32 = P.sbuf("ff_hnT32", [128, KC, 128], F32, st)
                rt = P.sbuf("ff_rt", [128, KC, NEXP], F32, st)
                P.dma("sp", rt[:], A["moe_router"][0].rearrange("(c p) e -> p c e", p=128), w=["rt"])
                gates = P.sbuf("ff_gates", [128, TPG, NEXP], F32, st)
                lgr = Ring(P, st, "ff_lg", 2, [128, 24], F32)
            for g in range(NG):
                for tt_ in range(TPG):
                    t = g * TPG + tt_
                    hb, hbk = hbr.next()
                    ss, sk = ssr.next()
                    P.dma("sp", hacc[:, tt_, :], Sc["h"][t * 128:(t + 1) * 128, :], w=[("hacc", tt_)])
                    P.act(junk[:], hacc[:, tt_, :], AF.Square, [("hacc", tt_)], ["ff_junk", sk], accum_out=ss[:, 0:1])
                    P.ts("dve", ss[:, 1:2], ss[:, 0:1], 1.0 / D, 1e-6, ALU.mult, ALU.add, [sk], [sk])
                    P.act(ss[:, 1:2], ss[:, 1:2], AF.Sqrt, [sk], [sk])
                    P.add("dve", lambda e, ss=ss: e.reciprocal(out=ss[:, 1:2], in_=ss[:, 1:2]), [sk], [sk])
                    P.ts("pool", hb[:], hacc[:, tt_, :], ss[:, 1:2], None, ALU.mult, None, [("hacc", tt_), sk], [hbk])
                    for c4 in range(0, KC, 4):
                        pb, pbk = pt[(c4 // 4) % 2], f"pt{(c4 // 4) % 2}"
                        pv = pb[:, 0:512].rearrange("p (a b) -> p a b", b=128)
                        for j in range(4):
                            P.tr(pv[:, j, :], hb[:, (c4 + j) * 128:(c4 + j + 1) * 128], c["ident"][:], [hbk, "const"], [pbk])
                        P.tt("dve", hnT[:, c4:c4 + 4, tt_ * 128:(tt_ + 1) * 128], pv[:, 0:4, :],
                             gcol[:, c4:c4 + 4].unsqueeze(2).to_broadcast([128, 4, 128]), ALU.mult, [pbk, "gcol"], ["hnT"])
                    if moe:
                        h32, h32k = hn32.next()
                        lg, lgk = lgr.next()
                        P.ts("pool", h32[:], hacc[:, tt_, :], ss[:, 1:2], None, ALU.mult, None, [("hacc", tt_), sk], [h32k])
                        for c4 in range(0, KC, 4):
                            pb, pbk = ps[(c4 // 4) % 2], f"ps{(c4 // 4) % 2}"
                            pv = pb[:, 0:512].rearrange("p (a b) -> p a b", b=128)
                            for j in range(4):
                                P.tr(pv[:, j, :], h32[:, (c4 + j) * 128:(c4 + j + 1) * 128], c["identf"][:], [h32k, "const"], [pbk])
                            P.tt("dve", hnT32[:, c4:c4 + 4, :], pv[:, 0:4, :],
                                 gcol[:, c4:c4 + 4].unsqueeze(2).to_broadcast([128, 4, 128]), ALU.mult, [pbk, "gcol"], ["hnT32"])
                        for k in range(KC):
                            P.mm(ps[2][:, 0:NEXP], hnT32[:, k, :], rt[:, k, :], k == 0, k == KC - 1, ["hnT32", "rt"], ["ps2"])
                        P.cp("dve", lg[:, 0:8], ps[2][:, 0:NEXP], ["ps2"], [lgk])
                        P.add("dve", lambda e, lg=lg: e.max(out=lg[:, 8:16], in_=lg[:, 0:8]), [lgk], [lgk])
                        P.ts("dve", lg[:, 16:24], lg[:, 0:8], lg[:, 8:9], None, ALU.subtract, None, [lgk], [lgk])
                        P.act(lg[:, 16:24], lg[:, 16:24], AF.Exp, [lgk], [lgk])
                        P.ts("dve", lg[:, 0:8], lg[:, 0:8], lg[:, 9:10], None, ALU.is_ge, None, [lgk], [lgk])
                        P.tt("dve", lg[:, 16:24], lg[:, 16:24], lg[:, 0:8], ALU.mult, [lgk], [lgk])
                        P.add("dve", lambda e, lg=lg: e.reduce_sum(out=lg[:, 10:11], in_=lg[:, 16:24], axis=AX.X), [lgk], [lgk])
                        P.add("dve", lambda e, lg=lg: e.reciprocal(out=lg[:, 10:11], in_=lg[:, 10:11]), [lgk], [lgk])
                        P.ts("dve", gates[:, tt_, :], lg[:, 16:24], lg[:, 10:11], None, ALU.mult, None, [lgk], [("gates", tt_)])
                for e_ in range(NEXP if moe else 1):
                    W13 = A["moe_w13"][0, e_] if moe else A["ffn_w13"][0]
                    W2 = A["moe_w2"][0, e_] if moe else A["ffn_w2"][0]
                    for fb in range(0, FC, 1):
                        w13, w13k = w13r.next()
                        P.dma("pool", w13[:, :, 0, :], W13[:, fb * 128:(fb + 1) * 128].rearrange("(c p) n -> p c n", p=128), w=[w13k])
                        P.dma("pool", w13[:, :, 1, :], W13[:, FFN + fb * 128:FFN + (fb + 1) * 128].rearrange("(c p) n -> p c n", p=128), w=[w13k])
                        for f2 in range(1):
                            f = fb + f2
                            for k in range(KC):
                                P.mm(ps[0][:, 0:GW], w13[:, k, 0, f2 * 128:(f2 + 1) * 128], hnT[:, k, :], k == 0, k == KC - 1, [w13k, "hnT"], ["ps0"])
                            for k in range(KC):
                                P.mm(ps[1][:, 0:GW], w13[:, k, 1, f2 * 128:(f2 + 1) * 128], hnT[:, k, :], k == 0, k == KC - 1, [w13k, "hnT"], ["ps1"])
                            si, sik = sir.next()
                            P.act(si[:], ps[0][:, 0:GW], AF.Silu, ["ps0"], [sik])
                            P.tt("dve", actT[:, f, :], si[:], ps[1][:, 0:GW], ALU.mult, [sik, "ps1"], [("actT", f)])
                    ar = [("actT", f) for f in range(FC)]
                    for dcb in range(4):
                        for f8 in range(0, FC, 8):
                            w2, w2k = w2r.next()
                            P.dma("pool", w2[:], W2[f8 * 128:(f8 + 8) * 128, dcb * 512:(dcb + 1) * 512].rearrange("(c p) n -> p c n", p=128), w=[w2k])
                            for f in range(8):
                                for tt_ in range(TPG):
                                    P.mm(ps[2 + tt_][:, 0:512], actT[:, f8 + f, tt_ * 128:(tt_ + 1) * 128], w2[:, f, :],
                                         f8 + f == 0, f8 + f == FC - 1, [w2k] + ar, [f"ps{2 + tt_}"])
                        for tt_ in range(TPG):
                            hs = hacc[:, tt_, dcb * 512:(dcb + 1) * 512]
                            if moe:
                                P.stt("dve", hs, ps[2 + tt_][:, 0:512], gates[:, tt_, e_:e_ + 1], hs, ALU.mult, ALU.add,
                                      [f"ps{2 + tt_}", ("gates", tt_), ("hacc", tt_)], [("hacc", tt_)])
                            else:
                                P.tt("dve", hs, hs, ps[2 + tt_][:, 0:512], ALU.add, [f"ps{2 + tt_}", ("hacc", tt_)], [("hacc", tt_)])
                for tt_ in range(TPG):
                    t = g * TPG + tt_
                    P.dma("sp", Sc["h"][t * 128:(t + 1) * 128, :], hacc[:, tt_, :], r=[("hacc", tt_)])


def make_consts(S):
    bf = ml_dtypes.bfloat16
    i = np.arange(128)
    tri = (i[:, None] <= i[None, :]).astype(np.float32)
    inv = 1.0 / (10000.0 ** (np.arange(0, 64, 2, dtype=np.float32) / 64.0))
    ang = np.arange(S, dtype=np.float32)[:, None] * inv[None, :]
    cos, sin = np.cos(ang).astype(np.float32).T, np.sin(ang).astype(np.float32).T
    return dict(
        c_ident=np.eye(128, dtype=np.float32).astype(bf), c_identf=np.eye(128, dtype=np.float32),
        c_tri=tri, c_trib=tri.astype(bf), c_lowb=(1.0 - tri).astype(bf),
        c_mneg=((1.0 - tri) * NEG).astype(np.float32), c_ones=np.ones((128, 128), np.float32),
        c_cos2=np.ascontiguousarray(np.concatenate([cos, cos], 0)),
        c_sin2=np.ascontiguousarray(np.concatenate([-sin, sin], 0)),
    )


_CACHE = {}


def kernel(**inputs):
    S = inputs["x"].shape[1]
    B = inputs["x"].shape[0]
    if S not in _CACHE:
        _CACHE[S] = Builder(S).build()
    nc = _CACHE[S]
    consts = make_consts(S)
    shared = {k: np.ascontiguousarray(v) for k, v in inputs.items() if k not in ("x", "mem")}
    in_maps = []
    for b in range(B):
        m = dict(shared)
        m.update(consts)
        m["x"] = np.ascontiguousarray(inputs["x"][b])
        m["mem"] = np.ascontiguousarray(inputs["mem"][b])
        in_maps.append(m)
    res = run_bass_kernel_spmd(nc, in_maps, core_ids=list(range(B)))
    return np.stack([r["out"] for r in res.results], axis=0).astype(np.float32)
```

```python
import contextlib
import math
import numpy as np
import ml_dtypes
import concourse.bass as bass
import concourse.mybir as mybir
from concourse.bass_utils import run_bass_kernel_spmd

F32 = mybir.dt.float32
BF16 = mybir.dt.bfloat16
AF = mybir.ActivationFunctionType
ALU = mybir.AluOpType
AX = mybir.AxisListType

EPOCH = 4000
N_DMA_SEMS = 40

D = 2048
KC = D // 128
IN_TOTAL = 16216
FFN = 7168
NEXP = 8
MEM = 256
O_MLQ, O_MLK, O_MLV, O_MLO, O_MLI, O_MLF = 0, 512, 1024, 2048, 3072, 3076
O_Z, O_XBC, O_DT, O_CQ, O_CKV, O_KR = 3080, 4104, 5640, 5656, 6168, 6424
O_SWQ, O_SWK, O_SWV, O_GATE = 6488, 7512, 7768, 8024
NEG = -30000.0


class Prog:
    ENGS = ("pe", "act", "dve", "pool", "sp")

    def __init__(self, nc):
        self.nc = nc
        self.ops = []
        self.last_w = {}
        self.readers = {}
        self.stack = contextlib.ExitStack()
        self.nd = 0
        self.last_on_eng = {}
        self.last_dma_slot = {}
        self.uid = 0

    def sbuf(self, name, shape, dtype, stack=None):
        self.uid += 1
        return (stack or self.stack).enter_context(
            self.nc.sbuf_tensor(f"{name}_{self.uid}", list(shape), dtype))

    def psum(self, name, shape, dtype):
        return self.stack.enter_context(self.nc.psum_tensor(name, list(shape), dtype))

    def dram(self, name, shape, dtype, kind="Internal"):
        return self.nc.dram_tensor(name, list(shape), dtype, kind=kind).ap()

    def add(self, eng, emit, reads=(), writes=(), dma=False):
        idx = len(self.ops)
        raw, other = set(), set()
        for r in reads:
            w = self.last_w.get(r)
            if w is not None:
                raw.add(w)
        for r in writes:
            w = self.last_w.get(r)
            if w is not None:
                other.add(w)
            for rd in self.readers.get(r, ()):
                other.add(rd)
        for r in reads:
            lst = self.readers.setdefault(r, [])
            if not dma:
                lst[:] = [j for j in lst if self.ops[j]["dma"] or self.ops[j]["eng"] != eng]
            lst.append(idx)
        for r in writes:
            self.last_w[r] = idx
            self.readers[r] = []
        raw.discard(idx)
        other.discard(idx)
        other -= raw
        op = dict(eng=eng, emit=emit, raw=raw, other=other, dma=dma, need_inc=False,
                  ticket=None, explicit=None)
        if dma:
            op["slot"] = self.nd % N_DMA_SEMS
            self.nd += 1
            self.last_dma_slot[op["slot"]] = idx
        self.last_on_eng[eng] = idx
        self.ops.append(op)
        return idx

    def barrier(self):
        targets = set(self.last_on_eng.values()) | set(self.last_dma_slot.values())
        for e in self.ENGS:
            self.ops.append(dict(eng=e, emit=None, raw=set(), other=set(), dma=False,
                                 need_inc=False, ticket=None, explicit=set(targets)))
        self.last_w = {}
        self.readers = {}

    @contextlib.contextmanager
    def phase(self):
        st = contextlib.ExitStack()
        try:
            yield st
        finally:
            self.barrier()
            st.close()

    def dma(self, eng, out, in_, r=(), w=(), **kw):
        return self.add(eng, lambda e: e.dma_start(out=out, in_=in_, **kw), r, w, dma=True)

    def mm(self, out, lhsT, rhs, start, stop, r, w):
        return self.add("pe", lambda e: e.matmul(out=out, lhsT=lhsT, rhs=rhs, start=start, stop=stop), r, w)

    def tr(self, out, in_, ident, r, w):
        return self.add("pe", lambda e: e.transpose(out=out, in_=in_, identity=ident), r, w)

    def act(self, out, in_, func, r, w, **kw):
        return self.add("act", lambda e: e.activation(out=out, in_=in_, func=func, **kw), r, w)

    def ts(self, eng, out, in0, s1, s2, op0, op1, r, w):
        if op1 is None:
            return self.add(eng, lambda e: e.tensor_scalar(out=out, in0=in0, scalar1=s1, scalar2=None, op0=op0), r, w)
        return self.add(eng, lambda e: e.tensor_scalar(out=out, in0=in0, scalar1=s1, scalar2=s2, op0=op0, op1=op1), r, w)

    def tt(self, eng, out, in0, in1, op, r, w):
        return self.add(eng, lambda e: e.tensor_tensor(out=out, in0=in0, in1=in1, op=op), r, w)

    def stt(self, eng, out, in0, scalar, in1, op0, op1, r, w):
        return self.add(eng, lambda e: e.scalar_tensor_tensor(out=out, in0=in0, scalar=scalar, in1=in1,
                                                              op0=op0, op1=op1), r, w)

    def cp(self, eng, out, in_, r, w):
        if eng == "act":
            return self.add(eng, lambda e: e.copy(out=out, in_=in_), r, w)
        return self.add(eng, lambda e: e.tensor_copy(out=out, in_=in_), r, w)

    def memset(self, eng, ap, val, w):
        return self.add(eng, lambda e: e.memset(ap, val), (), w)

    def emit_all(self):
        nc, ops = self.nc, self.ops
        self.barrier()
        for i, op in enumerate(ops):
            if op["explicit"] is not None:
                deps = [j for j in op["explicit"] if j < i]
            else:
                deps = []
                for j in op["raw"]:
                    pj = ops[j]
                    if pj["eng"] == op["eng"] and not pj["dma"] and op["eng"] == "pe" and not op["dma"]:
                        continue
                    deps.append(j)
                for j in op["other"]:
                    pj = ops[j]
                    if pj["eng"] == op["eng"] and not pj["dma"] and not op["dma"]:
                        continue
                    deps.append(j)
            op["deps"] = sorted(set(deps))
            for j in op["deps"]:
                ops[j]["need_inc"] = True
        counts = {e: 0 for e in self.ENGS}
        nsem = {e: 1 for e in self.ENGS}
        dma_uses = [0] * N_DMA_SEMS
        dma_prev = [None] * N_DMA_SEMS
        for i, op in enumerate(ops):
            if op["dma"]:
                s = op["slot"]
                dma_uses[s] += 1
                op["ticket"] = ("dma", s, dma_uses[s] * 16)
                op["dma_prev"] = dma_prev[s]
                dma_prev[s] = i
            elif op["need_inc"]:
                e = op["eng"]
                ep, v = divmod(counts[e], EPOCH)
                counts[e] += 1
                op["ticket"] = (e, ep, v + 1)
                nsem[e] = max(nsem[e], ep + 1)
        sems = {}
        for e in self.ENGS:
            for ep in range(nsem[e]):
                sems[(e, ep)] = self.stack.enter_context(nc.semaphore(f"s_{e}_{ep}"))
        for s in range(N_DMA_SEMS):
            sems[("dma", s)] = self.stack.enter_context(nc.semaphore(f"s_dma_{s}"))
        print("ops", len(ops), "incs", counts, "ndma", self.nd, "nsems", len(sems), flush=True)
        by_eng = {e: [] for e in self.ENGS}
        for i, op in enumerate(ops):
            by_eng[op["eng"]].append(i)

        def run_engine(ename, eng):
            waited = {}

            def wait(t):
                key = (t[0], t[1])
                if waited.get(key, 0) >= t[2]:
                    return
                eng.wait_ge(sems[key], t[2])
                waited[key] = t[2]

            for i in by_eng[ename]:
                op = ops[i]
                for j in op["deps"]:
                    wait(ops[j]["ticket"])
                if op["emit"] is None:
                    continue
                if op["dma"] and op["dma_prev"] is not None:
                    wait(ops[op["dma_prev"]]["ticket"])
                ins = op["emit"](eng)
                t = op["ticket"]
                if t is not None:
                    ins.then_inc(sems[(t[0], t[1])], 16 if op["dma"] else 1)

        with nc.Block() as block:
            @block.tensor
            def _(e):
                run_engine("pe", e)

            @block.scalar
            def _(e):
                run_engine("act", e)

            @block.vector
            def _(e):
                run_engine("dve", e)

            @block.gpsimd
            def _(e):
                run_engine("pool", e)

            @block.sync
            def _(e):
                run_engine("sp", e)
        self.stack.close()


class Ring:
    def __init__(self, P, st, name, n, shape, dtype):
        self.n = n
        self.bufs = [P.sbuf(name, shape, dtype, st) for _ in range(n)]
        P.uid += 1
        self.keys = [f"{name}#{P.uid}#{i}" for i in range(n)]
        self.i = 0

    def next(self):
        j = self.i % self.n
        self.i += 1
        return self.bufs[j], self.keys[j]


class KeyRing:
    def __init__(self, objs, keys):
        self.objs, self.keys, self.i = objs, keys, 0

    def next(self):
        j = self.i % len(self.objs)
        self.i += 1
        return self.objs[j], self.keys[j]


class Builder:
    def __init__(self, S, depth=2, debug=False, stop_after=None):
        self.S = S
        self.NT = S // 128
        self.GW = min(512, S)
        self.NG = S // self.GW
        self.depth = depth
        self.debug = debug
        self.stop_after = stop_after
        nc = bass.Bass("TRN2", target_bir_lowering=False)
        self.nc = nc
        self.P = Prog(nc)

    def norm_T(self, st, src, gamma, dstT, dkey, R, W, eps=1e-6, dt=BF16, rows_at=None):
        P, c = self.P, self.c
        kc = W // 128
        gcol = P.sbuf("gcol", [128, kc], F32, st)
        P.dma("sp", gcol[:], gamma.rearrange("(c p) -> p c", p=128), w=["gcol"], allow_slow_non_contiguous=True)
        hr = Ring(P, st, "nt_h", 2, [128, W], F32)
        hbr = Ring(P, st, "nt_hb", 2, [128, W], dt)
        junk = P.sbuf("nt_junk", [128, W], F32, st)
        ssr = Ring(P, st, "nt_ss", 2, [128, 2], F32)
        ptr = KeyRing(self.pt if dt == BF16 else self.ps[4:6], ["pt0", "pt1"] if dt == BF16 else ["ps4", "ps5"])
        ident = c["ident"] if dt == BF16 else c["identf"]
        for t in range(R // 128):
            ht, hk = hr.next()
            hb, hbk = hbr.next()
            ss, sk = ssr.next()
            P.dma("sp", ht[:], src[t * 128:(t + 1) * 128, :], w=[hk])
            P.act(junk[:], ht[:], AF.Square, [hk], ["nt_junk", sk], accum_out=ss[:, 0:1])
            P.ts("dve", ss[:, 1:2], ss[:, 0:1], 1.0 / W, eps, ALU.mult, ALU.add, [sk], [sk])
            P.act(ss[:, 1:2], ss[:, 1:2], AF.Sqrt, [sk], [sk])
            P.add("dve", lambda e, ss=ss: e.reciprocal(out=ss[:, 1:2], in_=ss[:, 1:2]), [sk], [sk])
            P.ts("pool", hb[:], ht[:], ss[:, 1:2], None, ALU.mult, None, [hk, sk], [hbk])
            for c4 in range(0, kc, 4):
                n4 = min(4, kc - c4)
                pst, pk = ptr.next()
                pv = pst[:, 0:512].rearrange("p (a b) -> p a b", b=128)
                for j in range(n4):
                    P.tr(pv[:, j, :], hb[:, (c4 + j) * 128:(c4 + j + 1) * 128], ident[:], [hbk, "const"], [pk])
                P.tt("dve", dstT[:, c4:c4 + n4, t * 128:(t + 1) * 128], pv[:, 0:n4, :],
                     gcol[:, c4:c4 + n4].unsqueeze(2).to_broadcast([128, n4, 128]), ALU.mult,
                     [pk, "gcol"], [(dkey, t)])

    def wload(self, wring, W_ap, c0, w, kc):
        wt, wk = wring.next()
        self.P.dma("pool", wt[:, 0:kc, 0:w], W_ap[:, c0:c0 + w].rearrange("(c p) n -> p c n", p=128), w=[wk])
        return wt, wk

    def proj_fm(self, st, xT, xkey, kc, W_ap, c0, ncols, dst, rings, func=None, dtype=F32):
        P, S, GW = self.P, self.S, self.GW
        wring, psr, stg32, stg16 = rings
        stgr = stg32 if dtype == F32 else stg16
        xr = [(xkey, t) for t in range(self.NT)]
        for cb in range(0, ncols, 512):
            w = min(512, ncols - cb)
            wt, wk = self.wload(wring, W_ap, c0 + cb, w, kc)
            for sb in range(0, w, 128):
                m = min(128, w - sb)
                stg, sk = stgr.next()
                for g in range(self.NG):
                    ps, pk = psr.next()
                    for k in range(kc):
                        P.mm(ps[0:m, 0:GW], wt[:, k, sb:sb + m], xT[:, k, g * GW:(g + 1) * GW],
                             k == 0, k == kc - 1, [wk] + xr, [pk])
                    if func is not None or (g % 2 == 0):
                        P.act(stg[0:m, g * GW:(g + 1) * GW], ps[0:m, 0:GW], func or AF.Copy, [pk], [sk])
                    else:
                        P.cp("dve", stg[0:m, g * GW:(g + 1) * GW], ps[0:m, 0:GW], [pk], [sk])
                P.dma("sp", dst[cb + sb:cb + sb + m, :], stg[0:m, 0:S], r=[sk])

    def proj_tm(self, st, xT, xkey, kc, W_ap, c0, ncols, dst, rings, dtype=F32):
        P, S = self.P, self.S
        wring, psr, stg32, stg16 = rings
        stgr = stg32 if dtype == F32 else stg16
        for cb in range(0, ncols, 512):
            w = min(512, ncols - cb)
            wt, wk = self.wload(wring, W_ap, c0 + cb, w, kc)
            for t in range(self.NT):
                ps, pk = psr.next()
                stg, sk = stgr.next()
                for k in range(kc):
                    P.mm(ps[:, 0:w], xT[:, k, t * 128:(t + 1) * 128], wt[:, k, 0:w], k == 0, k == kc - 1,
                         [wk, (xkey, t)], [pk])
                if t % 2 == 0:
                    P.act(stg[:, 0:w], ps[:, 0:w], AF.Copy, [pk], [sk])
                else:
                    P.cp("dve", stg[:, 0:w], ps[:, 0:w], [pk], [sk])
                P.dma("sp", dst[t * 128:(t + 1) * 128, cb:cb + w], stg[:, 0:w], r=[sk])

    def bcast_row(self, st, name, src_row, n):
        t = self.P.sbuf(name, [128, n], F32, st)
        self.P.dma("sp", t[:], src_row.partition_broadcast(128), w=[name])
        return t

    def build(self):
        P, nc, S, NT = self.P, self.nc, self.S, self.NT
        inp = lambda n, sh, dt=F32: P.dram(n, sh, dt, kind="ExternalInput")
        A = {}
        A["x"] = inp("x", [S, D])
        A["mem"] = inp("mem", [MEM, D])
        for n, sh in (("norm_mix", [2, D]), ("w_in", [2, D, IN_TOTAL]), ("ml_igate_bias", [2, 4]),
                      ("ml_fgate_bias", [2, 4]), ("ml_norm", [2, 1024]), ("ssm_conv_w", [2, 4, 1536]),
                      ("ssm_conv_b", [2, 1536]), ("ssm_dt_bias", [2, 16]), ("ssm_a_log", [2, 16]),
                      ("ssm_d", [2, 16]), ("ssm_norm", [2, 1024]), ("mla_q_norm", [2, 512]),
                      ("mla_w_uq", [2, 512, 1536]), ("mla_kv_norm", [2, 256]), ("mla_w_ukv", [2, 256, 2048]),
                      ("swa_sinks", [2, 16]), ("w_branch", [2, 4, 1024, D]), ("w_out", [2, D, D]),
                      ("norm_cross", [2, D]), ("norm_mem", [2, D]), ("xa_wq", [2, D, 512]),
                      ("xa_wkv", [2, D, 1024]), ("xa_wo", [2, 512, D]), ("norm_ffn", [2, D]),
                      ("ffn_w13", [1, D, 2 * FFN]), ("ffn_w2", [1, FFN, D]), ("moe_router", [1, D, NEXP]),
                      ("moe_w13", [1, NEXP, D, 2 * FFN]), ("moe_w2", [1, NEXP, FFN, D]), ("norm_final", [D])):
            A[n] = inp(n, sh)
        A["c_ident"] = inp("c_ident", [128, 128], BF16)
        A["c_identf"] = inp("c_identf", [128, 128])
        A["c_tri"] = inp("c_tri", [128, 128])
        A["c_trib"] = inp("c_trib", [128, 128], BF16)
        A["c_lowb"] = inp("c_lowb", [128, 128], BF16)
        A["c_mneg"] = inp("c_mneg", [128, 128])
        A["c_ones"] = inp("c_ones", [128, 128])
        A["c_sutri"] = inp("c_sutri", [128, 128])
        A["c_iota"] = inp("c_iota", [128, 1024])
        A["c_jcol"] = inp("c_jcol", [128, 8])
        A["c_cos2"] = inp("c_cos2", [64, S])
        A["c_sin2"] = inp("c_sin2", [64, S])
        self.A = A
        out = P.dram("out", [S, D], F32, kind="ExternalOutput")
        self.out = out
        Sc = {}
        Sc["h"] = P.dram("s_h", [S, D], F32)
        Sc["ufm32"] = P.dram("s_ufm32", [IN_TOTAL + 64, S], F32)
        Sc["ufm16"] = P.dram("s_ufm16", [IN_TOTAL, S], BF16)
        Sc["utm32"] = P.dram("s_utm32", [S, IN_TOTAL], F32)
        Sc["utm16"] = P.dram("s_utm16", [S, IN_TOTAL], BF16)
        Sc["xcT"] = P.dram("s_xcT", [1536, S], F32)
        Sc["hn16"] = P.dram("s_hn16", [S, D], BF16)
        kind = "ExternalOutput" if self.debug else "Internal"
        for i, n in enumerate("abcd"):
            Sc["y" + n] = P.dram("s_y" + n, [S, 1024], BF16, kind=kind)
        if self.debug:
            Sc["dbg_h"] = [P.dram(f"dbg_h{i}", [S, D], F32, kind="ExternalOutput") for i in range(3 * self.depth)]
        self.Sc = Sc

        c = {}
        self.c = c
        for n, dt in (("ident", BF16), ("identf", F32), ("tri", F32), ("trib", BF16), ("lowb", BF16),
                      ("mneg", F32), ("ones", F32)):
            c[n] = P.sbuf("c_" + n, [128, 128], dt)
            P.dma("sp", c[n][:], A["c_" + n], w=["const"])
        self.ps = [P.psum(f"ps{i}", [128, 512], F32) for i in range(6)]
        self.pt = [P.psum(f"pt{i}", [128, 1024], BF16) for i in range(2)]
        P.barrier()

        with P.phase() as st:
            r = Ring(P, st, "cpx", 3, [128, D], F32)
            for t in range(NT):
                b, k = r.next()
                P.dma("sp", b[:], A["x"][t * 128:(t + 1) * 128, :], w=[k])
                P.dma("sp", Sc["h"][t * 128:(t + 1) * 128, :], b[:], r=[k])

        for l in range(self.depth):
            self.layer(l)
            if self.stop_after is not None and l == self.stop_after[0]:
                break
        self.final_norm()
        P.emit_all()
        return nc

    def dbg_copy_h(self, idx):
        if not self.debug:
            return
        P, Sc = self.P, self.Sc
        with P.phase() as st:
            r = Ring(P, st, "cpd", 3, [128, D], F32)
            for t in range(self.NT):
                b, k = r.next()
                P.dma("sp", b[:], Sc["h"][t * 128:(t + 1) * 128, :], w=[k])
                P.dma("sp", Sc["dbg_h"][idx][t * 128:(t + 1) * 128, :], b[:], r=[k])

    def final_norm(self):
        P, S, NT, A, Sc = self.P, self.S, self.NT, self.A, self.Sc
        with P.phase() as st:
            g = self.bcast_row(st, "fn_g", A["norm_final"], D)
            hr = Ring(P, st, "fn_h", 2, [128, D], F32)
            orr = Ring(P, st, "fn_o", 2, [128, D], F32)
            junk = P.sbuf("fn_junk", [128, D], F32, st)
            ssr = Ring(P, st, "fn_ss", 2, [128, 2], F32)
            for t in range(NT):
                ht, hk = hr.next()
                ot, ok = orr.next()
                ss, sk = ssr.next()
                P.dma("sp", ht[:], Sc["h"][t * 128:(t + 1) * 128, :], w=[hk])
                P.act(junk[:], ht[:], AF.Square, [hk], ["fn_junk", sk], accum_out=ss[:, 0:1])
                P.ts("dve", ss[:, 1:2], ss[:, 0:1], 1.0 / D, 1e-6, ALU.mult, ALU.add, [sk], [sk])
                P.act(ss[:, 1:2], ss[:, 1:2], AF.Sqrt, [sk], [sk])
                P.add("dve", lambda e, ss=ss: e.reciprocal(out=ss[:, 1:2], in_=ss[:, 1:2]), [sk], [sk])
                P.stt("dve", ot[:], ht[:], ss[:, 1:2], g[:], ALU.mult, ALU.mult, [hk, sk, "fn_g"], [ok])
                P.dma("sp", self.out[t * 128:(t + 1) * 128, :], ot[:], r=[ok])

    def layer(self, l):
        sa = self.stop_after
        self.in_proj(l)
        self.mlstm(l)
        self.ssd(l)
        self.mla(l)
        self.swa(l)
        if sa is not None and sa == (l, "branches"):
            return
        self.merge(l)
        self.dbg_copy_h(3 * l + 0)
        if sa is not None and sa == (l, "mixer"):
            return
        self.xattn(l)
        self.dbg_copy_h(3 * l + 1)
        if sa is not None and sa == (l, "xattn"):
            return
        if l % 2 == 1:
            self.moe_layer(l)
        else:
            self.ffn_layer(l)
        self.dbg_copy_h(3 * l + 2)

    def in_proj(self, l):
        P, S, NT, A, Sc = self.P, self.S, self.NT, self.A, self.Sc
        W = A["w_in"][l]
        with P.phase() as st:
            xnT = P.sbuf("xnT", [128, KC, S], BF16, st)
            with P.phase() as st2:
                self.norm_T(st2, Sc["h"], A["norm_mix"][l], xnT, "xnT", S, D)
            wring = Ring(P, st, "ip_w", 3, [128, KC, 512], BF16)
            psr = KeyRing(self.ps[0:4], ["ps0", "ps1", "ps2", "ps3"])
            stg32 = Ring(P, st, "ip_s32", 3, [128, max(S, 512)], F32)
            stg16 = Ring(P, st, "ip_s16", 3, [128, max(S, 512)], BF16)
            rings = (wring, psr, stg32, stg16)
            fm = lambda c0, n, dt, func=None, drow=None: self.proj_fm(
                st, xnT, "xnT", KC, W, c0, n,
                (Sc["ufm32"] if dt == F32 else Sc["ufm16"])[(c0 if drow is None else drow):(c0 if drow is None else drow) + n, :],
                rings, func=func, dtype=dt)
            tm = lambda c0, n, dt: self.proj_tm(
                st, xnT, "xnT", KC, W, c0, n, (Sc["utm32"] if dt == F32 else Sc["utm16"])[:, c0:c0 + n], rings, dtype=dt)
            tm(O_MLK, 512, BF16)
            tm(O_MLV, 1024, BF16)
            tm(O_MLO, 1032, F32)
            tm(O_Z, 1024, F32)
            tm(O_DT, 784, F32)
            tm(O_SWV, 256, BF16)
            fm(O_MLQ, 1024, BF16)
            fm(O_XBC, 1536, F32)
            fm(O_KR, 64, F32)
            fm(O_KR + 32, 32, F32, drow=IN_TOTAL)
            fm(O_KR, 32, F32, drow=IN_TOTAL + 32)
            fm(O_SWQ, 1280, BF16)
            fm(O_GATE, 4 * D, BF16, func=AF.Sigmoid)

    def mlstm(self, l):
        P, S, NT, A, Sc, c = self.P, self.S, self.NT, self.A, self.Sc, self.c
        ps = self.ps
        with P.phase() as st:
            gif = P.sbuf("ml_gif", [128, NT, 8], F32, st)
            P.dma("sp", gif[:], Sc["utm32"][:, O_MLI:O_MLI + 8].rearrange("(t p) c -> p t c", p=128), w=["gif"])
            bi = self.bcast_row(st, "ml_bi", A["ml_igate_bias"][l], 4)
            bf = self.bcast_row(st, "ml_bf", A["ml_fgate_bias"][l], 4)
            nw = self.bcast_row(st, "ml_nw", A["ml_norm"][l], 1024)
            ig = P.sbuf("ml_ig", [128, NT, 4], F32, st)
            lf = P.sbuf("ml_lf", [128, NT, 4], F32, st)
            P.tt("dve", ig[:], gif[:, :, 0:4], bi[:].unsqueeze(1).to_broadcast([128, NT, 4]), ALU.add, ["gif", "ml_bi"], ["ig"])
            P.tt("dve", lf[:], gif[:, :, 4:8], bf[:].unsqueeze(1).to_broadcast([128, NT, 4]), ALU.add, ["gif", "ml_bf"], ["lf"])
            P.act(lf[:], lf[:], AF.Exp, ["lf"], ["lf"], scale=-1.0)
            P.act(lf[:], lf[:], AF.Ln, ["lf"], ["lf"], bias=1.0)
            P.ts("dve", lf[:], lf[:], -1.0, None, ALU.mult, None, ["lf"], ["lf"])
            bcs = P.sbuf("ml_b", [128, NT, 4], F32, st)
            ibm = P.sbuf("ml_ibm", [128, NT, 4], F32, st)
            eb = P.sbuf("ml_eb", [128, NT, 4], F32, st)
            wst = P.sbuf("ml_wst", [128, NT, 4], F32, st)
            ebt = P.sbuf("ml_ebt", [128, NT, 4], F32, st)
            for t in range(NT):
                P.mm(ps[0][:, 0:4], c["tri"][:], lf[:, t, :], True, True, ["lf", "const"], ["ps0"])
                P.mm(ps[0][:, 4:8], c["ones"][:], lf[:, t, :], True, True, ["lf", "const"], ["ps0"])
                P.cp("dve", bcs[:, t, :], ps[0][:, 0:4], ["ps0"], ["bcs"])
                P.cp("dve", ebt[:, t, :], ps[0][:, 4:8], ["ps0"], ["ebt"])
            P.tt("dve", ibm[:], ig[:], bcs[:], ALU.subtract, ["ig", "bcs"], ["ibm"])
            P.tt("dve", wst[:], ibm[:], ebt[:], ALU.add, ["ibm", "ebt"], ["wst"])
            P.act(wst[:], wst[:], AF.Exp, ["wst"], ["wst"])
            P.ts("dve", wst[:], wst[:], 128.0 ** -0.5, None, ALU.mult, None, ["wst"], ["wst"])
            P.act(eb[:], bcs[:], AF.Exp, ["bcs"], ["eb"])
            P.act(ebt[:], ebt[:], AF.Exp, ["ebt"], ["ebt"])

            qTr = Ring(P, st, "ml_qT", 2, [128, S], BF16)
            kTr = Ring(P, st, "ml_kT", 2, [128, S], BF16)
            ktr = Ring(P, st, "ml_kt", 2, [128, NT, 128], BF16)
            var = Ring(P, st, "ml_va", 2, [128, NT, 257], BF16)
            otr = Ring(P, st, "ml_o", 2, [128, NT, 256], F32)
            ltr = Ring(P, st, "ml_lt", 2, [128, 128], F32)
            etr = Ring(P, st, "ml_et", 2, [128, 128], F32)
            ptr_ = Ring(P, st, "ml_pt", 2, [128, 128], BF16)
            kwr = Ring(P, st, "ml_kw", 2, [128, 128], BF16)
            Cst = P.sbuf("ml_C", [128, 257], F32, st)
            Cbr = Ring(P, st, "ml_Cb", 2, [128, 257], BF16)
            tmr = Ring(P, st, "ml_tmp", 2, [128, 257], F32)
            hor = Ring(P, st, "ml_ho", 2, [128, 257], F32)
            smr = Ring(P, st, "ml_sm", 2, [128, 4], F32)
            yr = Ring(P, st, "ml_y", 2, [128, 256], BF16)
            junk = P.sbuf("ml_junk", [128, 256], F32, st)
            for hd in range(4):
                qT, qk = qTr.next()
                kT, kk = kTr.next()
                kt, ktk = ktr.next()
                va, vk = var.next()
                og, ogk = otr.next()
                P.dma("sp", qT[:], Sc["ufm16"][O_MLQ + hd * 128:O_MLQ + (hd + 1) * 128, :], w=[qk])
                P.dma("sp", kT[:], Sc["ufm16"][O_MLK + hd * 128:O_MLK + (hd + 1) * 128, :], w=[kk])
                P.dma("sp", kt[:], Sc["utm16"][:, O_MLK + hd * 128:O_MLK + (hd + 1) * 128].rearrange("(t p) c -> p t c", p=128), w=[ktk])
                P.dma("sp", va[:, :, 0:256], Sc["utm16"][:, O_MLV + hd * 256:O_MLV + (hd + 1) * 256].rearrange("(t p) c -> p t c", p=128), w=[vk])
                P.memset("pool", va[:, :, 256:257], 1.0, [vk])
                P.dma("sp", og[:], Sc["utm32"][:, O_MLO + hd * 256:O_MLO + (hd + 1) * 256].rearrange("(t p) c -> p t c", p=128), w=[ogk])
                P.act(og[:], og[:], AF.Sigmoid, [ogk], [ogk])
                P.memset("dve", Cst[:], 0.0, ["C"])
                Cb, cbk = Cbr.next()
                P.memset("pool", Cb[:], 0.0, [cbk])
                for t in range(NT):
                    sl = slice(t * 128, (t + 1) * 128)
                    lt, ltk = ltr.next()
                    et, etk = etr.next()
                    pt, ptk = ptr_.next()
                    kw, kwk = kwr.next()
                    tmp, tmk = tmr.next()
                    ho, hok = hor.next()
                    sm, smk = smr.next()
                    y, yk = yr.next()
                    P.ts("pool", lt[:], c["tri"][:], lf[:, t, hd:hd + 1], None, ALU.mult, None, ["lf", "const"], [ltk])
                    P.mm(ps[1][:, 0:128], c["ones"][:], lt[:], True, False, [ltk, "const"], ["ps1"])
                    P.mm(ps[1][:, 0:128], c["identf"][:], c["mneg"][:], False, True, ["const"], ["ps1"])
                    P.act(et[:], ps[1][:, 0:128], AF.Exp, ["ps1", "ibm"], [etk], bias=ibm[:, t, hd:hd + 1])
                    P.mm(ps[2][:, 0:128], kT[:, sl], qT[:, sl], True, True, [kk, qk], ["ps2"])
                    P.stt("dve", pt[:], ps[2][:, 0:128], 128.0 ** -0.5, et[:], ALU.mult, ALU.mult, ["ps2", etk], [ptk])
                    P.mm(ps[3][:, 0:257], pt[:], va[:, t, :], True, True, [ptk, vk], ["ps3"])
                    P.mm(ps[4][:, 0:257], qT[:, sl], Cb[:], True, True, [qk, cbk], ["ps4"])
                    P.act(tmp[:], ps[4][:, 0:257], AF.Copy, ["ps4", "eb"], [tmk], scale=eb[:, t, hd:hd + 1])
                    P.tt("dve", ho[:], tmp[:], ps[3][:, 0:257], ALU.add, [tmk, "ps3"], [hok])
                    P.act(sm[:, 0:1], ho[:, 256:257], AF.Abs, [hok], [smk])
                    P.ts("dve", sm[:, 0:1], sm[:, 0:1], 1.0, None, ALU.max, None, [smk], [smk])
                    P.add("dve", lambda e, sm=sm: e.reciprocal(out=sm[:, 0:1], in_=sm[:, 0:1]), [smk], [smk])
                    P.ts("dve", ho[:, 0:256], ho[:, 0:256], sm[:, 0:1], None, ALU.mult, None, [hok, smk], [hok])
                    P.act(junk[:], ho[:, 0:256], AF.Square, [hok], ["ml_junk", smk], accum_out=sm[:, 1:2])
                    P.ts("dve", sm[:, 2:3], sm[:, 1:2], 1.0 / 256, 1e-6, ALU.mult, ALU.add, [smk], [smk])
                    P.act(sm[:, 2:3], sm[:, 2:3], AF.Sqrt, [smk], [smk])
                    P.add("dve", lambda e, sm=sm: e.reciprocal(out=sm[:, 2:3], in_=sm[:, 2:3]), [smk], [smk])
                    P.stt("dve", ho[:, 0:256], ho[:, 0:256], sm[:, 2:3], nw[:, hd * 256:(hd + 1) * 256], ALU.mult, ALU.mult,
                          [hok, smk, "ml_nw"], [hok])
                    P.tt("pool", y[:], ho[:, 0:256], og[:, t, :], ALU.mult, [hok, ogk], [yk])
                    P.dma("sp", Sc["ya"][sl, hd * 256:(hd + 1) * 256], y[:], r=[yk])
                    if t < NT - 1:
                        P.ts("pool", kw[:], kt[:, t, :], wst[:, t, hd:hd + 1], None, ALU.mult, None, [ktk, "wst"], [kwk])
                        P.mm(ps[5][:, 0:257], kw[:], va[:, t, :], True, True, [kwk, vk], ["ps5"])
                        P.stt("dve", Cst[:], Cst[:], ebt[:, t, hd:hd + 1], ps[5][:, 0:257], ALU.mult, ALU.add,
                              ["C", "ebt", "ps5"], ["C"])
                        Cb, cbk = Cbr.next()
                        P.cp("act", Cb[:], Cst[:], ["C"], [cbk])

    def ssd(self, l):
        P, S, NT, A, Sc, c = self.P, self.S, self.NT, self.A, self.Sc, self.c
        ps, pt = self.ps, self.pt
        with P.phase() as st:
            cw = P.sbuf("sd_cw", [128, 12, 4], F32, st)
            cb = P.sbuf("sd_cb", [128, 12], F32, st)
            for j in range(4):
                P.dma("sp", cw[:, :, j], A["ssm_conv_w"][l, j].rearrange("(c p) -> p c", p=128), w=["cw"], allow_slow_non_contiguous=True)
            P.dma("sp", cb[:], A["ssm_conv_b"][l].rearrange("(c p) -> p c", p=128), w=["cb"], allow_slow_non_contiguous=True)
            xr = Ring(P, st, "sd_x", 2, [128, S + 3], F32)
            ar = Ring(P, st, "sd_a", 2, [128, S], F32)
            for ch in range(12):
                x, xk = xr.next()
                a, ak = ar.next()
                P.memset("pool", x[:, 0:3], 0.0, [xk])
                P.dma("sp", x[:, 3:S + 3], Sc["ufm32"][O_XBC + ch * 128:O_XBC + (ch + 1) * 128, :], w=[xk])
                P.ts("dve", a[:], x[:, 3:S + 3], cw[:, ch, 3:4], None, ALU.mult, None, [xk, "cw"], [ak])
                for j in range(3):
                    P.stt("dve", a[:], x[:, j:S + j], cw[:, ch, j:j + 1], a[:], ALU.mult, ALU.add, [xk, "cw", ak], [ak])
                P.act(a[:], a[:], AF.Silu, [ak, "cb"], [ak], bias=cb[:, ch:ch + 1])
                P.dma("sp", Sc["xcT"][ch * 128:(ch + 1) * 128, :], a[:], r=[ak])
        with P.phase() as st:
            dtb = self.bcast_row(st, "sd_dtb", A["ssm_dt_bias"][l], 16)
            alog = self.bcast_row(st, "sd_alog", A["ssm_a_log"][l], 16)
            dsk = self.bcast_row(st, "sd_dsk", A["ssm_d"][l], 16)
            nw = self.bcast_row(st, "sd_nw", A["ssm_norm"][l], 1024)
            P.act(alog[:], alog[:], AF.Exp, ["sd_alog"], ["sd_alog"])
            dt = P.sbuf("sd_dt", [128, NT, 16], F32, st)
            P.dma("sp", dt[:], Sc["utm32"][:, O_DT:O_DT + 16].rearrange("(t p) c -> p t c", p=128), w=["dt"])
            P.tt("dve", dt[:], dt[:], dtb[:].unsqueeze(1).to_broadcast([128, NT, 16]), ALU.add, ["dt", "sd_dtb"], ["dt"])
            P.act(dt[:], dt[:], AF.Exp, ["dt"], ["dt"])
            P.act(dt[:], dt[:], AF.Ln, ["dt"], ["dt"], bias=1.0)
            av = P.sbuf("sd_av", [128, NT, 16], F32, st)
            P.stt("dve", av[:], dt[:], -1.0, alog[:].unsqueeze(1).to_broadcast([128, NT, 16]), ALU.mult, ALU.mult,
                  ["dt", "sd_alog"], ["av"])
            acs = P.sbuf("sd_acs", [128, NT, 16], F32, st)
            nacs = P.sbuf("sd_nacs", [128, NT, 16], F32, st)
            eat = P.sbuf("sd_eat", [128, NT, 16], F32, st)
            ea = P.sbuf("sd_ea", [128, NT, 16], F32, st)
            dsc = P.sbuf("sd_dsc", [128, NT, 16], F32, st)
            for t in range(NT):
                P.mm(ps[0][:, 0:16], c["tri"][:], av[:, t, :], True, True, ["av", "const"], ["ps0"])
                P.mm(ps[0][:, 16:32], c["ones"][:], av[:, t, :], True, True, ["av", "const"], ["ps0"])
                P.cp("dve", acs[:, t, :], ps[0][:, 0:16], ["ps0"], ["acs"])
                P.cp("dve", eat[:, t, :], ps[0][:, 16:32], ["ps0"], ["eat"])
            P.ts("dve", nacs[:], acs[:], -1.0, None, ALU.mult, None, ["acs"], ["nacs"])
            P.tt("dve", dsc[:], eat[:], acs[:], ALU.subtract, ["eat", "acs"], ["dsc"])
            P.act(dsc[:], dsc[:], AF.Exp, ["dsc"], ["dsc"])
            P.tt("dve", dsc[:], dsc[:], dt[:], ALU.mult, ["dsc", "dt"], ["dsc"])
            P.act(ea[:], acs[:], AF.Exp, ["acs"], ["ea"])
            P.act(eat[:], eat[:], AF.Exp, ["eat"], ["eat"])

            xcr = Ring(P, st, "sd_xc", 2, [128, 12, 128], F32)
            xcbr = Ring(P, st, "sd_xcb", 2, [128, 12, 128], BF16)
            xtr = Ring(P, st, "sd_xt", 2, [128, 1280], BF16)
            xsr = Ring(P, st, "sd_xs", 2, [128, 1024], BF16)
            xdr = Ring(P, st, "sd_xd", 2, [128, 1024], BF16)
            cbr = Ring(P, st, "sd_cbT", 2, [128, 2, 128], F32)
            atr = Ring(P, st, "sd_at", 3, [128, 128], F32)
            der = Ring(P, st, "sd_de", 3, [128, 128], F32)
            mtr = Ring(P, st, "sd_mt", 3, [128, 128], BF16)
            zr = Ring(P, st, "sd_z", 2, [128, 1024], F32)
            yr = Ring(P, st, "sd_y", 2, [128, 1024], F32)
            tr_ = Ring(P, st, "sd_t", 2, [128, 1024], F32)
            ybr = Ring(P, st, "sd_yb", 2, [128, 1024], BF16)
            smr = Ring(P, st, "sd_sm", 2, [128, 4], F32)
            junk = P.sbuf("sd_junk", [128, 512], F32, st)
            state = P.sbuf("sd_state", [128, 2, 512], F32, st)
            stbr = Ring(P, st, "sd_stb", 2, [128, 2, 512], BF16)
            P.memset("dve", state[:], 0.0, ["state"])
            stb, stbk = stbr.next()
            P.memset("pool", stb[:], 0.0, [stbk])
            for t in range(NT):
                sl = slice(t * 128, (t + 1) * 128)
                xc, xck = xcr.next()
                xcb, xcbk = xcbr.next()
                xt, xtk = xtr.next()
                xs, xsk = xsr.next()
                xd, xdk = xdr.next()
                cbT, cbTk = cbr.next()
                z, zk = zr.next()
                y, yk = yr.next()
                tm_, tmk = tr_.next()
                yb, ybk = ybr.next()
                sm, smk = smr.next()
                P.dma("sp", xc[:], Sc["xcT"][:, sl].rearrange("(c p) s -> p c s", p=128), w=[xck])
                P.dma("sp", z[:], Sc["utm32"][sl, O_Z:O_Z + 1024], w=[zk])
                P.cp("pool", xcb[:], xc[:], [xck], [xcbk])
                for q in range(3):
                    n4 = 4 if q < 2 else 2
                    pb, pbk = pt[q % 2], ("pt0", "pt1")[q % 2]
                    pv = pb[:, 0:512].rearrange("p (a b) -> p a b", b=128)
                    for j in range(n4):
                        P.tr(pv[:, j, :], xcb[:, q * 4 + j, :], c["ident"][:], [xcbk, "const"], [pbk])
                    P.cp("dve" if q % 2 == 0 else "act", xt[:, q * 512:q * 512 + n4 * 128], pb[:, 0:n4 * 128], [pbk], [xtk])
                x3 = xt[:, 0:1024].rearrange("p (h d) -> p h d", d=64)
                P.tt("dve", xs[:].rearrange("p (h d) -> p h d", d=64), x3, dt[:, t, :].unsqueeze(2).to_broadcast([128, 16, 64]),
                     ALU.mult, [xtk, "dt"], [xsk])
                P.tt("pool", xd[:].rearrange("p (h d) -> p h d", d=64), x3, dsc[:, t, :].unsqueeze(2).to_broadcast([128, 16, 64]),
                     ALU.mult, [xtk, "dsc"], [xdk])
                for g in range(2):
                    P.mm(ps[1][:, g * 128:(g + 1) * 128], xcb[:, 8 + g, :], xcb[:, 10 + g, :], True, True, [xcbk], ["ps1"])
                P.cp("act", cbT[:].rearrange("p a b -> p (a b)"), ps[1][:, 0:256], ["ps1"], [cbTk])
                for g in range(2):
                    P.mm(ps[2 + g][:, 0:512], xcb[:, 10 + g, :], stb[:, g, :], True, True, [xcbk, stbk], [f"ps{2 + g}"])
                for hd in range(16):
                    g = hd // 8
                    at, atk = atr.next()
                    de, dek = der.next()
                    mt, mtk = mtr.next()
                    P.ts("pool", at[:], c["tri"][:], av[:, t, hd:hd + 1], None, ALU.mult, None, ["av", "const"], [atk])
                    P.mm(ps[0][:, 0:128], c["ones"][:], at[:], True, False, [atk, "const"], ["ps0"])
                    P.mm(ps[0][:, 0:128], c["identf"][:], c["mneg"][:], False, True, ["const"], ["ps0"])
                    P.act(de[:], ps[0][:, 0:128], AF.Exp, ["ps0", "nacs"], [dek], bias=nacs[:, t, hd:hd + 1])
                    P.tt("dve", mt[:], de[:], cbT[:, g, :], ALU.mult, [dek, cbTk], [mtk])
                    P.mm(ps[4 + g][:, (hd % 8) * 64:(hd % 8 + 1) * 64], mt[:], xs[:, hd * 64:(hd + 1) * 64], True, True,
                         [mtk, xsk], [f"ps{4 + g}"])
                for g in range(2):
                    P.tt("dve", tm_[:, g * 512:(g + 1) * 512].rearrange("p (h d) -> p h d", d=64),
                         ps[2 + g][:, 0:512].rearrange("p (h d) -> p h d", d=64),
                         ea[:, t, g * 8:(g + 1) * 8].unsqueeze(2).to_broadcast([128, 8, 64]), ALU.mult,
                         [f"ps{2 + g}", "ea"], [tmk])
                    P.tt("dve", y[:, g * 512:(g + 1) * 512], tm_[:, g * 512:(g + 1) * 512], ps[4 + g][:, 0:512], ALU.add,
                         [tmk, f"ps{4 + g}"], [yk])
                P.tt("pool", tm_[:].rearrange("p (h d) -> p h d", d=64), x3, dsk[:].unsqueeze(2).to_broadcast([128, 16, 64]),
                     ALU.mult, [xtk, "sd_dsk", yk], [tmk])
                P.tt("pool", y[:], y[:], tm_[:], ALU.add, [tmk, yk], [yk])
                P.act(z[:], z[:], AF.Silu, [zk], [zk])
                P.tt("dve", y[:], y[:], z[:], ALU.mult, [yk, zk], [yk])
                for g in range(2):
                    P.act(junk[:], y[:, g * 512:(g + 1) * 512], AF.Square, [yk], ["sd_junk", smk], accum_out=sm[:, g:g + 1])
                P.ts("dve", sm[:, 2:4], sm[:, 0:2], 1.0 / 512, 1e-6, ALU.mult, ALU.add, [smk], [smk])
                P.act(sm[:, 2:4], sm[:, 2:4], AF.Sqrt, [smk], [smk])
                P.add("dve", lambda e, sm=sm: e.reciprocal(out=sm[:, 2:4], in_=sm[:, 2:4]), [smk], [smk])
                for g in range(2):
                    P.stt("dve", yb[:, g * 512:(g + 1) * 512], y[:, g * 512:(g + 1) * 512], sm[:, 2 + g:3 + g],
                          nw[:, g * 512:(g + 1) * 512], ALU.mult, ALU.mult, [yk, smk, "sd_nw"], [ybk])
                P.dma("sp", Sc["yb"][sl, :], yb[:], r=[ybk])
                if t < NT - 1:
                    for g in range(2):
                        P.mm(ps[2 + g][:, 0:512], xt[:, 1024 + g * 128:1024 + (g + 1) * 128], xd[:, g * 512:(g + 1) * 512],
                             True, True, [xtk, xdk], [f"ps{2 + g}"])
                        P.tt("dve", state[:, g, :].rearrange("p (h d) -> p h d", d=64),
                             state[:, g, :].rearrange("p (h d) -> p h d", d=64),
                             eat[:, t, g * 8:(g + 1) * 8].unsqueeze(2).to_broadcast([128, 8, 64]), ALU.mult,
                             ["state", "eat"], ["state"])
                        P.tt("dve", state[:, g, :], state[:, g, :], ps[2 + g][:, 0:512], ALU.add, ["state", f"ps{2 + g}"], ["state"])
                    stb, stbk = stbr.next()
                    P.cp("act", stb[:], state[:], ["state"], [stbk])

    def mla(self, l):
        P, S, NT, A, Sc, c = self.P, self.S, self.NT, self.A, self.Sc, self.c
        ps, GW, NG = self.ps, self.GW, self.NG
        scale = 192.0 ** -0.5
        with P.phase() as st:
            cqT = P.sbuf("la_cqT", [128, 4, S], BF16, st)
            ckT = P.sbuf("la_ckT", [128, 2, S], BF16, st)
            with P.phase() as st2:
                self.norm_T(st2, Sc["utm32"][:, O_CQ:O_CQ + 512], A["mla_q_norm"][l], cqT, "cqT", S, 512)
            with P.phase() as st2:
                self.norm_T(st2, Sc["utm32"][:, O_CKV:O_CKV + 256], A["mla_kv_norm"][l], ckT, "ckT", S, 256)
            cos2 = P.sbuf("la_cos", [64, S], F32, st)
            sin2 = P.sbuf("la_sin", [64, S], F32, st)
            P.dma("sp", cos2[:], A["c_cos2"], w=["cos"])
            P.dma("sp", sin2[:], A["c_sin2"], w=["sin"])
            krT = P.sbuf("la_krT", [64, S], BF16, st)
            k1 = P.sbuf("la_k1", [64, S], F32, st)
            k2 = P.sbuf("la_k2", [64, S], F32, st)
            P.dma("sp", k1[:], Sc["ufm32"][O_KR:O_KR + 64, :], w=["k1"])
            P.dma("sp", k2[:], Sc["ufm32"][IN_TOTAL:IN_TOTAL + 64, :], w=["k2"])
            P.tt("dve", k1[:], k1[:], cos2[:], ALU.mult, ["k1", "cos"], ["k1"])
            P.tt("pool", k2[:], k2[:], sin2[:], ALU.mult, ["k2", "sin"], ["k2"])
            P.tt("dve", krT[:], k1[:], k2[:], ALU.add, ["k1", "k2"], ["krT"])
            Wq, Wkv = A["mla_w_uq"][l], A["mla_w_ukv"][l]
            wqr = Ring(P, st, "la_wq", 2, [128, 4, 256], BF16)
            wkr = Ring(P, st, "la_wk", 2, [128, 2, 256], BF16)
            qnr = Ring(P, st, "la_qn", 2, [128, S], BF16)
            qrr = Ring(P, st, "la_qr", 2, [64, S], BF16)
            knr = Ring(P, st, "la_kn", 2, [128, S], BF16)
            var = Ring(P, st, "la_va", 2, [128, NT, 129], BF16)
            t1r = Ring(P, st, "la_t1", 2, [64, GW], F32)
            t2r = Ring(P, st, "la_t2", 2, [64, GW], F32)
            ptr_ = Ring(P, st, "la_pt", 3, [128, GW], BF16)
            yr = Ring(P, st, "la_y", 4, [128, 128], BF16)
            smr = Ring(P, st, "la_sm", 4, [128, 2], F32)
            ycur = {}
            for hd in range(8):
                wq, wqk = wqr.next()
                wk, wkk = wkr.next()
                qn, qnk = qnr.next()
                qr, qrk = qrr.next()
                kn, knk = knr.next()
                va, vak = var.next()
                q0 = hd * 192
                vw = lambda a, b: Wq[:, a:b].rearrange("(c p) n -> p c n", p=128)
                P.dma("pool", wq[:, :, 0:192], vw(q0, q0 + 192), w=[wqk])
                P.dma("pool", wq[:, :, 192:224], vw(q0 + 160, q0 + 192), w=[wqk])
                P.dma("pool", wq[:, :, 224:256], vw(q0 + 128, q0 + 160), w=[wqk])
                P.dma("pool", wk[:], Wkv[:, hd * 256:(hd + 1) * 256].rearrange("(c p) n -> p c n", p=128), w=[wkk])
                cqr = [("cqT", t) for t in range(NT)]
                ckr = [("ckT", t) for t in range(NT)]
                for g in range(NG):
                    gs = slice(g * GW, (g + 1) * GW)
                    for k in range(4):
                        P.mm(ps[0][:, 0:GW], wq[:, k, 0:128], cqT[:, k, gs], k == 0, k == 3, [wqk] + cqr, ["ps0"])
                    P.cp("act", qn[:, gs], ps[0][:, 0:GW], ["ps0"], [qnk])
                    for k in range(4):
                        P.mm(ps[1][0:64, 0:GW], wq[:, k, 128:192], cqT[:, k, gs], k == 0, k == 3, [wqk] + cqr, ["ps1"])
                    for k in range(4):
                        P.mm(ps[2][0:64, 0:GW], wq[:, k, 192:256], cqT[:, k, gs], k == 0, k == 3, [wqk] + cqr, ["ps2"])
                    t1, t1k = t1r.next()
                    t2, t2k = t2r.next()
                    P.tt("dve", t1[:], ps[1][0:64, 0:GW], cos2[:, gs], ALU.mult, ["ps1", "cos"], [t1k])
                    P.tt("dve", t2[:], ps[2][0:64, 0:GW], sin2[:, gs], ALU.mult, ["ps2", "sin"], [t2k])
                    P.tt("pool", qr[:, gs], t1[:], t2[:], ALU.add, [t1k, t2k], [qrk])
                    for k in range(2):
                        P.mm(ps[3][:, 0:GW], wk[:, k, 0:128], ckT[:, k, gs], k == 0, k == 1, [wkk] + ckr, ["ps3"])
                    P.cp("act", kn[:, gs], ps[3][:, 0:GW], ["ps3"], [knk])
                for t in range(NT):
                    for k in range(2):
                        P.mm(ps[4][:, 0:128], ckT[:, k, t * 128:(t + 1) * 128], wk[:, k, 128:256], k == 0, k == 1,
                             [wkk, ("ckT", t)], ["ps4"])
                    P.cp("dve", va[:, t, 0:128], ps[4][:, 0:128], ["ps4"], [vak])
                P.memset("pool", va[:, :, 128:129], 1.0, [vak])
                TPG = GW // 128
                for g in range(NG):
                    gs0 = g * GW
                    nkt = (g + 1) * TPG
                    for j in range(nkt):
                        r0 = max(0, j - g * TPG)
                        c0 = r0 * 128
                        pt, ptk = ptr_.next()
                        sb = ps[j % 2]
                        sbk = f"ps{j % 2}"
                        P.mm(sb[:, c0:GW], kn[:, j * 128:(j + 1) * 128], qn[:, gs0 + c0:gs0 + GW], True, False, [knk, qnk], [sbk])
                        P.mm(sb[:, c0:GW], krT[:, j * 128:(j + 1) * 128], qr[:, gs0 + c0:gs0 + GW], False, True, ["krT", qrk], [sbk])
                        P.act(pt[:, c0:GW], sb[:, c0:GW], AF.Exp, [sbk], [ptk], scale=scale)
                        if j >= g * TPG:
                            P.tt("pool", pt[:, c0:c0 + 128], pt[:, c0:c0 + 128], c["trib"][:], ALU.mult, [ptk, "const"], [ptk])
                        for r in range(r0, TPG):
                            qi = g * TPG + r
                            P.mm(ps[2 + r][:, 0:129], pt[:, r * 128:(r + 1) * 128], va[:, j, :], j == 0, j == qi,
                                 [ptk, vak], [f"ps{2 + r}"])
                            if j == qi:
                                y, yk = yr.next()
                                sm, smk = smr.next()
                                P.add("dve", lambda e, sm=sm, r=r: e.reciprocal(out=sm[:, 0:1], in_=ps[2 + r][:, 128:129]),
                                      [f"ps{2 + r}"], [smk])
                                P.ts("dve", y[:], ps[2 + r][:, 0:128], sm[:, 0:1], None, ALU.mult, None,
                                     [f"ps{2 + r}", smk], [yk])
                                P.dma("sp", Sc["yc"][qi * 128:(qi + 1) * 128, hd * 128:(hd + 1) * 128], y[:], r=[yk])

    def swa(self, l):
        P, S, NT, A, Sc, c = self.P, self.S, self.NT, self.A, self.Sc, self.c
        ps = self.ps
        scale = 64.0 ** -0.5
        with P.phase() as st:
            sk_ = self.bcast_row(st, "sw_sink", A["swa_sinks"][l], 16)
            P.act(sk_[:], sk_[:], AF.Exp, ["sw_sink"], ["sw_sink"])
            qr = Ring(P, st, "sw_q", 2, [64, 4, S], BF16)
            kr = Ring(P, st, "sw_k", 2, [64, S], BF16)
            var = Ring(P, st, "sw_va", 2, [128, NT, 65], BF16)
            ptr_ = Ring(P, st, "sw_pt", 4, [128, 512], BF16)
            yr = Ring(P, st, "sw_y", 3, [128, 256], BF16)
            smr = Ring(P, st, "sw_sm", 3, [128, 4], F32)
            for g in range(4):
                q, qk = qr.next()
                k, kk = kr.next()
                va, vak = var.next()
                P.dma("sp", q[:], Sc["ufm16"][O_SWQ + g * 256:O_SWQ + (g + 1) * 256, :].rearrange("(r p) s -> p r s", p=64), w=[qk])
                P.dma("sp", k[:], Sc["ufm16"][O_SWK + g * 64:O_SWK + (g + 1) * 64, :], w=[kk])
                P.dma("sp", va[:, :, 0:64], Sc["utm16"][:, O_SWV + g * 64:O_SWV + (g + 1) * 64].rearrange("(t p) c -> p t c", p=128), w=[vak])
                P.memset("pool", va[:, :, 64:65], 1.0, [vak])
                for n in range(NT):
                    sl = slice(n * 128, (n + 1) * 128)
                    pc, pck = ptr_.next()
                    P.mm(ps[0][:, 0:512].rearrange("p (r s) -> p r s", r=4), k[:, sl], q[:, :, sl], True, True, [kk, qk], ["ps0"])
                    P.act(pc[:], ps[0][:, 0:512], AF.Exp, ["ps0"], [pck], scale=scale)
                    P.tt("pool", pc[:].rearrange("p (r s) -> p r s", r=4), pc[:].rearrange("p (r s) -> p r s", r=4),
                         c["trib"][:].unsqueeze(1).to_broadcast([128, 4, 128]), ALU.mult, [pck, "const"], [pck])
                    if n > 0:
                        pp, ppk = ptr_.next()
                        psl = slice((n - 1) * 128, n * 128)
                        P.mm(ps[1][:, 0:512].rearrange("p (r s) -> p r s", r=4), k[:, psl], q[:, :, sl], True, True, [kk, qk], ["ps1"])
                        P.act(pp[:], ps[1][:, 0:512], AF.Exp, ["ps1"], [ppk], scale=scale)
                        P.tt("pool", pp[:].rearrange("p (r s) -> p r s", r=4), pp[:].rearrange("p (r s) -> p r s", r=4),
                             c["lowb"][:].unsqueeze(1).to_broadcast([128, 4, 128]), ALU.mult, [ppk, "const"], [ppk])
                    ob = ps[2 + (n % 2)]
                    obk = f"ps{2 + (n % 2)}"
                    for r in range(4):
                        if n > 0:
                            P.mm(ob[:, r * 65:(r + 1) * 65], pp[:, r * 128:(r + 1) * 128], va[:, n - 1, :], True, False, [ppk, vak], [obk])
                        P.mm(ob[:, r * 65:(r + 1) * 65], pc[:, r * 128:(r + 1) * 128], va[:, n, :], n == 0, True, [pck, vak], [obk])
                    y, yk = yr.next()
                    sm, smk = smr.next()
                    o3 = ob[:, 0:260].rearrange("p (r d) -> p r d", d=65)
                    P.tt("dve", sm[:].unsqueeze(2), o3[:, :, 64:65], sk_[:, g * 4:(g + 1) * 4].unsqueeze(2), ALU.add, [obk, "sw_sink"], [smk])
                    P.add("dve", lambda e, sm=sm: e.reciprocal(out=sm[:], in_=sm[:]), [smk], [smk])
                    P.tt("dve", y[:].rearrange("p (r d) -> p r d", d=64), o3[:, :, 0:64], sm[:].unsqueeze(2).to_broadcast([128, 4, 64]),
                         ALU.mult, [obk, smk], [yk])
                    P.dma("sp", Sc["yd"][sl, g * 256:(g + 1) * 256], y[:], r=[yk])

    def merge(self, l):
        P, S, NT, A, Sc, c = self.P, self.S, self.NT, self.A, self.Sc, self.c
        ps, pt, GW, NG = self.ps, self.pt, self.GW, self.NG
        TPG = GW // 128
        with P.phase() as st:
            yTr = [P.sbuf(f"mg_yT{n}", [128, 8, GW], BF16, st) for n in range(4)]
            ytr = Ring(P, st, "mg_yt", 3, [128, 1024], BF16)
            wbr = Ring(P, st, "mg_wb", 2, [128, 8, 512], BF16)
            wor = Ring(P, st, "mg_wo", 2, [128, 16, 512], BF16)
            gtr = Ring(P, st, "mg_g", 3, [128, GW], BF16)
            tmr = Ring(P, st, "mg_tmp", 2, [128, GW], F32)
            mT = P.sbuf("mg_mT", [128, 16, GW], F32, st)
            mTb = P.sbuf("mg_mTb", [128, 16, GW], BF16, st)
            hr = Ring(P, st, "mg_h", 3, [128, 512], F32)
            ptr = KeyRing(pt, ["pt0", "pt1"])
            for g in range(NG):
                gs = slice(g * GW, (g + 1) * GW)
                for n, nm in enumerate("abcd"):
                    for tt_ in range(TPG):
                        t = g * TPG + tt_
                        yt, ytk = ytr.next()
                        P.dma("sp", yt[:], Sc["y" + nm][t * 128:(t + 1) * 128, :], w=[ytk])
                        for q in range(2):
                            pb, pbk = ptr.next()
                            pv = pb[:, 0:512].rearrange("p (a b) -> p a b", b=128)
                            for j in range(4):
                                P.tr(pv[:, j, :], yt[:, (q * 4 + j) * 128:(q * 4 + j + 1) * 128], c["ident"][:], [ytk, "const"], [pbk])
                            P.cp("dve" if q == 0 else "act", yTr[n][:, q * 4:(q + 1) * 4, tt_ * 128:(tt_ + 1) * 128], pv[:, 0:4, :],
                                 [pbk], [("yT", n)])
                for n in range(4):
                    for dcb in range(4):
                        wb, wbk = wbr.next()
                        P.dma("pool", wb[:], A["w_branch"][l, n][:, dcb * 512:(dcb + 1) * 512].rearrange("(c p) n -> p c n", p=128), w=[wbk])
                        for dc in range(4):
                            dch = dcb * 4 + dc
                            pb, pbk = ps[dc % 4], f"ps{dc % 4}"
                            for k in range(8):
                                P.mm(pb[:, 0:GW], wb[:, k, dc * 128:(dc + 1) * 128], yTr[n][:, k, :], k == 0, k == 7,
                                     [wbk, ("yT", n)], [pbk])
                            gt, gtk = gtr.next()
                            r0 = O_GATE + n * D + dch * 128
                            P.dma("sp", gt[:], Sc["ufm16"][r0:r0 + 128, gs], w=[gtk])
                            if n == 0:
                                P.tt("dve", mT[:, dch, :], pb[:, 0:GW], gt[:], ALU.mult, [pbk, gtk], [("mT", dch)])
                            else:
                                tmp, tmk = tmr.next()
                                P.tt("dve", tmp[:], pb[:, 0:GW], gt[:], ALU.mult, [pbk, gtk], [tmk])
                                P.tt("pool", mT[:, dch, :], mT[:, dch, :], tmp[:], ALU.add, [("mT", dch), tmk], [("mT", dch)])
                for dch in range(16):
                    P.cp("act", mTb[:, dch, :], mT[:, dch, :], [("mT", dch)], [("mTb", dch)])
                mr = [("mTb", d_) for d_ in range(16)]
                for ocb in range(4):
                    wo, wok = wor.next()
                    P.dma("pool", wo[:], A["w_out"][l][:, ocb * 512:(ocb + 1) * 512].rearrange("(c p) n -> p c n", p=128), w=[wok])
                    for tt_ in range(TPG):
                        t = g * TPG + tt_
                        pb, pbk = ps[4 + tt_ % 2], f"ps{4 + tt_ % 2}"
                        ht, hk = hr.next()
                        P.dma("sp", ht[:], Sc["h"][t * 128:(t + 1) * 128, ocb * 512:(ocb + 1) * 512], w=[hk])
                        for k in range(16):
                            P.mm(pb[:, 0:512], mTb[:, k, tt_ * 128:(tt_ + 1) * 128], wo[:, k, :], k == 0, k == 15, [wok] + mr, [pbk])
                        P.tt("dve", ht[:], ht[:], pb[:, 0:512], ALU.add, [hk, pbk], [hk])
                        P.dma("sp", Sc["h"][t * 128:(t + 1) * 128, ocb * 512:(ocb + 1) * 512], ht[:], r=[hk])

    def xattn(self, l):
        P, S, NT, A, Sc, c = self.P, self.S, self.NT, self.A, self.Sc, self.c
        ps, pt, GW, NG = self.ps, self.pt, self.GW, self.NG
        scale = 128.0 ** -0.5
        with P.phase() as st:
            hnT = P.sbuf("xa_hnT", [128, KC, S], BF16, st)
            mnT = P.sbuf("xa_mnT", [128, KC, MEM], BF16, st)
            with P.phase() as st2:
                self.norm_T(st2, Sc["h"], A["norm_cross"][l], hnT, "hnT", S, D)
            with P.phase() as st2:
                self.norm_T(st2, A["mem"], A["norm_mem"][l], mnT, "mnT", MEM, D)
            wkv = P.sbuf("xa_wkv", [128, KC, 1024], BF16, st)
            wq = P.sbuf("xa_wq", [128, KC, 512], BF16, st)
            wo = P.sbuf("xa_wo", [128, 4, D], BF16, st)
            rr = lambda ap: ap.rearrange("(c p) n -> p c n", p=128)
            P.dma("pool", wkv[:, :, 0:512], rr(A["xa_wkv"][l][:, 0:512]), w=["wkv"])
            P.dma("pool", wkv[:, :, 512:1024], rr(A["xa_wkv"][l][:, 512:1024]), w=["wkv"])
            P.dma("pool", wq[:], rr(A["xa_wq"][l]), w=["wq"])
            for q in range(4):
                P.dma("pool", wo[:, :, q * 512:(q + 1) * 512], rr(A["xa_wo"][l][:, q * 512:(q + 1) * 512]), w=["wo"])
            KT = P.sbuf("xa_KT", [128, 4, MEM], BF16, st)
            VA = P.sbuf("xa_VA", [128, 2, 4, 129], BF16, st)
            QT = P.sbuf("xa_QT", [128, 4, S], BF16, st)
            mnr = [("mnT", t) for t in range(MEM // 128)]
            hnr = [("hnT", t) for t in range(NT)]
            for hd in range(4):
                for k in range(KC):
                    P.mm(ps[0][:, 0:MEM], wkv[:, k, hd * 128:(hd + 1) * 128], mnT[:, k, :], k == 0, k == KC - 1, ["wkv"] + mnr, ["ps0"])
                P.cp("act", KT[:, hd, :], ps[0][:, 0:MEM], ["ps0"], ["KT"])
            for mt in range(2):
                for k in range(KC):
                    P.mm(ps[1][:, 0:512], mnT[:, k, mt * 128:(mt + 1) * 128], wkv[:, k, 512:1024], k == 0, k == KC - 1, ["wkv"] + mnr, ["ps1"])
                P.cp("dve", VA[:, mt, :, 0:128], ps[1][:, 0:512].rearrange("p (h d) -> p h d", d=128), ["ps1"], ["VA"])
            P.memset("pool", VA[:, :, :, 128:129], 1.0, ["VA"])
            for hd in range(4):
                for g in range(NG):
                    pb, pbk = ps[2 + g % 2], f"ps{2 + g % 2}"
                    for k in range(KC):
                        P.mm(pb[:, 0:GW], wq[:, k, hd * 128:(hd + 1) * 128], hnT[:, k, g * GW:(g + 1) * GW], k == 0, k == KC - 1,
                             ["wq"] + hnr, [pbk])
                    P.cp("act" if g % 2 == 0 else "dve", QT[:, hd, g * GW:(g + 1) * GW], pb[:, 0:GW], [pbk], ["QT"])
            ptr_ = Ring(P, st, "xa_pt", 4, [128, GW], BF16)
            oTr = Ring(P, st, "xa_oT", 2, [128, 4, 128], BF16)
            smr = Ring(P, st, "xa_sm", 4, [128, 1], F32)
            hr = Ring(P, st, "xa_h", 2, [128, D], F32)
            TPG = GW // 128
            for g in range(NG):
                gs = slice(g * GW, (g + 1) * GW)
                pts = {}
                for hd in range(4):
                    for mt in range(2):
                        pb, pbk = ps[mt], f"ps{mt}"
                        p_, pk_ = ptr_.next()
                        P.mm(pb[:, 0:GW], KT[:, hd, mt * 128:(mt + 1) * 128], QT[:, hd, gs], True, True, ["KT", "QT"], [pbk])
                        P.act(p_[:], pb[:, 0:GW], AF.Exp, [pbk], [pk_], scale=scale)
                        pts[(hd % 2, mt)] = (p_, pk_)
                    for tt_ in range(TPG):
                        t = g * TPG + tt_
                        ob, obk = ps[2 + tt_ % 2], f"ps{2 + tt_ % 2}"
                        for mt in range(2):
                            p_, pk_ = pts[(hd % 2, mt)]
                            P.mm(ob[:, 0:129], p_[:, tt_ * 128:(tt_ + 1) * 128], VA[:, mt, hd, :], mt == 0, mt == 1, [pk_, "VA"], [obk])
                        sm, smk = smr.next()
                        o_, ok_ = self._xa_o(st, t)
                        P.add("dve", lambda e, sm=sm, ob=ob: e.reciprocal(out=sm[:, 0:1], in_=ob[:, 128:129]), [obk], [smk])
                        P.ts("dve", o_[:, hd * 128:(hd + 1) * 128], ob[:, 0:128], sm[:, 0:1], None, ALU.mult, None, [obk, smk], [(ok_, hd)])
                for tt_ in range(TPG):
                    t = g * TPG + tt_
                    o_, ok_ = self._xa_o(st, t)
                    oT, oTk = oTr.next()
                    pb, pbk = pt[tt_ % 2], f"pt{tt_ % 2}"
                    pv = pb[:, 0:512].rearrange("p (a b) -> p a b", b=128)
                    for j in range(4):
                        P.tr(pv[:, j, :], o_[:, j * 128:(j + 1) * 128], c["ident"][:], [(ok_, j), "const"], [pbk])
                    P.cp("act", oT[:], pv[:, 0:4, :], [pbk], [oTk])
                    ht, hk = hr.next()
                    P.dma("sp", ht[:], Sc["h"][t * 128:(t + 1) * 128, :], w=[hk])
                    for ocb in range(4):
                        ob, obk = ps[4 + ocb % 2], f"ps{4 + ocb % 2}"
                        for k in range(4):
                            P.mm(ob[:, 0:512], oT[:, k, :], wo[:, k, ocb * 512:(ocb + 1) * 512], k == 0, k == 3, [oTk, "wo"], [obk])
                        P.tt("dve", ht[:, ocb * 512:(ocb + 1) * 512], ht[:, ocb * 512:(ocb + 1) * 512], ob[:, 0:512], ALU.add, [hk, obk], [hk])
                    P.dma("sp", Sc["h"][t * 128:(t + 1) * 128, :], ht[:], r=[hk])

    def _xa_o(self, st, t):
        if not hasattr(self, "_xao") or self._xao_st is not st:
            self._xao = {}
            self._xao_st = st
        if t not in self._xao:
            self.P.uid += 1
            self._xao[t] = (self.P.sbuf("xa_ot", [128, 512], BF16, st), f"xa_ot#{self.P.uid}")
        return self._xao[t]

    def ffn_layer(self, l):
        P, S, NT, A, Sc, c = self.P, self.S, self.NT, self.A, self.Sc, self.c
        ps, pt, GW, NG = self.ps, self.pt, self.GW, self.NG
        TPG = GW // 128
        moe = (l % 2 == 1)
        FC = FFN // 128
        with P.phase() as st:
            hacc = P.sbuf("ff_hacc", [128, TPG, D], F32, st)
            hnT = P.sbuf("ff_hnT", [128, KC, GW], BF16, st)
            actT = P.sbuf("ff_actT", [128, FC, GW], BF16, st)
            w13r = Ring(P, st, "ff_w13", 2, [128, KC, 2, 128], BF16)
            w2r = Ring(P, st, "ff_w2", 2, [128, 8, 512], BF16)
            sir = Ring(P, st, "ff_si", 2, [128, GW], F32)
            gcol = P.sbuf("ff_gcol", [128, KC], F32, st)
            P.dma("sp", gcol[:], A["norm_ffn"][l].rearrange("(c p) -> p c", p=128), w=["gcol"], allow_slow_non_contiguous=True)
            hbr = Ring(P, st, "ff_hb", 2, [128, D], BF16)
            junk = P.sbuf("ff_junk", [128, D], BF16, st)
            ssr = Ring(P, st, "ff_ss", 2, [128, 2], F32)
            if moe:
                hn32 = Ring(P, st, "ff_hn32", 1, [128, D], F32)
                hnT32 = P.sbuf("ff_hnT32", [128, KC, 128], F32, st)
                rt = P.sbuf("ff_rt", [128, KC, NEXP], F32, st)
                P.dma("sp", rt[:], A["moe_router"][0].rearrange("(c p) e -> p c e", p=128), w=["rt"])
                gates = P.sbuf("ff_gates", [128, TPG, NEXP], F32, st)
                lgr = Ring(P, st, "ff_lg", 2, [128, 24], F32)
            for g in range(NG):
                for tt_ in range(TPG):
                    t = g * TPG + tt_
                    hb, hbk = hbr.next()
                    ss, sk = ssr.next()
                    P.dma("sp", hacc[:, tt_, :], Sc["h"][t * 128:(t + 1) * 128, :], w=[("hacc", tt_)])
                    P.act(junk[:], hacc[:, tt_, :], AF.Square, [("hacc", tt_)], ["ff_junk", sk], accum_out=ss[:, 0:1])
                    P.ts("dve", ss[:, 1:2], ss[:, 0:1], 1.0 / D, 1e-6, ALU.mult, ALU.add, [sk], [sk])
                    P.act(ss[:, 1:2], ss[:, 1:2], AF.Sqrt, [sk], [sk])
                    P.add("dve", lambda e, ss=ss: e.reciprocal(out=ss[:, 1:2], in_=ss[:, 1:2]), [sk], [sk])
                    P.ts("pool", hb[:], hacc[:, tt_, :], ss[:, 1:2], None, ALU.mult, None, [("hacc", tt_), sk], [hbk])
                    for c4 in range(0, KC, 4):
                        pb, pbk = pt[(c4 // 4) % 2], f"pt{(c4 // 4) % 2}"
                        pv = pb[:, 0:512].rearrange("p (a b) -> p a b", b=128)
                        for j in range(4):
                            P.tr(pv[:, j, :], hb[:, (c4 + j) * 128:(c4 + j + 1) * 128], c["ident"][:], [hbk, "const"], [pbk])
                        P.tt("dve", hnT[:, c4:c4 + 4, tt_ * 128:(tt_ + 1) * 128], pv[:, 0:4, :],
                             gcol[:, c4:c4 + 4].unsqueeze(2).to_broadcast([128, 4, 128]), ALU.mult, [pbk, "gcol"], ["hnT"])
                    if moe:
                        h32, h32k = hn32.next()
                        lg, lgk = lgr.next()
                        P.ts("pool", h32[:], hacc[:, tt_, :], ss[:, 1:2], None, ALU.mult, None, [("hacc", tt_), sk], [h32k])
                        for c4 in range(0, KC, 4):
                            pb, pbk = ps[(c4 // 4) % 2], f"ps{(c4 // 4) % 2}"
                            pv = pb[:, 0:512].rearrange("p (a b) -> p a b", b=128)
                            for j in range(4):
                                P.tr(pv[:, j, :], h32[:, (c4 + j) * 128:(c4 + j + 1) * 128], c["identf"][:], [h32k, "const"], [pbk])
                            P.tt("dve", hnT32[:, c4:c4 + 4, :], pv[:, 0:4, :],
                                 gcol[:, c4:c4 + 4].unsqueeze(2).to_broadcast([128, 4, 128]), ALU.mult, [pbk, "gcol"], ["hnT32"])
                        for k in range(KC):
                            P.mm(ps[2][:, 0:NEXP], hnT32[:, k, :], rt[:, k, :], k == 0, k == KC - 1, ["hnT32", "rt"], ["ps2"])
                        P.cp("dve", lg[:, 0:8], ps[2][:, 0:NEXP], ["ps2"], [lgk])
                        P.add("dve", lambda e, lg=lg: e.max(out=lg[:, 8:16], in_=lg[:, 0:8]), [lgk], [lgk])
                        P.ts("dve", lg[:, 16:24], lg[:, 0:8], lg[:, 8:9], None, ALU.subtract, None, [lgk], [lgk])
                        P.act(lg[:, 16:24], lg[:, 16:24], AF.Exp, [lgk], [lgk])
                        P.ts("dve", lg[:, 0:8], lg[:, 0:8], lg[:, 9:10], None, ALU.is_ge, None, [lgk], [lgk])
                        P.tt("dve", lg[:, 16:24], lg[:, 16:24], lg[:, 0:8], ALU.mult, [lgk], [lgk])
                        P.add("dve", lambda e, lg=lg: e.reduce_sum(out=lg[:, 10:11], in_=lg[:, 16:24], axis=AX.X), [lgk], [lgk])
                        P.add("dve", lambda e, lg=lg: e.reciprocal(out=lg[:, 10:11], in_=lg[:, 10:11]), [lgk], [lgk])
                        P.ts("dve", gates[:, tt_, :], lg[:, 16:24], lg[:, 10:11], None, ALU.mult, None, [lgk], [("gates", tt_)])
                for e_ in range(NEXP if moe else 1):
                    W13 = A["moe_w13"][0, e_] if moe else A["ffn_w13"][0]
                    W2 = A["moe_w2"][0, e_] if moe else A["ffn_w2"][0]
                    for fb in range(0, FC, 1):
                        w13, w13k = w13r.next()
                        P.dma("pool", w13[:, :, 0, :], W13[:, fb * 128:(fb + 1) * 128].rearrange("(c p) n -> p c n", p=128), w=[w13k])
                        P.dma("pool", w13[:, :, 1, :], W13[:, FFN + fb * 128:FFN + (fb + 1) * 128].rearrange("(c p) n -> p c n", p=128), w=[w13k])
                        for f2 in range(1):
                            f = fb + f2
                            for k in range(KC):
                                P.mm(ps[0][:, 0:GW], w13[:, k, 0, f2 * 128:(f2 + 1) * 128], hnT[:, k, :], k == 0, k == KC - 1, [w13k, "hnT"], ["ps0"])
                            for k in range(KC):
                                P.mm(ps[1][:, 0:GW], w13[:, k, 1, f2 * 128:(f2 + 1) * 128], hnT[:, k, :], k == 0, k == KC - 1, [w13k, "hnT"], ["ps1"])
                            si, sik = sir.next()
                            P.act(si[:], ps[0][:, 0:GW], AF.Silu, ["ps0"], [sik])
                            P.tt("dve", actT[:, f, :], si[:], ps[1][:, 0:GW], ALU.mult, [sik, "ps1"], [("actT", f)])
                    ar = [("actT", f) for f in range(FC)]
                    for dcb in range(4):
                        for f8 in range(0, FC, 8):
                            w2, w2k = w2r.next()
                            P.dma("pool", w2[:], W2[f8 * 128:(f8 + 8) * 128, dcb * 512:(dcb + 1) * 512].rearrange("(c p) n -> p c n", p=128), w=[w2k])
                            for f in range(8):
                                for tt_ in range(TPG):
                                    P.mm(ps[2 + tt_][:, 0:512], actT[:, f8 + f, tt_ * 128:(tt_ + 1) * 128], w2[:, f, :],
                                         f8 + f == 0, f8 + f == FC - 1, [w2k] + ar, [f"ps{2 + tt_}"])
                        for tt_ in range(TPG):
                            hs = hacc[:, tt_, dcb * 512:(dcb + 1) * 512]
                            if moe:
                                P.stt("dve", hs, ps[2 + tt_][:, 0:512], gates[:, tt_, e_:e_ + 1], hs, ALU.mult, ALU.add,
                                      [f"ps{2 + tt_}", ("gates", tt_), ("hacc", tt_)], [("hacc", tt_)])
                            else:
                                P.tt("dve", hs, hs, ps[2 + tt_][:, 0:512], ALU.add, [f"ps{2 + tt_}", ("hacc", tt_)], [("hacc", tt_)])
                for tt_ in range(TPG):
                    t = g * TPG + tt_
                    P.dma("sp", Sc["h"][t * 128:(t + 1) * 128, :], hacc[:, tt_, :], r=[("hacc", tt_)])

    def moe_layer(self, l):
        P, S, NT, A, Sc, c = self.P, self.S, self.NT, self.A, self.Sc, self.c
        ps, pt = self.ps, self.pt
        FC = FFN // 128
        CAP = 128 * int(math.ceil(1.5 * S / 4 / 128))
        NJ = CAP // 128
        NP = int(math.ceil(CAP / 512))
        PW = CAP // NP
        NQ = 4
        FQ = FC // NQ
        with P.phase() as stg:
            gates = P.sbuf("mo_gates", [128, NT, NEXP], F32, stg)
            posm = P.sbuf("mo_posm", [128, NT, NEXP], F32, stg)
            iota = P.sbuf("mo_iota", [128, CAP], F32, stg)
            jcol = P.sbuf("mo_jcol", [128, 8], F32, stg)
            P.dma("sp", iota[:], A["c_iota"][:, 0:CAP], w=["iota"])
            P.dma("sp", jcol[:], A["c_jcol"], w=["jcol"])
            with P.phase() as st:
                grow = self.bcast_row(st, "mo_g", A["norm_ffn"][l], D)
                sutri = P.sbuf("mo_sutri", [128, 128], F32, st)
                P.dma("sp", sutri[:], A["c_sutri"], w=["sutri"])
                rt = P.sbuf("mo_rt", [128, KC, NEXP], F32, st)
                P.dma("sp", rt[:], A["moe_router"][0].rearrange("(c p) e -> p c e", p=128), w=["rt"])
                hr = Ring(P, st, "mo_h", 2, [128, D], F32)
                h32r = Ring(P, st, "mo_h32", 2, [128, D], F32)
                h16r = Ring(P, st, "mo_h16", 2, [128, D], BF16)
                junk = P.sbuf("mo_junk", [128, D], BF16, st)
                ssr = Ring(P, st, "mo_ss", 2, [128, 2], F32)
                hT32r = Ring(P, st, "mo_hT32", 2, [128, KC, 128], F32)
                lgr = Ring(P, st, "mo_lg", 2, [128, 32], F32)
                sel = P.sbuf("mo_sel", [128, NT, NEXP], F32, st)
                run = P.sbuf("mo_run", [128, NEXP], F32, st)
                P.memset("dve", run[:], 0.0, ["run"])
                for t in range(NT):
                    ht, hk = hr.next()
                    h32, h32k = h32r.next()
                    h16, h16k = h16r.next()
                    ss, sk = ssr.next()
                    hT32, hT32k = hT32r.next()
                    lg, lgk = lgr.next()
                    P.dma("sp", ht[:], Sc["h"][t * 128:(t + 1) * 128, :], w=[hk])
                    P.act(junk[:], ht[:], AF.Square, [hk], ["mo_junk", sk], accum_out=ss[:, 0:1])
                    P.ts("dve", ss[:, 1:2], ss[:, 0:1], 1.0 / D, 1e-6, ALU.mult, ALU.add, [sk], [sk])
                    P.act(ss[:, 1:2], ss[:, 1:2], AF.Sqrt, [sk], [sk])
                    P.add("dve", lambda e, ss=ss: e.reciprocal(out=ss[:, 1:2], in_=ss[:, 1:2]), [sk], [sk])
                    P.stt("dve", h32[:], ht[:], ss[:, 1:2], grow[:], ALU.mult, ALU.mult, [hk, sk, "mo_g"], [h32k])
                    P.cp("act", h16[:], h32[:], [h32k], [h16k])
                    P.dma("sp", Sc["hn16"][t * 128:(t + 1) * 128, :], h16[:], r=[h16k])
                    for c4 in range(0, KC, 4):
                        pb, pbk = ps[(c4 // 4) % 2], f"ps{(c4 // 4) % 2}"
                        pv = pb[:, 0:512].rearrange("p (a b) -> p a b", b=128)
                        for j in range(4):
                            P.tr(pv[:, j, :], h32[:, (c4 + j) * 128:(c4 + j + 1) * 128], c["identf"][:], [h32k, "const"], [pbk])
                        P.cp("dve" if (c4 // 4) % 2 == 0 else "act", hT32[:, c4:c4 + 4, :], pv[:, 0:4, :], [pbk], [hT32k])
                    for k in range(KC):
                        P.mm(ps[2][:, 0:NEXP], hT32[:, k, :], rt[:, k, :], k == 0, k == KC - 1, [hT32k, "rt"], ["ps2"])
                    P.cp("dve", lg[:, 0:8], ps[2][:, 0:NEXP], ["ps2"], [lgk])
                    P.add("dve", lambda e, lg=lg: e.max(out=lg[:, 8:16], in_=lg[:, 0:8]), [lgk], [lgk])
                    P.ts("dve", lg[:, 16:24], lg[:, 0:8], lg[:, 8:9], None, ALU.subtract, None, [lgk], [lgk])
                    P.act(lg[:, 16:24], lg[:, 16:24], AF.Exp, [lgk], [lgk])
                    P.ts("dve", sel[:, t, :], lg[:, 0:8], lg[:, 9:10], None, ALU.is_ge, None, [lgk], [("sel", t)])
                    P.tt("dve", lg[:, 16:24], lg[:, 16:24], sel[:, t, :], ALU.mult, [lgk, ("sel", t)], [lgk])
                    P.add("dve", lambda e, lg=lg: e.reduce_sum(out=lg[:, 10:11], in_=lg[:, 16:24], axis=AX.X), [lgk], [lgk])
                    P.add("dve", lambda e, lg=lg: e.reciprocal(out=lg[:, 10:11], in_=lg[:, 10:11]), [lgk], [lgk])
                    P.ts("dve", gates[:, t, :], lg[:, 16:24], lg[:, 10:11], None, ALU.mult, None, [lgk], [("gates", t)])
                    P.mm(ps[3][:, 0:NEXP], sutri[:], sel[:, t, :], True, True, ["sutri", ("sel", t)], ["ps3"])
                    P.mm(ps[3][:, 8:8 + NEXP], c["ones"][:], sel[:, t, :], True, True, ["const", ("sel", t)], ["ps3"])
                    P.tt("dve", lg[:, 24:32], ps[3][:, 0:NEXP], run[:], ALU.add, ["ps3", "run"], [lgk])
                    P.stt("dve", posm[:, t, :], lg[:, 24:32], 1.0, sel[:, t, :], ALU.add, ALU.mult, [lgk, ("sel", t)], [("posm", t)])
                    P.ts("dve", posm[:, t, :], posm[:, t, :], -1.0, None, ALU.add, None, [("posm", t)], [("posm", t)])
                    P.tt("dve", run[:], run[:], ps[3][:, 8:8 + NEXP], ALU.add, ["ps3", "run"], ["run"])
            st = stg
            xgo = P.sbuf("mo_xgo", [128, KC * CAP], BF16, st)
            xgT = xgo[:, :].rearrange("p (k j) -> p k j", j=CAP)
            outg16 = xgo[:, :].rearrange("p (j d) -> p j d", d=D)
            actT = P.sbuf("mo_actT", [128, FQ, CAP], BF16, st)
            outg32 = P.sbuf("mo_outg32", [128, NJ, D], F32, st)
            posbc = P.sbuf("mo_posbc", [128, S], F32, st)
            w13r = Ring(P, st, "mo_w13", 2, [128, KC, 2, 128], BF16)
            w2r = Ring(P, st, "mo_w2", 2, [128, 8, 512], BF16)
            hnr = Ring(P, st, "mo_hn", 3, [128, D], BF16)
            xgr = Ring(P, st, "mo_xg", 2, [128, D], BF16)
            htr = Ring(P, st, "mo_ht", 2, [128, D], F32)
            sir = Ring(P, st, "mo_si", 2, [128, PW], F32)
            pmr = Ring(P, st, "mo_pm", 4, [128, 128], BF16)
            pmTr = Ring(P, st, "mo_pmT", 2, [128, NJ, 128], BF16)
            dgr = Ring(P, st, "mo_dg", 2, [128, 128], F32)
            pmall = [("posm", t) for t in range(NT)]
            for e_ in range(NEXP):
                W13, W2 = A["moe_w13"][0, e_], A["moe_w2"][0, e_]
                for t in range(NT):
                    dg, dgk = dgr.next()
                    P.ts("pool", dg[:], c["identf"][:], posm[:, t, e_:e_ + 1], None, ALU.mult, None, ["const", ("posm", t)], [dgk])
                    pb, pbk = ps[4 + t % 2], f"ps{4 + t % 2}"
                    P.mm(pb[:, 0:128], c["ones"][:], dg[:], True, True, ["const", dgk], [pbk])
                    P.cp("act", posbc[:, t * 128:(t + 1) * 128], pb[:, 0:128], [pbk], ["posbc"])
                for J in range(NJ):
                    for t in range(NT):
                        hn, hnk = hnr.next()
                        pm, pmk = pmr.next()
                        P.dma("sp", hn[:], Sc["hn16"][t * 128:(t + 1) * 128, :], w=[hnk])
                        P.ts("dve" if t % 2 == 0 else "pool", pm[:], iota[:, J * 128:(J + 1) * 128], posm[:, t, e_:e_ + 1], None,
                             ALU.is_equal, None, ["iota", ("posm", t)], [pmk])
                        for q in range(4):
                            P.mm(ps[q][:, 0:512], pm[:], hn[:, q * 512:(q + 1) * 512], t == 0, t == NT - 1, [pmk, hnk], [f"ps{q}"])
                    xg, xgk = xgr.next()
                    for q in range(4):
                        P.cp("act" if q % 2 == 0 else "dve", xg[:, q * 512:(q + 1) * 512], ps[q][:, 0:512], [f"ps{q}"], [xgk])
                    for c4 in range(0, KC, 4):
                        pb, pbk = pt[(c4 // 4) % 2], f"pt{(c4 // 4) % 2}"
                        pv = pb[:, 0:512].rearrange("p (a b) -> p a b", b=128)
                        for j in range(4):
                            P.tr(pv[:, j, :], xg[:, (c4 + j) * 128:(c4 + j + 1) * 128], c["ident"][:], [xgk, "const"], [pbk])
                        P.cp("dve" if (c4 // 4) % 2 == 0 else "act", xgT[:, c4:c4 + 4, J * 128:(J + 1) * 128], pv[:, 0:4, :], [pbk], ["xgo"])
                for fq in range(NQ):
                    for fl in range(FQ):
                        f = fq * FQ + fl
                        w13, w13k = w13r.next()
                        P.dma("pool", w13[:, :, 0, :], W13[:, f * 128:(f + 1) * 128].rearrange("(c p) n -> p c n", p=128), w=[w13k])
                        P.dma("pool", w13[:, :, 1, :], W13[:, FFN + f * 128:FFN + (f + 1) * 128].rearrange("(c p) n -> p c n", p=128), w=[w13k])
                        for pc in range(NP):
                            cs = slice(pc * PW, (pc + 1) * PW)
                            b1, b1k = ps[pc], f"ps{pc}"
                            b3, b3k = ps[2 + pc], f"ps{2 + pc}"
                            for k in range(KC):
                                P.mm(b1[:, 0:PW], w13[:, k, 0, :], xgT[:, k, cs], k == 0, k == KC - 1, [w13k, "xgo"], [b1k])
                            for k in range(KC):
                                P.mm(b3[:, 0:PW], w13[:, k, 1, :], xgT[:, k, cs], k == 0, k == KC - 1, [w13k, "xgo"], [b3k])
                            si, sik = sir.next()
                            P.act(si[:], b1[:, 0:PW], AF.Silu, [b1k], [sik])
                            P.tt("dve", actT[:, fl, cs], si[:], b3[:, 0:PW], ALU.mult, [sik, b3k], [("actT", fl)])
                    ar = [("actT", fl) for fl in range(FQ)]
                    for dcb in range(4):
                        for f8 in range(0, FQ, 8):
                            nf = min(8, FQ - f8)
                            w2, w2k = w2r.next()
                            r0 = (fq * FQ + f8) * 128
                            P.dma("pool", w2[:, 0:nf, :], W2[r0:r0 + nf * 128, dcb * 512:(dcb + 1) * 512].rearrange("(c p) n -> p c n", p=128), w=[w2k])
                            for f in range(nf):
                                for J in range(NJ):
                                    P.mm(ps[J][:, 0:512], actT[:, f8 + f, J * 128:(J + 1) * 128], w2[:, f, :],
                                         f8 + f == 0, f8 + f == FQ - 1, [w2k] + ar, [f"ps{J}"])
                        for J in range(NJ):
                            dst32 = outg32[:, J, dcb * 512:(dcb + 1) * 512]
                            if fq == 0:
                                P.cp("act" if J % 2 == 0 else "dve", dst32, ps[J][:, 0:512], [f"ps{J}"], [("og32", J, dcb)])
                            elif fq < NQ - 1:
                                P.tt("dve", dst32, dst32, ps[J][:, 0:512], ALU.add, [f"ps{J}", ("og32", J, dcb)], [("og32", J, dcb)])
                            else:
                                P.tt("dve", outg16[:, J, dcb * 512:(dcb + 1) * 512], dst32, ps[J][:, 0:512], ALU.add,
                                     [f"ps{J}", ("og32", J, dcb)], ["xgo"])
                for t in range(NT):
                    ht, hk = htr.next()
                    pmT, pmTk = pmTr.next()
                    P.dma("sp", ht[:], Sc["h"][t * 128:(t + 1) * 128, :], r=[("hD", t)], w=[hk])
                    for J in range(NJ):
                        P.ts("dve" if J % 2 == 0 else "pool", pmT[:, J, :], posbc[:, t * 128:(t + 1) * 128], jcol[:, J:J + 1], None,
                             ALU.is_equal, None, ["posbc", "jcol"], [pmTk])
                    for dcb in range(4):
                        pb, pbk = ps[dcb], f"ps{dcb}"
                        for J in range(NJ):
                            P.mm(pb[:, 0:512], pmT[:, J, :], outg16[:, J, dcb * 512:(dcb + 1) * 512], J == 0, J == NJ - 1, [pmTk, "xgo"], [pbk])
                        hs = ht[:, dcb * 512:(dcb + 1) * 512]
                        P.stt("dve", hs, pb[:, 0:512], gates[:, t, e_:e_ + 1], hs, ALU.mult, ALU.add, [pbk, ("gates", t), hk], [hk])
                    P.dma("sp", Sc["h"][t * 128:(t + 1) * 128, :], ht[:], r=[hk], w=[("hD", t)])


def make_consts(S):
    bf = ml_dtypes.bfloat16
    i = np.arange(128)
    tri = (i[:, None] <= i[None, :]).astype(np.float32)
    inv = 1.0 / (10000.0 ** (np.arange(0, 64, 2, dtype=np.float32) / 64.0))
    ang = np.arange(S, dtype=np.float32)[:, None] * inv[None, :]
    cos, sin = np.cos(ang).astype(np.float32).T, np.sin(ang).astype(np.float32).T
    return dict(
        c_ident=np.eye(128, dtype=np.float32).astype(bf), c_identf=np.eye(128, dtype=np.float32),
        c_tri=tri, c_trib=tri.astype(bf), c_lowb=(1.0 - tri).astype(bf),
        c_mneg=((1.0 - tri) * NEG).astype(np.float32), c_ones=np.ones((128, 128), np.float32),
        c_sutri=(tri - np.eye(128, dtype=np.float32)).astype(np.float32),
        c_iota=np.ascontiguousarray(np.broadcast_to(np.arange(1024, dtype=np.float32)[None, :], (128, 1024))),
        c_jcol=np.ascontiguousarray((np.arange(8, dtype=np.float32)[None, :] * 128 + i[:, None]).astype(np.float32)),
        c_cos2=np.ascontiguousarray(np.concatenate([cos, cos], 0)),
        c_sin2=np.ascontiguousarray(np.concatenate([-sin, sin], 0)),
    )


_CACHE = {}


def kernel(**inputs):
    S = inputs["x"].shape[1]
    B = inputs["x"].shape[0]
    if S not in _CACHE:
        _CACHE[S] = Builder(S).build()
    nc = _CACHE[S]
    consts = make_consts(S)
    shared = {k: np.ascontiguousarray(v) for k, v in inputs.items() if k not in ("x", "mem")}
    in_maps = []
    for b in range(B):
        m = dict(shared)
        m.update(consts)
        m["x"] = np.ascontiguousarray(inputs["x"][b])
        m["mem"] = np.ascontiguousarray(inputs["mem"][b])
        in_maps.append(m)
    res = run_bass_kernel_spmd(nc, in_maps, core_ids=list(range(B)))
    return np.stack([r["out"] for r in res.results], axis=0).astype(np.float32)
```

```python
import contextlib
import math
import numpy as np
import ml_dtypes
import concourse.bass as bass
import concourse.mybir as mybir
from concourse.bass_utils import run_bass_kernel_spmd

F32 = mybir.dt.float32
BF16 = mybir.dt.bfloat16
AF = mybir.ActivationFunctionType
ALU = mybir.AluOpType
AX = mybir.AxisListType

EPOCH = 4000
N_DMA_SEMS = 40

D = 2048
KC = D // 128
IN_TOTAL = 16216
FFN = 7168
NEXP = 8
MEM = 256
O_MLQ, O_MLK, O_MLV, O_MLO, O_MLI, O_MLF = 0, 512, 1024, 2048, 3072, 3076
O_Z, O_XBC, O_DT, O_CQ, O_CKV, O_KR = 3080, 4104, 5640, 5656, 6168, 6424
O_SWQ, O_SWK, O_SWV, O_GATE = 6488, 7512, 7768, 8024
NEG = -30000.0


class Prog:
    ENGS = ("pe", "act", "dve", "pool", "sp")

    def __init__(self, nc):
        self.nc = nc
        self.ops = []
        self.last_w = {}
        self.readers = {}
        self.stack = contextlib.ExitStack()
        self.nd = 0
        self.last_on_eng = {}
        self.last_dma_slot = {}
        self.uid = 0

    def sbuf(self, name, shape, dtype, stack=None):
        self.uid += 1
        return (stack or self.stack).enter_context(
            self.nc.sbuf_tensor(f"{name}_{self.uid}", list(shape), dtype))

    def psum(self, name, shape, dtype):
        return self.stack.enter_context(self.nc.psum_tensor(name, list(shape), dtype))

    def dram(self, name, shape, dtype, kind="Internal"):
        return self.nc.dram_tensor(name, list(shape), dtype, kind=kind).ap()

    def add(self, eng, emit, reads=(), writes=(), dma=False):
        idx = len(self.ops)
        raw, other = set(), set()
        for r in reads:
            w = self.last_w.get(r)
            if w is not None:
                raw.add(w)
        for r in writes:
            w = self.last_w.get(r)
            if w is not None:
                other.add(w)
            for rd in self.readers.get(r, ()):
                other.add(rd)
        for r in reads:
            lst = self.readers.setdefault(r, [])
            if not dma:
                lst[:] = [j for j in lst if self.ops[j]["dma"] or self.ops[j]["eng"] != eng]
            lst.append(idx)
        for r in writes:
            self.last_w[r] = idx
            self.readers[r] = []
        raw.discard(idx)
        other.discard(idx)
        other -= raw
        op = dict(eng=eng, emit=emit, raw=raw, other=other, dma=dma, need_inc=False,
                  ticket=None, explicit=None)
        if dma:
            op["slot"] = self.nd % N_DMA_SEMS
            self.nd += 1
            self.last_dma_slot[op["slot"]] = idx
        self.last_on_eng[eng] = idx
        self.ops.append(op)
        return idx

    def barrier(self):
        targets = set(self.last_on_eng.values()) | set(self.last_dma_slot.values())
        for e in self.ENGS:
            self.ops.append(dict(eng=e, emit=None, raw=set(), other=set(), dma=False,
                                 need_inc=False, ticket=None, explicit=set(targets)))
        self.last_w = {}
        self.readers = {}

    @contextlib.contextmanager
    def phase(self):
        st = contextlib.ExitStack()
        try:
            yield st
        finally:
            self.barrier()
            st.close()

    def dma(self, eng, out, in_, r=(), w=(), **kw):
        return self.add(eng, lambda e: e.dma_start(out=out, in_=in_, **kw), r, w, dma=True)

    def mm(self, out, lhsT, rhs, start, stop, r, w):
        return self.add("pe", lambda e: e.matmul(out=out, lhsT=lhsT, rhs=rhs, start=start, stop=stop), r, w)

    def tr(self, out, in_, ident, r, w):
        return self.add("pe", lambda e: e.transpose(out=out, in_=in_, identity=ident), r, w)

    def act(self, out, in_, func, r, w, **kw):
        return self.add("act", lambda e: e.activation(out=out, in_=in_, func=func, **kw), r, w)

    def ts(self, eng, out, in0, s1, s2, op0, op1, r, w):
        if op1 is None:
            return self.add(eng, lambda e: e.tensor_scalar(out=out, in0=in0, scalar1=s1, scalar2=None, op0=op0), r, w)
        return self.add(eng, lambda e: e.tensor_scalar(out=out, in0=in0, scalar1=s1, scalar2=s2, op0=op0, op1=op1), r, w)

    def tt(self, eng, out, in0, in1, op, r, w):
        return self.add(eng, lambda e: e.tensor_tensor(out=out, in0=in0, in1=in1, op=op), r, w)

    def stt(self, eng, out, in0, scalar, in1, op0, op1, r, w):
        return self.add(eng, lambda e: e.scalar_tensor_tensor(out=out, in0=in0, scalar=scalar, in1=in1,
                                                              op0=op0, op1=op1), r, w)

    def cp(self, eng, out, in_, r, w):
        if eng == "act":
            return self.add(eng, lambda e: e.copy(out=out, in_=in_), r, w)
        return self.add(eng, lambda e: e.tensor_copy(out=out, in_=in_), r, w)

    def memset(self, eng, ap, val, w):
        return self.add(eng, lambda e: e.memset(ap, val), (), w)

    def emit_all(self):
        nc, ops = self.nc, self.ops
        self.barrier()
        for i, op in enumerate(ops):
            if op["explicit"] is not None:
                deps = [j for j in op["explicit"] if j < i]
            else:
                deps = []
                for j in op["raw"]:
                    pj = ops[j]
                    if pj["eng"] == op["eng"] and not pj["dma"] and op["eng"] == "pe" and not op["dma"]:
                        continue
                    deps.append(j)
                for j in op["other"]:
                    pj = ops[j]
                    if pj["eng"] == op["eng"] and not pj["dma"] and not op["dma"]:
                        continue
                    deps.append(j)
            op["deps"] = sorted(set(deps))
            for j in op["deps"]:
                ops[j]["need_inc"] = True
        counts = {e: 0 for e in self.ENGS}
        nsem = {e: 1 for e in self.ENGS}
        dma_uses = [0] * N_DMA_SEMS
        dma_prev = [None] * N_DMA_SEMS
        for i, op in enumerate(ops):
            if op["dma"]:
                s = op["slot"]
                dma_uses[s] += 1
                op["ticket"] = ("dma", s, dma_uses[s] * 16)
                op["dma_prev"] = dma_prev[s]
                dma_prev[s] = i
            elif op["need_inc"]:
                e = op["eng"]
                ep, v = divmod(counts[e], EPOCH)
                counts[e] += 1
                op["ticket"] = (e, ep, v + 1)
                nsem[e] = max(nsem[e], ep + 1)
        sems = {}
        for e in self.ENGS:
            for ep in range(nsem[e]):
                sems[(e, ep)] = self.stack.enter_context(nc.semaphore(f"s_{e}_{ep}"))
        for s in range(N_DMA_SEMS):
            sems[("dma", s)] = self.stack.enter_context(nc.semaphore(f"s_dma_{s}"))
        print("ops", len(ops), "incs", counts, "ndma", self.nd, "nsems", len(sems), flush=True)
        by_eng = {e: [] for e in self.ENGS}
        for i, op in enumerate(ops):
            by_eng[op["eng"]].append(i)

        def run_engine(ename, eng):
            waited = {}

            def wait(t):
                key = (t[0], t[1])
                if waited.get(key, 0) >= t[2]:
                    return
                eng.wait_ge(sems[key], t[2])
                waited[key] = t[2]

            for i in by_eng[ename]:
                op = ops[i]
                for j in op["deps"]:
                    wait(ops[j]["ticket"])
                if op["emit"] is None:
                    continue
                if op["dma"] and op["dma_prev"] is not None:
                    wait(ops[op["dma_prev"]]["ticket"])
                ins = op["emit"](eng)
                t = op["ticket"]
                if t is not None:
                    ins.then_inc(sems[(t[0], t[1])], 16 if op["dma"] else 1)

        with nc.Block() as block:
            @block.tensor
            def _(e):
                run_engine("pe", e)

            @block.scalar
            def _(e):
                run_engine("act", e)

            @block.vector
            def _(e):
                run_engine("dve", e)

            @block.gpsimd
            def _(e):
                run_engine("pool", e)

            @block.sync
            def _(e):
                run_engine("sp", e)
        self.stack.close()


class Ring:
    def __init__(self, P, st, name, n, shape, dtype):
        self.n = n
        self.bufs = [P.sbuf(name, shape, dtype, st) for _ in range(n)]
        P.uid += 1
        self.keys = [f"{name}#{P.uid}#{i}" for i in range(n)]
        self.i = 0

    def next(self):
        j = self.i % self.n
        self.i += 1
        return self.bufs[j], self.keys[j]


class KeyRing:
    def __init__(self, objs, keys):
        self.objs, self.keys, self.i = objs, keys, 0

    def next(self):
        j = self.i % len(self.objs)
        self.i += 1
        return self.objs[j], self.keys[j]


class Builder:
    def __init__(self, S, depth=2, debug=False, stop_after=None):
        self.S = S
        self.NT = S // 128
        self.GW = min(512, S)
        self.NG = S // self.GW
        self.depth = depth
        self.debug = debug
        self.stop_after = stop_after
        nc = bass.Bass("TRN2", target_bir_lowering=False)
        self.nc = nc
        self.P = Prog(nc)

    def norm_T(self, st, src, gamma, dstT, dkey, R, W, eps=1e-6, dt=BF16, rows_at=None):
        P, c = self.P, self.c
        kc = W // 128
        gcol = P.sbuf("gcol", [128, kc], F32, st)
        P.dma("sp", gcol[:], gamma.rearrange("(c p) -> p c", p=128), w=["gcol"], allow_slow_non_contiguous=True)
        hr = Ring(P, st, "nt_h", 2, [128, W], F32)
        hbr = Ring(P, st, "nt_hb", 2, [128, W], dt)
        junk = P.sbuf("nt_junk", [128, W], F32, st)
        ssr = Ring(P, st, "nt_ss", 2, [128, 2], F32)
        ptr = KeyRing(self.pt if dt == BF16 else self.ps[4:6], ["pt0", "pt1"] if dt == BF16 else ["ps4", "ps5"])
        ident = c["ident"] if dt == BF16 else c["identf"]
        for t in range(R // 128):
            ht, hk = hr.next()
            hb, hbk = hbr.next()
            ss, sk = ssr.next()
            P.dma("sp", ht[:], src[t * 128:(t + 1) * 128, :], w=[hk])
            P.act(junk[:], ht[:], AF.Square, [hk], ["nt_junk", sk], accum_out=ss[:, 0:1])
            P.ts("dve", ss[:, 1:2], ss[:, 0:1], 1.0 / W, eps, ALU.mult, ALU.add, [sk], [sk])
            P.act(ss[:, 1:2], ss[:, 1:2], AF.Sqrt, [sk], [sk])
            P.add("dve", lambda e, ss=ss: e.reciprocal(out=ss[:, 1:2], in_=ss[:, 1:2]), [sk], [sk])
            P.ts("pool", hb[:], ht[:], ss[:, 1:2], None, ALU.mult, None, [hk, sk], [hbk])
            for c4 in range(0, kc, 4):
                n4 = min(4, kc - c4)
                pst, pk = ptr.next()
                pv = pst[:, 0:512].rearrange("p (a b) -> p a b", b=128)
                for j in range(n4):
                    P.tr(pv[:, j, :], hb[:, (c4 + j) * 128:(c4 + j + 1) * 128], ident[:], [hbk, "const"], [pk])
                P.tt("dve", dstT[:, c4:c4 + n4, t * 128:(t + 1) * 128], pv[:, 0:n4, :],
                     gcol[:, c4:c4 + n4].unsqueeze(2).to_broadcast([128, n4, 128]), ALU.mult,
                     [pk, "gcol"], [(dkey, t)])

    def wload(self, wring, W_ap, c0, w, kc):
        wt, wk = wring.next()
        self.P.dma("pool", wt[:, 0:kc, 0:w], W_ap[:, c0:c0 + w].rearrange("(c p) n -> p c n", p=128), w=[wk])
        return wt, wk

    def proj_fm(self, st, xT, xkey, kc, W_ap, c0, ncols, dst, rings, func=None, dtype=F32):
        P, S, GW = self.P, self.S, self.GW
        wring, psr, stg32, stg16 = rings
        stgr = stg32 if dtype == F32 else stg16
        xr = [(xkey, t) for t in range(self.NT)]
        for cb in range(0, ncols, 512):
            w = min(512, ncols - cb)
            wt, wk = self.wload(wring, W_ap, c0 + cb, w, kc)
            for sb in range(0, w, 128):
                m = min(128, w - sb)
                stg, sk = stgr.next()
                for g in range(self.NG):
                    ps, pk = psr.next()
                    for k in range(kc):
                        P.mm(ps[0:m, 0:GW], wt[:, k, sb:sb + m], xT[:, k, g * GW:(g + 1) * GW],
                             k == 0, k == kc - 1, [wk] + xr, [pk])
                    if func is not None or (g % 2 == 0):
                        P.act(stg[0:m, g * GW:(g + 1) * GW], ps[0:m, 0:GW], func or AF.Copy, [pk], [sk])
                    else:
                        P.cp("dve", stg[0:m, g * GW:(g + 1) * GW], ps[0:m, 0:GW], [pk], [sk])
                P.dma("sp", dst[cb + sb:cb + sb + m, :], stg[0:m, 0:S], r=[sk])

    def proj_tm(self, st, xT, xkey, kc, W_ap, c0, ncols, dst, rings, dtype=F32):
        P, S = self.P, self.S
        wring, psr, stg32, stg16 = rings
        stgr = stg32 if dtype == F32 else stg16
        for cb in range(0, ncols, 512):
            w = min(512, ncols - cb)
            wt, wk = self.wload(wring, W_ap, c0 + cb, w, kc)
            for t in range(self.NT):
                ps, pk = psr.next()
                stg, sk = stgr.next()
                for k in range(kc):
                    P.mm(ps[:, 0:w], xT[:, k, t * 128:(t + 1) * 128], wt[:, k, 0:w], k == 0, k == kc - 1,
                         [wk, (xkey, t)], [pk])
                if t % 2 == 0:
                    P.act(stg[:, 0:w], ps[:, 0:w], AF.Copy, [pk], [sk])
                else:
                    P.cp("dve", stg[:, 0:w], ps[:, 0:w], [pk], [sk])
                P.dma("sp", dst[t * 128:(t + 1) * 128, cb:cb + w], stg[:, 0:w], r=[sk])

    def bcast_row(self, st, name, src_row, n):
        t = self.P.sbuf(name, [128, n], F32, st)
        self.P.dma("sp", t[:], src_row.partition_broadcast(128), w=[name])
        return t

    def build(self):
        P, nc, S, NT = self.P, self.nc, self.S, self.NT
        inp = lambda n, sh, dt=F32: P.dram(n, sh, dt, kind="ExternalInput")
        A = {}
        A["x"] = inp("x", [S, D])
        A["mem"] = inp("mem", [MEM, D])
        for n, sh in (("norm_mix", [2, D]), ("w_in", [2, D, IN_TOTAL]), ("ml_igate_bias", [2, 4]),
                      ("ml_fgate_bias", [2, 4]), ("ml_norm", [2, 1024]), ("ssm_conv_w", [2, 4, 1536]),
                      ("ssm_conv_b", [2, 1536]), ("ssm_dt_bias", [2, 16]), ("ssm_a_log", [2, 16]),
                      ("ssm_d", [2, 16]), ("ssm_norm", [2, 1024]), ("mla_q_norm", [2, 512]),
                      ("mla_w_uq", [2, 512, 1536]), ("mla_kv_norm", [2, 256]), ("mla_w_ukv", [2, 256, 2048]),
                      ("swa_sinks", [2, 16]), ("w_branch", [2, 4, 1024, D]), ("w_out", [2, D, D]),
                      ("norm_cross", [2, D]), ("norm_mem", [2, D]), ("xa_wq", [2, D, 512]),
                      ("xa_wkv", [2, D, 1024]), ("xa_wo", [2, 512, D]), ("norm_ffn", [2, D]),
                      ("ffn_w13", [1, D, 2 * FFN]), ("ffn_w2", [1, FFN, D]), ("moe_router", [1, D, NEXP]),
                      ("moe_w13", [1, NEXP, D, 2 * FFN]), ("moe_w2", [1, NEXP, FFN, D]), ("norm_final", [D])):
            A[n] = inp(n, sh)
        A["c_ident"] = inp("c_ident", [128, 128], BF16)
        A["c_identf"] = inp("c_identf", [128, 128])
        A["c_tri"] = inp("c_tri", [128, 128])
        A["c_trib"] = inp("c_trib", [128, 128], BF16)
        A["c_lowb"] = inp("c_lowb", [128, 128], BF16)
        A["c_mneg"] = inp("c_mneg", [128, 128])
        A["c_ones"] = inp("c_ones", [128, 128])
        A["c_sutri"] = inp("c_sutri", [128, 128])
        A["c_iota"] = inp("c_iota", [128, 1024])
        A["c_jcol"] = inp("c_jcol", [128, 8])
        A["c_cos2"] = inp("c_cos2", [64, S])
        A["c_sin2"] = inp("c_sin2", [64, S])
        self.A = A
        out = P.dram("out", [S, D], F32, kind="ExternalOutput")
        self.out = out
        Sc = {}
        Sc["h"] = P.dram("s_h", [S, D], F32)
        Sc["ufm32"] = P.dram("s_ufm32", [IN_TOTAL + 64, S], F32)
        Sc["ufm16"] = P.dram("s_ufm16", [IN_TOTAL, S], BF16)
        Sc["utm32"] = P.dram("s_utm32", [S, IN_TOTAL], F32)
        Sc["utm16"] = P.dram("s_utm16", [S, IN_TOTAL], BF16)
        Sc["xcT"] = P.dram("s_xcT", [1536, S], F32)
        Sc["hn16"] = P.dram("s_hn16", [S, D], BF16)
        kind = "ExternalOutput" if self.debug else "Internal"
        for i, n in enumerate("abcd"):
            Sc["y" + n] = P.dram("s_y" + n, [S, 1024], BF16, kind=kind)
        if self.debug:
            Sc["dbg_h"] = [P.dram(f"dbg_h{i}", [S, D], F32, kind="ExternalOutput") for i in range(3 * self.depth)]
        self.Sc = Sc

        c = {}
        self.c = c
        for n, dt in (("ident", BF16), ("identf", F32), ("tri", F32), ("trib", BF16), ("lowb", BF16),
                      ("mneg", F32), ("ones", F32)):
            c[n] = P.sbuf("c_" + n, [128, 128], dt)
            P.dma("sp", c[n][:], A["c_" + n], w=["const"])
        self.ps = [P.psum(f"ps{i}", [128, 512], F32) for i in range(6)]
        self.pt = [P.psum(f"pt{i}", [128, 1024], BF16) for i in range(2)]
        P.barrier()

        with P.phase() as st:
            r = Ring(P, st, "cpx", 3, [128, D], F32)
            for t in range(NT):
                b, k = r.next()
                P.dma("sp", b[:], A["x"][t * 128:(t + 1) * 128, :], w=[k])
                P.dma("sp", Sc["h"][t * 128:(t + 1) * 128, :], b[:], r=[k])

        for l in range(self.depth):
            self.layer(l)
            if self.stop_after is not None and l == self.stop_after[0]:
                break
        self.final_norm()
        P.emit_all()
        return nc

    def dbg_copy_h(self, idx):
        if not self.debug:
            return
        P, Sc = self.P, self.Sc
        with P.phase() as st:
            r = Ring(P, st, "cpd", 3, [128, D], F32)
            for t in range(self.NT):
                b, k = r.next()
                P.dma("sp", b[:], Sc["h"][t * 128:(t + 1) * 128, :], w=[k])
                P.dma("sp", Sc["dbg_h"][idx][t * 128:(t + 1) * 128, :], b[:], r=[k])

    def final_norm(self):
        P, S, NT, A, Sc = self.P, self.S, self.NT, self.A, self.Sc
        with P.phase() as st:
            g = self.bcast_row(st, "fn_g", A["norm_final"], D)
            hr = Ring(P, st, "fn_h", 2, [128, D], F32)
            orr = Ring(P, st, "fn_o", 2, [128, D], F32)
            junk = P.sbuf("fn_junk", [128, D], F32, st)
            ssr = Ring(P, st, "fn_ss", 2, [128, 2], F32)
            for t in range(NT):
                ht, hk = hr.next()
                ot, ok = orr.next()
                ss, sk = ssr.next()
                P.dma("sp", ht[:], Sc["h"][t * 128:(t + 1) * 128, :], w=[hk])
                P.act(junk[:], ht[:], AF.Square, [hk], ["fn_junk", sk], accum_out=ss[:, 0:1])
                P.ts("dve", ss[:, 1:2], ss[:, 0:1], 1.0 / D, 1e-6, ALU.mult, ALU.add, [sk], [sk])
                P.act(ss[:, 1:2], ss[:, 1:2], AF.Sqrt, [sk], [sk])
                P.add("dve", lambda e, ss=ss: e.reciprocal(out=ss[:, 1:2], in_=ss[:, 1:2]), [sk], [sk])
                P.stt("dve", ot[:], ht[:], ss[:, 1:2], g[:], ALU.mult, ALU.mult, [hk, sk, "fn_g"], [ok])
                P.dma("sp", self.out[t * 128:(t + 1) * 128, :], ot[:], r=[ok])

    def layer(self, l):
        sa = self.stop_after
        self.in_proj(l)
        self.mlstm(l)
        self.ssd(l)
        self.mla(l)
        self.swa(l)
        if sa is not None and sa == (l, "branches"):
            return
        self.merge(l)
        self.dbg_copy_h(3 * l + 0)
        if sa is not None and sa == (l, "mixer"):
            return
        self.xattn(l)
        self.dbg_copy_h(3 * l + 1)
        if sa is not None and sa == (l, "xattn"):
            return
        if l % 2 == 1:
            self.moe_layer(l)
        else:
            self.ffn_layer(l)
        self.dbg_copy_h(3 * l + 2)

    def in_proj(self, l):
        P, S, NT, A, Sc = self.P, self.S, self.NT, self.A, self.Sc
        W = A["w_in"][l]
        with P.phase() as st:
            xnT = P.sbuf("xnT", [128, KC, S], BF16, st)
            with P.phase() as st2:
                self.norm_T(st2, Sc["h"], A["norm_mix"][l], xnT, "xnT", S, D)
            wring = Ring(P, st, "ip_w", 3, [128, KC, 512], BF16)
            psr = KeyRing(self.ps[0:4], ["ps0", "ps1", "ps2", "ps3"])
            stg32 = Ring(P, st, "ip_s32", 3, [128, max(S, 512)], F32)
            stg16 = Ring(P, st, "ip_s16", 3, [128, max(S, 512)], BF16)
            rings = (wring, psr, stg32, stg16)
            fm = lambda c0, n, dt, func=None, drow=None: self.proj_fm(
                st, xnT, "xnT", KC, W, c0, n,
                (Sc["ufm32"] if dt == F32 else Sc["ufm16"])[(c0 if drow is None else drow):(c0 if drow is None else drow) + n, :],
                rings, func=func, dtype=dt)
            tm = lambda c0, n, dt: self.proj_tm(
                st, xnT, "xnT", KC, W, c0, n, (Sc["utm32"] if dt == F32 else Sc["utm16"])[:, c0:c0 + n], rings, dtype=dt)
            tm(O_MLK, 512, BF16)
            tm(O_MLV, 1024, BF16)
            tm(O_MLO, 1032, F32)
            tm(O_Z, 1024, F32)
            tm(O_DT, 784, F32)
            tm(O_SWV, 256, BF16)
            fm(O_MLQ, 1024, BF16)
            fm(O_XBC, 1536, F32)
            fm(O_KR, 64, F32)
            fm(O_KR + 32, 32, F32, drow=IN_TOTAL)
            fm(O_KR, 32, F32, drow=IN_TOTAL + 32)
            fm(O_SWQ, 1280, BF16)
            fm(O_GATE, 4 * D, BF16, func=AF.Sigmoid)

    def mlstm(self, l):
        P, S, NT, A, Sc, c = self.P, self.S, self.NT, self.A, self.Sc, self.c
        ps = self.ps
        with P.phase() as st:
            gif = P.sbuf("ml_gif", [128, NT, 8], F32, st)
            P.dma("sp", gif[:], Sc["utm32"][:, O_MLI:O_MLI + 8].rearrange("(t p) c -> p t c", p=128), w=["gif"])
            bi = self.bcast_row(st, "ml_bi", A["ml_igate_bias"][l], 4)
            bf = self.bcast_row(st, "ml_bf", A["ml_fgate_bias"][l], 4)
            nw = self.bcast_row(st, "ml_nw", A["ml_norm"][l], 1024)
            ig = P.sbuf("ml_ig", [128, NT, 4], F32, st)
            lf = P.sbuf("ml_lf", [128, NT, 4], F32, st)
            P.tt("dve", ig[:], gif[:, :, 0:4], bi[:].unsqueeze(1).to_broadcast([128, NT, 4]), ALU.add, ["gif", "ml_bi"], ["ig"])
            P.tt("dve", lf[:], gif[:, :, 4:8], bf[:].unsqueeze(1).to_broadcast([128, NT, 4]), ALU.add, ["gif", "ml_bf"], ["lf"])
            P.act(lf[:], lf[:], AF.Exp, ["lf"], ["lf"], scale=-1.0)
            P.act(lf[:], lf[:], AF.Ln, ["lf"], ["lf"], bias=1.0)
            P.ts("dve", lf[:], lf[:], -1.0, None, ALU.mult, None, ["lf"], ["lf"])
            bcs = P.sbuf("ml_b", [128, NT, 4], F32, st)
            ibm = P.sbuf("ml_ibm", [128, NT, 4], F32, st)
            eb = P.sbuf("ml_eb", [128, NT, 4], F32, st)
            wst = P.sbuf("ml_wst", [128, NT, 4], F32, st)
            ebt = P.sbuf("ml_ebt", [128, NT, 4], F32, st)
            for t in range(NT):
                P.mm(ps[0][:, 0:4], c["tri"][:], lf[:, t, :], True, True, ["lf", "const"], ["ps0"])
                P.mm(ps[0][:, 4:8], c["ones"][:], lf[:, t, :], True, True, ["lf", "const"], ["ps0"])
                P.cp("dve", bcs[:, t, :], ps[0][:, 0:4], ["ps0"], ["bcs"])
                P.cp("dve", ebt[:, t, :], ps[0][:, 4:8], ["ps0"], ["ebt"])
            P.tt("dve", ibm[:], ig[:], bcs[:], ALU.subtract, ["ig", "bcs"], ["ibm"])
            P.tt("dve", wst[:], ibm[:], ebt[:], ALU.add, ["ibm", "ebt"], ["wst"])
            P.act(wst[:], wst[:], AF.Exp, ["wst"], ["wst"])
            P.ts("dve", wst[:], wst[:], 128.0 ** -0.5, None, ALU.mult, None, ["wst"], ["wst"])
            P.act(eb[:], bcs[:], AF.Exp, ["bcs"], ["eb"])
            P.act(ebt[:], ebt[:], AF.Exp, ["ebt"], ["ebt"])

            heads = []
            for hd in range(4):
                H = {}
                H["qT"] = P.sbuf("ml_qT", [128, S], BF16, st)
                H["kT"] = P.sbuf("ml_kT", [128, S], BF16, st)
                H["kt"] = P.sbuf("ml_kt", [128, NT, 128], BF16, st)
                H["va"] = P.sbuf("ml_va", [128, NT, 257], BF16, st)
                H["C"] = P.sbuf("ml_C", [128, 257], F32, st)
                H["Cbr"] = Ring(P, st, "ml_Cb", 2, [128, 257], BF16)
                hk = f"mlh{hd}"
                P.dma("sp", H["qT"][:], Sc["ufm16"][O_MLQ + hd * 128:O_MLQ + (hd + 1) * 128, :], w=[hk + "q"])
                P.dma("sp", H["kT"][:], Sc["ufm16"][O_MLK + hd * 128:O_MLK + (hd + 1) * 128, :], w=[hk + "k"])
                P.dma("sp", H["kt"][:], Sc["utm16"][:, O_MLK + hd * 128:O_MLK + (hd + 1) * 128].rearrange("(t p) c -> p t c", p=128), w=[hk + "kt"])
                P.dma("sp", H["va"][:, :, 0:256], Sc["utm16"][:, O_MLV + hd * 256:O_MLV + (hd + 1) * 256].rearrange("(t p) c -> p t c", p=128), w=[hk + "v"])
                P.memset("pool", H["va"][:, :, 256:257], 1.0, [hk + "v"])
                P.memset("dve", H["C"][:], 0.0, [hk + "C"])
                H["Cb"] = H["Cbr"].next()
                P.memset("pool", H["Cb"][0][:], 0.0, [H["Cb"][1]])
                heads.append(H)
            ogr = Ring(P, st, "ml_og", 6, [128, 256], F32)
            ltr = Ring(P, st, "ml_lt", 6, [128, 128], F32)
            etr = Ring(P, st, "ml_et", 6, [128, 128], F32)
            ptr_ = Ring(P, st, "ml_pt", 6, [128, 128], BF16)
            kwr = Ring(P, st, "ml_kw", 6, [128, 128], BF16)
            tmr = Ring(P, st, "ml_tmp", 6, [128, 257], F32)
            hor = Ring(P, st, "ml_ho", 6, [128, 257], F32)
            smr = Ring(P, st, "ml_sm", 6, [128, 4], F32)
            yr = Ring(P, st, "ml_y", 6, [128, 256], BF16)
            junk = P.sbuf("ml_junk", [128, 256], BF16, st)
            for t in range(NT):
                sl = slice(t * 128, (t + 1) * 128)
                for hd in range(4):
                    H = heads[hd]
                    hk = f"mlh{hd}"
                    qT, qk, kT, kk, kt, ktk, va, vk = H["qT"], hk + "q", H["kT"], hk + "k", H["kt"], hk + "kt", H["va"], hk + "v"
                    Cst, Ck = H["C"], hk + "C"
                    Cb, cbk = H["Cb"]
                    og, ogk = ogr.next()
                    lt, ltk = ltr.next()
                    et, etk = etr.next()
                    pt, ptk = ptr_.next()
                    kw, kwk = kwr.next()
                    tmp, tmk = tmr.next()
                    ho, hok = hor.next()
                    sm, smk = smr.next()
                    y, yk = yr.next()
                    P.dma("sp", og[:], Sc["utm32"][sl, O_MLO + hd * 256:O_MLO + (hd + 1) * 256], w=[ogk])
                    P.act(og[:], og[:], AF.Sigmoid, [ogk], [ogk])
                    P.ts("pool", lt[:], c["tri"][:], lf[:, t, hd:hd + 1], None, ALU.mult, None, ["lf", "const"], [ltk])
                    P.mm(ps[1][:, 0:128], c["ones"][:], lt[:], True, False, [ltk, "const"], ["ps1"])
                    P.mm(ps[1][:, 0:128], c["identf"][:], c["mneg"][:], False, True, ["const"], ["ps1"])
                    P.act(et[:], ps[1][:, 0:128], AF.Exp, ["ps1", "ibm"], [etk], bias=ibm[:, t, hd:hd + 1])
                    P.mm(ps[2][:, 0:128], kT[:, sl], qT[:, sl], True, True, [kk, qk], ["ps2"])
                    P.stt("dve", pt[:], ps[2][:, 0:128], 128.0 ** -0.5, et[:], ALU.mult, ALU.mult, ["ps2", etk], [ptk])
                    P.mm(ps[3][:, 0:257], pt[:], va[:, t, :], True, True, [ptk, vk], ["ps3"])
                    P.mm(ps[4][:, 0:257], qT[:, sl], Cb[:], True, True, [qk, cbk], ["ps4"])
                    P.act(tmp[:], ps[4][:, 0:257], AF.Copy, ["ps4", "eb"], [tmk], scale=eb[:, t, hd:hd + 1])
                    P.tt("dve", ho[:], tmp[:], ps[3][:, 0:257], ALU.add, [tmk, "ps3"], [hok])
                    P.act(sm[:, 0:1], ho[:, 256:257], AF.Abs, [hok], [smk])
                    P.ts("dve", sm[:, 0:1], sm[:, 0:1], 1.0, None, ALU.max, None, [smk], [smk])
                    P.add("dve", lambda e, sm=sm: e.reciprocal(out=sm[:, 0:1], in_=sm[:, 0:1]), [smk], [smk])
                    P.ts("dve", ho[:, 0:256], ho[:, 0:256], sm[:, 0:1], None, ALU.mult, None, [hok, smk], [hok])
                    P.act(junk[:], ho[:, 0:256], AF.Square, [hok], ["ml_junk", smk], accum_out=sm[:, 1:2])
                    P.ts("dve", sm[:, 2:3], sm[:, 1:2], 1.0 / 256, 1e-6, ALU.mult, ALU.add, [smk], [smk])
                    P.act(sm[:, 2:3], sm[:, 2:3], AF.Sqrt, [smk], [smk])
                    P.add("dve", lambda e, sm=sm: e.reciprocal(out=sm[:, 2:3], in_=sm[:, 2:3]), [smk], [smk])
                    P.stt("dve", ho[:, 0:256], ho[:, 0:256], sm[:, 2:3], nw[:, hd * 256:(hd + 1) * 256], ALU.mult, ALU.mult,
                          [hok, smk, "ml_nw"], [hok])
                    P.tt("pool", y[:], ho[:, 0:256], og[:], ALU.mult, [hok, ogk], [yk])
                    P.dma("sp", Sc["ya"][sl, hd * 256:(hd + 1) * 256], y[:], r=[yk])
                    if t < NT - 1:
                        P.ts("pool", kw[:], kt[:, t, :], wst[:, t, hd:hd + 1], None, ALU.mult, None, [ktk, "wst"], [kwk])
                        P.mm(ps[5][:, 0:257], kw[:], va[:, t, :], True, True, [kwk, vk], ["ps5"])
                        P.stt("dve", Cst[:], Cst[:], ebt[:, t, hd:hd + 1], ps[5][:, 0:257], ALU.mult, ALU.add,
                              [Ck, "ebt", "ps5"], [Ck])
                        H["Cb"] = H["Cbr"].next()
                        P.cp("act", H["Cb"][0][:], Cst[:], [Ck], [H["Cb"][1]])

    def ssd(self, l):
        P, S, NT, A, Sc, c = self.P, self.S, self.NT, self.A, self.Sc, self.c
        ps, pt = self.ps, self.pt
        with P.phase() as st:
            cw = P.sbuf("sd_cw", [128, 12, 4], F32, st)
            cb = P.sbuf("sd_cb", [128, 12], F32, st)
            for j in range(4):
                P.dma("sp", cw[:, :, j], A["ssm_conv_w"][l, j].rearrange("(c p) -> p c", p=128), w=["cw"], allow_slow_non_contiguous=True)
            P.dma("sp", cb[:], A["ssm_conv_b"][l].rearrange("(c p) -> p c", p=128), w=["cb"], allow_slow_non_contiguous=True)
            xr = Ring(P, st, "sd_x", 2, [128, S + 3], F32)
            ar = Ring(P, st, "sd_a", 2, [128, S], F32)
            for ch in range(12):
                x, xk = xr.next()
                a, ak = ar.next()
                P.memset("pool", x[:, 0:3], 0.0, [xk])
                P.dma("sp", x[:, 3:S + 3], Sc["ufm32"][O_XBC + ch * 128:O_XBC + (ch + 1) * 128, :], w=[xk])
                P.ts("dve", a[:], x[:, 3:S + 3], cw[:, ch, 3:4], None, ALU.mult, None, [xk, "cw"], [ak])
                for j in range(3):
                    P.stt("dve", a[:], x[:, j:S + j], cw[:, ch, j:j + 1], a[:], ALU.mult, ALU.add, [xk, "cw", ak], [ak])
                P.act(a[:], a[:], AF.Silu, [ak, "cb"], [ak], bias=cb[:, ch:ch + 1])
                P.dma("sp", Sc["xcT"][ch * 128:(ch + 1) * 128, :], a[:], r=[ak])
        with P.phase() as st:
            dtb = self.bcast_row(st, "sd_dtb", A["ssm_dt_bias"][l], 16)
            alog = self.bcast_row(st, "sd_alog", A["ssm_a_log"][l], 16)
            dsk = self.bcast_row(st, "sd_dsk", A["ssm_d"][l], 16)
            nw = self.bcast_row(st, "sd_nw", A["ssm_norm"][l], 1024)
            P.act(alog[:], alog[:], AF.Exp, ["sd_alog"], ["sd_alog"])
            dt = P.sbuf("sd_dt", [128, NT, 16], F32, st)
            P.dma("sp", dt[:], Sc["utm32"][:, O_DT:O_DT + 16].rearrange("(t p) c -> p t c", p=128), w=["dt"])
            P.tt("dve", dt[:], dt[:], dtb[:].unsqueeze(1).to_broadcast([128, NT, 16]), ALU.add, ["dt", "sd_dtb"], ["dt"])
            P.act(dt[:], dt[:], AF.Exp, ["dt"], ["dt"])
            P.act(dt[:], dt[:], AF.Ln, ["dt"], ["dt"], bias=1.0)
            av = P.sbuf("sd_av", [128, NT, 16], F32, st)
            P.stt("dve", av[:], dt[:], -1.0, alog[:].unsqueeze(1).to_broadcast([128, NT, 16]), ALU.mult, ALU.mult,
                  ["dt", "sd_alog"], ["av"])
            acs = P.sbuf("sd_acs", [128, NT, 16], F32, st)
            nacs = P.sbuf("sd_nacs", [128, NT, 16], F32, st)
            eat = P.sbuf("sd_eat", [128, NT, 16], F32, st)
            ea = P.sbuf("sd_ea", [128, NT, 16], F32, st)
            dsc = P.sbuf("sd_dsc", [128, NT, 16], F32, st)
            for t in range(NT):
                P.mm(ps[0][:, 0:16], c["tri"][:], av[:, t, :], True, True, ["av", "const"], ["ps0"])
                P.mm(ps[0][:, 16:32], c["ones"][:], av[:, t, :], True, True, ["av", "const"], ["ps0"])
                P.cp("dve", acs[:, t, :], ps[0][:, 0:16], ["ps0"], ["acs"])
                P.cp("dve", eat[:, t, :], ps[0][:, 16:32], ["ps0"], ["eat"])
            P.ts("dve", nacs[:], acs[:], -1.0, None, ALU.mult, None, ["acs"], ["nacs"])
            P.tt("dve", dsc[:], eat[:], acs[:], ALU.subtract, ["eat", "acs"], ["dsc"])
            P.act(dsc[:], dsc[:], AF.Exp, ["dsc"], ["dsc"])
            P.tt("dve", dsc[:], dsc[:], dt[:], ALU.mult, ["dsc", "dt"], ["dsc"])
            P.act(ea[:], acs[:], AF.Exp, ["acs"], ["ea"])
            P.act(eat[:], eat[:], AF.Exp, ["eat"], ["eat"])

            xcr = Ring(P, st, "sd_xc", 2, [128, 12, 128], F32)
            xcbr = Ring(P, st, "sd_xcb", 2, [128, 12, 128], BF16)
            xtr = Ring(P, st, "sd_xt", 2, [128, 1280], BF16)
            xsr = Ring(P, st, "sd_xs", 2, [128, 1024], BF16)
            xdr = Ring(P, st, "sd_xd", 2, [128, 1024], BF16)
            cbr = Ring(P, st, "sd_cbT", 2, [128, 2, 128], F32)
            atr = Ring(P, st, "sd_at", 3, [128, 128], F32)
            der = Ring(P, st, "sd_de", 3, [128, 128], F32)
            mtr = Ring(P, st, "sd_mt", 3, [128, 128], BF16)
            zr = Ring(P, st, "sd_z", 2, [128, 1024], F32)
            yr = Ring(P, st, "sd_y", 2, [128, 1024], F32)
            tr_ = Ring(P, st, "sd_t", 2, [128, 1024], F32)
            ybr = Ring(P, st, "sd_yb", 2, [128, 1024], BF16)
            smr = Ring(P, st, "sd_sm", 2, [128, 4], F32)
            junk = P.sbuf("sd_junk", [128, 512], F32, st)
            state = P.sbuf("sd_state", [128, 2, 512], F32, st)
            stbr = Ring(P, st, "sd_stb", 2, [128, 2, 512], BF16)
            P.memset("dve", state[:], 0.0, ["state"])
            stb, stbk = stbr.next()
            P.memset("pool", stb[:], 0.0, [stbk])
            for t in range(NT):
                sl = slice(t * 128, (t + 1) * 128)
                xc, xck = xcr.next()
                xcb, xcbk = xcbr.next()
                xt, xtk = xtr.next()
                xs, xsk = xsr.next()
                xd, xdk = xdr.next()
                cbT, cbTk = cbr.next()
                z, zk = zr.next()
                y, yk = yr.next()
                tm_, tmk = tr_.next()
                yb, ybk = ybr.next()
                sm, smk = smr.next()
                P.dma("sp", xc[:], Sc["xcT"][:, sl].rearrange("(c p) s -> p c s", p=128), w=[xck])
                P.dma("sp", z[:], Sc["utm32"][sl, O_Z:O_Z + 1024], w=[zk])
                P.cp("pool", xcb[:], xc[:], [xck], [xcbk])
                for q in range(3):
                    n4 = 4 if q < 2 else 2
                    pb, pbk = pt[q % 2], ("pt0", "pt1")[q % 2]
                    pv = pb[:, 0:512].rearrange("p (a b) -> p a b", b=128)
                    for j in range(n4):
                        P.tr(pv[:, j, :], xcb[:, q * 4 + j, :], c["ident"][:], [xcbk, "const"], [pbk])
                    P.cp("dve" if q % 2 == 0 else "act", xt[:, q * 512:q * 512 + n4 * 128], pb[:, 0:n4 * 128], [pbk], [xtk])
                x3 = xt[:, 0:1024].rearrange("p (h d) -> p h d", d=64)
                P.tt("dve", xs[:].rearrange("p (h d) -> p h d", d=64), x3, dt[:, t, :].unsqueeze(2).to_broadcast([128, 16, 64]),
                     ALU.mult, [xtk, "dt"], [xsk])
                P.tt("pool", xd[:].rearrange("p (h d) -> p h d", d=64), x3, dsc[:, t, :].unsqueeze(2).to_broadcast([128, 16, 64]),
                     ALU.mult, [xtk, "dsc"], [xdk])
                for g in range(2):
                    P.mm(ps[1][:, g * 128:(g + 1) * 128], xcb[:, 8 + g, :], xcb[:, 10 + g, :], True, True, [xcbk], ["ps1"])
                P.cp("act", cbT[:].rearrange("p a b -> p (a b)"), ps[1][:, 0:256], ["ps1"], [cbTk])
                for g in range(2):
                    P.mm(ps[2 + g][:, 0:512], xcb[:, 10 + g, :], stb[:, g, :], True, True, [xcbk, stbk], [f"ps{2 + g}"])
                for hd in range(16):
                    g = hd // 8
                    at, atk = atr.next()
                    de, dek = der.next()
                    mt, mtk = mtr.next()
                    P.ts("pool", at[:], c["tri"][:], av[:, t, hd:hd + 1], None, ALU.mult, None, ["av", "const"], [atk])
                    P.mm(ps[0][:, 0:128], c["ones"][:], at[:], True, False, [atk, "const"], ["ps0"])
                    P.mm(ps[0][:, 0:128], c["identf"][:], c["mneg"][:], False, True, ["const"], ["ps0"])
                    P.act(de[:], ps[0][:, 0:128], AF.Exp, ["ps0", "nacs"], [dek], bias=nacs[:, t, hd:hd + 1])
                    P.tt("dve", mt[:], de[:], cbT[:, g, :], ALU.mult, [dek, cbTk], [mtk])
                    P.mm(ps[4 + g][:, (hd % 8) * 64:(hd % 8 + 1) * 64], mt[:], xs[:, hd * 64:(hd + 1) * 64], True, True,
                         [mtk, xsk], [f"ps{4 + g}"])
                for g in range(2):
                    P.tt("dve", tm_[:, g * 512:(g + 1) * 512].rearrange("p (h d) -> p h d", d=64),
                         ps[2 + g][:, 0:512].rearrange("p (h d) -> p h d", d=64),
                         ea[:, t, g * 8:(g + 1) * 8].unsqueeze(2).to_broadcast([128, 8, 64]), ALU.mult,
                         [f"ps{2 + g}", "ea"], [tmk])
                    P.tt("dve", y[:, g * 512:(g + 1) * 512], tm_[:, g * 512:(g + 1) * 512], ps[4 + g][:, 0:512], ALU.add,
                         [tmk, f"ps{4 + g}"], [yk])
                P.tt("pool", tm_[:].rearrange("p (h d) -> p h d", d=64), x3, dsk[:].unsqueeze(2).to_broadcast([128, 16, 64]),
                     ALU.mult, [xtk, "sd_dsk", yk], [tmk])
                P.tt("pool", y[:], y[:], tm_[:], ALU.add, [tmk, yk], [yk])
                P.act(z[:], z[:], AF.Silu, [zk], [zk])
                P.tt("dve", y[:], y[:], z[:], ALU.mult, [yk, zk], [yk])
                for g in range(2):
                    P.act(junk[:], y[:, g * 512:(g + 1) * 512], AF.Square, [yk], ["sd_junk", smk], accum_out=sm[:, g:g + 1])
                P.ts("dve", sm[:, 2:4], sm[:, 0:2], 1.0 / 512, 1e-6, ALU.mult, ALU.add, [smk], [smk])
                P.act(sm[:, 2:4], sm[:, 2:4], AF.Sqrt, [smk], [smk])
                P.add("dve", lambda e, sm=sm: e.reciprocal(out=sm[:, 2:4], in_=sm[:, 2:4]), [smk], [smk])
                for g in range(2):
                    P.stt("dve", yb[:, g * 512:(g + 1) * 512], y[:, g * 512:(g + 1) * 512], sm[:, 2 + g:3 + g],
                          nw[:, g * 512:(g + 1) * 512], ALU.mult, ALU.mult, [yk, smk, "sd_nw"], [ybk])
                P.dma("sp", Sc["yb"][sl, :], yb[:], r=[ybk])
                if t < NT - 1:
                    for g in range(2):
                        P.mm(ps[2 + g][:, 0:512], xt[:, 1024 + g * 128:1024 + (g + 1) * 128], xd[:, g * 512:(g + 1) * 512],
                             True, True, [xtk, xdk], [f"ps{2 + g}"])
                        P.tt("dve", state[:, g, :].rearrange("p (h d) -> p h d", d=64),
                             state[:, g, :].rearrange("p (h d) -> p h d", d=64),
                             eat[:, t, g * 8:(g + 1) * 8].unsqueeze(2).to_broadcast([128, 8, 64]), ALU.mult,
                             ["state", "eat"], ["state"])
                        P.tt("dve", state[:, g, :], state[:, g, :], ps[2 + g][:, 0:512], ALU.add, ["state", f"ps{2 + g}"], ["state"])
                    stb, stbk = stbr.next()
                    P.cp("act", stb[:], state[:], ["state"], [stbk])

    def mla(self, l):
        P, S, NT, A, Sc, c = self.P, self.S, self.NT, self.A, self.Sc, self.c
        ps, GW, NG = self.ps, self.GW, self.NG
        scale = 192.0 ** -0.5
        with P.phase() as st:
            cqT = P.sbuf("la_cqT", [128, 4, S], BF16, st)
            ckT = P.sbuf("la_ckT", [128, 2, S], BF16, st)
            with P.phase() as st2:
                self.norm_T(st2, Sc["utm32"][:, O_CQ:O_CQ + 512], A["mla_q_norm"][l], cqT, "cqT", S, 512)
            with P.phase() as st2:
                self.norm_T(st2, Sc["utm32"][:, O_CKV:O_CKV + 256], A["mla_kv_norm"][l], ckT, "ckT", S, 256)
            cos2 = P.sbuf("la_cos", [64, S], F32, st)
            sin2 = P.sbuf("la_sin", [64, S], F32, st)
            P.dma("sp", cos2[:], A["c_cos2"], w=["cos"])
            P.dma("sp", sin2[:], A["c_sin2"], w=["sin"])
            krT = P.sbuf("la_krT", [64, S], BF16, st)
            k1 = P.sbuf("la_k1", [64, S], F32, st)
            k2 = P.sbuf("la_k2", [64, S], F32, st)
            P.dma("sp", k1[:], Sc["ufm32"][O_KR:O_KR + 64, :], w=["k1"])
            P.dma("sp", k2[:], Sc["ufm32"][IN_TOTAL:IN_TOTAL + 64, :], w=["k2"])
            P.tt("dve", k1[:], k1[:], cos2[:], ALU.mult, ["k1", "cos"], ["k1"])
            P.tt("pool", k2[:], k2[:], sin2[:], ALU.mult, ["k2", "sin"], ["k2"])
            P.tt("dve", krT[:], k1[:], k2[:], ALU.add, ["k1", "k2"], ["krT"])
            Wq, Wkv = A["mla_w_uq"][l], A["mla_w_ukv"][l]
            wqr = Ring(P, st, "la_wq", 2, [128, 4, 256], BF16)
            wkr = Ring(P, st, "la_wk", 2, [128, 2, 256], BF16)
            qnr = Ring(P, st, "la_qn", 2, [128, S], BF16)
            qrr = Ring(P, st, "la_qr", 2, [64, S], BF16)
            knr = Ring(P, st, "la_kn", 2, [128, S], BF16)
            var = Ring(P, st, "la_va", 2, [128, NT, 129], BF16)
            t1r = Ring(P, st, "la_t1", 2, [64, GW], F32)
            t2r = Ring(P, st, "la_t2", 2, [64, GW], F32)
            ptr_ = Ring(P, st, "la_pt", 4, [128, GW], BF16)
            yr = Ring(P, st, "la_y", 4, [128, 128], BF16)
            smr = Ring(P, st, "la_sm", 4, [128, 2], F32)
            ycur = {}
            for hd in range(8):
                wq, wqk = wqr.next()
                wk, wkk = wkr.next()
                qn, qnk = qnr.next()
                qr, qrk = qrr.next()
                kn, knk = knr.next()
                va, vak = var.next()
                q0 = hd * 192
                vw = lambda a, b: Wq[:, a:b].rearrange("(c p) n -> p c n", p=128)
                P.dma("pool", wq[:, :, 0:192], vw(q0, q0 + 192), w=[wqk])
                P.dma("pool", wq[:, :, 192:224], vw(q0 + 160, q0 + 192), w=[wqk])
                P.dma("pool", wq[:, :, 224:256], vw(q0 + 128, q0 + 160), w=[wqk])
                P.dma("pool", wk[:], Wkv[:, hd * 256:(hd + 1) * 256].rearrange("(c p) n -> p c n", p=128), w=[wkk])
                cqr = [("cqT", t) for t in range(NT)]
                ckr = [("ckT", t) for t in range(NT)]
                for g in range(NG):
                    gs = slice(g * GW, (g + 1) * GW)
                    for k in range(4):
                        P.mm(ps[0][:, 0:GW], wq[:, k, 0:128], cqT[:, k, gs], k == 0, k == 3, [wqk] + cqr, ["ps0"])
                    P.cp("act", qn[:, gs], ps[0][:, 0:GW], ["ps0"], [qnk])
                    for k in range(4):
                        P.mm(ps[1][0:64, 0:GW], wq[:, k, 128:192], cqT[:, k, gs], k == 0, k == 3, [wqk] + cqr, ["ps1"])
                    for k in range(4):
                        P.mm(ps[2][0:64, 0:GW], wq[:, k, 192:256], cqT[:, k, gs], k == 0, k == 3, [wqk] + cqr, ["ps2"])
                    t1, t1k = t1r.next()
                    t2, t2k = t2r.next()
                    P.tt("dve", t1[:], ps[1][0:64, 0:GW], cos2[:, gs], ALU.mult, ["ps1", "cos"], [t1k])
                    P.tt("dve", t2[:], ps[2][0:64, 0:GW], sin2[:, gs], ALU.mult, ["ps2", "sin"], [t2k])
                    P.tt("pool", qr[:, gs], t1[:], t2[:], ALU.add, [t1k, t2k], [qrk])
                    for k in range(2):
                        P.mm(ps[3][:, 0:GW], wk[:, k, 0:128], ckT[:, k, gs], k == 0, k == 1, [wkk] + ckr, ["ps3"])
                    P.cp("act", kn[:, gs], ps[3][:, 0:GW], ["ps3"], [knk])
                for t in range(NT):
                    for k in range(2):
                        P.mm(ps[4][:, 0:128], ckT[:, k, t * 128:(t + 1) * 128], wk[:, k, 128:256], k == 0, k == 1,
                             [wkk, ("ckT", t)], ["ps4"])
                    P.cp("dve", va[:, t, 0:128], ps[4][:, 0:128], ["ps4"], [vak])
                P.memset("pool", va[:, :, 128:129], 1.0, [vak])
                TPG = GW // 128
                its = [(g, j) for g in range(NG) for j in range((g + 1) * TPG)]

                def emit_S(i):
                    g, j = its[i]
                    gs0 = g * GW
                    c0 = max(0, j - g * TPG) * 128
                    sb, sbk = ps[i % 2], f"ps{i % 2}"
                    P.mm(sb[:, c0:GW], kn[:, j * 128:(j + 1) * 128], qn[:, gs0 + c0:gs0 + GW], True, False, [knk, qnk], [sbk])
                    P.mm(sb[:, c0:GW], krT[:, j * 128:(j + 1) * 128], qr[:, gs0 + c0:gs0 + GW], False, True, ["krT", qrk], [sbk])

                emit_S(0)
                for i, (g, j) in enumerate(its):
                    if i + 1 < len(its):
                        emit_S(i + 1)
                    r0 = max(0, j - g * TPG)
                    c0 = r0 * 128
                    sb, sbk = ps[i % 2], f"ps{i % 2}"
                    pt, ptk = ptr_.next()
                    P.act(pt[:, c0:GW], sb[:, c0:GW], AF.Exp, [sbk], [ptk], scale=scale)
                    if j >= g * TPG:
                        P.tt("dve", pt[:, c0:c0 + 128], pt[:, c0:c0 + 128], c["trib"][:], ALU.mult, [ptk, "const"], [ptk])
                    for r in range(r0, TPG):
                        qi = g * TPG + r
                        P.mm(ps[2 + r][:, 0:129], pt[:, r * 128:(r + 1) * 128], va[:, j, :], j == 0, j == qi,
                             [ptk, vak], [f"ps{2 + r}"])
                        if j == qi:
                            y, yk = yr.next()
                            sm, smk = smr.next()
                            P.add("dve", lambda e, sm=sm, r=r: e.reciprocal(out=sm[:, 0:1], in_=ps[2 + r][:, 128:129]),
                                  [f"ps{2 + r}"], [smk])
                            P.ts("dve", y[:], ps[2 + r][:, 0:128], sm[:, 0:1], None, ALU.mult, None,
                                 [f"ps{2 + r}", smk], [yk])
                            P.dma("sp", Sc["yc"][qi * 128:(qi + 1) * 128, hd * 128:(hd + 1) * 128], y[:], r=[yk])

    def swa(self, l):
        P, S, NT, A, Sc, c = self.P, self.S, self.NT, self.A, self.Sc, self.c
        ps = self.ps
        scale = 64.0 ** -0.5
        with P.phase() as st:
            sk_ = self.bcast_row(st, "sw_sink", A["swa_sinks"][l], 16)
            P.act(sk_[:], sk_[:], AF.Exp, ["sw_sink"], ["sw_sink"])
            qr = Ring(P, st, "sw_q", 2, [64, 4, S], BF16)
            kr = Ring(P, st, "sw_k", 2, [64, S], BF16)
            var = Ring(P, st, "sw_va", 2, [128, NT, 65], BF16)
            ptr_ = Ring(P, st, "sw_pt", 4, [128, 512], BF16)
            yr = Ring(P, st, "sw_y", 3, [128, 256], BF16)
            smr = Ring(P, st, "sw_sm", 3, [128, 4], F32)
            for g in range(4):
                q, qk = qr.next()
                k, kk = kr.next()
                va, vak = var.next()
                P.dma("sp", q[:], Sc["ufm16"][O_SWQ + g * 256:O_SWQ + (g + 1) * 256, :].rearrange("(r p) s -> p r s", p=64), w=[qk])
                P.dma("sp", k[:], Sc["ufm16"][O_SWK + g * 64:O_SWK + (g + 1) * 64, :], w=[kk])
                P.dma("sp", va[:, :, 0:64], Sc["utm16"][:, O_SWV + g * 64:O_SWV + (g + 1) * 64].rearrange("(t p) c -> p t c", p=128), w=[vak])
                P.memset("pool", va[:, :, 64:65], 1.0, [vak])
                for n in range(NT):
                    sl = slice(n * 128, (n + 1) * 128)
                    pc, pck = ptr_.next()
                    P.mm(ps[0][:, 0:512].rearrange("p (r s) -> p r s", r=4), k[:, sl], q[:, :, sl], True, True, [kk, qk], ["ps0"])
                    P.act(pc[:], ps[0][:, 0:512], AF.Exp, ["ps0"], [pck], scale=scale)
                    P.tt("pool", pc[:].rearrange("p (r s) -> p r s", r=4), pc[:].rearrange("p (r s) -> p r s", r=4),
                         c["trib"][:].unsqueeze(1).to_broadcast([128, 4, 128]), ALU.mult, [pck, "const"], [pck])
                    if n > 0:
                        pp, ppk = ptr_.next()
                        psl = slice((n - 1) * 128, n * 128)
                        P.mm(ps[1][:, 0:512].rearrange("p (r s) -> p r s", r=4), k[:, psl], q[:, :, sl], True, True, [kk, qk], ["ps1"])
                        P.act(pp[:], ps[1][:, 0:512], AF.Exp, ["ps1"], [ppk], scale=scale)
                        P.tt("pool", pp[:].rearrange("p (r s) -> p r s", r=4), pp[:].rearrange("p (r s) -> p r s", r=4),
                             c["lowb"][:].unsqueeze(1).to_broadcast([128, 4, 128]), ALU.mult, [ppk, "const"], [ppk])
                    ob = ps[2 + (n % 2)]
                    obk = f"ps{2 + (n % 2)}"
                    for r in range(4):
                        if n > 0:
                            P.mm(ob[:, r * 65:(r + 1) * 65], pp[:, r * 128:(r + 1) * 128], va[:, n - 1, :], True, False, [ppk, vak], [obk])
                        P.mm(ob[:, r * 65:(r + 1) * 65], pc[:, r * 128:(r + 1) * 128], va[:, n, :], n == 0, True, [pck, vak], [obk])
                    y, yk = yr.next()
                    sm, smk = smr.next()
                    o3 = ob[:, 0:260].rearrange("p (r d) -> p r d", d=65)
                    P.tt("dve", sm[:].unsqueeze(2), o3[:, :, 64:65], sk_[:, g * 4:(g + 1) * 4].unsqueeze(2), ALU.add, [obk, "sw_sink"], [smk])
                    P.add("dve", lambda e, sm=sm: e.reciprocal(out=sm[:], in_=sm[:]), [smk], [smk])
                    P.tt("dve", y[:].rearrange("p (r d) -> p r d", d=64), o3[:, :, 0:64], sm[:].unsqueeze(2).to_broadcast([128, 4, 64]),
                         ALU.mult, [obk, smk], [yk])
                    P.dma("sp", Sc["yd"][sl, g * 256:(g + 1) * 256], y[:], r=[yk])

    def merge(self, l):
        P, S, NT, A, Sc, c = self.P, self.S, self.NT, self.A, self.Sc, self.c
        ps, pt, GW, NG = self.ps, self.pt, self.GW, self.NG
        TPG = GW // 128
        with P.phase() as st:
            yTr = [P.sbuf(f"mg_yT{n}", [128, 8, GW], BF16, st) for n in range(4)]
            ytr = Ring(P, st, "mg_yt", 3, [128, 1024], BF16)
            wbr = Ring(P, st, "mg_wb", 3, [128, 8, 512], BF16)
            wor = Ring(P, st, "mg_wo", 2, [128, 16, 512], BF16)
            gtr = Ring(P, st, "mg_g", 3, [128, GW], BF16)
            tmr = Ring(P, st, "mg_tmp", 2, [128, GW], F32)
            mT = P.sbuf("mg_mT", [128, 16, GW], F32, st)
            mTb = P.sbuf("mg_mTb", [128, 16, GW], BF16, st)
            hr = Ring(P, st, "mg_h", 3, [128, 512], F32)
            ptr = KeyRing(pt, ["pt0", "pt1"])
            for g in range(NG):
                gs = slice(g * GW, (g + 1) * GW)
                for n, nm in enumerate("abcd"):
                    for tt_ in range(TPG):
                        t = g * TPG + tt_
                        yt, ytk = ytr.next()
                        P.dma("sp", yt[:], Sc["y" + nm][t * 128:(t + 1) * 128, :], w=[ytk])
                        for q in range(2):
                            pb, pbk = ptr.next()
                            pv = pb[:, 0:512].rearrange("p (a b) -> p a b", b=128)
                            for j in range(4):
                                P.tr(pv[:, j, :], yt[:, (q * 4 + j) * 128:(q * 4 + j + 1) * 128], c["ident"][:], [ytk, "const"], [pbk])
                            P.cp("dve" if q == 0 else "act", yTr[n][:, q * 4:(q + 1) * 4, tt_ * 128:(tt_ + 1) * 128], pv[:, 0:4, :],
                                 [pbk], [("yT", n)])
                for n in range(4):
                    for dcb in range(4):
                        wb, wbk = wbr.next()
                        P.dma("pool", wb[:], A["w_branch"][l, n][:, dcb * 512:(dcb + 1) * 512].rearrange("(c p) n -> p c n", p=128), w=[wbk])
                        for dc in range(4):
                            dch = dcb * 4 + dc
                            pb, pbk = ps[dc % 4], f"ps{dc % 4}"
                            for k in range(8):
                                P.mm(pb[:, 0:GW], wb[:, k, dc * 128:(dc + 1) * 128], yTr[n][:, k, :], k == 0, k == 7,
                                     [wbk, ("yT", n)], [pbk])
                            gt, gtk = gtr.next()
                            r0 = O_GATE + n * D + dch * 128
                            P.dma("sp", gt[:], Sc["ufm16"][r0:r0 + 128, gs], w=[gtk])
                            if n == 0:
                                P.tt("dve", mT[:, dch, :], pb[:, 0:GW], gt[:], ALU.mult, [pbk, gtk], [("mT", dch)])
                            else:
                                tmp, tmk = tmr.next()
                                P.tt("dve", tmp[:], pb[:, 0:GW], gt[:], ALU.mult, [pbk, gtk], [tmk])
                                P.tt("dve", mT[:, dch, :], mT[:, dch, :], tmp[:], ALU.add, [("mT", dch), tmk], [("mT", dch)])
                for dch in range(16):
                    P.cp("act", mTb[:, dch, :], mT[:, dch, :], [("mT", dch)], [("mTb", dch)])
                mr = [("mTb", d_) for d_ in range(16)]
                for ocb in range(4):
                    wo, wok = wor.next()
                    P.dma("pool", wo[:], A["w_out"][l][:, ocb * 512:(ocb + 1) * 512].rearrange("(c p) n -> p c n", p=128), w=[wok])
                    for tt_ in range(TPG):
                        t = g * TPG + tt_
                        pb, pbk = ps[4 + tt_ % 2], f"ps{4 + tt_ % 2}"
                        ht, hk = hr.next()
                        P.dma("sp", ht[:], Sc["h"][t * 128:(t + 1) * 128, ocb * 512:(ocb + 1) * 512], w=[hk])
                        for k in range(16):
                            P.mm(pb[:, 0:512], mTb[:, k, tt_ * 128:(tt_ + 1) * 128], wo[:, k, :], k == 0, k == 15, [wok] + mr, [pbk])
                        P.tt("dve", ht[:], ht[:], pb[:, 0:512], ALU.add, [hk, pbk], [hk])
                        P.dma("sp", Sc["h"][t * 128:(t + 1) * 128, ocb * 512:(ocb + 1) * 512], ht[:], r=[hk])

    def xattn(self, l):
        P, S, NT, A, Sc, c = self.P, self.S, self.NT, self.A, self.Sc, self.c
        ps, pt, GW, NG = self.ps, self.pt, self.GW, self.NG
        scale = 128.0 ** -0.5
        with P.phase() as st:
            hnT = P.sbuf("xa_hnT", [128, KC, S], BF16, st)
            mnT = P.sbuf("xa_mnT", [128, KC, MEM], BF16, st)
            with P.phase() as st2:
                self.norm_T(st2, Sc["h"], A["norm_cross"][l], hnT, "hnT", S, D)
            with P.phase() as st2:
                self.norm_T(st2, A["mem"], A["norm_mem"][l], mnT, "mnT", MEM, D)
            wkv = P.sbuf("xa_wkv", [128, KC, 1024], BF16, st)
            wq = P.sbuf("xa_wq", [128, KC, 512], BF16, st)
            wo = P.sbuf("xa_wo", [128, 4, D], BF16, st)
            rr = lambda ap: ap.rearrange("(c p) n -> p c n", p=128)
            P.dma("pool", wkv[:, :, 0:512], rr(A["xa_wkv"][l][:, 0:512]), w=["wkv"])
            P.dma("pool", wkv[:, :, 512:1024], rr(A["xa_wkv"][l][:, 512:1024]), w=["wkv"])
            P.dma("pool", wq[:], rr(A["xa_wq"][l]), w=["wq"])
            for q in range(4):
                P.dma("pool", wo[:, :, q * 512:(q + 1) * 512], rr(A["xa_wo"][l][:, q * 512:(q + 1) * 512]), w=["wo"])
            KT = P.sbuf("xa_KT", [128, 4, MEM], BF16, st)
            VA = P.sbuf("xa_VA", [128, 2, 4, 129], BF16, st)
            QT = P.sbuf("xa_QT", [128, 4, S], BF16, st)
            mnr = [("mnT", t) for t in range(MEM // 128)]
            hnr = [("hnT", t) for t in range(NT)]
            for hd in range(4):
                for k in range(KC):
                    P.mm(ps[0][:, 0:MEM], wkv[:, k, hd * 128:(hd + 1) * 128], mnT[:, k, :], k == 0, k == KC - 1, ["wkv"] + mnr, ["ps0"])
                P.cp("act", KT[:, hd, :], ps[0][:, 0:MEM], ["ps0"], ["KT"])
            for mt in range(2):
                for k in range(KC):
                    P.mm(ps[1][:, 0:512], mnT[:, k, mt * 128:(mt + 1) * 128], wkv[:, k, 512:1024], k == 0, k == KC - 1, ["wkv"] + mnr, ["ps1"])
                P.cp("dve", VA[:, mt, :, 0:128], ps[1][:, 0:512].rearrange("p (h d) -> p h d", d=128), ["ps1"], ["VA"])
            P.memset("pool", VA[:, :, :, 128:129], 1.0, ["VA"])
            for hd in range(4):
                for g in range(NG):
                    pb, pbk = ps[2 + g % 2], f"ps{2 + g % 2}"
                    for k in range(KC):
                        P.mm(pb[:, 0:GW], wq[:, k, hd * 128:(hd + 1) * 128], hnT[:, k, g * GW:(g + 1) * GW], k == 0, k == KC - 1,
                             ["wq"] + hnr, [pbk])
                    P.cp("act" if g % 2 == 0 else "dve", QT[:, hd, g * GW:(g + 1) * GW], pb[:, 0:GW], [pbk], ["QT"])
            ptr_ = Ring(P, st, "xa_pt", 4, [128, GW], BF16)
            oTr = Ring(P, st, "xa_oT", 2, [128, 4, 128], BF16)
            smr = Ring(P, st, "xa_sm", 4, [128, 1], F32)
            hr = Ring(P, st, "xa_h", 2, [128, D], F32)
            TPG = GW // 128
            for g in range(NG):
                gs = slice(g * GW, (g + 1) * GW)
                pts = {}
                for hd in range(4):
                    for mt in range(2):
                        pb, pbk = ps[mt], f"ps{mt}"
                        p_, pk_ = ptr_.next()
                        P.mm(pb[:, 0:GW], KT[:, hd, mt * 128:(mt + 1) * 128], QT[:, hd, gs], True, True, ["KT", "QT"], [pbk])
                        P.act(p_[:], pb[:, 0:GW], AF.Exp, [pbk], [pk_], scale=scale)
                        pts[(hd % 2, mt)] = (p_, pk_)
                    for tt_ in range(TPG):
                        t = g * TPG + tt_
                        ob, obk = ps[2 + tt_ % 2], f"ps{2 + tt_ % 2}"
                        for mt in range(2):
                            p_, pk_ = pts[(hd % 2, mt)]
                            P.mm(ob[:, 0:129], p_[:, tt_ * 128:(tt_ + 1) * 128], VA[:, mt, hd, :], mt == 0, mt == 1, [pk_, "VA"], [obk])
                        sm, smk = smr.next()
                        o_, ok_ = self._xa_o(st, t)
                        P.add("dve", lambda e, sm=sm, ob=ob: e.reciprocal(out=sm[:, 0:1], in_=ob[:, 128:129]), [obk], [smk])
                        P.ts("dve", o_[:, hd * 128:(hd + 1) * 128], ob[:, 0:128], sm[:, 0:1], None, ALU.mult, None, [obk, smk], [(ok_, hd)])
                for tt_ in range(TPG):
                    t = g * TPG + tt_
                    o_, ok_ = self._xa_o(st, t)
                    oT, oTk = oTr.next()
                    pb, pbk = pt[tt_ % 2], f"pt{tt_ % 2}"
                    pv = pb[:, 0:512].rearrange("p (a b) -> p a b", b=128)
                    for j in range(4):
                        P.tr(pv[:, j, :], o_[:, j * 128:(j + 1) * 128], c["ident"][:], [(ok_, j), "const"], [pbk])
                    P.cp("act", oT[:], pv[:, 0:4, :], [pbk], [oTk])
                    ht, hk = hr.next()
                    P.dma("sp", ht[:], Sc["h"][t * 128:(t + 1) * 128, :], w=[hk])
                    for ocb in range(4):
                        ob, obk = ps[4 + ocb % 2], f"ps{4 + ocb % 2}"
                        for k in range(4):
                            P.mm(ob[:, 0:512], oT[:, k, :], wo[:, k, ocb * 512:(ocb + 1) * 512], k == 0, k == 3, [oTk, "wo"], [obk])
                        P.tt("dve", ht[:, ocb * 512:(ocb + 1) * 512], ht[:, ocb * 512:(ocb + 1) * 512], ob[:, 0:512], ALU.add, [hk, obk], [hk])
                    P.dma("sp", Sc["h"][t * 128:(t + 1) * 128, :], ht[:], r=[hk])

    def _xa_o(self, st, t):
        if not hasattr(self, "_xao") or self._xao_st is not st:
            self._xao = {}
            self._xao_st = st
        if t not in self._xao:
            self.P.uid += 1
            self._xao[t] = (self.P.sbuf("xa_ot", [128, 512], BF16, st), f"xa_ot#{self.P.uid}")
        return self._xao[t]

    def ffn_layer(self, l):
        P, S, NT, A, Sc, c = self.P, self.S, self.NT, self.A, self.Sc, self.c
        ps, pt, GW, NG = self.ps, self.pt, self.GW, self.NG
        TPG = GW // 128
        moe = (l % 2 == 1)
        FC = FFN // 128
        with P.phase() as st:
            hacc = P.sbuf("ff_hacc", [128, TPG, D], F32, st)
            hnT = P.sbuf("ff_hnT", [128, KC, GW], BF16, st)
            actT = P.sbuf("ff_actT", [128, FC, GW], BF16, st)
            w13r = Ring(P, st, "ff_w13", 2, [128, KC, 2, 128], BF16)
            w2r = Ring(P, st, "ff_w2", 2, [128, 8, 512], BF16)
            sir = Ring(P, st, "ff_si", 2, [128, GW], F32)
            gcol = P.sbuf("ff_gcol", [128, KC], F32, st)
            P.dma("sp", gcol[:], A["norm_ffn"][l].rearrange("(c p) -> p c", p=128), w=["gcol"], allow_slow_non_contiguous=True)
            hbr = Ring(P, st, "ff_hb", 2, [128, D], BF16)
            junk = P.sbuf("ff_junk", [128, D], BF16, st)
            ssr = Ring(P, st, "ff_ss", 2, [128, 2], F32)
            if moe:
                hn32 = Ring(P, st, "ff_hn32", 1, [128, D], F32)
                hnT32 = P.sbuf("ff_hnT32", [128, KC, 128], F32, st)
                rt = P.sbuf("ff_rt", [128, KC, NEXP], F32, st)
                P.dma("sp", rt[:], A["moe_router"][0].rearrange("(c p) e -> p c e", p=128), w=["rt"])
                gates = P.sbuf("ff_gates", [128, TPG, NEXP], F32, st)
                lgr = Ring(P, st, "ff_lg", 2, [128, 24], F32)
            for g in range(NG):
                for tt_ in range(TPG):
                    t = g * TPG + tt_
                    hb, hbk = hbr.next()
                    ss, sk = ssr.next()
                    P.dma("sp", hacc[:, tt_, :], Sc["h"][t * 128:(t + 1) * 128, :], w=[("hacc", tt_)])
                    P.act(junk[:], hacc[:, tt_, :], AF.Square, [("hacc", tt_)], ["ff_junk", sk], accum_out=ss[:, 0:1])
                    P.ts("dve", ss[:, 1:2], ss[:, 0:1], 1.0 / D, 1e-6, ALU.mult, ALU.add, [sk], [sk])
                    P.act(ss[:, 1:2], ss[:, 1:2], AF.Sqrt, [sk], [sk])
                    P.add("dve", lambda e, ss=ss: e.reciprocal(out=ss[:, 1:2], in_=ss[:, 1:2]), [sk], [sk])
                    P.ts("pool", hb[:], hacc[:, tt_, :], ss[:, 1:2], None, ALU.mult, None, [("hacc", tt_), sk], [hbk])
                    for c4 in range(0, KC, 4):
                        pb, pbk = pt[(c4 // 4) % 2], f"pt{(c4 // 4) % 2}"
                        pv = pb[:, 0:512].rearrange("p (a b) -> p a b", b=128)
                        for j in range(4):
                            P.tr(pv[:, j, :], hb[:, (c4 + j) * 128:(c4 + j + 1) * 128], c["ident"][:], [hbk, "const"], [pbk])
                        P.tt("dve", hnT[:, c4:c4 + 4, tt_ * 128:(tt_ + 1) * 128], pv[:, 0:4, :],
                             gcol[:, c4:c4 + 4].unsqueeze(2).to_broadcast([128, 4, 128]), ALU.mult, [pbk, "gcol"], ["hnT"])
                    if moe:
                        h32, h32k = hn32.next()
                        lg, lgk = lgr.next()
                        P.ts("pool", h32[:], hacc[:, tt_, :], ss[:, 1:2], None, ALU.mult, None, [("hacc", tt_), sk], [h32k])
                        for c4 in range(0, KC, 4):
                            pb, pbk = ps[(c4 // 4) % 2], f"ps{(c4 // 4) % 2}"
                            pv = pb[:, 0:512].rearrange("p (a b) -> p a b", b=128)
                            for j in range(4):
                                P.tr(pv[:, j, :], h32[:, (c4 + j) * 128:(c4 + j + 1) * 128], c["identf"][:], [h32k, "const"], [pbk])
                            P.tt("dve", hnT32[:, c4:c4 + 4, :], pv[:, 0:4, :],
                                 gcol[:, c4:c4 + 4].unsqueeze(2).to_broadcast([128, 4, 128]), ALU.mult, [pbk, "gcol"], ["hnT32"])
                        for k in range(KC):
                            P.mm(ps[2][:, 0:NEXP], hnT32[:, k, :], rt[:, k, :], k == 0, k == KC - 1, ["hnT32", "rt"], ["ps2"])
                        P.cp("dve", lg[:, 0:8], ps[2][:, 0:NEXP], ["ps2"], [lgk])
                        P.add("dve", lambda e, lg=lg: e.max(out=lg[:, 8:16], in_=lg[:, 0:8]), [lgk], [lgk])
                        P.ts("dve", lg[:, 16:24], lg[:, 0:8], lg[:, 8:9], None, ALU.subtract, None, [lgk], [lgk])
                        P.act(lg[:, 16:24], lg[:, 16:24], AF.Exp, [lgk], [lgk])
                        P.ts("dve", lg[:, 0:8], lg[:, 0:8], lg[:, 9:10], None, ALU.is_ge, None, [lgk], [lgk])
                        P.tt("dve", lg[:, 16:24], lg[:, 16:24], lg[:, 0:8], ALU.mult, [lgk], [lgk])
                        P.add("dve", lambda e, lg=lg: e.reduce_sum(out=lg[:, 10:11], in_=lg[:, 16:24], axis=AX.X), [lgk], [lgk])
                        P.add("dve", lambda e, lg=lg: e.reciprocal(out=lg[:, 10:11], in_=lg[:, 10:11]), [lgk], [lgk])
                        P.ts("dve", gates[:, tt_, :], lg[:, 16:24], lg[:, 10:11], None, ALU.mult, None, [lgk], [("gates", tt_)])
                for e_ in range(NEXP if moe else 1):
                    W13 = A["moe_w13"][0, e_] if moe else A["ffn_w13"][0]
                    W2 = A["moe_w2"][0, e_] if moe else A["ffn_w2"][0]
                    for fb in range(0, FC, 1):
                        w13, w13k = w13r.next()
                        P.dma("pool", w13[:, :, 0, :], W13[:, fb * 128:(fb + 1) * 128].rearrange("(c p) n -> p c n", p=128), w=[w13k])
                        P.dma("pool", w13[:, :, 1, :], W13[:, FFN + fb * 128:FFN + (fb + 1) * 128].rearrange("(c p) n -> p c n", p=128), w=[w13k])
                        for f2 in range(1):
                            f = fb + f2
                            for k in range(KC):
                                P.mm(ps[0][:, 0:GW], w13[:, k, 0, f2 * 128:(f2 + 1) * 128], hnT[:, k, :], k == 0, k == KC - 1, [w13k, "hnT"], ["ps0"])
                            for k in range(KC):
                                P.mm(ps[1][:, 0:GW], w13[:, k, 1, f2 * 128:(f2 + 1) * 128], hnT[:, k, :], k == 0, k == KC - 1, [w13k, "hnT"], ["ps1"])
                            si, sik = sir.next()
                            P.act(si[:], ps[0][:, 0:GW], AF.Silu, ["ps0"], [sik])
                            P.tt("dve", actT[:, f, :], si[:], ps[1][:, 0:GW], ALU.mult, [sik, "ps1"], [("actT", f)])
                    ar = [("actT", f) for f in range(FC)]
                    for dcb in range(4):
                        for f8 in range(0, FC, 8):
                            w2, w2k = w2r.next()
                            P.dma("pool", w2[:], W2[f8 * 128:(f8 + 8) * 128, dcb * 512:(dcb + 1) * 512].rearrange("(c p) n -> p c n", p=128), w=[w2k])
                            for f in range(8):
                                for tt_ in range(TPG):
                                    P.mm(ps[2 + tt_][:, 0:512], actT[:, f8 + f, tt_ * 128:(tt_ + 1) * 128], w2[:, f, :],
                                         f8 + f == 0, f8 + f == FC - 1, [w2k] + ar, [f"ps{2 + tt_}"])
                        for tt_ in range(TPG):
                            hs = hacc[:, tt_, dcb * 512:(dcb + 1) * 512]
                            if moe:
                                P.stt("dve", hs, ps[2 + tt_][:, 0:512], gates[:, tt_, e_:e_ + 1], hs, ALU.mult, ALU.add,
                                      [f"ps{2 + tt_}", ("gates", tt_), ("hacc", tt_)], [("hacc", tt_)])
                            else:
                                P.tt("dve", hs, hs, ps[2 + tt_][:, 0:512], ALU.add, [f"ps{2 + tt_}", ("hacc", tt_)], [("hacc", tt_)])
                for tt_ in range(TPG):
                    t = g * TPG + tt_
                    P.dma("sp", Sc["h"][t * 128:(t + 1) * 128, :], hacc[:, tt_, :], r=[("hacc", tt_)])

    def moe_layer(self, l):
        P, S, NT, A, Sc, c = self.P, self.S, self.NT, self.A, self.Sc, self.c
        ps, pt = self.ps, self.pt
        FC = FFN // 128
        CAP = 128 * int(math.ceil(1.5 * S / 4 / 128))
        NJ = CAP // 128
        NP = int(math.ceil(CAP / 512))
        PW = CAP // NP
        NQ = 4
        FQ = FC // NQ
        with P.phase() as stg:
            gates = P.sbuf("mo_gates", [128, NT, NEXP], F32, stg)
            posm = P.sbuf("mo_posm", [128, NT, NEXP], F32, stg)
            iota = P.sbuf("mo_iota", [128, CAP], F32, stg)
            jcol = P.sbuf("mo_jcol", [128, 8], F32, stg)
            P.dma("sp", iota[:], A["c_iota"][:, 0:CAP], w=["iota"])
            P.dma("sp", jcol[:], A["c_jcol"], w=["jcol"])
            with P.phase() as st:
                grow = self.bcast_row(st, "mo_g", A["norm_ffn"][l], D)
                sutri = P.sbuf("mo_sutri", [128, 128], F32, st)
                P.dma("sp", sutri[:], A["c_sutri"], w=["sutri"])
                rt = P.sbuf("mo_rt", [128, KC, NEXP], F32, st)
                P.dma("sp", rt[:], A["moe_router"][0].rearrange("(c p) e -> p c e", p=128), w=["rt"])
                hr = Ring(P, st, "mo_h", 2, [128, D], F32)
                h32r = Ring(P, st, "mo_h32", 2, [128, D], F32)
                h16r = Ring(P, st, "mo_h16", 2, [128, D], BF16)
                junk = P.sbuf("mo_junk", [128, D], BF16, st)
                ssr = Ring(P, st, "mo_ss", 2, [128, 2], F32)
                hT32r = Ring(P, st, "mo_hT32", 2, [128, KC, 128], F32)
                lgr = Ring(P, st, "mo_lg", 2, [128, 32], F32)
                sel = P.sbuf("mo_sel", [128, NT, NEXP], F32, st)
                run = P.sbuf("mo_run", [128, NEXP], F32, st)
                P.memset("dve", run[:], 0.0, ["run"])
                for t in range(NT):
                    ht, hk = hr.next()
                    h32, h32k = h32r.next()
                    h16, h16k = h16r.next()
                    ss, sk = ssr.next()
                    hT32, hT32k = hT32r.next()
                    lg, lgk = lgr.next()
                    P.dma("sp", ht[:], Sc["h"][t * 128:(t + 1) * 128, :], w=[hk])
                    P.act(junk[:], ht[:], AF.Square, [hk], ["mo_junk", sk], accum_out=ss[:, 0:1])
                    P.ts("dve", ss[:, 1:2], ss[:, 0:1], 1.0 / D, 1e-6, ALU.mult, ALU.add, [sk], [sk])
                    P.act(ss[:, 1:2], ss[:, 1:2], AF.Sqrt, [sk], [sk])
                    P.add("dve", lambda e, ss=ss: e.reciprocal(out=ss[:, 1:2], in_=ss[:, 1:2]), [sk], [sk])
                    P.stt("dve", h32[:], ht[:], ss[:, 1:2], grow[:], ALU.mult, ALU.mult, [hk, sk, "mo_g"], [h32k])
                    P.cp("act", h16[:], h32[:], [h32k], [h16k])
                    P.dma("sp", Sc["hn16"][t * 128:(t + 1) * 128, :], h16[:], r=[h16k])
                    for c4 in range(0, KC, 4):
                        pb, pbk = ps[(c4 // 4) % 2], f"ps{(c4 // 4) % 2}"
                        pv = pb[:, 0:512].rearrange("p (a b) -> p a b", b=128)
                        for j in range(4):
                            P.tr(pv[:, j, :], h32[:, (c4 + j) * 128:(c4 + j + 1) * 128], c["identf"][:], [h32k, "const"], [pbk])
                        P.cp("dve" if (c4 // 4) % 2 == 0 else "act", hT32[:, c4:c4 + 4, :], pv[:, 0:4, :], [pbk], [hT32k])
                    for k in range(KC):
                        P.mm(ps[2][:, 0:NEXP], hT32[:, k, :], rt[:, k, :], k == 0, k == KC - 1, [hT32k, "rt"], ["ps2"])
                    P.cp("dve", lg[:, 0:8], ps[2][:, 0:NEXP], ["ps2"], [lgk])
                    P.add("dve", lambda e, lg=lg: e.max(out=lg[:, 8:16], in_=lg[:, 0:8]), [lgk], [lgk])
                    P.ts("dve", lg[:, 16:24], lg[:, 0:8], lg[:, 8:9], None, ALU.subtract, None, [lgk], [lgk])
                    P.act(lg[:, 16:24], lg[:, 16:24], AF.Exp, [lgk], [lgk])
                    P.ts("dve", sel[:, t, :], lg[:, 0:8], lg[:, 9:10], None, ALU.is_ge, None, [lgk], [("sel", t)])
                    P.tt("dve", lg[:, 16:24], lg[:, 16:24], sel[:, t, :], ALU.mult, [lgk, ("sel", t)], [lgk])
                    P.add("dve", lambda e, lg=lg: e.reduce_sum(out=lg[:, 10:11], in_=lg[:, 16:24], axis=AX.X), [lgk], [lgk])
                    P.add("dve", lambda e, lg=lg: e.reciprocal(out=lg[:, 10:11], in_=lg[:, 10:11]), [lgk], [lgk])
                    P.ts("dve", gates[:, t, :], lg[:, 16:24], lg[:, 10:11], None, ALU.mult, None, [lgk], [("gates", t)])
                    P.mm(ps[3][:, 0:NEXP], sutri[:], sel[:, t, :], True, True, ["sutri", ("sel", t)], ["ps3"])
                    P.mm(ps[3][:, 8:8 + NEXP], c["ones"][:], sel[:, t, :], True, True, ["const", ("sel", t)], ["ps3"])
                    P.tt("dve", lg[:, 24:32], ps[3][:, 0:NEXP], run[:], ALU.add, ["ps3", "run"], [lgk])
                    P.stt("dve", posm[:, t, :], lg[:, 24:32], 1.0, sel[:, t, :], ALU.add, ALU.mult, [lgk, ("sel", t)], [("posm", t)])
                    P.ts("dve", posm[:, t, :], posm[:, t, :], -1.0, None, ALU.add, None, [("posm", t)], [("posm", t)])
                    P.tt("dve", run[:], run[:], ps[3][:, 8:8 + NEXP], ALU.add, ["ps3", "run"], ["run"])
            st = stg
            xgo = P.sbuf("mo_xgo", [128, KC * CAP], BF16, st)
            xgT = xgo[:, :].rearrange("p (k j) -> p k j", j=CAP)
            outg16 = xgo[:, :].rearrange("p (j d) -> p j d", d=D)
            actT = P.sbuf("mo_actT", [128, FQ, CAP], BF16, st)
            outg32 = P.sbuf("mo_outg32", [128, NJ, D], F32, st)
            posbc = P.sbuf("mo_posbc", [128, S], F32, st)
            w13r = Ring(P, st, "mo_w13", 2, [128, KC, 2, 128], BF16)
            w2r = Ring(P, st, "mo_w2", 2, [128, 8, 512], BF16)
            hnr = Ring(P, st, "mo_hn", 3, [128, D], BF16)
            xgr = Ring(P, st, "mo_xg", 2, [128, D], BF16)
            htr = Ring(P, st, "mo_ht", 2, [128, D], F32)
            sir = Ring(P, st, "mo_si", 2, [128, PW], F32)
            pmr = Ring(P, st, "mo_pm", 4, [128, 128], BF16)
            pmTr = Ring(P, st, "mo_pmT", 2, [128, NJ, 128], BF16)
            dgr = Ring(P, st, "mo_dg", 2, [128, 128], F32)
            pmall = [("posm", t) for t in range(NT)]
            for e_ in range(NEXP):
                W13, W2 = A["moe_w13"][0, e_], A["moe_w2"][0, e_]
                for t in range(NT):
                    dg, dgk = dgr.next()
                    P.ts("dve", dg[:], c["identf"][:], posm[:, t, e_:e_ + 1], None, ALU.mult, None, ["const", ("posm", t)], [dgk])
                    pb, pbk = ps[4 + t % 2], f"ps{4 + t % 2}"
                    P.mm(pb[:, 0:128], c["ones"][:], dg[:], True, True, ["const", dgk], [pbk])
                    P.cp("act", posbc[:, t * 128:(t + 1) * 128], pb[:, 0:128], [pbk], ["posbc"])
                for J in range(NJ):
                    for t in range(NT):
                        hn, hnk = hnr.next()
                        pm, pmk = pmr.next()
                        P.dma("sp", hn[:], Sc["hn16"][t * 128:(t + 1) * 128, :], w=[hnk])
                        P.ts("dve", pm[:], iota[:, J * 128:(J + 1) * 128], posm[:, t, e_:e_ + 1], None,
                             ALU.is_equal, None, ["iota", ("posm", t)], [pmk])
                        for q in range(4):
                            P.mm(ps[q][:, 0:512], pm[:], hn[:, q * 512:(q + 1) * 512], t == 0, t == NT - 1, [pmk, hnk], [f"ps{q}"])
                    xg, xgk = xgr.next()
                    for q in range(4):
                        P.cp("act" if q % 2 == 0 else "dve", xg[:, q * 512:(q + 1) * 512], ps[q][:, 0:512], [f"ps{q}"], [xgk])
                    for c4 in range(0, KC, 4):
                        pb, pbk = pt[(c4 // 4) % 2], f"pt{(c4 // 4) % 2}"
                        pv = pb[:, 0:512].rearrange("p (a b) -> p a b", b=128)
                        for j in range(4):
                            P.tr(pv[:, j, :], xg[:, (c4 + j) * 128:(c4 + j + 1) * 128], c["ident"][:], [xgk, "const"], [pbk])
                        P.cp("dve" if (c4 // 4) % 2 == 0 else "act", xgT[:, c4:c4 + 4, J * 128:(J + 1) * 128], pv[:, 0:4, :], [pbk], ["xgo"])
                for fq in range(NQ):
                    for fl in range(FQ):
                        f = fq * FQ + fl
                        w13, w13k = w13r.next()
                        P.dma("pool", w13[:, :, 0, :], W13[:, f * 128:(f + 1) * 128].rearrange("(c p) n -> p c n", p=128), w=[w13k])
                        P.dma("pool", w13[:, :, 1, :], W13[:, FFN + f * 128:FFN + (f + 1) * 128].rearrange("(c p) n -> p c n", p=128), w=[w13k])
                        for pc in range(NP):
                            cs = slice(pc * PW, (pc + 1) * PW)
                            b1, b1k = ps[pc], f"ps{pc}"
                            b3, b3k = ps[2 + pc], f"ps{2 + pc}"
                            for k in range(KC):
                                P.mm(b1[:, 0:PW], w13[:, k, 0, :], xgT[:, k, cs], k == 0, k == KC - 1, [w13k, "xgo"], [b1k])
                            for k in range(KC):
                                P.mm(b3[:, 0:PW], w13[:, k, 1, :], xgT[:, k, cs], k == 0, k == KC - 1, [w13k, "xgo"], [b3k])
                            si, sik = sir.next()
                            P.act(si[:], b1[:, 0:PW], AF.Silu, [b1k], [sik])
                            P.tt("dve", actT[:, fl, cs], si[:], b3[:, 0:PW], ALU.mult, [sik, b3k], [("actT", fl)])
                    ar = [("actT", fl) for fl in range(FQ)]
                    for dcb in range(4):
                        for f8 in range(0, FQ, 8):
                            nf = min(8, FQ - f8)
                            w2, w2k = w2r.next()
                            r0 = (fq * FQ + f8) * 128
                            P.dma("pool", w2[:, 0:nf, :], W2[r0:r0 + nf * 128, dcb * 512:(dcb + 1) * 512].rearrange("(c p) n -> p c n", p=128), w=[w2k])
                            for f in range(nf):
                                for J in range(NJ):
                                    P.mm(ps[J][:, 0:512], actT[:, f8 + f, J * 128:(J + 1) * 128], w2[:, f, :],
                                         f8 + f == 0, f8 + f == FQ - 1, [w2k] + ar, [f"ps{J}"])
                        for J in range(NJ):
                            dst32 = outg32[:, J, dcb * 512:(dcb + 1) * 512]
                            if fq == 0:
                                P.cp("act" if J % 2 == 0 else "dve", dst32, ps[J][:, 0:512], [f"ps{J}"], [("og32", J, dcb)])
                            elif fq < NQ - 1:
                                P.tt("dve", dst32, dst32, ps[J][:, 0:512], ALU.add, [f"ps{J}", ("og32", J, dcb)], [("og32", J, dcb)])
                            else:
                                P.tt("dve", outg16[:, J, dcb * 512:(dcb + 1) * 512], dst32, ps[J][:, 0:512], ALU.add,
                                     [f"ps{J}", ("og32", J, dcb)], ["xgo"])
                for t in range(NT):
                    ht, hk = htr.next()
                    pmT, pmTk = pmTr.next()
                    P.dma("sp", ht[:], Sc["h"][t * 128:(t + 1) * 128, :], r=[("hD", t)], w=[hk])
                    for J in range(NJ):
                        P.ts("dve", pmT[:, J, :], posbc[:, t * 128:(t + 1) * 128], jcol[:, J:J + 1], None,
                             ALU.is_equal, None, ["posbc", "jcol"], [pmTk])
                    for dcb in range(4):
                        pb, pbk = ps[dcb], f"ps{dcb}"
                        for J in range(NJ):
                            P.mm(pb[:, 0:512], pmT[:, J, :], outg16[:, J, dcb * 512:(dcb + 1) * 512], J == 0, J == NJ - 1, [pmTk, "xgo"], [pbk])
                        hs = ht[:, dcb * 512:(dcb + 1) * 512]
                        P.stt("dve", hs, pb[:, 0:512], gates[:, t, e_:e_ + 1], hs, ALU.mult, ALU.add, [pbk, ("gates", t), hk], [hk])
                    P.dma("sp", Sc["h"][t * 128:(t + 1) * 128, :], ht[:], r=[hk], w=[("hD", t)])


def make_consts(S):
    bf = ml_dtypes.bfloat16
    i = np.arange(128)
    tri = (i[:, None] <= i[None, :]).astype(np.float32)
    inv = 1.0 / (10000.0 ** (np.arange(0, 64, 2, dtype=np.float32) / 64.0))
    ang = np.arange(S, dtype=np.float32)[:, None] * inv[None, :]
    cos, sin = np.cos(ang).astype(np.float32).T, np.sin(ang).astype(np.float32).T
    return dict(
        c_ident=np.eye(128, dtype=np.float32).astype(bf), c_identf=np.eye(128, dtype=np.float32),
        c_tri=tri, c_trib=tri.astype(bf), c_lowb=(1.0 - tri).astype(bf),
        c_mneg=((1.0 - tri) * NEG).astype(np.float32), c_ones=np.ones((128, 128), np.float32),
        c_sutri=(tri - np.eye(128, dtype=np.float32)).astype(np.float32),
        c_iota=np.ascontiguousarray(np.broadcast_to(np.arange(1024, dtype=np.float32)[None, :], (128, 1024))),
        c_jcol=np.ascontiguousarray((np.arange(8, dtype=np.float32)[None, :] * 128 + i[:, None]).astype(np.float32)),
        c_cos2=np.ascontiguousarray(np.concatenate([cos, cos], 0)),
        c_sin2=np.ascontiguousarray(np.concatenate([-sin, sin], 0)),
    )


_CACHE = {}


def kernel(**inputs):
    S = inputs["x"].shape[1]
    B = inputs["x"].shape[0]
    if S not in _CACHE:
        _CACHE[S] = Builder(S).build()
    nc = _CACHE[S]
    consts = make_consts(S)
    shared = {k: np.ascontiguousarray(v) for k, v in inputs.items() if k not in ("x", "mem")}
    in_maps = []
    for b in range(B):
        m = dict(shared)
        m.update(consts)
        m["x"] = np.ascontiguousarray(inputs["x"][b])
        m["mem"] = np.ascontiguousarray(inputs["mem"][b])
        in_maps.append(m)
    res = run_bass_kernel_spmd(nc, in_maps, core_ids=list(range(B)))
    return np.stack([r["out"] for r in res.results], axis=0).astype(np.float32)
```

```python
import contextlib
import math
import numpy as np
import ml_dtypes
import concourse.bass as bass
import concourse.mybir as mybir
from concourse.bass_utils import run_bass_kernel_spmd

F32 = mybir.dt.float32
BF16 = mybir.dt.bfloat16
AF = mybir.ActivationFunctionType
ALU = mybir.AluOpType
AX = mybir.AxisListType

EPOCH = 4000
N_DMA_SEMS = 40

D = 2048
KC = D // 128
IN_TOTAL = 16216
FFN = 7168
NEXP = 8
MEM = 256
O_MLQ, O_MLK, O_MLV, O_MLO, O_MLI, O_MLF = 0, 512, 1024, 2048, 3072, 3076
O_Z, O_XBC, O_DT, O_CQ, O_CKV, O_KR = 3080, 4104, 5640, 5656, 6168, 6424
O_SWQ, O_SWK, O_SWV, O_GATE = 6488, 7512, 7768, 8024
NEG = -30000.0


class Prog:
    ENGS = ("pe", "act", "dve", "pool", "sp")

    def __init__(self, nc):
        self.nc = nc
        self.ops = []
        self.last_w = {}
        self.readers = {}
        self.stack = contextlib.ExitStack()
        self.nd = 0
        self.last_on_eng = {}
        self.last_dma_slot = {}
        self.uid = 0

    def sbuf(self, name, shape, dtype, stack=None):
        self.uid += 1
        return (stack or self.stack).enter_context(
            self.nc.sbuf_tensor(f"{name}_{self.uid}", list(shape), dtype))

    def psum(self, name, shape, dtype):
        return self.stack.enter_context(self.nc.psum_tensor(name, list(shape), dtype))

    def dram(self, name, shape, dtype, kind="Internal"):
        return self.nc.dram_tensor(name, list(shape), dtype, kind=kind).ap()

    def add(self, eng, emit, reads=(), writes=(), dma=False):
        idx = len(self.ops)
        raw, other = set(), set()
        for r in reads:
            w = self.last_w.get(r)
            if w is not None:
                raw.add(w)
        for r in writes:
            w = self.last_w.get(r)
            if w is not None:
                other.add(w)
            for rd in self.readers.get(r, ()):
                other.add(rd)
        for r in reads:
            lst = self.readers.setdefault(r, [])
            if not dma:
                lst[:] = [j for j in lst if self.ops[j]["dma"] or self.ops[j]["eng"] != eng]
            lst.append(idx)
        for r in writes:
            self.last_w[r] = idx
            self.readers[r] = []
        raw.discard(idx)
        other.discard(idx)
        other -= raw
        op = dict(eng=eng, emit=emit, raw=raw, other=other, dma=dma, need_inc=False,
                  ticket=None, explicit=None)
        if dma:
            op["slot"] = self.nd % N_DMA_SEMS
            self.nd += 1
            self.last_dma_slot[op["slot"]] = idx
        self.last_on_eng[eng] = idx
        self.ops.append(op)
        return idx

    def barrier(self):
        targets = set(self.last_on_eng.values()) | set(self.last_dma_slot.values())
        for e in self.ENGS:
            self.ops.append(dict(eng=e, emit=None, raw=set(), other=set(), dma=False,
                                 need_inc=False, ticket=None, explicit=set(targets)))
        self.last_w = {}
        self.readers = {}

    @contextlib.contextmanager
    def phase(self):
        st = contextlib.ExitStack()
        try:
            yield st
        finally:
            self.barrier()
            st.close()

    def dma(self, eng, out, in_, r=(), w=(), **kw):
        return self.add(eng, lambda e: e.dma_start(out=out, in_=in_, **kw), r, w, dma=True)

    def mm(self, out, lhsT, rhs, start, stop, r, w):
        return self.add("pe", lambda e: e.matmul(out=out, lhsT=lhsT, rhs=rhs, start=start, stop=stop), r, w)

    def tr(self, out, in_, ident, r, w):
        return self.add("pe", lambda e: e.transpose(out=out, in_=in_, identity=ident), r, w)

    def act(self, out, in_, func, r, w, **kw):
        return self.add("act", lambda e: e.activation(out=out, in_=in_, func=func, **kw), r, w)

    def ts(self, eng, out, in0, s1, s2, op0, op1, r, w):
        if op1 is None:
            return self.add(eng, lambda e: e.tensor_scalar(out=out, in0=in0, scalar1=s1, scalar2=None, op0=op0), r, w)
        return self.add(eng, lambda e: e.tensor_scalar(out=out, in0=in0, scalar1=s1, scalar2=s2, op0=op0, op1=op1), r, w)

    def tt(self, eng, out, in0, in1, op, r, w):
        return self.add(eng, lambda e: e.tensor_tensor(out=out, in0=in0, in1=in1, op=op), r, w)

    def stt(self, eng, out, in0, scalar, in1, op0, op1, r, w):
        return self.add(eng, lambda e: e.scalar_tensor_tensor(out=out, in0=in0, scalar=scalar, in1=in1,
                                                              op0=op0, op1=op1), r, w)

    def cp(self, eng, out, in_, r, w):
        if eng == "act":
            return self.add(eng, lambda e: e.copy(out=out, in_=in_), r, w)
        return self.add(eng, lambda e: e.tensor_copy(out=out, in_=in_), r, w)

    def memset(self, eng, ap, val, w):
        return self.add(eng, lambda e: e.memset(ap, val), (), w)

    def emit_all(self):
        nc, ops = self.nc, self.ops
        self.barrier()
        for i, op in enumerate(ops):
            if op["explicit"] is not None:
                deps = [j for j in op["explicit"] if j < i]
            else:
                deps = []
                for j in op["raw"]:
                    pj = ops[j]
                    if pj["eng"] == op["eng"] and not pj["dma"] and op["eng"] == "pe" and not op["dma"]:
                        continue
                    deps.append(j)
                for j in op["other"]:
                    pj = ops[j]
                    if pj["eng"] == op["eng"] and not pj["dma"] and not op["dma"]:
                        continue
                    deps.append(j)
            op["deps"] = sorted(set(deps))
            for j in op["deps"]:
                ops[j]["need_inc"] = True
        counts = {e: 0 for e in self.ENGS}
        nsem = {e: 1 for e in self.ENGS}
        dma_uses = [0] * N_DMA_SEMS
        dma_prev = [None] * N_DMA_SEMS
        for i, op in enumerate(ops):
            if op["dma"]:
                s = op["slot"]
                dma_uses[s] += 1
                op["ticket"] = ("dma", s, dma_uses[s] * 16)
                op["dma_prev"] = dma_prev[s]
                dma_prev[s] = i
            elif op["need_inc"]:
                e = op["eng"]
                ep, v = divmod(counts[e], EPOCH)
                counts[e] += 1
                op["ticket"] = (e, ep, v + 1)
                nsem[e] = max(nsem[e], ep + 1)
        sems = {}
        for e in self.ENGS:
            for ep in range(nsem[e]):
                sems[(e, ep)] = self.stack.enter_context(nc.semaphore(f"s_{e}_{ep}"))
        for s in range(N_DMA_SEMS):
            sems[("dma", s)] = self.stack.enter_context(nc.semaphore(f"s_dma_{s}"))
        print("ops", len(ops), "incs", counts, "ndma", self.nd, "nsems", len(sems), flush=True)
        by_eng = {e: [] for e in self.ENGS}
        for i, op in enumerate(ops):
            by_eng[op["eng"]].append(i)

        def run_engine(ename, eng):
            waited = {}

            def wait(t):
                key = (t[0], t[1])
                if waited.get(key, 0) >= t[2]:
                    return
                eng.wait_ge(sems[key], t[2])
                waited[key] = t[2]

            for i in by_eng[ename]:
                op = ops[i]
                for j in op["deps"]:
                    wait(ops[j]["ticket"])
                if op["emit"] is None:
                    continue
                if op["dma"] and op["dma_prev"] is not None:
                    wait(ops[op["dma_prev"]]["ticket"])
                ins = op["emit"](eng)
                t = op["ticket"]
                if t is not None:
                    ins.then_inc(sems[(t[0], t[1])], 16 if op["dma"] else 1)

        with nc.Block() as block:
            @block.tensor
            def _(e):
                run_engine("pe", e)

            @block.scalar
            def _(e):
                run_engine("act", e)

            @block.vector
            def _(e):
                run_engine("dve", e)

            @block.gpsimd
            def _(e):
                run_engine("pool", e)

            @block.sync
            def _(e):
                run_engine("sp", e)
        self.stack.close()


class Ring:
    def __init__(self, P, st, name, n, shape, dtype):
        self.n = n
        self.bufs = [P.sbuf(name, shape, dtype, st) for _ in range(n)]
        P.uid += 1
        self.keys = [f"{name}#{P.uid}#{i}" for i in range(n)]
        self.i = 0

    def next(self):
        j = self.i % self.n
        self.i += 1
        return self.bufs[j], self.keys[j]


class KeyRing:
    def __init__(self, objs, keys):
        self.objs, self.keys, self.i = objs, keys, 0

    def next(self):
        j = self.i % len(self.objs)
        self.i += 1
        return self.objs[j], self.keys[j]


class Builder:
    def __init__(self, S, depth=2, debug=False, stop_after=None):
        self.S = S
        self.NT = S // 128
        self.GW = min(512, S)
        self.NG = S // self.GW
        self.depth = depth
        self.debug = debug
        self.stop_after = stop_after
        nc = bass.Bass("TRN2", target_bir_lowering=False)
        self.nc = nc
        self.P = Prog(nc)

    def norm_T(self, st, src, gamma, dstT, dkey, R, W, eps=1e-6, dt=BF16, rows_at=None):
        P, c = self.P, self.c
        kc = W // 128
        gcol = P.sbuf("gcol", [128, kc], F32, st)
        P.dma("sp", gcol[:], gamma.rearrange("(c p) -> p c", p=128), w=["gcol"], allow_slow_non_contiguous=True)
        hr = Ring(P, st, "nt_h", 2, [128, W], F32)
        hbr = Ring(P, st, "nt_hb", 2, [128, W], dt)
        junk = P.sbuf("nt_junk", [128, W], F32, st)
        ssr = Ring(P, st, "nt_ss", 2, [128, 2], F32)
        ptr = KeyRing(self.pt if dt == BF16 else self.ps[4:6], ["pt0", "pt1"] if dt == BF16 else ["ps4", "ps5"])
        ident = c["ident"] if dt == BF16 else c["identf"]
        for t in range(R // 128):
            ht, hk = hr.next()
            hb, hbk = hbr.next()
            ss, sk = ssr.next()
            P.dma("sp", ht[:], src[t * 128:(t + 1) * 128, :], w=[hk])
            P.act(junk[:], ht[:], AF.Square, [hk], ["nt_junk", sk], accum_out=ss[:, 0:1])
            P.ts("dve", ss[:, 1:2], ss[:, 0:1], 1.0 / W, eps, ALU.mult, ALU.add, [sk], [sk])
            P.act(ss[:, 1:2], ss[:, 1:2], AF.Sqrt, [sk], [sk])
            P.add("dve", lambda e, ss=ss: e.reciprocal(out=ss[:, 1:2], in_=ss[:, 1:2]), [sk], [sk])
            P.ts("pool", hb[:], ht[:], ss[:, 1:2], None, ALU.mult, None, [hk, sk], [hbk])
            for c4 in range(0, kc, 4):
                n4 = min(4, kc - c4)
                pst, pk = ptr.next()
                pv = pst[:, 0:512].rearrange("p (a b) -> p a b", b=128)
                for j in range(n4):
                    P.tr(pv[:, j, :], hb[:, (c4 + j) * 128:(c4 + j + 1) * 128], ident[:], [hbk, "const"], [pk])
                P.tt("dve", dstT[:, c4:c4 + n4, t * 128:(t + 1) * 128], pv[:, 0:n4, :],
                     gcol[:, c4:c4 + n4].unsqueeze(2).to_broadcast([128, n4, 128]), ALU.mult,
                     [pk, "gcol"], [(dkey, t)])

    def wload(self, wring, W_ap, c0, w, kc):
        wt, wk = wring.next()
        self.P.dma("pool", wt[:, 0:kc, 0:w], W_ap[:, c0:c0 + w].rearrange("(c p) n -> p c n", p=128), w=[wk])
        return wt, wk

    def proj_fm(self, st, xT, xkey, kc, W_ap, c0, ncols, dst, rings, func=None, dtype=F32):
        P, S, GW = self.P, self.S, self.GW
        wring, psr, stg32, stg16 = rings
        stgr = stg32 if dtype == F32 else stg16
        xr = [(xkey, t) for t in range(self.NT)]
        for cb in range(0, ncols, 512):
            w = min(512, ncols - cb)
            wt, wk = self.wload(wring, W_ap, c0 + cb, w, kc)
            for sb in range(0, w, 128):
                m = min(128, w - sb)
                stg, sk = stgr.next()
                for g in range(self.NG):
                    ps, pk = psr.next()
                    for k in range(kc):
                        P.mm(ps[0:m, 0:GW], wt[:, k, sb:sb + m], xT[:, k, g * GW:(g + 1) * GW],
                             k == 0, k == kc - 1, [wk] + xr, [pk])
                    if func is not None or (g % 2 == 0):
                        P.act(stg[0:m, g * GW:(g + 1) * GW], ps[0:m, 0:GW], func or AF.Copy, [pk], [sk])
                    else:
                        P.cp("dve", stg[0:m, g * GW:(g + 1) * GW], ps[0:m, 0:GW], [pk], [sk])
                P.dma("sp", dst[cb + sb:cb + sb + m, :], stg[0:m, 0:S], r=[sk])

    def proj_tm(self, st, xT, xkey, kc, W_ap, c0, ncols, dst, rings, dtype=F32):
        P, S = self.P, self.S
        wring, psr, stg32, stg16 = rings
        stgr = stg32 if dtype == F32 else stg16
        for cb in range(0, ncols, 512):
            w = min(512, ncols - cb)
            wt, wk = self.wload(wring, W_ap, c0 + cb, w, kc)
            for t in range(self.NT):
                ps, pk = psr.next()
                stg, sk = stgr.next()
                for k in range(kc):
                    P.mm(ps[:, 0:w], xT[:, k, t * 128:(t + 1) * 128], wt[:, k, 0:w], k == 0, k == kc - 1,
                         [wk, (xkey, t)], [pk])
                if t % 2 == 0:
                    P.act(stg[:, 0:w], ps[:, 0:w], AF.Copy, [pk], [sk])
                else:
                    P.cp("dve", stg[:, 0:w], ps[:, 0:w], [pk], [sk])
                P.dma("sp", dst[t * 128:(t + 1) * 128, cb:cb + w], stg[:, 0:w], r=[sk])

    def bcast_row(self, st, name, src_row, n):
        t = self.P.sbuf(name, [128, n], F32, st)
        self.P.dma("sp", t[:], src_row.partition_broadcast(128), w=[name])
        return t

    def build(self):
        P, nc, S, NT = self.P, self.nc, self.S, self.NT
        inp = lambda n, sh, dt=F32: P.dram(n, sh, dt, kind="ExternalInput")
        A = {}
        A["x"] = inp("x", [S, D])
        A["mem"] = inp("mem", [MEM, D])
        for n, sh in (("norm_mix", [2, D]), ("w_in", [2, D, IN_TOTAL]), ("ml_igate_bias", [2, 4]),
                      ("ml_fgate_bias", [2, 4]), ("ml_norm", [2, 1024]), ("ssm_conv_w", [2, 4, 1536]),
                      ("ssm_conv_b", [2, 1536]), ("ssm_dt_bias", [2, 16]), ("ssm_a_log", [2, 16]),
                      ("ssm_d", [2, 16]), ("ssm_norm", [2, 1024]), ("mla_q_norm", [2, 512]),
                      ("mla_w_uq", [2, 512, 1536]), ("mla_kv_norm", [2, 256]), ("mla_w_ukv", [2, 256, 2048]),
                      ("swa_sinks", [2, 16]), ("w_branch", [2, 4, 1024, D]), ("w_out", [2, D, D]),
                      ("norm_cross", [2, D]), ("norm_mem", [2, D]), ("xa_wq", [2, D, 512]),
                      ("xa_wkv", [2, D, 1024]), ("xa_wo", [2, 512, D]), ("norm_ffn", [2, D]),
                      ("ffn_w13", [1, D, 2 * FFN]), ("ffn_w2", [1, FFN, D]), ("moe_router", [1, D, NEXP]),
                      ("moe_w13", [1, NEXP, D, 2 * FFN]), ("moe_w2", [1, NEXP, FFN, D]), ("norm_final", [D])):
            A[n] = inp(n, sh)
        A["c_ident"] = inp("c_ident", [128, 128], BF16)
        A["c_identf"] = inp("c_identf", [128, 128])
        A["c_tri"] = inp("c_tri", [128, 128])
        A["c_trib"] = inp("c_trib", [128, 128], BF16)
        A["c_lowb"] = inp("c_lowb", [128, 128], BF16)
        A["c_mneg"] = inp("c_mneg", [128, 128])
        A["c_ones"] = inp("c_ones", [128, 128])
        A["c_sutri"] = inp("c_sutri", [128, 128])
        A["c_iota"] = inp("c_iota", [128, 1024])
        A["c_jcol"] = inp("c_jcol", [128, 8])
        A["c_cos2"] = inp("c_cos2", [64, S])
        A["c_sin2"] = inp("c_sin2", [64, S])
        self.A = A
        out = P.dram("out", [S, D], F32, kind="ExternalOutput")
        self.out = out
        Sc = {}
        Sc["h"] = P.dram("s_h", [S, D], F32)
        Sc["ufm32"] = P.dram("s_ufm32", [IN_TOTAL + 64, S], F32)
        Sc["ufm16"] = P.dram("s_ufm16", [IN_TOTAL, S], BF16)
        Sc["utm32"] = P.dram("s_utm32", [S, IN_TOTAL], F32)
        Sc["utm16"] = P.dram("s_utm16", [S, IN_TOTAL], BF16)
        Sc["xcT"] = P.dram("s_xcT", [1536, S], F32)
        Sc["hn16"] = P.dram("s_hn16", [S, D], BF16)
        kind = "ExternalOutput" if self.debug else "Internal"
        for i, n in enumerate("abcd"):
            Sc["y" + n] = P.dram("s_y" + n, [S, 1024], BF16, kind=kind)
        if self.debug:
            Sc["dbg_h"] = [P.dram(f"dbg_h{i}", [S, D], F32, kind="ExternalOutput") for i in range(3 * self.depth)]
        self.Sc = Sc

        c = {}
        self.c = c
        for n, dt in (("ident", BF16), ("identf", F32), ("tri", F32), ("trib", BF16), ("lowb", BF16),
                      ("mneg", F32), ("ones", F32)):
            c[n] = P.sbuf("c_" + n, [128, 128], dt)
            P.dma("sp", c[n][:], A["c_" + n], w=["const"])
        self.ps = [P.psum(f"ps{i}", [128, 512], F32) for i in range(6)]
        self.pt = [P.psum(f"pt{i}", [128, 1024], BF16) for i in range(2)]
        P.barrier()

        with P.phase() as st:
            r = Ring(P, st, "cpx", 3, [128, D], F32)
            for t in range(NT):
                b, k = r.next()
                P.dma("sp", b[:], A["x"][t * 128:(t + 1) * 128, :], w=[k])
                P.dma("sp", Sc["h"][t * 128:(t + 1) * 128, :], b[:], r=[k])

        for l in range(self.depth):
            self.layer(l)
            if self.stop_after is not None and l == self.stop_after[0]:
                break
        self.final_norm()
        P.emit_all()
        return nc

    def dbg_copy_h(self, idx):
        if not self.debug:
            return
        P, Sc = self.P, self.Sc
        with P.phase() as st:
            r = Ring(P, st, "cpd", 3, [128, D], F32)
            for t in range(self.NT):
                b, k = r.next()
                P.dma("sp", b[:], Sc["h"][t * 128:(t + 1) * 128, :], w=[k])
                P.dma("sp", Sc["dbg_h"][idx][t * 128:(t + 1) * 128, :], b[:], r=[k])

    def final_norm(self):
        P, S, NT, A, Sc = self.P, self.S, self.NT, self.A, self.Sc
        with P.phase() as st:
            g = self.bcast_row(st, "fn_g", A["norm_final"], D)
            hr = Ring(P, st, "fn_h", 2, [128, D], F32)
            orr = Ring(P, st, "fn_o", 2, [128, D], F32)
            junk = P.sbuf("fn_junk", [128, D], F32, st)
            ssr = Ring(P, st, "fn_ss", 2, [128, 2], F32)
            for t in range(NT):
                ht, hk = hr.next()
                ot, ok = orr.next()
                ss, sk = ssr.next()
                P.dma("sp", ht[:], Sc["h"][t * 128:(t + 1) * 128, :], w=[hk])
                P.act(junk[:], ht[:], AF.Square, [hk], ["fn_junk", sk], accum_out=ss[:, 0:1])
                P.ts("dve", ss[:, 1:2], ss[:, 0:1], 1.0 / D, 1e-6, ALU.mult, ALU.add, [sk], [sk])
                P.act(ss[:, 1:2], ss[:, 1:2], AF.Sqrt, [sk], [sk])
                P.add("dve", lambda e, ss=ss: e.reciprocal(out=ss[:, 1:2], in_=ss[:, 1:2]), [sk], [sk])
                P.stt("dve", ot[:], ht[:], ss[:, 1:2], g[:], ALU.mult, ALU.mult, [hk, sk, "fn_g"], [ok])
                P.dma("sp", self.out[t * 128:(t + 1) * 128, :], ot[:], r=[ok])

    def layer(self, l):
        sa = self.stop_after
        self.in_proj(l)
        self.mlstm(l)
        self.ssd(l)
        self.mla(l)
        self.swa(l)
        if sa is not None and sa == (l, "branches"):
            return
        self.merge(l)
        self.dbg_copy_h(3 * l + 0)
        if sa is not None and sa == (l, "mixer"):
            return
        self.xattn(l)
        self.dbg_copy_h(3 * l + 1)
        if sa is not None and sa == (l, "xattn"):
            return
        if l % 2 == 1:
            self.moe_layer(l)
        else:
            self.ffn_layer(l)
        self.dbg_copy_h(3 * l + 2)

    def in_proj(self, l):
        P, S, NT, A, Sc = self.P, self.S, self.NT, self.A, self.Sc
        W = A["w_in"][l]
        with P.phase() as st:
            xnT = P.sbuf("xnT", [128, KC, S], BF16, st)
            with P.phase() as st2:
                self.norm_T(st2, Sc["h"], A["norm_mix"][l], xnT, "xnT", S, D)
            wring = Ring(P, st, "ip_w", 3, [128, KC, 512], BF16)
            psr = KeyRing(self.ps[0:4], ["ps0", "ps1", "ps2", "ps3"])
            stg32 = Ring(P, st, "ip_s32", 3, [128, max(S, 512)], F32)
            stg16 = Ring(P, st, "ip_s16", 3, [128, max(S, 512)], BF16)
            rings = (wring, psr, stg32, stg16)
            fm = lambda c0, n, dt, func=None, drow=None: self.proj_fm(
                st, xnT, "xnT", KC, W, c0, n,
                (Sc["ufm32"] if dt == F32 else Sc["ufm16"])[(c0 if drow is None else drow):(c0 if drow is None else drow) + n, :],
                rings, func=func, dtype=dt)
            tm = lambda c0, n, dt: self.proj_tm(
                st, xnT, "xnT", KC, W, c0, n, (Sc["utm32"] if dt == F32 else Sc["utm16"])[:, c0:c0 + n], rings, dtype=dt)
            tm(O_MLK, 512, BF16)
            tm(O_MLV, 1024, BF16)
            tm(O_MLO, 1032, F32)
            tm(O_Z, 1024, F32)
            tm(O_DT, 784, F32)
            tm(O_SWV, 256, BF16)
            fm(O_MLQ, 1024, BF16)
            fm(O_XBC, 1536, F32)
            fm(O_KR, 64, F32)
            fm(O_KR + 32, 32, F32, drow=IN_TOTAL)
            fm(O_KR, 32, F32, drow=IN_TOTAL + 32)
            fm(O_SWQ, 1280, BF16)
            fm(O_GATE, 4 * D, BF16, func=AF.Sigmoid)

    def mlstm(self, l):
        P, S, NT, A, Sc, c = self.P, self.S, self.NT, self.A, self.Sc, self.c
        ps = self.ps
        with P.phase() as st:
            gif = P.sbuf("ml_gif", [128, NT, 8], F32, st)
            P.dma("sp", gif[:], Sc["utm32"][:, O_MLI:O_MLI + 8].rearrange("(t p) c -> p t c", p=128), w=["gif"])
            bi = self.bcast_row(st, "ml_bi", A["ml_igate_bias"][l], 4)
            bf = self.bcast_row(st, "ml_bf", A["ml_fgate_bias"][l], 4)
            nw = self.bcast_row(st, "ml_nw", A["ml_norm"][l], 1024)
            ig = P.sbuf("ml_ig", [128, NT, 4], F32, st)
            lf = P.sbuf("ml_lf", [128, NT, 4], F32, st)
            P.tt("dve", ig[:], gif[:, :, 0:4], bi[:].unsqueeze(1).to_broadcast([128, NT, 4]), ALU.add, ["gif", "ml_bi"], ["ig"])
            P.tt("dve", lf[:], gif[:, :, 4:8], bf[:].unsqueeze(1).to_broadcast([128, NT, 4]), ALU.add, ["gif", "ml_bf"], ["lf"])
            P.act(lf[:], lf[:], AF.Exp, ["lf"], ["lf"], scale=-1.0)
            P.act(lf[:], lf[:], AF.Ln, ["lf"], ["lf"], bias=1.0)
            P.ts("dve", lf[:], lf[:], -1.0, None, ALU.mult, None, ["lf"], ["lf"])
            bcs = P.sbuf("ml_b", [128, NT, 4], F32, st)
            ibm = P.sbuf("ml_ibm", [128, NT, 4], F32, st)
            eb = P.sbuf("ml_eb", [128, NT, 4], F32, st)
            wst = P.sbuf("ml_wst", [128, NT, 4], F32, st)
            ebt = P.sbuf("ml_ebt", [128, NT, 4], F32, st)
            for t in range(NT):
                P.mm(ps[0][:, 0:4], c["tri"][:], lf[:, t, :], True, True, ["lf", "const"], ["ps0"])
                P.mm(ps[0][:, 4:8], c["ones"][:], lf[:, t, :], True, True, ["lf", "const"], ["ps0"])
                P.cp("dve", bcs[:, t, :], ps[0][:, 0:4], ["ps0"], ["bcs"])
                P.cp("dve", ebt[:, t, :], ps[0][:, 4:8], ["ps0"], ["ebt"])
            P.tt("dve", ibm[:], ig[:], bcs[:], ALU.subtract, ["ig", "bcs"], ["ibm"])
            P.tt("dve", wst[:], ibm[:], ebt[:], ALU.add, ["ibm", "ebt"], ["wst"])
            P.act(wst[:], wst[:], AF.Exp, ["wst"], ["wst"])
            P.ts("dve", wst[:], wst[:], 128.0 ** -0.5, None, ALU.mult, None, ["wst"], ["wst"])
            P.act(eb[:], bcs[:], AF.Exp, ["bcs"], ["eb"])
            P.act(ebt[:], ebt[:], AF.Exp, ["ebt"], ["ebt"])

            heads = []
            for hd in range(4):
                H = {}
                H["qT"] = P.sbuf("ml_qT", [128, S], BF16, st)
                H["kT"] = P.sbuf("ml_kT", [128, S], BF16, st)
                H["kt"] = P.sbuf("ml_kt", [128, NT, 128], BF16, st)
                H["va"] = P.sbuf("ml_va", [128, NT, 257], BF16, st)
                H["C"] = P.sbuf("ml_C", [128, 257], F32, st)
                H["Cbr"] = Ring(P, st, "ml_Cb", 2, [128, 257], BF16)
                hk = f"mlh{hd}"
                P.dma("sp", H["qT"][:], Sc["ufm16"][O_MLQ + hd * 128:O_MLQ + (hd + 1) * 128, :], w=[hk + "q"])
                P.dma("sp", H["kT"][:], Sc["ufm16"][O_MLK + hd * 128:O_MLK + (hd + 1) * 128, :], w=[hk + "k"])
                P.dma("sp", H["kt"][:], Sc["utm16"][:, O_MLK + hd * 128:O_MLK + (hd + 1) * 128].rearrange("(t p) c -> p t c", p=128), w=[hk + "kt"])
                P.dma("sp", H["va"][:, :, 0:256], Sc["utm16"][:, O_MLV + hd * 256:O_MLV + (hd + 1) * 256].rearrange("(t p) c -> p t c", p=128), w=[hk + "v"])
                P.memset("pool", H["va"][:, :, 256:257], 1.0, [hk + "v"])
                P.memset("dve", H["C"][:], 0.0, [hk + "C"])
                H["Cb"] = H["Cbr"].next()
                P.memset("pool", H["Cb"][0][:], 0.0, [H["Cb"][1]])
                heads.append(H)
            ogr = Ring(P, st, "ml_og", 6, [128, 256], F32)
            ltr = Ring(P, st, "ml_lt", 6, [128, 128], F32)
            etr = Ring(P, st, "ml_et", 6, [128, 128], F32)
            ptr_ = Ring(P, st, "ml_pt", 6, [128, 128], BF16)
            kwr = Ring(P, st, "ml_kw", 6, [128, 128], BF16)
            tmr = Ring(P, st, "ml_tmp", 6, [128, 257], F32)
            hor = Ring(P, st, "ml_ho", 6, [128, 257], F32)
            smr = Ring(P, st, "ml_sm", 6, [128, 4], F32)
            yr = Ring(P, st, "ml_y", 6, [128, 256], BF16)
            junk = P.sbuf("ml_junk", [128, 256], BF16, st)
            for t in range(NT):
                sl = slice(t * 128, (t + 1) * 128)
                for hd in range(4):
                    H = heads[hd]
                    hk = f"mlh{hd}"
                    qT, qk, kT, kk, kt, ktk, va, vk = H["qT"], hk + "q", H["kT"], hk + "k", H["kt"], hk + "kt", H["va"], hk + "v"
                    Cst, Ck = H["C"], hk + "C"
                    Cb, cbk = H["Cb"]
                    og, ogk = ogr.next()
                    lt, ltk = ltr.next()
                    et, etk = etr.next()
                    pt, ptk = ptr_.next()
                    kw, kwk = kwr.next()
                    tmp, tmk = tmr.next()
                    ho, hok = hor.next()
                    sm, smk = smr.next()
                    y, yk = yr.next()
                    P.dma("sp", og[:], Sc["utm32"][sl, O_MLO + hd * 256:O_MLO + (hd + 1) * 256], w=[ogk])
                    P.act(og[:], og[:], AF.Sigmoid, [ogk], [ogk])
                    P.ts("pool", lt[:], c["tri"][:], lf[:, t, hd:hd + 1], None, ALU.mult, None, ["lf", "const"], [ltk])
                    P.mm(ps[1][:, 0:128], c["ones"][:], lt[:], True, False, [ltk, "const"], ["ps1"])
                    P.mm(ps[1][:, 0:128], c["identf"][:], c["mneg"][:], False, True, ["const"], ["ps1"])
                    P.act(et[:], ps[1][:, 0:128], AF.Exp, ["ps1", "ibm"], [etk], bias=ibm[:, t, hd:hd + 1])
                    P.mm(ps[2][:, 0:128], kT[:, sl], qT[:, sl], True, True, [kk, qk], ["ps2"])
                    P.stt("dve", pt[:], ps[2][:, 0:128], 128.0 ** -0.5, et[:], ALU.mult, ALU.mult, ["ps2", etk], [ptk])
                    P.mm(ps[3][:, 0:257], pt[:], va[:, t, :], True, True, [ptk, vk], ["ps3"])
                    P.mm(ps[4][:, 0:257], qT[:, sl], Cb[:], True, True, [qk, cbk], ["ps4"])
                    P.act(tmp[:], ps[4][:, 0:257], AF.Copy, ["ps4", "eb"], [tmk], scale=eb[:, t, hd:hd + 1])
                    P.tt("dve", ho[:], tmp[:], ps[3][:, 0:257], ALU.add, [tmk, "ps3"], [hok])
                    P.act(sm[:, 0:1], ho[:, 256:257], AF.Abs, [hok], [smk])
                    P.ts("dve", sm[:, 0:1], sm[:, 0:1], 1.0, None, ALU.max, None, [smk], [smk])
                    P.add("dve", lambda e, sm=sm: e.reciprocal(out=sm[:, 0:1], in_=sm[:, 0:1]), [smk], [smk])
                    P.ts("dve", ho[:, 0:256], ho[:, 0:256], sm[:, 0:1], None, ALU.mult, None, [hok, smk], [hok])
                    P.act(junk[:], ho[:, 0:256], AF.Square, [hok], ["ml_junk", smk], accum_out=sm[:, 1:2])
                    P.ts("dve", sm[:, 2:3], sm[:, 1:2], 1.0 / 256, 1e-6, ALU.mult, ALU.add, [smk], [smk])
                    P.act(sm[:, 2:3], sm[:, 2:3], AF.Sqrt, [smk], [smk])
                    P.add("dve", lambda e, sm=sm: e.reciprocal(out=sm[:, 2:3], in_=sm[:, 2:3]), [smk], [smk])
                    P.stt("dve", ho[:, 0:256], ho[:, 0:256], sm[:, 2:3], nw[:, hd * 256:(hd + 1) * 256], ALU.mult, ALU.mult,
                          [hok, smk, "ml_nw"], [hok])
                    P.tt("pool", y[:], ho[:, 0:256], og[:], ALU.mult, [hok, ogk], [yk])
                    P.dma("sp", Sc["ya"][sl, hd * 256:(hd + 1) * 256], y[:], r=[yk])
                    if t < NT - 1:
                        P.ts("pool", kw[:], kt[:, t, :], wst[:, t, hd:hd + 1], None, ALU.mult, None, [ktk, "wst"], [kwk])
                        P.mm(ps[5][:, 0:257], kw[:], va[:, t, :], True, True, [kwk, vk], ["ps5"])
                        P.stt("dve", Cst[:], Cst[:], ebt[:, t, hd:hd + 1], ps[5][:, 0:257], ALU.mult, ALU.add,
                              [Ck, "ebt", "ps5"], [Ck])
                        H["Cb"] = H["Cbr"].next()
                        P.cp("act", H["Cb"][0][:], Cst[:], [Ck], [H["Cb"][1]])

    def ssd(self, l):
        P, S, NT, A, Sc, c = self.P, self.S, self.NT, self.A, self.Sc, self.c
        ps, pt = self.ps, self.pt
        with P.phase() as st:
            cw = P.sbuf("sd_cw", [128, 12, 4], F32, st)
            cb = P.sbuf("sd_cb", [128, 12], F32, st)
            for j in range(4):
                P.dma("sp", cw[:, :, j], A["ssm_conv_w"][l, j].rearrange("(c p) -> p c", p=128), w=["cw"], allow_slow_non_contiguous=True)
            P.dma("sp", cb[:], A["ssm_conv_b"][l].rearrange("(c p) -> p c", p=128), w=["cb"], allow_slow_non_contiguous=True)
            xr = Ring(P, st, "sd_x", 2, [128, S + 3], F32)
            ar = Ring(P, st, "sd_a", 2, [128, S], F32)
            for ch in range(12):
                x, xk = xr.next()
                a, ak = ar.next()
                P.memset("pool", x[:, 0:3], 0.0, [xk])
                P.dma("sp", x[:, 3:S + 3], Sc["ufm32"][O_XBC + ch * 128:O_XBC + (ch + 1) * 128, :], w=[xk])
                P.ts("dve", a[:], x[:, 3:S + 3], cw[:, ch, 3:4], None, ALU.mult, None, [xk, "cw"], [ak])
                for j in range(3):
                    P.stt("dve", a[:], x[:, j:S + j], cw[:, ch, j:j + 1], a[:], ALU.mult, ALU.add, [xk, "cw", ak], [ak])
                P.act(a[:], a[:], AF.Silu, [ak, "cb"], [ak], bias=cb[:, ch:ch + 1])
                P.dma("sp", Sc["xcT"][ch * 128:(ch + 1) * 128, :], a[:], r=[ak])
        with P.phase() as st:
            dtb = self.bcast_row(st, "sd_dtb", A["ssm_dt_bias"][l], 16)
            alog = self.bcast_row(st, "sd_alog", A["ssm_a_log"][l], 16)
            dsk = self.bcast_row(st, "sd_dsk", A["ssm_d"][l], 16)
            nw = self.bcast_row(st, "sd_nw", A["ssm_norm"][l], 1024)
            P.act(alog[:], alog[:], AF.Exp, ["sd_alog"], ["sd_alog"])
            dt = P.sbuf("sd_dt", [128, NT, 16], F32, st)
            P.dma("sp", dt[:], Sc["utm32"][:, O_DT:O_DT + 16].rearrange("(t p) c -> p t c", p=128), w=["dt"])
            P.tt("dve", dt[:], dt[:], dtb[:].unsqueeze(1).to_broadcast([128, NT, 16]), ALU.add, ["dt", "sd_dtb"], ["dt"])
            P.act(dt[:], dt[:], AF.Exp, ["dt"], ["dt"])
            P.act(dt[:], dt[:], AF.Ln, ["dt"], ["dt"], bias=1.0)
            av = P.sbuf("sd_av", [128, NT, 16], F32, st)
            P.stt("dve", av[:], dt[:], -1.0, alog[:].unsqueeze(1).to_broadcast([128, NT, 16]), ALU.mult, ALU.mult,
                  ["dt", "sd_alog"], ["av"])
            acs = P.sbuf("sd_acs", [128, NT, 16], F32, st)
            nacs = P.sbuf("sd_nacs", [128, NT, 16], F32, st)
            eat = P.sbuf("sd_eat", [128, NT, 16], F32, st)
            ea = P.sbuf("sd_ea", [128, NT, 16], F32, st)
            dsc = P.sbuf("sd_dsc", [128, NT, 16], F32, st)
            for t in range(NT):
                P.mm(ps[0][:, 0:16], c["tri"][:], av[:, t, :], True, True, ["av", "const"], ["ps0"])
                P.mm(ps[0][:, 16:32], c["ones"][:], av[:, t, :], True, True, ["av", "const"], ["ps0"])
                P.cp("dve", acs[:, t, :], ps[0][:, 0:16], ["ps0"], ["acs"])
                P.cp("dve", eat[:, t, :], ps[0][:, 16:32], ["ps0"], ["eat"])
            P.ts("dve", nacs[:], acs[:], -1.0, None, ALU.mult, None, ["acs"], ["nacs"])
            P.tt("dve", dsc[:], eat[:], acs[:], ALU.subtract, ["eat", "acs"], ["dsc"])
            P.act(dsc[:], dsc[:], AF.Exp, ["dsc"], ["dsc"])
            P.tt("dve", dsc[:], dsc[:], dt[:], ALU.mult, ["dsc", "dt"], ["dsc"])
            P.act(ea[:], acs[:], AF.Exp, ["acs"], ["ea"])
            P.act(eat[:], eat[:], AF.Exp, ["eat"], ["eat"])

            xcr = Ring(P, st, "sd_xc", 2, [128, 12, 128], F32)
            xcbr = Ring(P, st, "sd_xcb", 2, [128, 12, 128], BF16)
            xtr = Ring(P, st, "sd_xt", 2, [128, 1280], BF16)
            xsr = Ring(P, st, "sd_xs", 2, [128, 1024], BF16)
            xdr = Ring(P, st, "sd_xd", 2, [128, 1024], BF16)
            cbr = Ring(P, st, "sd_cbT", 2, [128, 2, 128], F32)
            atr = Ring(P, st, "sd_at", 3, [128, 128], F32)
            der = Ring(P, st, "sd_de", 3, [128, 128], F32)
            mtr = Ring(P, st, "sd_mt", 3, [128, 128], BF16)
            zr = Ring(P, st, "sd_z", 2, [128, 1024], F32)
            yr = Ring(P, st, "sd_y", 2, [128, 1024], F32)
            tr_ = Ring(P, st, "sd_t", 2, [128, 1024], F32)
            ybr = Ring(P, st, "sd_yb", 2, [128, 1024], BF16)
            smr = Ring(P, st, "sd_sm", 2, [128, 4], F32)
            junk = P.sbuf("sd_junk", [128, 512], F32, st)
            state = P.sbuf("sd_state", [128, 2, 512], F32, st)
            stbr = Ring(P, st, "sd_stb", 2, [128, 2, 512], BF16)
            P.memset("dve", state[:], 0.0, ["state"])
            stb, stbk = stbr.next()
            P.memset("pool", stb[:], 0.0, [stbk])
            for t in range(NT):
                sl = slice(t * 128, (t + 1) * 128)
                xc, xck = xcr.next()
                xcb, xcbk = xcbr.next()
                xt, xtk = xtr.next()
                xs, xsk = xsr.next()
                xd, xdk = xdr.next()
                cbT, cbTk = cbr.next()
                z, zk = zr.next()
                y, yk = yr.next()
                tm_, tmk = tr_.next()
                yb, ybk = ybr.next()
                sm, smk = smr.next()
                P.dma("sp", xc[:], Sc["xcT"][:, sl].rearrange("(c p) s -> p c s", p=128), w=[xck])
                P.dma("sp", z[:], Sc["utm32"][sl, O_Z:O_Z + 1024], w=[zk])
                P.cp("pool", xcb[:], xc[:], [xck], [xcbk])
                for q in range(3):
                    n4 = 4 if q < 2 else 2
                    pb, pbk = pt[q % 2], ("pt0", "pt1")[q % 2]
                    pv = pb[:, 0:512].rearrange("p (a b) -> p a b", b=128)
                    for j in range(n4):
                        P.tr(pv[:, j, :], xcb[:, q * 4 + j, :], c["ident"][:], [xcbk, "const"], [pbk])
                    P.cp("dve" if q % 2 == 0 else "act", xt[:, q * 512:q * 512 + n4 * 128], pb[:, 0:n4 * 128], [pbk], [xtk])
                x3 = xt[:, 0:1024].rearrange("p (h d) -> p h d", d=64)
                P.tt("dve", xs[:].rearrange("p (h d) -> p h d", d=64), x3, dt[:, t, :].unsqueeze(2).to_broadcast([128, 16, 64]),
                     ALU.mult, [xtk, "dt"], [xsk])
                P.tt("pool", xd[:].rearrange("p (h d) -> p h d", d=64), x3, dsc[:, t, :].unsqueeze(2).to_broadcast([128, 16, 64]),
                     ALU.mult, [xtk, "dsc"], [xdk])
                for g in range(2):
                    P.mm(ps[1][:, g * 128:(g + 1) * 128], xcb[:, 8 + g, :], xcb[:, 10 + g, :], True, True, [xcbk], ["ps1"])
                P.cp("act", cbT[:].rearrange("p a b -> p (a b)"), ps[1][:, 0:256], ["ps1"], [cbTk])
                for g in range(2):
                    P.mm(ps[2 + g][:, 0:512], xcb[:, 10 + g, :], stb[:, g, :], True, True, [xcbk, stbk], [f"ps{2 + g}"])
                stg1 = {}

                def stage1(hd):
                    g = hd // 8
                    at, atk = atr.next()
                    de, dek = der.next()
                    mt, mtk = mtr.next()
                    bb, bbk = ps[hd % 2], f"ps{hd % 2}"
                    P.ts("pool", at[:], c["tri"][:], av[:, t, hd:hd + 1], None, ALU.mult, None, ["av", "const"], [atk])
                    P.mm(bb[:, 0:128], c["ones"][:], at[:], True, False, [atk, "const"], [bbk])
                    P.mm(bb[:, 0:128], c["identf"][:], c["mneg"][:], False, True, ["const"], [bbk])
                    P.act(de[:], bb[:, 0:128], AF.Exp, [bbk, "nacs"], [dek], bias=nacs[:, t, hd:hd + 1])
                    P.tt("dve", mt[:], de[:], cbT[:, g, :], ALU.mult, [dek, cbTk], [mtk])
                    stg1[hd] = (mt, mtk)

                stage1(0)
                for hd in range(16):
                    g = hd // 8
                    if hd + 1 < 16:
                        stage1(hd + 1)
                    mt, mtk = stg1[hd]
                    P.mm(ps[4 + g][:, (hd % 8) * 64:(hd % 8 + 1) * 64], mt[:], xs[:, hd * 64:(hd + 1) * 64], True, True,
                         [mtk, xsk], [f"ps{4 + g}"])
                for g in range(2):
                    P.tt("dve", tm_[:, g * 512:(g + 1) * 512].rearrange("p (h d) -> p h d", d=64),
                         ps[2 + g][:, 0:512].rearrange("p (h d) -> p h d", d=64),
                         ea[:, t, g * 8:(g + 1) * 8].unsqueeze(2).to_broadcast([128, 8, 64]), ALU.mult,
                         [f"ps{2 + g}", "ea"], [tmk])
                    P.tt("dve", y[:, g * 512:(g + 1) * 512], tm_[:, g * 512:(g + 1) * 512], ps[4 + g][:, 0:512], ALU.add,
                         [tmk, f"ps{4 + g}"], [yk])
                P.tt("pool", tm_[:].rearrange("p (h d) -> p h d", d=64), x3, dsk[:].unsqueeze(2).to_broadcast([128, 16, 64]),
                     ALU.mult, [xtk, "sd_dsk", yk], [tmk])
                P.tt("pool", y[:], y[:], tm_[:], ALU.add, [tmk, yk], [yk])
                P.act(z[:], z[:], AF.Silu, [zk], [zk])
                P.tt("dve", y[:], y[:], z[:], ALU.mult, [yk, zk], [yk])
                for g in range(2):
                    P.act(junk[:], y[:, g * 512:(g + 1) * 512], AF.Square, [yk], ["sd_junk", smk], accum_out=sm[:, g:g + 1])
                P.ts("dve", sm[:, 2:4], sm[:, 0:2], 1.0 / 512, 1e-6, ALU.mult, ALU.add, [smk], [smk])
                P.act(sm[:, 2:4], sm[:, 2:4], AF.Sqrt, [smk], [smk])
                P.add("dve", lambda e, sm=sm: e.reciprocal(out=sm[:, 2:4], in_=sm[:, 2:4]), [smk], [smk])
                for g in range(2):
                    P.stt("dve", yb[:, g * 512:(g + 1) * 512], y[:, g * 512:(g + 1) * 512], sm[:, 2 + g:3 + g],
                          nw[:, g * 512:(g + 1) * 512], ALU.mult, ALU.mult, [yk, smk, "sd_nw"], [ybk])
                P.dma("sp", Sc["yb"][sl, :], yb[:], r=[ybk])
                if t < NT - 1:
                    for g in range(2):
                        P.mm(ps[2 + g][:, 0:512], xt[:, 1024 + g * 128:1024 + (g + 1) * 128], xd[:, g * 512:(g + 1) * 512],
                             True, True, [xtk, xdk], [f"ps{2 + g}"])
                        P.tt("dve", state[:, g, :].rearrange("p (h d) -> p h d", d=64),
                             state[:, g, :].rearrange("p (h d) -> p h d", d=64),
                             eat[:, t, g * 8:(g + 1) * 8].unsqueeze(2).to_broadcast([128, 8, 64]), ALU.mult,
                             ["state", "eat"], ["state"])
                        P.tt("dve", state[:, g, :], state[:, g, :], ps[2 + g][:, 0:512], ALU.add, ["state", f"ps{2 + g}"], ["state"])
                    stb, stbk = stbr.next()
                    P.cp("act", stb[:], state[:], ["state"], [stbk])

    def mla(self, l):
        P, S, NT, A, Sc, c = self.P, self.S, self.NT, self.A, self.Sc, self.c
        ps, GW, NG = self.ps, self.GW, self.NG
        scale = 192.0 ** -0.5
        with P.phase() as st:
            cqT = P.sbuf("la_cqT", [128, 4, S], BF16, st)
            ckT = P.sbuf("la_ckT", [128, 2, S], BF16, st)
            with P.phase() as st2:
                self.norm_T(st2, Sc["utm32"][:, O_CQ:O_CQ + 512], A["mla_q_norm"][l], cqT, "cqT", S, 512)
            with P.phase() as st2:
                self.norm_T(st2, Sc["utm32"][:, O_CKV:O_CKV + 256], A["mla_kv_norm"][l], ckT, "ckT", S, 256)
            cos2 = P.sbuf("la_cos", [64, S], F32, st)
            sin2 = P.sbuf("la_sin", [64, S], F32, st)
            P.dma("sp", cos2[:], A["c_cos2"], w=["cos"])
            P.dma("sp", sin2[:], A["c_sin2"], w=["sin"])
            krT = P.sbuf("la_krT", [64, S], BF16, st)
            k1 = P.sbuf("la_k1", [64, S], F32, st)
            k2 = P.sbuf("la_k2", [64, S], F32, st)
            P.dma("sp", k1[:], Sc["ufm32"][O_KR:O_KR + 64, :], w=["k1"])
            P.dma("sp", k2[:], Sc["ufm32"][IN_TOTAL:IN_TOTAL + 64, :], w=["k2"])
            P.tt("dve", k1[:], k1[:], cos2[:], ALU.mult, ["k1", "cos"], ["k1"])
            P.tt("pool", k2[:], k2[:], sin2[:], ALU.mult, ["k2", "sin"], ["k2"])
            P.tt("dve", krT[:], k1[:], k2[:], ALU.add, ["k1", "k2"], ["krT"])
            Wq, Wkv = A["mla_w_uq"][l], A["mla_w_ukv"][l]
            wqr = Ring(P, st, "la_wq", 2, [128, 4, 256], BF16)
            wkr = Ring(P, st, "la_wk", 2, [128, 2, 256], BF16)
            qnr = Ring(P, st, "la_qn", 2, [128, S], BF16)
            qrr = Ring(P, st, "la_qr", 2, [64, S], BF16)
            knr = Ring(P, st, "la_kn", 2, [128, S], BF16)
            var = Ring(P, st, "la_va", 2, [128, NT, 129], BF16)
            t1r = Ring(P, st, "la_t1", 2, [64, GW], F32)
            t2r = Ring(P, st, "la_t2", 2, [64, GW], F32)
            ptr_ = Ring(P, st, "la_pt", 4, [128, GW], BF16)
            yr = Ring(P, st, "la_y", 4, [128, 128], BF16)
            smr = Ring(P, st, "la_sm", 4, [128, 2], F32)
            ycur = {}
            for hd in range(8):
                wq, wqk = wqr.next()
                wk, wkk = wkr.next()
                qn, qnk = qnr.next()
                qr, qrk = qrr.next()
                kn, knk = knr.next()
                va, vak = var.next()
                q0 = hd * 192
                vw = lambda a, b: Wq[:, a:b].rearrange("(c p) n -> p c n", p=128)
                P.dma("pool", wq[:, :, 0:192], vw(q0, q0 + 192), w=[wqk])
                P.dma("pool", wq[:, :, 192:224], vw(q0 + 160, q0 + 192), w=[wqk])
                P.dma("pool", wq[:, :, 224:256], vw(q0 + 128, q0 + 160), w=[wqk])
                P.dma("pool", wk[:], Wkv[:, hd * 256:(hd + 1) * 256].rearrange("(c p) n -> p c n", p=128), w=[wkk])
                cqr = [("cqT", t) for t in range(NT)]
                ckr = [("ckT", t) for t in range(NT)]
                for g in range(NG):
                    gs = slice(g * GW, (g + 1) * GW)
                    for k in range(4):
                        P.mm(ps[0][:, 0:GW], wq[:, k, 0:128], cqT[:, k, gs], k == 0, k == 3, [wqk] + cqr, ["ps0"])
                    P.cp("act", qn[:, gs], ps[0][:, 0:GW], ["ps0"], [qnk])
                    for k in range(4):
                        P.mm(ps[1][0:64, 0:GW], wq[:, k, 128:192], cqT[:, k, gs], k == 0, k == 3, [wqk] + cqr, ["ps1"])
                    for k in range(4):
                        P.mm(ps[2][0:64, 0:GW], wq[:, k, 192:256], cqT[:, k, gs], k == 0, k == 3, [wqk] + cqr, ["ps2"])
                    t1, t1k = t1r.next()
                    t2, t2k = t2r.next()
                    P.tt("dve", t1[:], ps[1][0:64, 0:GW], cos2[:, gs], ALU.mult, ["ps1", "cos"], [t1k])
                    P.tt("dve", t2[:], ps[2][0:64, 0:GW], sin2[:, gs], ALU.mult, ["ps2", "sin"], [t2k])
                    P.tt("pool", qr[:, gs], t1[:], t2[:], ALU.add, [t1k, t2k], [qrk])
                    for k in range(2):
                        P.mm(ps[3][:, 0:GW], wk[:, k, 0:128], ckT[:, k, gs], k == 0, k == 1, [wkk] + ckr, ["ps3"])
                    P.cp("act", kn[:, gs], ps[3][:, 0:GW], ["ps3"], [knk])
                for t in range(NT):
                    for k in range(2):
                        P.mm(ps[4][:, 0:128], ckT[:, k, t * 128:(t + 1) * 128], wk[:, k, 128:256], k == 0, k == 1,
                             [wkk, ("ckT", t)], ["ps4"])
                    P.cp("dve", va[:, t, 0:128], ps[4][:, 0:128], ["ps4"], [vak])
                P.memset("pool", va[:, :, 128:129], 1.0, [vak])
                TPG = GW // 128
                its = [(g, j) for g in range(NG) for j in range((g + 1) * TPG)]

                def emit_S(i):
                    g, j = its[i]
                    gs0 = g * GW
                    c0 = max(0, j - g * TPG) * 128
                    sb, sbk = ps[i % 2], f"ps{i % 2}"
                    P.mm(sb[:, c0:GW], kn[:, j * 128:(j + 1) * 128], qn[:, gs0 + c0:gs0 + GW], True, False, [knk, qnk], [sbk])
                    P.mm(sb[:, c0:GW], krT[:, j * 128:(j + 1) * 128], qr[:, gs0 + c0:gs0 + GW], False, True, ["krT", qrk], [sbk])

                emit_S(0)
                for i, (g, j) in enumerate(its):
                    if i + 1 < len(its):
                        emit_S(i + 1)
                    r0 = max(0, j - g * TPG)
                    c0 = r0 * 128
                    sb, sbk = ps[i % 2], f"ps{i % 2}"
                    pt, ptk = ptr_.next()
                    P.act(pt[:, c0:GW], sb[:, c0:GW], AF.Exp, [sbk], [ptk], scale=scale)
                    if j >= g * TPG:
                        P.tt("dve", pt[:, c0:c0 + 128], pt[:, c0:c0 + 128], c["trib"][:], ALU.mult, [ptk, "const"], [ptk])
                    for r in range(r0, TPG):
                        qi = g * TPG + r
                        P.mm(ps[2 + r][:, 0:129], pt[:, r * 128:(r + 1) * 128], va[:, j, :], j == 0, j == qi,
                             [ptk, vak], [f"ps{2 + r}"])
                        if j == qi:
                            y, yk = yr.next()
                            sm, smk = smr.next()
                            P.add("dve", lambda e, sm=sm, r=r: e.reciprocal(out=sm[:, 0:1], in_=ps[2 + r][:, 128:129]),
                                  [f"ps{2 + r}"], [smk])
                            P.ts("dve", y[:], ps[2 + r][:, 0:128], sm[:, 0:1], None, ALU.mult, None,
                                 [f"ps{2 + r}", smk], [yk])
                            P.dma("sp", Sc["yc"][qi * 128:(qi + 1) * 128, hd * 128:(hd + 1) * 128], y[:], r=[yk])

    def swa(self, l):
        P, S, NT, A, Sc, c = self.P, self.S, self.NT, self.A, self.Sc, self.c
        ps = self.ps
        scale = 64.0 ** -0.5
        with P.phase() as st:
            sk_ = self.bcast_row(st, "sw_sink", A["swa_sinks"][l], 16)
            P.act(sk_[:], sk_[:], AF.Exp, ["sw_sink"], ["sw_sink"])
            qr = Ring(P, st, "sw_q", 2, [64, 4, S], BF16)
            kr = Ring(P, st, "sw_k", 2, [64, S], BF16)
            var = Ring(P, st, "sw_va", 2, [128, NT, 65], BF16)
            ptr_ = Ring(P, st, "sw_pt", 4, [128, 512], BF16)
            yr = Ring(P, st, "sw_y", 3, [128, 256], BF16)
            smr = Ring(P, st, "sw_sm", 3, [128, 4], F32)
            for g in range(4):
                q, qk = qr.next()
                k, kk = kr.next()
                va, vak = var.next()
                P.dma("sp", q[:], Sc["ufm16"][O_SWQ + g * 256:O_SWQ + (g + 1) * 256, :].rearrange("(r p) s -> p r s", p=64), w=[qk])
                P.dma("sp", k[:], Sc["ufm16"][O_SWK + g * 64:O_SWK + (g + 1) * 64, :], w=[kk])
                P.dma("sp", va[:, :, 0:64], Sc["utm16"][:, O_SWV + g * 64:O_SWV + (g + 1) * 64].rearrange("(t p) c -> p t c", p=128), w=[vak])
                P.memset("pool", va[:, :, 64:65], 1.0, [vak])
                for n in range(NT):
                    sl = slice(n * 128, (n + 1) * 128)
                    pc, pck = ptr_.next()
                    P.mm(ps[0][:, 0:512].rearrange("p (r s) -> p r s", r=4), k[:, sl], q[:, :, sl], True, True, [kk, qk], ["ps0"])
                    P.act(pc[:], ps[0][:, 0:512], AF.Exp, ["ps0"], [pck], scale=scale)
                    P.tt("pool", pc[:].rearrange("p (r s) -> p r s", r=4), pc[:].rearrange("p (r s) -> p r s", r=4),
                         c["trib"][:].unsqueeze(1).to_broadcast([128, 4, 128]), ALU.mult, [pck, "const"], [pck])
                    if n > 0:
                        pp, ppk = ptr_.next()
                        psl = slice((n - 1) * 128, n * 128)
                        P.mm(ps[1][:, 0:512].rearrange("p (r s) -> p r s", r=4), k[:, psl], q[:, :, sl], True, True, [kk, qk], ["ps1"])
                        P.act(pp[:], ps[1][:, 0:512], AF.Exp, ["ps1"], [ppk], scale=scale)
                        P.tt("pool", pp[:].rearrange("p (r s) -> p r s", r=4), pp[:].rearrange("p (r s) -> p r s", r=4),
                             c["lowb"][:].unsqueeze(1).to_broadcast([128, 4, 128]), ALU.mult, [ppk, "const"], [ppk])
                    ob = ps[2 + (n % 2)]
                    obk = f"ps{2 + (n % 2)}"
                    for r in range(4):
                        if n > 0:
                            P.mm(ob[:, r * 65:(r + 1) * 65], pp[:, r * 128:(r + 1) * 128], va[:, n - 1, :], True, False, [ppk, vak], [obk])
                        P.mm(ob[:, r * 65:(r + 1) * 65], pc[:, r * 128:(r + 1) * 128], va[:, n, :], n == 0, True, [pck, vak], [obk])
                    y, yk = yr.next()
                    sm, smk = smr.next()
                    o3 = ob[:, 0:260].rearrange("p (r d) -> p r d", d=65)
                    P.tt("dve", sm[:].unsqueeze(2), o3[:, :, 64:65], sk_[:, g * 4:(g + 1) * 4].unsqueeze(2), ALU.add, [obk, "sw_sink"], [smk])
                    P.add("dve", lambda e, sm=sm: e.reciprocal(out=sm[:], in_=sm[:]), [smk], [smk])
                    P.tt("dve", y[:].rearrange("p (r d) -> p r d", d=64), o3[:, :, 0:64], sm[:].unsqueeze(2).to_broadcast([128, 4, 64]),
                         ALU.mult, [obk, smk], [yk])
                    P.dma("sp", Sc["yd"][sl, g * 256:(g + 1) * 256], y[:], r=[yk])

    def merge(self, l):
        P, S, NT, A, Sc, c = self.P, self.S, self.NT, self.A, self.Sc, self.c
        ps, pt, GW, NG = self.ps, self.pt, self.GW, self.NG
        TPG = GW // 128
        with P.phase() as st:
            yTr = [P.sbuf(f"mg_yT{n}", [128, 8, GW], BF16, st) for n in range(4)]
            ytr = Ring(P, st, "mg_yt", 3, [128, 1024], BF16)
            wbr = Ring(P, st, "mg_wb", 3, [128, 8, 512], BF16)
            wor = Ring(P, st, "mg_wo", 2, [128, 16, 512], BF16)
            gtr = Ring(P, st, "mg_g", 3, [128, GW], BF16)
            tmr = Ring(P, st, "mg_tmp", 2, [128, GW], F32)
            mT = P.sbuf("mg_mT", [128, 16, GW], F32, st)
            mTb = P.sbuf("mg_mTb", [128, 16, GW], BF16, st)
            hr = Ring(P, st, "mg_h", 3, [128, 512], F32)
            ptr = KeyRing(pt, ["pt0", "pt1"])
            for g in range(NG):
                gs = slice(g * GW, (g + 1) * GW)
                for n, nm in enumerate("abcd"):
                    for tt_ in range(TPG):
                        t = g * TPG + tt_
                        yt, ytk = ytr.next()
                        P.dma("sp", yt[:], Sc["y" + nm][t * 128:(t + 1) * 128, :], w=[ytk])
                        for q in range(2):
                            pb, pbk = ptr.next()
                            pv = pb[:, 0:512].rearrange("p (a b) -> p a b", b=128)
                            for j in range(4):
                                P.tr(pv[:, j, :], yt[:, (q * 4 + j) * 128:(q * 4 + j + 1) * 128], c["ident"][:], [ytk, "const"], [pbk])
                            P.cp("dve" if q == 0 else "act", yTr[n][:, q * 4:(q + 1) * 4, tt_ * 128:(tt_ + 1) * 128], pv[:, 0:4, :],
                                 [pbk], [("yT", n)])
                for n in range(4):
                    for dcb in range(4):
                        wb, wbk = wbr.next()
                        P.dma("pool", wb[:], A["w_branch"][l, n][:, dcb * 512:(dcb + 1) * 512].rearrange("(c p) n -> p c n", p=128), w=[wbk])
                        for dc in range(4):
                            dch = dcb * 4 + dc
                            pb, pbk = ps[dc % 4], f"ps{dc % 4}"
                            for k in range(8):
                                P.mm(pb[:, 0:GW], wb[:, k, dc * 128:(dc + 1) * 128], yTr[n][:, k, :], k == 0, k == 7,
                                     [wbk, ("yT", n)], [pbk])
                            gt, gtk = gtr.next()
                            r0 = O_GATE + n * D + dch * 128
                            P.dma("sp", gt[:], Sc["ufm16"][r0:r0 + 128, gs], w=[gtk])
                            if n == 0:
                                P.tt("dve", mT[:, dch, :], pb[:, 0:GW], gt[:], ALU.mult, [pbk, gtk], [("mT", dch)])
                            else:
                                tmp, tmk = tmr.next()
                                P.tt("dve", tmp[:], pb[:, 0:GW], gt[:], ALU.mult, [pbk, gtk], [tmk])
                                P.tt("dve", mT[:, dch, :], mT[:, dch, :], tmp[:], ALU.add, [("mT", dch), tmk], [("mT", dch)])
                for dch in range(16):
                    P.cp("act", mTb[:, dch, :], mT[:, dch, :], [("mT", dch)], [("mTb", dch)])
                mr = [("mTb", d_) for d_ in range(16)]
                for ocb in range(4):
                    wo, wok = wor.next()
                    P.dma("pool", wo[:], A["w_out"][l][:, ocb * 512:(ocb + 1) * 512].rearrange("(c p) n -> p c n", p=128), w=[wok])
                    for tt_ in range(TPG):
                        t = g * TPG + tt_
                        pb, pbk = ps[4 + tt_ % 2], f"ps{4 + tt_ % 2}"
                        ht, hk = hr.next()
                        P.dma("sp", ht[:], Sc["h"][t * 128:(t + 1) * 128, ocb * 512:(ocb + 1) * 512], w=[hk])
                        for k in range(16):
                            P.mm(pb[:, 0:512], mTb[:, k, tt_ * 128:(tt_ + 1) * 128], wo[:, k, :], k == 0, k == 15, [wok] + mr, [pbk])
                        P.tt("dve", ht[:], ht[:], pb[:, 0:512], ALU.add, [hk, pbk], [hk])
                        P.dma("sp", Sc["h"][t * 128:(t + 1) * 128, ocb * 512:(ocb + 1) * 512], ht[:], r=[hk])

    def xattn(self, l):
        P, S, NT, A, Sc, c = self.P, self.S, self.NT, self.A, self.Sc, self.c
        ps, pt, GW, NG = self.ps, self.pt, self.GW, self.NG
        scale = 128.0 ** -0.5
        with P.phase() as st:
            hnT = P.sbuf("xa_hnT", [128, KC, S], BF16, st)
            mnT = P.sbuf("xa_mnT", [128, KC, MEM], BF16, st)
            with P.phase() as st2:
                self.norm_T(st2, Sc["h"], A["norm_cross"][l], hnT, "hnT", S, D)
            with P.phase() as st2:
                self.norm_T(st2, A["mem"], A["norm_mem"][l], mnT, "mnT", MEM, D)
            wkv = P.sbuf("xa_wkv", [128, KC, 1024], BF16, st)
            wq = P.sbuf("xa_wq", [128, KC, 512], BF16, st)
            wo = P.sbuf("xa_wo", [128, 4, D], BF16, st)
            rr = lambda ap: ap.rearrange("(c p) n -> p c n", p=128)
            P.dma("pool", wkv[:, :, 0:512], rr(A["xa_wkv"][l][:, 0:512]), w=["wkv"])
            P.dma("pool", wkv[:, :, 512:1024], rr(A["xa_wkv"][l][:, 512:1024]), w=["wkv"])
            P.dma("pool", wq[:], rr(A["xa_wq"][l]), w=["wq"])
            for q in range(4):
                P.dma("pool", wo[:, :, q * 512:(q + 1) * 512], rr(A["xa_wo"][l][:, q * 512:(q + 1) * 512]), w=["wo"])
            KT = P.sbuf("xa_KT", [128, 4, MEM], BF16, st)
            VA = P.sbuf("xa_VA", [128, 2, 4, 129], BF16, st)
            QT = P.sbuf("xa_QT", [128, 4, S], BF16, st)
            mnr = [("mnT", t) for t in range(MEM // 128)]
            hnr = [("hnT", t) for t in range(NT)]
            for hd in range(4):
                for k in range(KC):
                    P.mm(ps[0][:, 0:MEM], wkv[:, k, hd * 128:(hd + 1) * 128], mnT[:, k, :], k == 0, k == KC - 1, ["wkv"] + mnr, ["ps0"])
                P.cp("act", KT[:, hd, :], ps[0][:, 0:MEM], ["ps0"], ["KT"])
            for mt in range(2):
                for k in range(KC):
                    P.mm(ps[1][:, 0:512], mnT[:, k, mt * 128:(mt + 1) * 128], wkv[:, k, 512:1024], k == 0, k == KC - 1, ["wkv"] + mnr, ["ps1"])
                P.cp("dve", VA[:, mt, :, 0:128], ps[1][:, 0:512].rearrange("p (h d) -> p h d", d=128), ["ps1"], ["VA"])
            P.memset("pool", VA[:, :, :, 128:129], 1.0, ["VA"])
            for hd in range(4):
                for g in range(NG):
                    pb, pbk = ps[2 + g % 2], f"ps{2 + g % 2}"
                    for k in range(KC):
                        P.mm(pb[:, 0:GW], wq[:, k, hd * 128:(hd + 1) * 128], hnT[:, k, g * GW:(g + 1) * GW], k == 0, k == KC - 1,
                             ["wq"] + hnr, [pbk])
                    P.cp("act" if g % 2 == 0 else "dve", QT[:, hd, g * GW:(g + 1) * GW], pb[:, 0:GW], [pbk], ["QT"])
            ptr_ = Ring(P, st, "xa_pt", 4, [128, GW], BF16)
            oTr = Ring(P, st, "xa_oT", 2, [128, 4, 128], BF16)
            smr = Ring(P, st, "xa_sm", 4, [128, 1], F32)
            hr = Ring(P, st, "xa_h", 2, [128, D], F32)
            TPG = GW // 128
            for g in range(NG):
                gs = slice(g * GW, (g + 1) * GW)
                pts = {}
                for hd in range(4):
                    for mt in range(2):
                        pb, pbk = ps[mt], f"ps{mt}"
                        p_, pk_ = ptr_.next()
                        P.mm(pb[:, 0:GW], KT[:, hd, mt * 128:(mt + 1) * 128], QT[:, hd, gs], True, True, ["KT", "QT"], [pbk])
                        P.act(p_[:], pb[:, 0:GW], AF.Exp, [pbk], [pk_], scale=scale)
                        pts[(hd % 2, mt)] = (p_, pk_)
                    for tt_ in range(TPG):
                        t = g * TPG + tt_
                        ob, obk = ps[2 + tt_ % 2], f"ps{2 + tt_ % 2}"
                        for mt in range(2):
                            p_, pk_ = pts[(hd % 2, mt)]
                            P.mm(ob[:, 0:129], p_[:, tt_ * 128:(tt_ + 1) * 128], VA[:, mt, hd, :], mt == 0, mt == 1, [pk_, "VA"], [obk])
                        sm, smk = smr.next()
                        o_, ok_ = self._xa_o(st, t)
                        P.add("dve", lambda e, sm=sm, ob=ob: e.reciprocal(out=sm[:, 0:1], in_=ob[:, 128:129]), [obk], [smk])
                        P.ts("dve", o_[:, hd * 128:(hd + 1) * 128], ob[:, 0:128], sm[:, 0:1], None, ALU.mult, None, [obk, smk], [(ok_, hd)])
                for tt_ in range(TPG):
                    t = g * TPG + tt_
                    o_, ok_ = self._xa_o(st, t)
                    oT, oTk = oTr.next()
                    pb, pbk = pt[tt_ % 2], f"pt{tt_ % 2}"
                    pv = pb[:, 0:512].rearrange("p (a b) -> p a b", b=128)
                    for j in range(4):
                        P.tr(pv[:, j, :], o_[:, j * 128:(j + 1) * 128], c["ident"][:], [(ok_, j), "const"], [pbk])
                    P.cp("act", oT[:], pv[:, 0:4, :], [pbk], [oTk])
                    ht, hk = hr.next()
                    P.dma("sp", ht[:], Sc["h"][t * 128:(t + 1) * 128, :], w=[hk])
                    for ocb in range(4):
                        ob, obk = ps[4 + ocb % 2], f"ps{4 + ocb % 2}"
                        for k in range(4):
                            P.mm(ob[:, 0:512], oT[:, k, :], wo[:, k, ocb * 512:(ocb + 1) * 512], k == 0, k == 3, [oTk, "wo"], [obk])
                        P.tt("dve", ht[:, ocb * 512:(ocb + 1) * 512], ht[:, ocb * 512:(ocb + 1) * 512], ob[:, 0:512], ALU.add, [hk, obk], [hk])
                    P.dma("sp", Sc["h"][t * 128:(t + 1) * 128, :], ht[:], r=[hk])

    def _xa_o(self, st, t):
        if not hasattr(self, "_xao") or self._xao_st is not st:
            self._xao = {}
            self._xao_st = st
        if t not in self._xao:
            self.P.uid += 1
            self._xao[t] = (self.P.sbuf("xa_ot", [128, 512], BF16, st), f"xa_ot#{self.P.uid}")
        return self._xao[t]

    def ffn_layer(self, l):
        P, S, NT, A, Sc, c = self.P, self.S, self.NT, self.A, self.Sc, self.c
        ps, pt, GW, NG = self.ps, self.pt, self.GW, self.NG
        TPG = GW // 128
        moe = (l % 2 == 1)
        FC = FFN // 128
        with P.phase() as st:
            hacc = P.sbuf("ff_hacc", [128, TPG, D], F32, st)
            hnT = P.sbuf("ff_hnT", [128, KC, GW], BF16, st)
            actT = P.sbuf("ff_actT", [128, FC, GW], BF16, st)
            w13r = Ring(P, st, "ff_w13", 2, [128, KC, 2, 256], BF16)
            w2r = Ring(P, st, "ff_w2", 2, [128, 8, 512], BF16)
            sir = Ring(P, st, "ff_si", 2, [128, GW], F32)
            gcol = P.sbuf("ff_gcol", [128, KC], F32, st)
            P.dma("sp", gcol[:], A["norm_ffn"][l].rearrange("(c p) -> p c", p=128), w=["gcol"], allow_slow_non_contiguous=True)
            hbr = Ring(P, st, "ff_hb", 2, [128, D], BF16)
            junk = P.sbuf("ff_junk", [128, D], BF16, st)
            ssr = Ring(P, st, "ff_ss", 2, [128, 2], F32)
            if moe:
                hn32 = Ring(P, st, "ff_hn32", 1, [128, D], F32)
                hnT32 = P.sbuf("ff_hnT32", [128, KC, 128], F32, st)
                rt = P.sbuf("ff_rt", [128, KC, NEXP], F32, st)
                P.dma("sp", rt[:], A["moe_router"][0].rearrange("(c p) e -> p c e", p=128), w=["rt"])
                gates = P.sbuf("ff_gates", [128, TPG, NEXP], F32, st)
                lgr = Ring(P, st, "ff_lg", 2, [128, 24], F32)
            for g in range(NG):
                for tt_ in range(TPG):
                    t = g * TPG + tt_
                    hb, hbk = hbr.next()
                    ss, sk = ssr.next()
                    P.dma("sp", hacc[:, tt_, :], Sc["h"][t * 128:(t + 1) * 128, :], w=[("hacc", tt_)])
                    P.act(junk[:], hacc[:, tt_, :], AF.Square, [("hacc", tt_)], ["ff_junk", sk], accum_out=ss[:, 0:1])
                    P.ts("dve", ss[:, 1:2], ss[:, 0:1], 1.0 / D, 1e-6, ALU.mult, ALU.add, [sk], [sk])
                    P.act(ss[:, 1:2], ss[:, 1:2], AF.Sqrt, [sk], [sk])
                    P.add("dve", lambda e, ss=ss: e.reciprocal(out=ss[:, 1:2], in_=ss[:, 1:2]), [sk], [sk])
                    P.ts("pool", hb[:], hacc[:, tt_, :], ss[:, 1:2], None, ALU.mult, None, [("hacc", tt_), sk], [hbk])
                    for c4 in range(0, KC, 4):
                        pb, pbk = pt[(c4 // 4) % 2], f"pt{(c4 // 4) % 2}"
                        pv = pb[:, 0:512].rearrange("p (a b) -> p a b", b=128)
                        for j in range(4):
                            P.tr(pv[:, j, :], hb[:, (c4 + j) * 128:(c4 + j + 1) * 128], c["ident"][:], [hbk, "const"], [pbk])
                        P.tt("dve", hnT[:, c4:c4 + 4, tt_ * 128:(tt_ + 1) * 128], pv[:, 0:4, :],
                             gcol[:, c4:c4 + 4].unsqueeze(2).to_broadcast([128, 4, 128]), ALU.mult, [pbk, "gcol"], ["hnT"])
                    if moe:
                        h32, h32k = hn32.next()
                        lg, lgk = lgr.next()
                        P.ts("pool", h32[:], hacc[:, tt_, :], ss[:, 1:2], None, ALU.mult, None, [("hacc", tt_), sk], [h32k])
                        for c4 in range(0, KC, 4):
                            pb, pbk = ps[(c4 // 4) % 2], f"ps{(c4 // 4) % 2}"
                            pv = pb[:, 0:512].rearrange("p (a b) -> p a b", b=128)
                            for j in range(4):
                                P.tr(pv[:, j, :], h32[:, (c4 + j) * 128:(c4 + j + 1) * 128], c["identf"][:], [h32k, "const"], [pbk])
                            P.tt("dve", hnT32[:, c4:c4 + 4, :], pv[:, 0:4, :],
                                 gcol[:, c4:c4 + 4].unsqueeze(2).to_broadcast([128, 4, 128]), ALU.mult, [pbk, "gcol"], ["hnT32"])
                        for k in range(KC):
                            P.mm(ps[2][:, 0:NEXP], hnT32[:, k, :], rt[:, k, :], k == 0, k == KC - 1, ["hnT32", "rt"], ["ps2"])
                        P.cp("dve", lg[:, 0:8], ps[2][:, 0:NEXP], ["ps2"], [lgk])
                        P.add("dve", lambda e, lg=lg: e.max(out=lg[:, 8:16], in_=lg[:, 0:8]), [lgk], [lgk])
                        P.ts("dve", lg[:, 16:24], lg[:, 0:8], lg[:, 8:9], None, ALU.subtract, None, [lgk], [lgk])
                        P.act(lg[:, 16:24], lg[:, 16:24], AF.Exp, [lgk], [lgk])
                        P.ts("dve", lg[:, 0:8], lg[:, 0:8], lg[:, 9:10], None, ALU.is_ge, None, [lgk], [lgk])
                        P.tt("dve", lg[:, 16:24], lg[:, 16:24], lg[:, 0:8], ALU.mult, [lgk], [lgk])
                        P.add("dve", lambda e, lg=lg: e.reduce_sum(out=lg[:, 10:11], in_=lg[:, 16:24], axis=AX.X), [lgk], [lgk])
                        P.add("dve", lambda e, lg=lg: e.reciprocal(out=lg[:, 10:11], in_=lg[:, 10:11]), [lgk], [lgk])
                        P.ts("dve", gates[:, tt_, :], lg[:, 16:24], lg[:, 10:11], None, ALU.mult, None, [lgk], [("gates", tt_)])
                for e_ in range(NEXP if moe else 1):
                    W13 = A["moe_w13"][0, e_] if moe else A["ffn_w13"][0]
                    W2 = A["moe_w2"][0, e_] if moe else A["ffn_w2"][0]
                    for fb in range(0, FC, 2):
                        w13, w13k = w13r.next()
                        P.dma("pool", w13[:, :, 0, :], W13[:, fb * 128:(fb + 2) * 128].rearrange("(c p) n -> p c n", p=128), w=[w13k])
                        P.dma("pool", w13[:, :, 1, :], W13[:, FFN + fb * 128:FFN + (fb + 2) * 128].rearrange("(c p) n -> p c n", p=128), w=[w13k])
                        for f2 in range(2):
                            f = fb + f2
                            for k in range(KC):
                                P.mm(ps[0][:, 0:GW], w13[:, k, 0, f2 * 128:(f2 + 1) * 128], hnT[:, k, :], k == 0, k == KC - 1, [w13k, "hnT"], ["ps0"])
                            for k in range(KC):
                                P.mm(ps[1][:, 0:GW], w13[:, k, 1, f2 * 128:(f2 + 1) * 128], hnT[:, k, :], k == 0, k == KC - 1, [w13k, "hnT"], ["ps1"])
                            si, sik = sir.next()
                            P.act(si[:], ps[0][:, 0:GW], AF.Silu, ["ps0"], [sik])
                            P.tt("dve", actT[:, f, :], si[:], ps[1][:, 0:GW], ALU.mult, [sik, "ps1"], [("actT", f)])
                    ar = [("actT", f) for f in range(FC)]
                    for dcb in range(4):
                        for f8 in range(0, FC, 8):
                            w2, w2k = w2r.next()
                            P.dma("pool", w2[:], W2[f8 * 128:(f8 + 8) * 128, dcb * 512:(dcb + 1) * 512].rearrange("(c p) n -> p c n", p=128), w=[w2k])
                            for f in range(8):
                                for tt_ in range(TPG):
                                    P.mm(ps[2 + tt_][:, 0:512], actT[:, f8 + f, tt_ * 128:(tt_ + 1) * 128], w2[:, f, :],
                                         f8 + f == 0, f8 + f == FC - 1, [w2k] + ar, [f"ps{2 + tt_}"])
                        for tt_ in range(TPG):
                            hs = hacc[:, tt_, dcb * 512:(dcb + 1) * 512]
                            if moe:
                                P.stt("dve", hs, ps[2 + tt_][:, 0:512], gates[:, tt_, e_:e_ + 1], hs, ALU.mult, ALU.add,
                                      [f"ps{2 + tt_}", ("gates", tt_), ("hacc", tt_)], [("hacc", tt_)])
                            else:
                                P.tt("dve", hs, hs, ps[2 + tt_][:, 0:512], ALU.add, [f"ps{2 + tt_}", ("hacc", tt_)], [("hacc", tt_)])
                for tt_ in range(TPG):
                    t = g * TPG + tt_
                    P.dma("sp", Sc["h"][t * 128:(t + 1) * 128, :], hacc[:, tt_, :], r=[("hacc", tt_)])

    def moe_layer(self, l):
        P, S, NT, A, Sc, c = self.P, self.S, self.NT, self.A, self.Sc, self.c
        ps, pt = self.ps, self.pt
        FC = FFN // 128
        CAP = 128 * int(math.ceil(1.5 * S / 4 / 128))
        NJ = CAP // 128
        NP = int(math.ceil(CAP / 512))
        PW = CAP // NP
        NQ = 7
        FQ = FC // NQ
        with P.phase() as stg:
            gates = P.sbuf("mo_gates", [128, NT, NEXP], F32, stg)
            posm = P.sbuf("mo_posm", [128, NT, NEXP], F32, stg)
            iota = P.sbuf("mo_iota", [128, CAP], F32, stg)
            jcol = P.sbuf("mo_jcol", [128, 8], F32, stg)
            P.dma("sp", iota[:], A["c_iota"][:, 0:CAP], w=["iota"])
            P.dma("sp", jcol[:], A["c_jcol"], w=["jcol"])
            with P.phase() as st:
                grow = self.bcast_row(st, "mo_g", A["norm_ffn"][l], D)
                sutri = P.sbuf("mo_sutri", [128, 128], F32, st)
                P.dma("sp", sutri[:], A["c_sutri"], w=["sutri"])
                rt = P.sbuf("mo_rt", [128, KC, NEXP], F32, st)
                P.dma("sp", rt[:], A["moe_router"][0].rearrange("(c p) e -> p c e", p=128), w=["rt"])
                hr = Ring(P, st, "mo_h", 2, [128, D], F32)
                h32r = Ring(P, st, "mo_h32", 2, [128, D], F32)
                h16r = Ring(P, st, "mo_h16", 2, [128, D], BF16)
                junk = P.sbuf("mo_junk", [128, D], BF16, st)
                ssr = Ring(P, st, "mo_ss", 2, [128, 2], F32)
                hT32r = Ring(P, st, "mo_hT32", 2, [128, KC, 128], F32)
                lgr = Ring(P, st, "mo_lg", 2, [128, 32], F32)
                sel = P.sbuf("mo_sel", [128, NT, NEXP], F32, st)
                run = P.sbuf("mo_run", [128, NEXP], F32, st)
                P.memset("dve", run[:], 0.0, ["run"])
                for t in range(NT):
                    ht, hk = hr.next()
                    h32, h32k = h32r.next()
                    h16, h16k = h16r.next()
                    ss, sk = ssr.next()
                    hT32, hT32k = hT32r.next()
                    lg, lgk = lgr.next()
                    P.dma("sp", ht[:], Sc["h"][t * 128:(t + 1) * 128, :], w=[hk])
                    P.act(junk[:], ht[:], AF.Square, [hk], ["mo_junk", sk], accum_out=ss[:, 0:1])
                    P.ts("dve", ss[:, 1:2], ss[:, 0:1], 1.0 / D, 1e-6, ALU.mult, ALU.add, [sk], [sk])
                    P.act(ss[:, 1:2], ss[:, 1:2], AF.Sqrt, [sk], [sk])
                    P.add("dve", lambda e, ss=ss: e.reciprocal(out=ss[:, 1:2], in_=ss[:, 1:2]), [sk], [sk])
                    P.stt("dve", h32[:], ht[:], ss[:, 1:2], grow[:], ALU.mult, ALU.mult, [hk, sk, "mo_g"], [h32k])
                    P.cp("act", h16[:], h32[:], [h32k], [h16k])
                    P.dma("sp", Sc["hn16"][t * 128:(t + 1) * 128, :], h16[:], r=[h16k])
                    for c4 in range(0, KC, 4):
                        pb, pbk = ps[(c4 // 4) % 2], f"ps{(c4 // 4) % 2}"
                        pv = pb[:, 0:512].rearrange("p (a b) -> p a b", b=128)
                        for j in range(4):
                            P.tr(pv[:, j, :], h32[:, (c4 + j) * 128:(c4 + j + 1) * 128], c["identf"][:], [h32k, "const"], [pbk])
                        P.cp("dve" if (c4 // 4) % 2 == 0 else "act", hT32[:, c4:c4 + 4, :], pv[:, 0:4, :], [pbk], [hT32k])
                    for k in range(KC):
                        P.mm(ps[2][:, 0:NEXP], hT32[:, k, :], rt[:, k, :], k == 0, k == KC - 1, [hT32k, "rt"], ["ps2"])
                    P.cp("dve", lg[:, 0:8], ps[2][:, 0:NEXP], ["ps2"], [lgk])
                    P.add("dve", lambda e, lg=lg: e.max(out=lg[:, 8:16], in_=lg[:, 0:8]), [lgk], [lgk])
                    P.ts("dve", lg[:, 16:24], lg[:, 0:8], lg[:, 8:9], None, ALU.subtract, None, [lgk], [lgk])
                    P.act(lg[:, 16:24], lg[:, 16:24], AF.Exp, [lgk], [lgk])
                    P.ts("dve", sel[:, t, :], lg[:, 0:8], lg[:, 9:10], None, ALU.is_ge, None, [lgk], [("sel", t)])
                    P.tt("dve", lg[:, 16:24], lg[:, 16:24], sel[:, t, :], ALU.mult, [lgk, ("sel", t)], [lgk])
                    P.add("dve", lambda e, lg=lg: e.reduce_sum(out=lg[:, 10:11], in_=lg[:, 16:24], axis=AX.X), [lgk], [lgk])
                    P.add("dve", lambda e, lg=lg: e.reciprocal(out=lg[:, 10:11], in_=lg[:, 10:11]), [lgk], [lgk])
                    P.ts("dve", gates[:, t, :], lg[:, 16:24], lg[:, 10:11], None, ALU.mult, None, [lgk], [("gates", t)])
                    P.mm(ps[3][:, 0:NEXP], sutri[:], sel[:, t, :], True, True, ["sutri", ("sel", t)], ["ps3"])
                    P.mm(ps[3][:, 8:8 + NEXP], c["ones"][:], sel[:, t, :], True, True, ["const", ("sel", t)], ["ps3"])
                    P.tt("dve", lg[:, 24:32], ps[3][:, 0:NEXP], run[:], ALU.add, ["ps3", "run"], [lgk])
                    P.stt("dve", posm[:, t, :], lg[:, 24:32], 1.0, sel[:, t, :], ALU.add, ALU.mult, [lgk, ("sel", t)], [("posm", t)])
                    P.ts("dve", posm[:, t, :], posm[:, t, :], -1.0, None, ALU.add, None, [("posm", t)], [("posm", t)])
                    P.tt("dve", run[:], run[:], ps[3][:, 8:8 + NEXP], ALU.add, ["ps3", "run"], ["run"])
            st = stg
            xgo = P.sbuf("mo_xgo", [128, KC * CAP], BF16, st)
            xgT = xgo[:, :].rearrange("p (k j) -> p k j", j=CAP)
            outg16 = xgo[:, :].rearrange("p (j d) -> p j d", d=D)
            actT = P.sbuf("mo_actT", [128, FQ, CAP], BF16, st)
            outg32 = P.sbuf("mo_outg32", [128, NJ, D], F32, st)
            posbc = P.sbuf("mo_posbc", [128, S], F32, st)
            w13r = Ring(P, st, "mo_w13", 2, [128, KC, 2, 256], BF16)
            w2r = Ring(P, st, "mo_w2", 2, [128, 8, 512], BF16)
            hnr = Ring(P, st, "mo_hn", 2, [128, D], BF16)
            xgr = Ring(P, st, "mo_xg", 1, [128, D], BF16)
            htr = Ring(P, st, "mo_ht", 2, [128, D], F32)
            sir = Ring(P, st, "mo_si", 2, [128, PW], F32)
            pmr = Ring(P, st, "mo_pm", 4, [128, 128], BF16)
            pmTr = Ring(P, st, "mo_pmT", 2, [128, NJ, 128], BF16)
            dgr = Ring(P, st, "mo_dg", 2, [128, 128], F32)
            pmall = [("posm", t) for t in range(NT)]
            for e_ in range(NEXP):
                W13, W2 = A["moe_w13"][0, e_], A["moe_w2"][0, e_]
                for t in range(NT):
                    dg, dgk = dgr.next()
                    P.ts("dve", dg[:], c["identf"][:], posm[:, t, e_:e_ + 1], None, ALU.mult, None, ["const", ("posm", t)], [dgk])
                    pb, pbk = ps[4 + t % 2], f"ps{4 + t % 2}"
                    P.mm(pb[:, 0:128], c["ones"][:], dg[:], True, True, ["const", dgk], [pbk])
                    P.cp("act", posbc[:, t * 128:(t + 1) * 128], pb[:, 0:128], [pbk], ["posbc"])
                for J in range(NJ):
                    for t in range(NT):
                        hn, hnk = hnr.next()
                        pm, pmk = pmr.next()
                        P.dma("sp", hn[:], Sc["hn16"][t * 128:(t + 1) * 128, :], w=[hnk])
                        P.ts("dve", pm[:], iota[:, J * 128:(J + 1) * 128], posm[:, t, e_:e_ + 1], None,
                             ALU.is_equal, None, ["iota", ("posm", t)], [pmk])
                        for q in range(4):
                            P.mm(ps[q][:, 0:512], pm[:], hn[:, q * 512:(q + 1) * 512], t == 0, t == NT - 1, [pmk, hnk], [f"ps{q}"])
                    xg, xgk = xgr.next()
                    for q in range(4):
                        P.cp("act" if q % 2 == 0 else "dve", xg[:, q * 512:(q + 1) * 512], ps[q][:, 0:512], [f"ps{q}"], [xgk])
                    for c4 in range(0, KC, 4):
                        pb, pbk = pt[(c4 // 4) % 2], f"pt{(c4 // 4) % 2}"
                        pv = pb[:, 0:512].rearrange("p (a b) -> p a b", b=128)
                        for j in range(4):
                            P.tr(pv[:, j, :], xg[:, (c4 + j) * 128:(c4 + j + 1) * 128], c["ident"][:], [xgk, "const"], [pbk])
                        P.cp("dve" if (c4 // 4) % 2 == 0 else "act", xgT[:, c4:c4 + 4, J * 128:(J + 1) * 128], pv[:, 0:4, :], [pbk], ["xgo"])
                for fq in range(NQ):
                    for fl in range(FQ):
                        f = fq * FQ + fl
                        f2 = fl % 2
                        if f2 == 0:
                            w13, w13k = w13r.next()
                            P.dma("pool", w13[:, :, 0, :], W13[:, f * 128:(f + 2) * 128].rearrange("(c p) n -> p c n", p=128), w=[w13k])
                            P.dma("pool", w13[:, :, 1, :], W13[:, FFN + f * 128:FFN + (f + 2) * 128].rearrange("(c p) n -> p c n", p=128), w=[w13k])
                        ws = slice(f2 * 128, (f2 + 1) * 128)
                        for pc in range(NP):
                            cs = slice(pc * PW, (pc + 1) * PW)
                            b1, b1k = ps[pc], f"ps{pc}"
                            b3, b3k = ps[2 + pc], f"ps{2 + pc}"
                            for k in range(KC):
                                P.mm(b1[:, 0:PW], w13[:, k, 0, ws], xgT[:, k, cs], k == 0, k == KC - 1, [w13k, "xgo"], [b1k])
                            for k in range(KC):
                                P.mm(b3[:, 0:PW], w13[:, k, 1, ws], xgT[:, k, cs], k == 0, k == KC - 1, [w13k, "xgo"], [b3k])
                            si, sik = sir.next()
                            P.act(si[:], b1[:, 0:PW], AF.Silu, [b1k], [sik])
                            P.tt("dve", actT[:, fl, cs], si[:], b3[:, 0:PW], ALU.mult, [sik, b3k], [("actT", fl)])
                    ar = [("actT", fl) for fl in range(FQ)]
                    for dcb in range(4):
                        for f8 in range(0, FQ, 8):
                            nf = min(8, FQ - f8)
                            w2, w2k = w2r.next()
                            r0 = (fq * FQ + f8) * 128
                            P.dma("pool", w2[:, 0:nf, :], W2[r0:r0 + nf * 128, dcb * 512:(dcb + 1) * 512].rearrange("(c p) n -> p c n", p=128), w=[w2k])
                            for f in range(nf):
                                for J in range(NJ):
                                    P.mm(ps[J][:, 0:512], actT[:, f8 + f, J * 128:(J + 1) * 128], w2[:, f, :],
                                         f8 + f == 0, f8 + f == FQ - 1, [w2k] + ar, [f"ps{J}"])
                        for J in range(NJ):
                            dst32 = outg32[:, J, dcb * 512:(dcb + 1) * 512]
                            if fq == 0:
                                P.cp("act" if J % 2 == 0 else "dve", dst32, ps[J][:, 0:512], [f"ps{J}"], [("og32", J, dcb)])
                            elif fq < NQ - 1:
                                P.tt("dve", dst32, dst32, ps[J][:, 0:512], ALU.add, [f"ps{J}", ("og32", J, dcb)], [("og32", J, dcb)])
                            else:
                                P.tt("dve", outg16[:, J, dcb * 512:(dcb + 1) * 512], dst32, ps[J][:, 0:512], ALU.add,
                                     [f"ps{J}", ("og32", J, dcb)], ["xgo"])
                for t in range(NT):
                    ht, hk = htr.next()
                    pmT, pmTk = pmTr.next()
                    P.dma("sp", ht[:], Sc["h"][t * 128:(t + 1) * 128, :], r=[("hD", t)], w=[hk])
                    for J in range(NJ):
                        P.ts("dve", pmT[:, J, :], posbc[:, t * 128:(t + 1) * 128], jcol[:, J:J + 1], None,
                             ALU.is_equal, None, ["posbc", "jcol"], [pmTk])
                    for dcb in range(4):
                        pb, pbk = ps[dcb], f"ps{dcb}"
                        for J in range(NJ):
                            P.mm(pb[:, 0:512], pmT[:, J, :], outg16[:, J, dcb * 512:(dcb + 1) * 512], J == 0, J == NJ - 1, [pmTk, "xgo"], [pbk])
                        hs = ht[:, dcb * 512:(dcb + 1) * 512]
                        P.stt("dve", hs, pb[:, 0:512], gates[:, t, e_:e_ + 1], hs, ALU.mult, ALU.add, [pbk, ("gates", t), hk], [hk])
                    P.dma("sp", Sc["h"][t * 128:(t + 1) * 128, :], ht[:], r=[hk], w=[("hD", t)])


def make_consts(S):
    bf = ml_dtypes.bfloat16
    i = np.arange(128)
    tri = (i[:, None] <= i[None, :]).astype(np.float32)
    inv = 1.0 / (10000.0 ** (np.arange(0, 64, 2, dtype=np.float32) / 64.0))
    ang = np.arange(S, dtype=np.float32)[:, None] * inv[None, :]
    cos, sin = np.cos(ang).astype(np.float32).T, np.sin(ang).astype(np.float32).T
    return dict(
        c_ident=np.eye(128, dtype=np.float32).astype(bf), c_identf=np.eye(128, dtype=np.float32),
        c_tri=tri, c_trib=tri.astype(bf), c_lowb=(1.0 - tri).astype(bf),
        c_mneg=((1.0 - tri) * NEG).astype(np.float32), c_ones=np.ones((128, 128), np.float32),
        c_sutri=(tri - np.eye(128, dtype=np.float32)).astype(np.float32),
        c_iota=np.ascontiguousarray(np.broadcast_to(np.arange(1024, dtype=np.float32)[None, :], (128, 1024))),
        c_jcol=np.ascontiguousarray((np.arange(8, dtype=np.float32)[None, :] * 128 + i[:, None]).astype(np.float32)),
        c_cos2=np.ascontiguousarray(np.concatenate([cos, cos], 0)),
        c_sin2=np.ascontiguousarray(np.concatenate([-sin, sin], 0)),
    )


_CACHE = {}


def kernel(**inputs):
    S = inputs["x"].shape[1]
    B = inputs["x"].shape[0]
    if S not in _CACHE:
        _CACHE[S] = Builder(S).build()
    nc = _CACHE[S]
    consts = make_consts(S)
    shared = {k: np.ascontiguousarray(v) for k, v in inputs.items() if k not in ("x", "mem")}
    in_maps = []
    for b in range(B):
        m = dict(shared)
        m.update(consts)
        m["x"] = np.ascontiguousarray(inputs["x"][b])
        m["mem"] = np.ascontiguousarray(inputs["mem"][b])
        in_maps.append(m)
    res = run_bass_kernel_spmd(nc, in_maps, core_ids=list(range(B)))
    return np.stack([r["out"] for r in res.results], axis=0).astype(np.float32)
```

```python
import contextlib
import math
import numpy as np
import ml_dtypes
import concourse.bass as bass
import concourse.mybir as mybir
from concourse.bass_utils import run_bass_kernel_spmd

F32 = mybir.dt.float32
BF16 = mybir.dt.bfloat16
AF = mybir.ActivationFunctionType
ALU = mybir.AluOpType
AX = mybir.AxisListType

EPOCH = 4000
N_DMA_SEMS = 40

D = 2048
KC = D // 128
IN_TOTAL = 16216
FFN = 7168
NEXP = 8
MEM = 256
O_MLQ, O_MLK, O_MLV, O_MLO, O_MLI, O_MLF = 0, 512, 1024, 2048, 3072, 3076
O_Z, O_XBC, O_DT, O_CQ, O_CKV, O_KR = 3080, 4104, 5640, 5656, 6168, 6424
O_SWQ, O_SWK, O_SWV, O_GATE = 6488, 7512, 7768, 8024
NEG = -30000.0


class Prog:
    ENGS = ("pe", "act", "dve", "pool", "sp")

    def __init__(self, nc):
        self.nc = nc
        self.ops = []
        self.last_w = {}
        self.readers = {}
        self.stack = contextlib.ExitStack()
        self.nd = 0
        self.last_on_eng = {}
        self.last_dma_slot = {}
        self.uid = 0

    def sbuf(self, name, shape, dtype, stack=None):
        self.uid += 1
        return (stack or self.stack).enter_context(
            self.nc.sbuf_tensor(f"{name}_{self.uid}", list(shape), dtype))

    def psum(self, name, shape, dtype):
        return self.stack.enter_context(self.nc.psum_tensor(name, list(shape), dtype))

    def dram(self, name, shape, dtype, kind="Internal"):
        return self.nc.dram_tensor(name, list(shape), dtype, kind=kind).ap()

    def add(self, eng, emit, reads=(), writes=(), dma=False):
        idx = len(self.ops)
        raw, other = set(), set()
        for r in reads:
            w = self.last_w.get(r)
            if w is not None:
                raw.add(w)
        for r in writes:
            w = self.last_w.get(r)
            if w is not None:
                other.add(w)
            for rd in self.readers.get(r, ()):
                other.add(rd)
        for r in reads:
            lst = self.readers.setdefault(r, [])
            if not dma:
                lst[:] = [j for j in lst if self.ops[j]["dma"] or self.ops[j]["eng"] != eng]
            lst.append(idx)
        for r in writes:
            self.last_w[r] = idx
            self.readers[r] = []
        raw.discard(idx)
        other.discard(idx)
        other -= raw
        op = dict(eng=eng, emit=emit, raw=raw, other=other, dma=dma, need_inc=False,
                  ticket=None, explicit=None)
        if dma:
            op["slot"] = self.nd % N_DMA_SEMS
            self.nd += 1
            self.last_dma_slot[op["slot"]] = idx
        self.last_on_eng[eng] = idx
        self.ops.append(op)
        return idx

    def barrier(self):
        targets = set(self.last_on_eng.values()) | set(self.last_dma_slot.values())
        for e in self.ENGS:
            self.ops.append(dict(eng=e, emit=None, raw=set(), other=set(), dma=False,
                                 need_inc=False, ticket=None, explicit=set(targets)))
        self.last_w = {}
        self.readers = {}

    @contextlib.contextmanager
    def phase(self):
        st = contextlib.ExitStack()
        try:
            yield st
        finally:
            self.barrier()
            st.close()

    def dma(self, eng, out, in_, r=(), w=(), **kw):
        return self.add(eng, lambda e: e.dma_start(out=out, in_=in_, **kw), r, w, dma=True)

    def mm(self, out, lhsT, rhs, start, stop, r, w):
        return self.add("pe", lambda e: e.matmul(out=out, lhsT=lhsT, rhs=rhs, start=start, stop=stop), r, w)

    def tr(self, out, in_, ident, r, w):
        return self.add("pe", lambda e: e.transpose(out=out, in_=in_, identity=ident), r, w)

    def act(self, out, in_, func, r, w, **kw):
        return self.add("act", lambda e: e.activation(out=out, in_=in_, func=func, **kw), r, w)

    def ts(self, eng, out, in0, s1, s2, op0, op1, r, w):
        if op1 is None:
            return self.add(eng, lambda e: e.tensor_scalar(out=out, in0=in0, scalar1=s1, scalar2=None, op0=op0), r, w)
        return self.add(eng, lambda e: e.tensor_scalar(out=out, in0=in0, scalar1=s1, scalar2=s2, op0=op0, op1=op1), r, w)

    def tt(self, eng, out, in0, in1, op, r, w):
        return self.add(eng, lambda e: e.tensor_tensor(out=out, in0=in0, in1=in1, op=op), r, w)

    def stt(self, eng, out, in0, scalar, in1, op0, op1, r, w):
        return self.add(eng, lambda e: e.scalar_tensor_tensor(out=out, in0=in0, scalar=scalar, in1=in1,
                                                              op0=op0, op1=op1), r, w)

    def cp(self, eng, out, in_, r, w):
        if eng == "act":
            return self.add(eng, lambda e: e.copy(out=out, in_=in_), r, w)
        return self.add(eng, lambda e: e.tensor_copy(out=out, in_=in_), r, w)

    def memset(self, eng, ap, val, w):
        return self.add(eng, lambda e: e.memset(ap, val), (), w)

    def emit_all(self):
        nc, ops = self.nc, self.ops
        self.barrier()
        for i, op in enumerate(ops):
            if op["explicit"] is not None:
                deps = [j for j in op["explicit"] if j < i]
            else:
                deps = []
                for j in op["raw"]:
                    pj = ops[j]
                    if pj["eng"] == op["eng"] and not pj["dma"] and op["eng"] == "pe" and not op["dma"]:
                        continue
                    deps.append(j)
                for j in op["other"]:
                    pj = ops[j]
                    if pj["eng"] == op["eng"] and not pj["dma"] and not op["dma"]:
                        continue
                    deps.append(j)
            op["deps"] = sorted(set(deps))
            for j in op["deps"]:
                ops[j]["need_inc"] = True
        counts = {e: 0 for e in self.ENGS}
        nsem = {e: 1 for e in self.ENGS}
        dma_uses = [0] * N_DMA_SEMS
        dma_prev = [None] * N_DMA_SEMS
        for i, op in enumerate(ops):
            if op["dma"]:
                s = op["slot"]
                dma_uses[s] += 1
                op["ticket"] = ("dma", s, dma_uses[s] * 16)
                op["dma_prev"] = dma_prev[s]
                dma_prev[s] = i
            elif op["need_inc"]:
                e = op["eng"]
                ep, v = divmod(counts[e], EPOCH)
                counts[e] += 1
                op["ticket"] = (e, ep, v + 1)
                nsem[e] = max(nsem[e], ep + 1)
        sems = {}
        for e in self.ENGS:
            for ep in range(nsem[e]):
                sems[(e, ep)] = self.stack.enter_context(nc.semaphore(f"s_{e}_{ep}"))
        for s in range(N_DMA_SEMS):
            sems[("dma", s)] = self.stack.enter_context(nc.semaphore(f"s_dma_{s}"))
        print("ops", len(ops), "incs", counts, "ndma", self.nd, "nsems", len(sems), flush=True)
        by_eng = {e: [] for e in self.ENGS}
        for i, op in enumerate(ops):
            by_eng[op["eng"]].append(i)

        def run_engine(ename, eng):
            waited = {}

            def wait(t):
                key = (t[0], t[1])
                if waited.get(key, 0) >= t[2]:
                    return
                eng.wait_ge(sems[key], t[2])
                waited[key] = t[2]

            for i in by_eng[ename]:
                op = ops[i]
                for j in op["deps"]:
                    wait(ops[j]["ticket"])
                if op["emit"] is None:
                    continue
                if op["dma"] and op["dma_prev"] is not None:
                    wait(ops[op["dma_prev"]]["ticket"])
                ins = op["emit"](eng)
                t = op["ticket"]
                if t is not None:
                    ins.then_inc(sems[(t[0], t[1])], 16 if op["dma"] else 1)

        with nc.Block() as block:
            @block.tensor
            def _(e):
                run_engine("pe", e)

            @block.scalar
            def _(e):
                run_engine("act", e)

            @block.vector
            def _(e):
                run_engine("dve", e)

            @block.gpsimd
            def _(e):
                run_engine("pool", e)

            @block.sync
            def _(e):
                run_engine("sp", e)
        self.stack.close()


class Ring:
    def __init__(self, P, st, name, n, shape, dtype):
        self.n = n
        self.bufs = [P.sbuf(name, shape, dtype, st) for _ in range(n)]
        P.uid += 1
        self.keys = [f"{name}#{P.uid}#{i}" for i in range(n)]
        self.i = 0

    def next(self):
        j = self.i % self.n
        self.i += 1
        return self.bufs[j], self.keys[j]


class KeyRing:
    def __init__(self, objs, keys):
        self.objs, self.keys, self.i = objs, keys, 0

    def next(self):
        j = self.i % len(self.objs)
        self.i += 1
        return self.objs[j], self.keys[j]


class Builder:
    def __init__(self, S, depth=2, debug=False, stop_after=None):
        self.S = S
        self.NT = S // 128
        self.GW = min(512, S)
        self.NG = S // self.GW
        self.depth = depth
        self.debug = debug
        self.stop_after = stop_after
        nc = bass.Bass("TRN2", target_bir_lowering=False)
        self.nc = nc
        self.P = Prog(nc)

    def norm_T(self, st, src, gamma, dstT, dkey, R, W, eps=1e-6, dt=BF16, rows_at=None):
        P, c = self.P, self.c
        kc = W // 128
        gcol = P.sbuf("gcol", [128, kc], F32, st)
        P.dma("sp", gcol[:], gamma.rearrange("(c p) -> p c", p=128), w=["gcol"], allow_slow_non_contiguous=True)
        hr = Ring(P, st, "nt_h", 2, [128, W], F32)
        hbr = Ring(P, st, "nt_hb", 2, [128, W], dt)
        junk = P.sbuf("nt_junk", [128, W], F32, st)
        ssr = Ring(P, st, "nt_ss", 2, [128, 2], F32)
        ptr = KeyRing(self.pt if dt == BF16 else self.ps[4:6], ["pt0", "pt1"] if dt == BF16 else ["ps4", "ps5"])
        ident = c["ident"] if dt == BF16 else c["identf"]
        for t in range(R // 128):
            ht, hk = hr.next()
            hb, hbk = hbr.next()
            ss, sk = ssr.next()
            P.dma("sp", ht[:], src[t * 128:(t + 1) * 128, :], w=[hk])
            P.act(junk[:], ht[:], AF.Square, [hk], ["nt_junk", sk], accum_out=ss[:, 0:1])
            P.ts("dve", ss[:, 1:2], ss[:, 0:1], 1.0 / W, eps, ALU.mult, ALU.add, [sk], [sk])
            P.act(ss[:, 1:2], ss[:, 1:2], AF.Sqrt, [sk], [sk])
            P.add("dve", lambda e, ss=ss: e.reciprocal(out=ss[:, 1:2], in_=ss[:, 1:2]), [sk], [sk])
            P.ts("pool", hb[:], ht[:], ss[:, 1:2], None, ALU.mult, None, [hk, sk], [hbk])
            for c4 in range(0, kc, 4):
                n4 = min(4, kc - c4)
                pst, pk = ptr.next()
                pv = pst[:, 0:512].rearrange("p (a b) -> p a b", b=128)
                for j in range(n4):
                    P.tr(pv[:, j, :], hb[:, (c4 + j) * 128:(c4 + j + 1) * 128], ident[:], [hbk, "const"], [pk])
                P.tt("dve", dstT[:, c4:c4 + n4, t * 128:(t + 1) * 128], pv[:, 0:n4, :],
                     gcol[:, c4:c4 + n4].unsqueeze(2).to_broadcast([128, n4, 128]), ALU.mult,
                     [pk, "gcol"], [(dkey, t)])

    def wload(self, wring, W_ap, c0, w, kc):
        wt, wk = wring.next()
        self.P.dma("pool", wt[:, 0:kc, 0:w], W_ap[:, c0:c0 + w].rearrange("(c p) n -> p c n", p=128), w=[wk])
        return wt, wk

    def proj_fm(self, st, xT, xkey, kc, W_ap, c0, ncols, dst, rings, func=None, dtype=F32):
        P, S, GW = self.P, self.S, self.GW
        wring, psr, stg32, stg16 = rings
        stgr = stg32 if dtype == F32 else stg16
        xr = [(xkey, t) for t in range(self.NT)]
        for cb in range(0, ncols, 512):
            w = min(512, ncols - cb)
            wt, wk = self.wload(wring, W_ap, c0 + cb, w, kc)
            for sb in range(0, w, 128):
                m = min(128, w - sb)
                stg, sk = stgr.next()
                for g in range(self.NG):
                    ps, pk = psr.next()
                    for k in range(kc):
                        P.mm(ps[0:m, 0:GW], wt[:, k, sb:sb + m], xT[:, k, g * GW:(g + 1) * GW],
                             k == 0, k == kc - 1, [wk] + xr, [pk])
                    if func is not None or (g % 2 == 0):
                        P.act(stg[0:m, g * GW:(g + 1) * GW], ps[0:m, 0:GW], func or AF.Copy, [pk], [sk])
                    else:
                        P.cp("dve", stg[0:m, g * GW:(g + 1) * GW], ps[0:m, 0:GW], [pk], [sk])
                P.dma("sp", dst[cb + sb:cb + sb + m, :], stg[0:m, 0:S], r=[sk])

    def proj_tm(self, st, xT, xkey, kc, W_ap, c0, ncols, dst, rings, dtype=F32):
        P, S = self.P, self.S
        wring, psr, stg32, stg16 = rings
        stgr = stg32 if dtype == F32 else stg16
        for cb in range(0, ncols, 512):
            w = min(512, ncols - cb)
            wt, wk = self.wload(wring, W_ap, c0 + cb, w, kc)
            for t in range(self.NT):
                ps, pk = psr.next()
                stg, sk = stgr.next()
                for k in range(kc):
                    P.mm(ps[:, 0:w], xT[:, k, t * 128:(t + 1) * 128], wt[:, k, 0:w], k == 0, k == kc - 1,
                         [wk, (xkey, t)], [pk])
                if t % 2 == 0:
                    P.act(stg[:, 0:w], ps[:, 0:w], AF.Copy, [pk], [sk])
                else:
                    P.cp("dve", stg[:, 0:w], ps[:, 0:w], [pk], [sk])
                P.dma("sp", dst[t * 128:(t + 1) * 128, cb:cb + w], stg[:, 0:w], r=[sk])

    def bcast_row(self, st, name, src_row, n):
        t = self.P.sbuf(name, [128, n], F32, st)
        self.P.dma("sp", t[:], src_row.partition_broadcast(128), w=[name])
        return t

    def build(self):
        P, nc, S, NT = self.P, self.nc, self.S, self.NT
        inp = lambda n, sh, dt=F32: P.dram(n, sh, dt, kind="ExternalInput")
        A = {}
        A["x"] = inp("x", [S, D])
        A["mem"] = inp("mem", [MEM, D])
        for n, sh in (("norm_mix", [2, D]), ("w_in", [2, D, IN_TOTAL]), ("ml_igate_bias", [2, 4]),
                      ("ml_fgate_bias", [2, 4]), ("ml_norm", [2, 1024]), ("ssm_conv_w", [2, 4, 1536]),
                      ("ssm_conv_b", [2, 1536]), ("ssm_dt_bias", [2, 16]), ("ssm_a_log", [2, 16]),
                      ("ssm_d", [2, 16]), ("ssm_norm", [2, 1024]), ("mla_q_norm", [2, 512]),
                      ("mla_w_uq", [2, 512, 1536]), ("mla_kv_norm", [2, 256]), ("mla_w_ukv", [2, 256, 2048]),
                      ("swa_sinks", [2, 16]), ("w_branch", [2, 4, 1024, D]), ("w_out", [2, D, D]),
                      ("norm_cross", [2, D]), ("norm_mem", [2, D]), ("xa_wq", [2, D, 512]),
                      ("xa_wkv", [2, D, 1024]), ("xa_wo", [2, 512, D]), ("norm_ffn", [2, D]),
                      ("ffn_w13", [1, D, 2 * FFN]), ("ffn_w2", [1, FFN, D]), ("moe_router", [1, D, NEXP]),
                      ("moe_w13", [1, NEXP, D, 2 * FFN]), ("moe_w2", [1, NEXP, FFN, D]), ("norm_final", [D])):
            A[n] = inp(n, sh)
        A["c_ident"] = inp("c_ident", [128, 128], BF16)
        A["c_identf"] = inp("c_identf", [128, 128])
        A["c_tri"] = inp("c_tri", [128, 128])
        A["c_trib"] = inp("c_trib", [128, 128], BF16)
        A["c_lowb"] = inp("c_lowb", [128, 128], BF16)
        A["c_mneg"] = inp("c_mneg", [128, 128])
        A["c_ones"] = inp("c_ones", [128, 128])
        A["c_sutri"] = inp("c_sutri", [128, 128])
        A["c_iota"] = inp("c_iota", [128, 1024])
        A["c_jcol"] = inp("c_jcol", [128, 8])
        A["c_cos2"] = inp("c_cos2", [64, S])
        A["c_sin2"] = inp("c_sin2", [64, S])
        self.A = A
        out = P.dram("out", [S, D], F32, kind="ExternalOutput")
        self.out = out
        Sc = {}
        Sc["h"] = P.dram("s_h", [S, D], F32)
        Sc["ufm32"] = P.dram("s_ufm32", [IN_TOTAL + 64, S], F32)
        Sc["ufm16"] = P.dram("s_ufm16", [IN_TOTAL, S], BF16)
        Sc["utm32"] = P.dram("s_utm32", [S, IN_TOTAL], F32)
        Sc["utm16"] = P.dram("s_utm16", [S, IN_TOTAL], BF16)
        Sc["xcT"] = P.dram("s_xcT", [1536, S], F32)
        Sc["hn16"] = P.dram("s_hn16", [S, D], BF16)
        kind = "ExternalOutput" if self.debug else "Internal"
        for i, n in enumerate("abcd"):
            Sc["y" + n] = P.dram("s_y" + n, [S, 1024], BF16, kind=kind)
        if self.debug:
            Sc["dbg_h"] = [P.dram(f"dbg_h{i}", [S, D], F32, kind="ExternalOutput") for i in range(3 * self.depth)]
        self.Sc = Sc

        c = {}
        self.c = c
        for n, dt in (("ident", BF16), ("identf", F32), ("tri", F32), ("trib", BF16), ("lowb", BF16),
                      ("mneg", F32), ("ones", F32)):
            c[n] = P.sbuf("c_" + n, [128, 128], dt)
            P.dma("sp", c[n][:], A["c_" + n], w=["const"])
        self.ps = [P.psum(f"ps{i}", [128, 512], F32) for i in range(6)]
        self.pt = [P.psum(f"pt{i}", [128, 1024], BF16) for i in range(2)]
        P.barrier()

        with P.phase() as st:
            r = Ring(P, st, "cpx", 3, [128, D], F32)
            for t in range(NT):
                b, k = r.next()
                P.dma("sp", b[:], A["x"][t * 128:(t + 1) * 128, :], w=[k])
                P.dma("sp", Sc["h"][t * 128:(t + 1) * 128, :], b[:], r=[k])

        for l in range(self.depth):
            self.layer(l)
            if self.stop_after is not None and l == self.stop_after[0]:
                break
        self.final_norm()
        P.emit_all()
        return nc

    def dbg_copy_h(self, idx):
        if not self.debug:
            return
        P, Sc = self.P, self.Sc
        with P.phase() as st:
            r = Ring(P, st, "cpd", 3, [128, D], F32)
            for t in range(self.NT):
                b, k = r.next()
                P.dma("sp", b[:], Sc["h"][t * 128:(t + 1) * 128, :], w=[k])
                P.dma("sp", Sc["dbg_h"][idx][t * 128:(t + 1) * 128, :], b[:], r=[k])

    def final_norm(self):
        P, S, NT, A, Sc = self.P, self.S, self.NT, self.A, self.Sc
        with P.phase() as st:
            g = self.bcast_row(st, "fn_g", A["norm_final"], D)
            hr = Ring(P, st, "fn_h", 2, [128, D], F32)
            orr = Ring(P, st, "fn_o", 2, [128, D], F32)
            junk = P.sbuf("fn_junk", [128, D], F32, st)
            ssr = Ring(P, st, "fn_ss", 2, [128, 2], F32)
            for t in range(NT):
                ht, hk = hr.next()
                ot, ok = orr.next()
                ss, sk = ssr.next()
                P.dma("sp", ht[:], Sc["h"][t * 128:(t + 1) * 128, :], w=[hk])
                P.act(junk[:], ht[:], AF.Square, [hk], ["fn_junk", sk], accum_out=ss[:, 0:1])
                P.ts("dve", ss[:, 1:2], ss[:, 0:1], 1.0 / D, 1e-6, ALU.mult, ALU.add, [sk], [sk])
                P.act(ss[:, 1:2], ss[:, 1:2], AF.Sqrt, [sk], [sk])
                P.add("dve", lambda e, ss=ss: e.reciprocal(out=ss[:, 1:2], in_=ss[:, 1:2]), [sk], [sk])
                P.stt("dve", ot[:], ht[:], ss[:, 1:2], g[:], ALU.mult, ALU.mult, [hk, sk, "fn_g"], [ok])
                P.dma("sp", self.out[t * 128:(t + 1) * 128, :], ot[:], r=[ok])

    def layer(self, l):
        sa = self.stop_after
        self.in_proj(l)
        self.mlstm(l)
        self.ssd(l)
        self.mla(l)
        self.swa(l)
        if sa is not None and sa == (l, "branches"):
            return
        self.merge(l)
        self.dbg_copy_h(3 * l + 0)
        if sa is not None and sa == (l, "mixer"):
            return
        self.xattn(l)
        self.dbg_copy_h(3 * l + 1)
        if sa is not None and sa == (l, "xattn"):
            return
        if l % 2 == 1:
            self.moe_layer(l)
        else:
            self.ffn_layer(l)
        self.dbg_copy_h(3 * l + 2)

    def in_proj(self, l):
        P, S, NT, A, Sc = self.P, self.S, self.NT, self.A, self.Sc
        W = A["w_in"][l]
        with P.phase() as st:
            xnT = P.sbuf("xnT", [128, KC, S], BF16, st)
            with P.phase() as st2:
                self.norm_T(st2, Sc["h"], A["norm_mix"][l], xnT, "xnT", S, D)
            wring = Ring(P, st, "ip_w", 3, [128, KC, 512], BF16)
            psr = KeyRing(self.ps[0:4], ["ps0", "ps1", "ps2", "ps3"])
            stg32 = Ring(P, st, "ip_s32", 3, [128, max(S, 512)], F32)
            stg16 = Ring(P, st, "ip_s16", 3, [128, max(S, 512)], BF16)
            rings = (wring, psr, stg32, stg16)
            fm = lambda c0, n, dt, func=None, drow=None: self.proj_fm(
                st, xnT, "xnT", KC, W, c0, n,
                (Sc["ufm32"] if dt == F32 else Sc["ufm16"])[(c0 if drow is None else drow):(c0 if drow is None else drow) + n, :],
                rings, func=func, dtype=dt)
            tm = lambda c0, n, dt: self.proj_tm(
                st, xnT, "xnT", KC, W, c0, n, (Sc["utm32"] if dt == F32 else Sc["utm16"])[:, c0:c0 + n], rings, dtype=dt)
            tm(O_MLK, 512, BF16)
            tm(O_MLV, 1024, BF16)
            tm(O_MLO, 1032, F32)
            tm(O_Z, 1024, F32)
            tm(O_DT, 784, F32)
            tm(O_SWV, 256, BF16)
            fm(O_MLQ, 1024, BF16)
            fm(O_XBC, 1536, F32)
            fm(O_KR, 64, F32)
            fm(O_KR + 32, 32, F32, drow=IN_TOTAL)
            fm(O_KR, 32, F32, drow=IN_TOTAL + 32)
            fm(O_SWQ, 1280, BF16)
            fm(O_GATE, 4 * D, BF16, func=AF.Sigmoid)

    def mlstm(self, l):
        P, S, NT, A, Sc, c = self.P, self.S, self.NT, self.A, self.Sc, self.c
        ps = self.ps
        with P.phase() as st:
            gif = P.sbuf("ml_gif", [128, NT, 8], F32, st)
            P.dma("sp", gif[:], Sc["utm32"][:, O_MLI:O_MLI + 8].rearrange("(t p) c -> p t c", p=128), w=["gif"])
            bi = self.bcast_row(st, "ml_bi", A["ml_igate_bias"][l], 4)
            bf = self.bcast_row(st, "ml_bf", A["ml_fgate_bias"][l], 4)
            nw = self.bcast_row(st, "ml_nw", A["ml_norm"][l], 1024)
            ig = P.sbuf("ml_ig", [128, NT, 4], F32, st)
            lf = P.sbuf("ml_lf", [128, NT, 4], F32, st)
            P.tt("dve", ig[:], gif[:, :, 0:4], bi[:].unsqueeze(1).to_broadcast([128, NT, 4]), ALU.add, ["gif", "ml_bi"], ["ig"])
            P.tt("dve", lf[:], gif[:, :, 4:8], bf[:].unsqueeze(1).to_broadcast([128, NT, 4]), ALU.add, ["gif", "ml_bf"], ["lf"])
            P.act(lf[:], lf[:], AF.Exp, ["lf"], ["lf"], scale=-1.0)
            P.act(lf[:], lf[:], AF.Ln, ["lf"], ["lf"], bias=1.0)
            P.ts("dve", lf[:], lf[:], -1.0, None, ALU.mult, None, ["lf"], ["lf"])
            bcs = P.sbuf("ml_b", [128, NT, 4], F32, st)
            ibm = P.sbuf("ml_ibm", [128, NT, 4], F32, st)
            eb = P.sbuf("ml_eb", [128, NT, 4], F32, st)
            wst = P.sbuf("ml_wst", [128, NT, 4], F32, st)
            ebt = P.sbuf("ml_ebt", [128, NT, 4], F32, st)
            for t in range(NT):
                P.mm(ps[0][:, 0:4], c["tri"][:], lf[:, t, :], True, True, ["lf", "const"], ["ps0"])
                P.mm(ps[0][:, 4:8], c["ones"][:], lf[:, t, :], True, True, ["lf", "const"], ["ps0"])
                P.cp("dve", bcs[:, t, :], ps[0][:, 0:4], ["ps0"], ["bcs"])
                P.cp("dve", ebt[:, t, :], ps[0][:, 4:8], ["ps0"], ["ebt"])
            P.tt("dve", ibm[:], ig[:], bcs[:], ALU.subtract, ["ig", "bcs"], ["ibm"])
            P.tt("dve", wst[:], ibm[:], ebt[:], ALU.add, ["ibm", "ebt"], ["wst"])
            P.act(wst[:], wst[:], AF.Exp, ["wst"], ["wst"])
            P.ts("dve", wst[:], wst[:], 128.0 ** -0.5, None, ALU.mult, None, ["wst"], ["wst"])
            P.act(eb[:], bcs[:], AF.Exp, ["bcs"], ["eb"])
            P.act(ebt[:], ebt[:], AF.Exp, ["ebt"], ["ebt"])

            heads = []
            for hd in range(4):
                H = {}
                H["qT"] = P.sbuf("ml_qT", [128, S], BF16, st)
                H["kT"] = P.sbuf("ml_kT", [128, S], BF16, st)
                H["kt"] = P.sbuf("ml_kt", [128, NT, 128], BF16, st)
                H["va"] = P.sbuf("ml_va", [128, NT, 257], BF16, st)
                H["C"] = P.sbuf("ml_C", [128, 257], F32, st)
                H["Cbr"] = Ring(P, st, "ml_Cb", 2, [128, 257], BF16)
                hk = f"mlh{hd}"
                P.dma("sp", H["qT"][:], Sc["ufm16"][O_MLQ + hd * 128:O_MLQ + (hd + 1) * 128, :], w=[hk + "q"])
                P.dma("sp", H["kT"][:], Sc["ufm16"][O_MLK + hd * 128:O_MLK + (hd + 1) * 128, :], w=[hk + "k"])
                P.dma("sp", H["kt"][:], Sc["utm16"][:, O_MLK + hd * 128:O_MLK + (hd + 1) * 128].rearrange("(t p) c -> p t c", p=128), w=[hk + "kt"])
                P.dma("sp", H["va"][:, :, 0:256], Sc["utm16"][:, O_MLV + hd * 256:O_MLV + (hd + 1) * 256].rearrange("(t p) c -> p t c", p=128), w=[hk + "v"])
                P.memset("pool", H["va"][:, :, 256:257], 1.0, [hk + "v"])
                P.memset("dve", H["C"][:], 0.0, [hk + "C"])
                H["Cb"] = H["Cbr"].next()
                P.memset("pool", H["Cb"][0][:], 0.0, [H["Cb"][1]])
                heads.append(H)
            ogr = Ring(P, st, "ml_og", 6, [128, 256], F32)
            ltr = Ring(P, st, "ml_lt", 6, [128, 128], F32)
            etr = Ring(P, st, "ml_et", 6, [128, 128], F32)
            ptr_ = Ring(P, st, "ml_pt", 6, [128, 128], BF16)
            kwr = Ring(P, st, "ml_kw", 6, [128, 128], BF16)
            tmr = Ring(P, st, "ml_tmp", 6, [128, 257], F32)
            hor = Ring(P, st, "ml_ho", 6, [128, 257], F32)
            smr = Ring(P, st, "ml_sm", 6, [128, 4], F32)
            yr = Ring(P, st, "ml_y", 6, [128, 256], BF16)
            junk = P.sbuf("ml_junk", [128, 256], BF16, st)
            for t in range(NT):
                sl = slice(t * 128, (t + 1) * 128)
                for hd in range(4):
                    H = heads[hd]
                    hk = f"mlh{hd}"
                    qT, qk, kT, kk, kt, ktk, va, vk = H["qT"], hk + "q", H["kT"], hk + "k", H["kt"], hk + "kt", H["va"], hk + "v"
                    Cst, Ck = H["C"], hk + "C"
                    Cb, cbk = H["Cb"]
                    og, ogk = ogr.next()
                    lt, ltk = ltr.next()
                    et, etk = etr.next()
                    pt, ptk = ptr_.next()
                    kw, kwk = kwr.next()
                    tmp, tmk = tmr.next()
                    ho, hok = hor.next()
                    sm, smk = smr.next()
                    y, yk = yr.next()
                    P.dma("sp", og[:], Sc["utm32"][sl, O_MLO + hd * 256:O_MLO + (hd + 1) * 256], w=[ogk])
                    P.act(og[:], og[:], AF.Sigmoid, [ogk], [ogk])
                    P.ts("pool", lt[:], c["tri"][:], lf[:, t, hd:hd + 1], None, ALU.mult, None, ["lf", "const"], [ltk])
                    P.mm(ps[1][:, 0:128], c["ones"][:], lt[:], True, False, [ltk, "const"], ["ps1"])
                    P.mm(ps[1][:, 0:128], c["identf"][:], c["mneg"][:], False, True, ["const"], ["ps1"])
                    P.act(et[:], ps[1][:, 0:128], AF.Exp, ["ps1", "ibm"], [etk], bias=ibm[:, t, hd:hd + 1])
                    P.mm(ps[2][:, 0:128], kT[:, sl], qT[:, sl], True, True, [kk, qk], ["ps2"])
                    P.stt("dve", pt[:], ps[2][:, 0:128], 128.0 ** -0.5, et[:], ALU.mult, ALU.mult, ["ps2", etk], [ptk])
                    P.mm(ps[3][:, 0:257], pt[:], va[:, t, :], True, True, [ptk, vk], ["ps3"])
                    P.mm(ps[4][:, 0:257], qT[:, sl], Cb[:], True, True, [qk, cbk], ["ps4"])
                    P.act(tmp[:], ps[4][:, 0:257], AF.Copy, ["ps4", "eb"], [tmk], scale=eb[:, t, hd:hd + 1])
                    P.tt("dve", ho[:], tmp[:], ps[3][:, 0:257], ALU.add, [tmk, "ps3"], [hok])
                    P.act(sm[:, 0:1], ho[:, 256:257], AF.Abs, [hok], [smk])
                    P.ts("dve", sm[:, 0:1], sm[:, 0:1], 1.0, None, ALU.max, None, [smk], [smk])
                    P.add("dve", lambda e, sm=sm: e.reciprocal(out=sm[:, 0:1], in_=sm[:, 0:1]), [smk], [smk])
                    P.ts("dve", ho[:, 0:256], ho[:, 0:256], sm[:, 0:1], None, ALU.mult, None, [hok, smk], [hok])
                    P.act(junk[:], ho[:, 0:256], AF.Square, [hok], ["ml_junk", smk], accum_out=sm[:, 1:2])
                    P.ts("dve", sm[:, 2:3], sm[:, 1:2], 1.0 / 256, 1e-6, ALU.mult, ALU.add, [smk], [smk])
                    P.act(sm[:, 2:3], sm[:, 2:3], AF.Sqrt, [smk], [smk])
                    P.add("dve", lambda e, sm=sm: e.reciprocal(out=sm[:, 2:3], in_=sm[:, 2:3]), [smk], [smk])
                    P.stt("dve", ho[:, 0:256], ho[:, 0:256], sm[:, 2:3], nw[:, hd * 256:(hd + 1) * 256], ALU.mult, ALU.mult,
                          [hok, smk, "ml_nw"], [hok])
                    P.tt("pool", y[:], ho[:, 0:256], og[:], ALU.mult, [hok, ogk], [yk])
                    P.dma("sp", Sc["ya"][sl, hd * 256:(hd + 1) * 256], y[:], r=[yk])
                    if t < NT - 1:
                        P.ts("pool", kw[:], kt[:, t, :], wst[:, t, hd:hd + 1], None, ALU.mult, None, [ktk, "wst"], [kwk])
                        P.mm(ps[5][:, 0:257], kw[:], va[:, t, :], True, True, [kwk, vk], ["ps5"])
                        P.stt("dve", Cst[:], Cst[:], ebt[:, t, hd:hd + 1], ps[5][:, 0:257], ALU.mult, ALU.add,
                              [Ck, "ebt", "ps5"], [Ck])
                        H["Cb"] = H["Cbr"].next()
                        P.cp("act", H["Cb"][0][:], Cst[:], [Ck], [H["Cb"][1]])

    def ssd(self, l):
        P, S, NT, A, Sc, c = self.P, self.S, self.NT, self.A, self.Sc, self.c
        ps, pt = self.ps, self.pt
        with P.phase() as st:
            cw = P.sbuf("sd_cw", [128, 12, 4], F32, st)
            cb = P.sbuf("sd_cb", [128, 12], F32, st)
            for j in range(4):
                P.dma("sp", cw[:, :, j], A["ssm_conv_w"][l, j].rearrange("(c p) -> p c", p=128), w=["cw"], allow_slow_non_contiguous=True)
            P.dma("sp", cb[:], A["ssm_conv_b"][l].rearrange("(c p) -> p c", p=128), w=["cb"], allow_slow_non_contiguous=True)
            xr = Ring(P, st, "sd_x", 2, [128, S + 3], F32)
            ar = Ring(P, st, "sd_a", 2, [128, S], F32)
            for ch in range(12):
                x, xk = xr.next()
                a, ak = ar.next()
                P.memset("pool", x[:, 0:3], 0.0, [xk])
                P.dma("sp", x[:, 3:S + 3], Sc["ufm32"][O_XBC + ch * 128:O_XBC + (ch + 1) * 128, :], w=[xk])
                P.ts("dve", a[:], x[:, 3:S + 3], cw[:, ch, 3:4], None, ALU.mult, None, [xk, "cw"], [ak])
                for j in range(3):
                    P.stt("dve", a[:], x[:, j:S + j], cw[:, ch, j:j + 1], a[:], ALU.mult, ALU.add, [xk, "cw", ak], [ak])
                P.act(a[:], a[:], AF.Silu, [ak, "cb"], [ak], bias=cb[:, ch:ch + 1])
                P.dma("sp", Sc["xcT"][ch * 128:(ch + 1) * 128, :], a[:], r=[ak])
        with P.phase() as st:
            dtb = self.bcast_row(st, "sd_dtb", A["ssm_dt_bias"][l], 16)
            alog = self.bcast_row(st, "sd_alog", A["ssm_a_log"][l], 16)
            dsk = self.bcast_row(st, "sd_dsk", A["ssm_d"][l], 16)
            nw = self.bcast_row(st, "sd_nw", A["ssm_norm"][l], 1024)
            P.act(alog[:], alog[:], AF.Exp, ["sd_alog"], ["sd_alog"])
            dt = P.sbuf("sd_dt", [128, NT, 16], F32, st)
            P.dma("sp", dt[:], Sc["utm32"][:, O_DT:O_DT + 16].rearrange("(t p) c -> p t c", p=128), w=["dt"])
            P.tt("dve", dt[:], dt[:], dtb[:].unsqueeze(1).to_broadcast([128, NT, 16]), ALU.add, ["dt", "sd_dtb"], ["dt"])
            P.act(dt[:], dt[:], AF.Exp, ["dt"], ["dt"])
            P.act(dt[:], dt[:], AF.Ln, ["dt"], ["dt"], bias=1.0)
            av = P.sbuf("sd_av", [128, NT, 16], F32, st)
            P.stt("dve", av[:], dt[:], -1.0, alog[:].unsqueeze(1).to_broadcast([128, NT, 16]), ALU.mult, ALU.mult,
                  ["dt", "sd_alog"], ["av"])
            acs = P.sbuf("sd_acs", [128, NT, 16], F32, st)
            nacs = P.sbuf("sd_nacs", [128, NT, 16], F32, st)
            eat = P.sbuf("sd_eat", [128, NT, 16], F32, st)
            ea = P.sbuf("sd_ea", [128, NT, 16], F32, st)
            dsc = P.sbuf("sd_dsc", [128, NT, 16], F32, st)
            for t in range(NT):
                P.mm(ps[0][:, 0:16], c["tri"][:], av[:, t, :], True, True, ["av", "const"], ["ps0"])
                P.mm(ps[0][:, 16:32], c["ones"][:], av[:, t, :], True, True, ["av", "const"], ["ps0"])
                P.cp("dve", acs[:, t, :], ps[0][:, 0:16], ["ps0"], ["acs"])
                P.cp("dve", eat[:, t, :], ps[0][:, 16:32], ["ps0"], ["eat"])
            P.ts("dve", nacs[:], acs[:], -1.0, None, ALU.mult, None, ["acs"], ["nacs"])
            P.tt("dve", dsc[:], eat[:], acs[:], ALU.subtract, ["eat", "acs"], ["dsc"])
            P.act(dsc[:], dsc[:], AF.Exp, ["dsc"], ["dsc"])
            P.tt("dve", dsc[:], dsc[:], dt[:], ALU.mult, ["dsc", "dt"], ["dsc"])
            P.act(ea[:], acs[:], AF.Exp, ["acs"], ["ea"])
            P.act(eat[:], eat[:], AF.Exp, ["eat"], ["eat"])

            xcr = Ring(P, st, "sd_xc", 2, [128, 12, 128], F32)
            xcbr = Ring(P, st, "sd_xcb", 2, [128, 12, 128], BF16)
            xtr = Ring(P, st, "sd_xt", 2, [128, 1280], BF16)
            xsr = Ring(P, st, "sd_xs", 2, [128, 1024], BF16)
            xdr = Ring(P, st, "sd_xd", 2, [128, 1024], BF16)
            cbr = Ring(P, st, "sd_cbT", 2, [128, 2, 128], F32)
            atr = Ring(P, st, "sd_at", 3, [128, 128], F32)
            der = Ring(P, st, "sd_de", 3, [128, 128], F32)
            mtr = Ring(P, st, "sd_mt", 3, [128, 128], BF16)
            zr = Ring(P, st, "sd_z", 2, [128, 1024], F32)
            yr = Ring(P, st, "sd_y", 2, [128, 1024], F32)
            tr_ = Ring(P, st, "sd_t", 2, [128, 1024], F32)
            ybr = Ring(P, st, "sd_yb", 2, [128, 1024], BF16)
            smr = Ring(P, st, "sd_sm", 2, [128, 4], F32)
            junk = P.sbuf("sd_junk", [128, 512], F32, st)
            state = P.sbuf("sd_state", [128, 2, 512], F32, st)
            stbr = Ring(P, st, "sd_stb", 2, [128, 2, 512], BF16)
            P.memset("dve", state[:], 0.0, ["state"])
            stb, stbk = stbr.next()
            P.memset("pool", stb[:], 0.0, [stbk])
            for t in range(NT):
                sl = slice(t * 128, (t + 1) * 128)
                xc, xck = xcr.next()
                xcb, xcbk = xcbr.next()
                xt, xtk = xtr.next()
                xs, xsk = xsr.next()
                xd, xdk = xdr.next()
                cbT, cbTk = cbr.next()
                z, zk = zr.next()
                y, yk = yr.next()
                tm_, tmk = tr_.next()
                yb, ybk = ybr.next()
                sm, smk = smr.next()
                P.dma("sp", xc[:], Sc["xcT"][:, sl].rearrange("(c p) s -> p c s", p=128), w=[xck])
                P.dma("sp", z[:], Sc["utm32"][sl, O_Z:O_Z + 1024], w=[zk])
                P.cp("pool", xcb[:], xc[:], [xck], [xcbk])
                for q in range(3):
                    n4 = 4 if q < 2 else 2
                    pb, pbk = pt[q % 2], ("pt0", "pt1")[q % 2]
                    pv = pb[:, 0:512].rearrange("p (a b) -> p a b", b=128)
                    for j in range(n4):
                        P.tr(pv[:, j, :], xcb[:, q * 4 + j, :], c["ident"][:], [xcbk, "const"], [pbk])
                    P.cp("dve" if q % 2 == 0 else "act", xt[:, q * 512:q * 512 + n4 * 128], pb[:, 0:n4 * 128], [pbk], [xtk])
                x3 = xt[:, 0:1024].rearrange("p (h d) -> p h d", d=64)
                P.tt("dve", xs[:].rearrange("p (h d) -> p h d", d=64), x3, dt[:, t, :].unsqueeze(2).to_broadcast([128, 16, 64]),
                     ALU.mult, [xtk, "dt"], [xsk])
                P.tt("pool", xd[:].rearrange("p (h d) -> p h d", d=64), x3, dsc[:, t, :].unsqueeze(2).to_broadcast([128, 16, 64]),
                     ALU.mult, [xtk, "dsc"], [xdk])
                for g in range(2):
                    P.mm(ps[1][:, g * 128:(g + 1) * 128], xcb[:, 8 + g, :], xcb[:, 10 + g, :], True, True, [xcbk], ["ps1"])
                P.cp("act", cbT[:].rearrange("p a b -> p (a b)"), ps[1][:, 0:256], ["ps1"], [cbTk])
                for g in range(2):
                    P.mm(ps[2 + g][:, 0:512], xcb[:, 10 + g, :], stb[:, g, :], True, True, [xcbk, stbk], [f"ps{2 + g}"])
                stg1 = {}

                def stage1(hd):
                    g = hd // 8
                    at, atk = atr.next()
                    de, dek = der.next()
                    mt, mtk = mtr.next()
                    bb, bbk = ps[hd % 2], f"ps{hd % 2}"
                    P.ts("pool", at[:], c["tri"][:], av[:, t, hd:hd + 1], None, ALU.mult, None, ["av", "const"], [atk])
                    P.mm(bb[:, 0:128], c["ones"][:], at[:], True, False, [atk, "const"], [bbk])
                    P.mm(bb[:, 0:128], c["identf"][:], c["mneg"][:], False, True, ["const"], [bbk])
                    P.act(de[:], bb[:, 0:128], AF.Exp, [bbk, "nacs"], [dek], bias=nacs[:, t, hd:hd + 1])
                    P.tt("dve", mt[:], de[:], cbT[:, g, :], ALU.mult, [dek, cbTk], [mtk])
                    stg1[hd] = (mt, mtk)

                stage1(0)
                for hd in range(16):
                    g = hd // 8
                    if hd + 1 < 16:
                        stage1(hd + 1)
                    mt, mtk = stg1[hd]
                    P.mm(ps[4 + g][:, (hd % 8) * 64:(hd % 8 + 1) * 64], mt[:], xs[:, hd * 64:(hd + 1) * 64], True, True,
                         [mtk, xsk], [f"ps{4 + g}"])
                for g in range(2):
                    P.tt("dve", tm_[:, g * 512:(g + 1) * 512].rearrange("p (h d) -> p h d", d=64),
                         ps[2 + g][:, 0:512].rearrange("p (h d) -> p h d", d=64),
                         ea[:, t, g * 8:(g + 1) * 8].unsqueeze(2).to_broadcast([128, 8, 64]), ALU.mult,
                         [f"ps{2 + g}", "ea"], [tmk])
                    P.tt("dve", y[:, g * 512:(g + 1) * 512], tm_[:, g * 512:(g + 1) * 512], ps[4 + g][:, 0:512], ALU.add,
                         [tmk, f"ps{4 + g}"], [yk])
                P.tt("pool", tm_[:].rearrange("p (h d) -> p h d", d=64), x3, dsk[:].unsqueeze(2).to_broadcast([128, 16, 64]),
                     ALU.mult, [xtk, "sd_dsk", yk], [tmk])
                P.tt("pool", y[:], y[:], tm_[:], ALU.add, [tmk, yk], [yk])
                P.act(z[:], z[:], AF.Silu, [zk], [zk])
                P.tt("dve", y[:], y[:], z[:], ALU.mult, [yk, zk], [yk])
                for g in range(2):
                    P.act(junk[:], y[:, g * 512:(g + 1) * 512], AF.Square, [yk], ["sd_junk", smk], accum_out=sm[:, g:g + 1])
                P.ts("dve", sm[:, 2:4], sm[:, 0:2], 1.0 / 512, 1e-6, ALU.mult, ALU.add, [smk], [smk])
                P.act(sm[:, 2:4], sm[:, 2:4], AF.Sqrt, [smk], [smk])
                P.add("dve", lambda e, sm=sm: e.reciprocal(out=sm[:, 2:4], in_=sm[:, 2:4]), [smk], [smk])
                for g in range(2):
                    P.stt("dve", yb[:, g * 512:(g + 1) * 512], y[:, g * 512:(g + 1) * 512], sm[:, 2 + g:3 + g],
                          nw[:, g * 512:(g + 1) * 512], ALU.mult, ALU.mult, [yk, smk, "sd_nw"], [ybk])
                P.dma("sp", Sc["yb"][sl, :], yb[:], r=[ybk])
                if t < NT - 1:
                    for g in range(2):
                        P.mm(ps[2 + g][:, 0:512], xt[:, 1024 + g * 128:1024 + (g + 1) * 128], xd[:, g * 512:(g + 1) * 512],
                             True, True, [xtk, xdk], [f"ps{2 + g}"])
                        P.tt("dve", state[:, g, :].rearrange("p (h d) -> p h d", d=64),
                             state[:, g, :].rearrange("p (h d) -> p h d", d=64),
                             eat[:, t, g * 8:(g + 1) * 8].unsqueeze(2).to_broadcast([128, 8, 64]), ALU.mult,
                             ["state", "eat"], ["state"])
                        P.tt("dve", state[:, g, :], state[:, g, :], ps[2 + g][:, 0:512], ALU.add, ["state", f"ps{2 + g}"], ["state"])
                    stb, stbk = stbr.next()
                    P.cp("act", stb[:], state[:], ["state"], [stbk])

    def mla(self, l):
        P, S, NT, A, Sc, c = self.P, self.S, self.NT, self.A, self.Sc, self.c
        ps, GW, NG = self.ps, self.GW, self.NG
        scale = 192.0 ** -0.5
        with P.phase() as st:
            cqT = P.sbuf("la_cqT", [128, 4, S], BF16, st)
            ckT = P.sbuf("la_ckT", [128, 2, S], BF16, st)
            with P.phase() as st2:
                self.norm_T(st2, Sc["utm32"][:, O_CQ:O_CQ + 512], A["mla_q_norm"][l], cqT, "cqT", S, 512)
            with P.phase() as st2:
                self.norm_T(st2, Sc["utm32"][:, O_CKV:O_CKV + 256], A["mla_kv_norm"][l], ckT, "ckT", S, 256)
            cos2 = P.sbuf("la_cos", [64, S], F32, st)
            sin2 = P.sbuf("la_sin", [64, S], F32, st)
            P.dma("sp", cos2[:], A["c_cos2"], w=["cos"])
            P.dma("sp", sin2[:], A["c_sin2"], w=["sin"])
            krT = P.sbuf("la_krT", [64, S], BF16, st)
            k1 = P.sbuf("la_k1", [64, S], F32, st)
            k2 = P.sbuf("la_k2", [64, S], F32, st)
            P.dma("sp", k1[:], Sc["ufm32"][O_KR:O_KR + 64, :], w=["k1"])
            P.dma("sp", k2[:], Sc["ufm32"][IN_TOTAL:IN_TOTAL + 64, :], w=["k2"])
            P.tt("dve", k1[:], k1[:], cos2[:], ALU.mult, ["k1", "cos"], ["k1"])
            P.tt("pool", k2[:], k2[:], sin2[:], ALU.mult, ["k2", "sin"], ["k2"])
            P.tt("dve", krT[:], k1[:], k2[:], ALU.add, ["k1", "k2"], ["krT"])
            Wq, Wkv = A["mla_w_uq"][l], A["mla_w_ukv"][l]
            wqr = Ring(P, st, "la_wq", 2, [128, 4, 256], BF16)
            wkr = Ring(P, st, "la_wk", 2, [128, 2, 256], BF16)
            qnr = Ring(P, st, "la_qn", 2, [128, S], BF16)
            qrr = Ring(P, st, "la_qr", 2, [64, S], BF16)
            knr = Ring(P, st, "la_kn", 2, [128, S], BF16)
            var = Ring(P, st, "la_va", 2, [128, NT, 129], BF16)
            t1r = Ring(P, st, "la_t1", 2, [64, GW], F32)
            t2r = Ring(P, st, "la_t2", 2, [64, GW], F32)
            ptr_ = Ring(P, st, "la_pt", 4, [128, GW], BF16)
            yr = Ring(P, st, "la_y", 4, [128, 128], BF16)
            smr = Ring(P, st, "la_sm", 4, [128, 2], F32)
            ycur = {}
            for hd in range(8):
                wq, wqk = wqr.next()
                wk, wkk = wkr.next()
                qn, qnk = qnr.next()
                qr, qrk = qrr.next()
                kn, knk = knr.next()
                va, vak = var.next()
                q0 = hd * 192
                vw = lambda a, b: Wq[:, a:b].rearrange("(c p) n -> p c n", p=128)
                P.dma("pool", wq[:, :, 0:192], vw(q0, q0 + 192), w=[wqk])
                P.dma("pool", wq[:, :, 192:224], vw(q0 + 160, q0 + 192), w=[wqk])
                P.dma("pool", wq[:, :, 224:256], vw(q0 + 128, q0 + 160), w=[wqk])
                P.dma("pool", wk[:], Wkv[:, hd * 256:(hd + 1) * 256].rearrange("(c p) n -> p c n", p=128), w=[wkk])
                cqr = [("cqT", t) for t in range(NT)]
                ckr = [("ckT", t) for t in range(NT)]
                for g in range(NG):
                    gs = slice(g * GW, (g + 1) * GW)
                    for k in range(4):
                        P.mm(ps[0][:, 0:GW], wq[:, k, 0:128], cqT[:, k, gs], k == 0, k == 3, [wqk] + cqr, ["ps0"])
                    P.cp("act", qn[:, gs], ps[0][:, 0:GW], ["ps0"], [qnk])
                    for k in range(4):
                        P.mm(ps[1][0:64, 0:GW], wq[:, k, 128:192], cqT[:, k, gs], k == 0, k == 3, [wqk] + cqr, ["ps1"])
                    for k in range(4):
                        P.mm(ps[2][0:64, 0:GW], wq[:, k, 192:256], cqT[:, k, gs], k == 0, k == 3, [wqk] + cqr, ["ps2"])
                    t1, t1k = t1r.next()
                    t2, t2k = t2r.next()
                    P.tt("dve", t1[:], ps[1][0:64, 0:GW], cos2[:, gs], ALU.mult, ["ps1", "cos"], [t1k])
                    P.tt("dve", t2[:], ps[2][0:64, 0:GW], sin2[:, gs], ALU.mult, ["ps2", "sin"], [t2k])
                    P.tt("pool", qr[:, gs], t1[:], t2[:], ALU.add, [t1k, t2k], [qrk])
                    for k in range(2):
                        P.mm(ps[3][:, 0:GW], wk[:, k, 0:128], ckT[:, k, gs], k == 0, k == 1, [wkk] + ckr, ["ps3"])
                    P.cp("act", kn[:, gs], ps[3][:, 0:GW], ["ps3"], [knk])
                for t in range(NT):
                    for k in range(2):
                        P.mm(ps[4][:, 0:128], ckT[:, k, t * 128:(t + 1) * 128], wk[:, k, 128:256], k == 0, k == 1,
                             [wkk, ("ckT", t)], ["ps4"])
                    P.cp("dve", va[:, t, 0:128], ps[4][:, 0:128], ["ps4"], [vak])
                P.memset("pool", va[:, :, 128:129], 1.0, [vak])
                TPG = GW // 128
                its = [(g, j) for g in range(NG) for j in range((g + 1) * TPG)]

                def emit_S(i):
                    g, j = its[i]
                    gs0 = g * GW
                    c0 = max(0, j - g * TPG) * 128
                    sb, sbk = ps[i % 2], f"ps{i % 2}"
                    P.mm(sb[:, c0:GW], kn[:, j * 128:(j + 1) * 128], qn[:, gs0 + c0:gs0 + GW], True, False, [knk, qnk], [sbk])
                    P.mm(sb[:, c0:GW], krT[:, j * 128:(j + 1) * 128], qr[:, gs0 + c0:gs0 + GW], False, True, ["krT", qrk], [sbk])

                emit_S(0)
                for i, (g, j) in enumerate(its):
                    if i + 1 < len(its):
                        emit_S(i + 1)
                    r0 = max(0, j - g * TPG)
                    c0 = r0 * 128
                    sb, sbk = ps[i % 2], f"ps{i % 2}"
                    pt, ptk = ptr_.next()
                    P.act(pt[:, c0:GW], sb[:, c0:GW], AF.Exp, [sbk], [ptk], scale=scale)
                    if j >= g * TPG:
                        P.tt("dve", pt[:, c0:c0 + 128], pt[:, c0:c0 + 128], c["trib"][:], ALU.mult, [ptk, "const"], [ptk])
                    for r in range(r0, TPG):
                        qi = g * TPG + r
                        P.mm(ps[2 + r][:, 0:129], pt[:, r * 128:(r + 1) * 128], va[:, j, :], j == 0, j == qi,
                             [ptk, vak], [f"ps{2 + r}"])
                        if j == qi:
                            y, yk = yr.next()
                            sm, smk = smr.next()
                            P.add("dve", lambda e, sm=sm, r=r: e.reciprocal(out=sm[:, 0:1], in_=ps[2 + r][:, 128:129]),
                                  [f"ps{2 + r}"], [smk])
                            P.ts("dve", y[:], ps[2 + r][:, 0:128], sm[:, 0:1], None, ALU.mult, None,
                                 [f"ps{2 + r}", smk], [yk])
                            P.dma("sp", Sc["yc"][qi * 128:(qi + 1) * 128, hd * 128:(hd + 1) * 128], y[:], r=[yk])

    def swa(self, l):
        P, S, NT, A, Sc, c = self.P, self.S, self.NT, self.A, self.Sc, self.c
        ps = self.ps
        scale = 64.0 ** -0.5
        with P.phase() as st:
            sk_ = self.bcast_row(st, "sw_sink", A["swa_sinks"][l], 16)
            P.act(sk_[:], sk_[:], AF.Exp, ["sw_sink"], ["sw_sink"])
            qr = Ring(P, st, "sw_q", 2, [64, 4, S], BF16)
            kr = Ring(P, st, "sw_k", 2, [64, S], BF16)
            var = Ring(P, st, "sw_va", 2, [128, NT, 65], BF16)
            ptr_ = Ring(P, st, "sw_pt", 4, [128, 512], BF16)
            yr = Ring(P, st, "sw_y", 3, [128, 256], BF16)
            smr = Ring(P, st, "sw_sm", 3, [128, 4], F32)
            for g in range(4):
                q, qk = qr.next()
                k, kk = kr.next()
                va, vak = var.next()
                P.dma("sp", q[:], Sc["ufm16"][O_SWQ + g * 256:O_SWQ + (g + 1) * 256, :].rearrange("(r p) s -> p r s", p=64), w=[qk])
                P.dma("sp", k[:], Sc["ufm16"][O_SWK + g * 64:O_SWK + (g + 1) * 64, :], w=[kk])
                P.dma("sp", va[:, :, 0:64], Sc["utm16"][:, O_SWV + g * 64:O_SWV + (g + 1) * 64].rearrange("(t p) c -> p t c", p=128), w=[vak])
                P.memset("pool", va[:, :, 64:65], 1.0, [vak])
                for n in range(NT):
                    sl = slice(n * 128, (n + 1) * 128)
                    pc, pck = ptr_.next()
                    P.mm(ps[0][:, 0:512].rearrange("p (r s) -> p r s", r=4), k[:, sl], q[:, :, sl], True, True, [kk, qk], ["ps0"])
                    P.act(pc[:], ps[0][:, 0:512], AF.Exp, ["ps0"], [pck], scale=scale)
                    P.tt("pool", pc[:].rearrange("p (r s) -> p r s", r=4), pc[:].rearrange("p (r s) -> p r s", r=4),
                         c["trib"][:].unsqueeze(1).to_broadcast([128, 4, 128]), ALU.mult, [pck, "const"], [pck])
                    if n > 0:
                        pp, ppk = ptr_.next()
                        psl = slice((n - 1) * 128, n * 128)
                        P.mm(ps[1][:, 0:512].rearrange("p (r s) -> p r s", r=4), k[:, psl], q[:, :, sl], True, True, [kk, qk], ["ps1"])
                        P.act(pp[:], ps[1][:, 0:512], AF.Exp, ["ps1"], [ppk], scale=scale)
                        P.tt("pool", pp[:].rearrange("p (r s) -> p r s", r=4), pp[:].rearrange("p (r s) -> p r s", r=4),
                             c["lowb"][:].unsqueeze(1).to_broadcast([128, 4, 128]), ALU.mult, [ppk, "const"], [ppk])
                    ob = ps[2 + (n % 2)]
                    obk = f"ps{2 + (n % 2)}"
                    for r in range(4):
                        if n > 0:
                            P.mm(ob[:, r * 65:(r + 1) * 65], pp[:, r * 128:(r + 1) * 128], va[:, n - 1, :], True, False, [ppk, vak], [obk])
                        P.mm(ob[:, r * 65:(r + 1) * 65], pc[:, r * 128:(r + 1) * 128], va[:, n, :], n == 0, True, [pck, vak], [obk])
                    y, yk = yr.next()
                    sm, smk = smr.next()
                    o3 = ob[:, 0:260].rearrange("p (r d) -> p r d", d=65)
                    P.tt("dve", sm[:].unsqueeze(2), o3[:, :, 64:65], sk_[:, g * 4:(g + 1) * 4].unsqueeze(2), ALU.add, [obk, "sw_sink"], [smk])
                    P.add("dve", lambda e, sm=sm: e.reciprocal(out=sm[:], in_=sm[:]), [smk], [smk])
                    P.tt("dve", y[:].rearrange("p (r d) -> p r d", d=64), o3[:, :, 0:64], sm[:].unsqueeze(2).to_broadcast([128, 4, 64]),
                         ALU.mult, [obk, smk], [yk])
                    P.dma("sp", Sc["yd"][sl, g * 256:(g + 1) * 256], y[:], r=[yk])

    def merge(self, l):
        P, S, NT, A, Sc, c = self.P, self.S, self.NT, self.A, self.Sc, self.c
        ps, pt, GW, NG = self.ps, self.pt, self.GW, self.NG
        TPG = GW // 128
        with P.phase() as st:
            yTr = [P.sbuf(f"mg_yT{n}", [128, 8, GW], BF16, st) for n in range(4)]
            ytr = Ring(P, st, "mg_yt", 3, [128, 1024], BF16)
            wbr = Ring(P, st, "mg_wb", 3, [128, 8, 512], BF16)
            wor = Ring(P, st, "mg_wo", 2, [128, 16, 512], BF16)
            gtr = Ring(P, st, "mg_g", 3, [128, GW], BF16)
            tmr = Ring(P, st, "mg_tmp", 2, [128, GW], F32)
            mT = P.sbuf("mg_mT", [128, 16, GW], F32, st)
            mTb = P.sbuf("mg_mTb", [128, 16, GW], BF16, st)
            hr = Ring(P, st, "mg_h", 3, [128, 512], F32)
            ptr = KeyRing(pt, ["pt0", "pt1"])
            for g in range(NG):
                gs = slice(g * GW, (g + 1) * GW)
                for n, nm in enumerate("abcd"):
                    for tt_ in range(TPG):
                        t = g * TPG + tt_
                        yt, ytk = ytr.next()
                        P.dma("sp", yt[:], Sc["y" + nm][t * 128:(t + 1) * 128, :], w=[ytk])
                        for q in range(2):
                            pb, pbk = ptr.next()
                            pv = pb[:, 0:512].rearrange("p (a b) -> p a b", b=128)
                            for j in range(4):
                                P.tr(pv[:, j, :], yt[:, (q * 4 + j) * 128:(q * 4 + j + 1) * 128], c["ident"][:], [ytk, "const"], [pbk])
                            P.cp("dve" if q == 0 else "act", yTr[n][:, q * 4:(q + 1) * 4, tt_ * 128:(tt_ + 1) * 128], pv[:, 0:4, :],
                                 [pbk], [("yT", n)])
                for n in range(4):
                    for dcb in range(4):
                        wb, wbk = wbr.next()
                        P.dma("pool", wb[:], A["w_branch"][l, n][:, dcb * 512:(dcb + 1) * 512].rearrange("(c p) n -> p c n", p=128), w=[wbk])
                        for dc in range(4):
                            dch = dcb * 4 + dc
                            pb, pbk = ps[dc % 4], f"ps{dc % 4}"
                            for k in range(8):
                                P.mm(pb[:, 0:GW], wb[:, k, dc * 128:(dc + 1) * 128], yTr[n][:, k, :], k == 0, k == 7,
                                     [wbk, ("yT", n)], [pbk])
                            gt, gtk = gtr.next()
                            r0 = O_GATE + n * D + dch * 128
                            P.dma("sp", gt[:], Sc["ufm16"][r0:r0 + 128, gs], w=[gtk])
                            if n == 0:
                                P.tt("dve", mT[:, dch, :], pb[:, 0:GW], gt[:], ALU.mult, [pbk, gtk], [("mT", dch)])
                            else:
                                tmp, tmk = tmr.next()
                                P.tt("dve", tmp[:], pb[:, 0:GW], gt[:], ALU.mult, [pbk, gtk], [tmk])
                                P.tt("dve", mT[:, dch, :], mT[:, dch, :], tmp[:], ALU.add, [("mT", dch), tmk], [("mT", dch)])
                for dch in range(16):
                    P.cp("act", mTb[:, dch, :], mT[:, dch, :], [("mT", dch)], [("mTb", dch)])
                mr = [("mTb", d_) for d_ in range(16)]
                for ocb in range(4):
                    wo, wok = wor.next()
                    P.dma("pool", wo[:], A["w_out"][l][:, ocb * 512:(ocb + 1) * 512].rearrange("(c p) n -> p c n", p=128), w=[wok])
                    for tt_ in range(TPG):
                        t = g * TPG + tt_
                        pb, pbk = ps[4 + tt_ % 2], f"ps{4 + tt_ % 2}"
                        ht, hk = hr.next()
                        P.dma("sp", ht[:], Sc["h"][t * 128:(t + 1) * 128, ocb * 512:(ocb + 1) * 512], w=[hk])
                        for k in range(16):
                            P.mm(pb[:, 0:512], mTb[:, k, tt_ * 128:(tt_ + 1) * 128], wo[:, k, :], k == 0, k == 15, [wok] + mr, [pbk])
                        P.tt("dve", ht[:], ht[:], pb[:, 0:512], ALU.add, [hk, pbk], [hk])
                        P.dma("sp", Sc["h"][t * 128:(t + 1) * 128, ocb * 512:(ocb + 1) * 512], ht[:], r=[hk])

    def xattn(self, l):
        P, S, NT, A, Sc, c = self.P, self.S, self.NT, self.A, self.Sc, self.c
        ps, pt, GW, NG = self.ps, self.pt, self.GW, self.NG
        scale = 128.0 ** -0.5
        with P.phase() as st:
            hnT = P.sbuf("xa_hnT", [128, KC, S], BF16, st)
            mnT = P.sbuf("xa_mnT", [128, KC, MEM], BF16, st)
            with P.phase() as st2:
                self.norm_T(st2, Sc["h"], A["norm_cross"][l], hnT, "hnT", S, D)
            with P.phase() as st2:
                self.norm_T(st2, A["mem"], A["norm_mem"][l], mnT, "mnT", MEM, D)
            wkv = P.sbuf("xa_wkv", [128, KC, 1024], BF16, st)
            wq = P.sbuf("xa_wq", [128, KC, 512], BF16, st)
            wo = P.sbuf("xa_wo", [128, 4, D], BF16, st)
            rr = lambda ap: ap.rearrange("(c p) n -> p c n", p=128)
            P.dma("pool", wkv[:, :, 0:512], rr(A["xa_wkv"][l][:, 0:512]), w=["wkv"])
            P.dma("pool", wkv[:, :, 512:1024], rr(A["xa_wkv"][l][:, 512:1024]), w=["wkv"])
            P.dma("pool", wq[:], rr(A["xa_wq"][l]), w=["wq"])
            for q in range(4):
                P.dma("pool", wo[:, :, q * 512:(q + 1) * 512], rr(A["xa_wo"][l][:, q * 512:(q + 1) * 512]), w=["wo"])
            KT = P.sbuf("xa_KT", [128, 4, MEM], BF16, st)
            VA = P.sbuf("xa_VA", [128, 2, 4, 129], BF16, st)
            QT = P.sbuf("xa_QT", [128, 4, S], BF16, st)
            mnr = [("mnT", t) for t in range(MEM // 128)]
            hnr = [("hnT", t) for t in range(NT)]
            for hd in range(4):
                for k in range(KC):
                    P.mm(ps[0][:, 0:MEM], wkv[:, k, hd * 128:(hd + 1) * 128], mnT[:, k, :], k == 0, k == KC - 1, ["wkv"] + mnr, ["ps0"])
                P.cp("act", KT[:, hd, :], ps[0][:, 0:MEM], ["ps0"], ["KT"])
            for mt in range(2):
                for k in range(KC):
                    P.mm(ps[1][:, 0:512], mnT[:, k, mt * 128:(mt + 1) * 128], wkv[:, k, 512:1024], k == 0, k == KC - 1, ["wkv"] + mnr, ["ps1"])
                P.cp("dve", VA[:, mt, :, 0:128], ps[1][:, 0:512].rearrange("p (h d) -> p h d", d=128), ["ps1"], ["VA"])
            P.memset("pool", VA[:, :, :, 128:129], 1.0, ["VA"])
            for hd in range(4):
                for g in range(NG):
                    pb, pbk = ps[2 + g % 2], f"ps{2 + g % 2}"
                    for k in range(KC):
                        P.mm(pb[:, 0:GW], wq[:, k, hd * 128:(hd + 1) * 128], hnT[:, k, g * GW:(g + 1) * GW], k == 0, k == KC - 1,
                             ["wq"] + hnr, [pbk])
                    P.cp("act" if g % 2 == 0 else "dve", QT[:, hd, g * GW:(g + 1) * GW], pb[:, 0:GW], [pbk], ["QT"])
            ptr_ = Ring(P, st, "xa_pt", 4, [128, GW], BF16)
            oTr = Ring(P, st, "xa_oT", 2, [128, 4, 128], BF16)
            smr = Ring(P, st, "xa_sm", 4, [128, 1], F32)
            hr = Ring(P, st, "xa_h", 2, [128, D], F32)
            TPG = GW // 128
            for g in range(NG):
                gs = slice(g * GW, (g + 1) * GW)
                pts = {}
                for hd in range(4):
                    for mt in range(2):
                        pb, pbk = ps[mt], f"ps{mt}"
                        p_, pk_ = ptr_.next()
                        P.mm(pb[:, 0:GW], KT[:, hd, mt * 128:(mt + 1) * 128], QT[:, hd, gs], True, True, ["KT", "QT"], [pbk])
                        P.act(p_[:], pb[:, 0:GW], AF.Exp, [pbk], [pk_], scale=scale)
                        pts[(hd % 2, mt)] = (p_, pk_)
                    for tt_ in range(TPG):
                        t = g * TPG + tt_
                        ob, obk = ps[2 + tt_ % 2], f"ps{2 + tt_ % 2}"
                        for mt in range(2):
                            p_, pk_ = pts[(hd % 2, mt)]
                            P.mm(ob[:, 0:129], p_[:, tt_ * 128:(tt_ + 1) * 128], VA[:, mt, hd, :], mt == 0, mt == 1, [pk_, "VA"], [obk])
                        sm, smk = smr.next()
                        o_, ok_ = self._xa_o(st, t)
                        P.add("dve", lambda e, sm=sm, ob=ob: e.reciprocal(out=sm[:, 0:1], in_=ob[:, 128:129]), [obk], [smk])
                        P.ts("dve", o_[:, hd * 128:(hd + 1) * 128], ob[:, 0:128], sm[:, 0:1], None, ALU.mult, None, [obk, smk], [(ok_, hd)])
                for tt_ in range(TPG):
                    t = g * TPG + tt_
                    o_, ok_ = self._xa_o(st, t)
                    oT, oTk = oTr.next()
                    pb, pbk = pt[tt_ % 2], f"pt{tt_ % 2}"
                    pv = pb[:, 0:512].rearrange("p (a b) -> p a b", b=128)
                    for j in range(4):
                        P.tr(pv[:, j, :], o_[:, j * 128:(j + 1) * 128], c["ident"][:], [(ok_, j), "const"], [pbk])
                    P.cp("act", oT[:], pv[:, 0:4, :], [pbk], [oTk])
                    ht, hk = hr.next()
                    P.dma("sp", ht[:], Sc["h"][t * 128:(t + 1) * 128, :], w=[hk])
                    for ocb in range(4):
                        ob, obk = ps[4 + ocb % 2], f"ps{4 + ocb % 2}"
                        for k in range(4):
                            P.mm(ob[:, 0:512], oT[:, k, :], wo[:, k, ocb * 512:(ocb + 1) * 512], k == 0, k == 3, [oTk, "wo"], [obk])
                        P.tt("dve", ht[:, ocb * 512:(ocb + 1) * 512], ht[:, ocb * 512:(ocb + 1) * 512], ob[:, 0:512], ALU.add, [hk, obk], [hk])
                    P.dma("sp", Sc["h"][t * 128:(t + 1) * 128, :], ht[:], r=[hk])

    def _xa_o(self, st, t):
        if not hasattr(self, "_xao") or self._xao_st is not st:
            self._xao = {}
            self._xao_st = st
        if t not in self._xao:
            self.P.uid += 1
            self._xao[t] = (self.P.sbuf("xa_ot", [128, 512], BF16, st), f"xa_ot#{self.P.uid}")
        return self._xao[t]

    def ffn_layer(self, l):
        P, S, NT, A, Sc, c = self.P, self.S, self.NT, self.A, self.Sc, self.c
        ps, pt, GW, NG = self.ps, self.pt, self.GW, self.NG
        TPG = GW // 128
        moe = (l % 2 == 1)
        FC = FFN // 128
        with P.phase() as st:
            hacc = P.sbuf("ff_hacc", [128, TPG, D], F32, st)
            hnT = P.sbuf("ff_hnT", [128, KC, GW], BF16, st)
            actT = P.sbuf("ff_actT", [128, FC, GW], BF16, st)
            w13r = Ring(P, st, "ff_w13", 2, [128, KC, 2, 256], BF16)
            w2r = Ring(P, st, "ff_w2", 2, [128, 8, 512], BF16)
            sir = Ring(P, st, "ff_si", 2, [128, GW], F32)
            gcol = P.sbuf("ff_gcol", [128, KC], F32, st)
            P.dma("sp", gcol[:], A["norm_ffn"][l].rearrange("(c p) -> p c", p=128), w=["gcol"], allow_slow_non_contiguous=True)
            hbr = Ring(P, st, "ff_hb", 2, [128, D], BF16)
            junk = P.sbuf("ff_junk", [128, D], BF16, st)
            ssr = Ring(P, st, "ff_ss", 2, [128, 2], F32)
            if moe:
                hn32 = Ring(P, st, "ff_hn32", 1, [128, D], F32)
                hnT32 = P.sbuf("ff_hnT32", [128, KC, 128], F32, st)
                rt = P.sbuf("ff_rt", [128, KC, NEXP], F32, st)
                P.dma("sp", rt[:], A["moe_router"][0].rearrange("(c p) e -> p c e", p=128), w=["rt"])
                gates = P.sbuf("ff_gates", [128, TPG, NEXP], F32, st)
                lgr = Ring(P, st, "ff_lg", 2, [128, 24], F32)
            for g in range(NG):
                for tt_ in range(TPG):
                    t = g * TPG + tt_
                    hb, hbk = hbr.next()
                    ss, sk = ssr.next()
                    P.dma("sp", hacc[:, tt_, :], Sc["h"][t * 128:(t + 1) * 128, :], w=[("hacc", tt_)])
                    P.act(junk[:], hacc[:, tt_, :], AF.Square, [("hacc", tt_)], ["ff_junk", sk], accum_out=ss[:, 0:1])
                    P.ts("dve", ss[:, 1:2], ss[:, 0:1], 1.0 / D, 1e-6, ALU.mult, ALU.add, [sk], [sk])
                    P.act(ss[:, 1:2], ss[:, 1:2], AF.Sqrt, [sk], [sk])
                    P.add("dve", lambda e, ss=ss: e.reciprocal(out=ss[:, 1:2], in_=ss[:, 1:2]), [sk], [sk])
                    P.ts("pool", hb[:], hacc[:, tt_, :], ss[:, 1:2], None, ALU.mult, None, [("hacc", tt_), sk], [hbk])
                    for c4 in range(0, KC, 4):
                        pb, pbk = pt[(c4 // 4) % 2], f"pt{(c4 // 4) % 2}"
                        pv = pb[:, 0:512].rearrange("p (a b) -> p a b", b=128)
                        for j in range(4):
                            P.tr(pv[:, j, :], hb[:, (c4 + j) * 128:(c4 + j + 1) * 128], c["ident"][:], [hbk, "const"], [pbk])
                        P.tt("dve", hnT[:, c4:c4 + 4, tt_ * 128:(tt_ + 1) * 128], pv[:, 0:4, :],
                             gcol[:, c4:c4 + 4].unsqueeze(2).to_broadcast([128, 4, 128]), ALU.mult, [pbk, "gcol"], ["hnT"])
                    if moe:
                        h32, h32k = hn32.next()
                        lg, lgk = lgr.next()
                        P.ts("pool", h32[:], hacc[:, tt_, :], ss[:, 1:2], None, ALU.mult, None, [("hacc", tt_), sk], [h32k])
                        for c4 in range(0, KC, 4):
                            pb, pbk = ps[(c4 // 4) % 2], f"ps{(c4 // 4) % 2}"
                            pv = pb[:, 0:512].rearrange("p (a b) -> p a b", b=128)
                            for j in range(4):
                                P.tr(pv[:, j, :], h32[:, (c4 + j) * 128:(c4 + j + 1) * 128], c["identf"][:], [h32k, "const"], [pbk])
                            P.tt("dve", hnT32[:, c4:c4 + 4, :], pv[:, 0:4, :],
                                 gcol[:, c4:c4 + 4].unsqueeze(2).to_broadcast([128, 4, 128]), ALU.mult, [pbk, "gcol"], ["hnT32"])
                        for k in range(KC):
                            P.mm(ps[2][:, 0:NEXP], hnT32[:, k, :], rt[:, k, :], k == 0, k == KC - 1, ["hnT32", "rt"], ["ps2"])
                        P.cp("dve", lg[:, 0:8], ps[2][:, 0:NEXP], ["ps2"], [lgk])
                        P.add("dve", lambda e, lg=lg: e.max(out=lg[:, 8:16], in_=lg[:, 0:8]), [lgk], [lgk])
                        P.ts("dve", lg[:, 16:24], lg[:, 0:8], lg[:, 8:9], None, ALU.subtract, None, [lgk], [lgk])
                        P.act(lg[:, 16:24], lg[:, 16:24], AF.Exp, [lgk], [lgk])
                        P.ts("dve", lg[:, 0:8], lg[:, 0:8], lg[:, 9:10], None, ALU.is_ge, None, [lgk], [lgk])
                        P.tt("dve", lg[:, 16:24], lg[:, 16:24], lg[:, 0:8], ALU.mult, [lgk], [lgk])
                        P.add("dve", lambda e, lg=lg: e.reduce_sum(out=lg[:, 10:11], in_=lg[:, 16:24], axis=AX.X), [lgk], [lgk])
                        P.add("dve", lambda e, lg=lg: e.reciprocal(out=lg[:, 10:11], in_=lg[:, 10:11]), [lgk], [lgk])
                        P.ts("dve", gates[:, tt_, :], lg[:, 16:24], lg[:, 10:11], None, ALU.mult, None, [lgk], [("gates", tt_)])
                for e_ in range(NEXP if moe else 1):
                    W13 = A["moe_w13"][0, e_] if moe else A["ffn_w13"][0]
                    W2 = A["moe_w2"][0, e_] if moe else A["ffn_w2"][0]
                    for fb in range(0, FC, 2):
                        w13, w13k = w13r.next()
                        P.dma("pool", w13[:, :, 0, :], W13[:, fb * 128:(fb + 2) * 128].rearrange("(c p) n -> p c n", p=128), w=[w13k])
                        P.dma("pool", w13[:, :, 1, :], W13[:, FFN + fb * 128:FFN + (fb + 2) * 128].rearrange("(c p) n -> p c n", p=128), w=[w13k])
                        for f2 in range(2):
                            f = fb + f2
                            for k in range(KC):
                                P.mm(ps[0][:, 0:GW], w13[:, k, 0, f2 * 128:(f2 + 1) * 128], hnT[:, k, :], k == 0, k == KC - 1, [w13k, "hnT"], ["ps0"])
                            for k in range(KC):
                                P.mm(ps[1][:, 0:GW], w13[:, k, 1, f2 * 128:(f2 + 1) * 128], hnT[:, k, :], k == 0, k == KC - 1, [w13k, "hnT"], ["ps1"])
                            si, sik = sir.next()
                            P.act(si[:], ps[0][:, 0:GW], AF.Silu, ["ps0"], [sik])
                            P.tt("dve", actT[:, f, :], si[:], ps[1][:, 0:GW], ALU.mult, [sik, "ps1"], [("actT", f)])
                    ar = [("actT", f) for f in range(FC)]
                    for dcb in range(4):
                        for f8 in range(0, FC, 8):
                            w2, w2k = w2r.next()
                            P.dma("pool", w2[:], W2[f8 * 128:(f8 + 8) * 128, dcb * 512:(dcb + 1) * 512].rearrange("(c p) n -> p c n", p=128), w=[w2k])
                            for f in range(8):
                                for tt_ in range(TPG):
                                    P.mm(ps[2 + tt_][:, 0:512], actT[:, f8 + f, tt_ * 128:(tt_ + 1) * 128], w2[:, f, :],
                                         f8 + f == 0, f8 + f == FC - 1, [w2k] + ar, [f"ps{2 + tt_}"])
                        for tt_ in range(TPG):
                            hs = hacc[:, tt_, dcb * 512:(dcb + 1) * 512]
                            if moe:
                                P.stt("dve", hs, ps[2 + tt_][:, 0:512], gates[:, tt_, e_:e_ + 1], hs, ALU.mult, ALU.add,
                                      [f"ps{2 + tt_}", ("gates", tt_), ("hacc", tt_)], [("hacc", tt_)])
                            else:
                                P.tt("dve", hs, hs, ps[2 + tt_][:, 0:512], ALU.add, [f"ps{2 + tt_}", ("hacc", tt_)], [("hacc", tt_)])
                for tt_ in range(TPG):
                    t = g * TPG + tt_
                    P.dma("sp", Sc["h"][t * 128:(t + 1) * 128, :], hacc[:, tt_, :], r=[("hacc", tt_)])

    def moe_layer(self, l):
        P, S, NT, A, Sc, c = self.P, self.S, self.NT, self.A, self.Sc, self.c
        ps, pt = self.ps, self.pt
        FC = FFN // 128
        CAP = 128 * int(math.ceil(1.5 * S / 4 / 128))
        NJ = CAP // 128
        NP = int(math.ceil(CAP / 512))
        PW = CAP // NP
        NQ = 7
        FQ = FC // NQ
        with P.phase() as stg:
            gates = P.sbuf("mo_gates", [128, NT, NEXP], F32, stg)
            posm = P.sbuf("mo_posm", [128, NT, NEXP], F32, stg)
            iota = P.sbuf("mo_iota", [128, CAP], F32, stg)
            jcol = P.sbuf("mo_jcol", [128, 8], F32, stg)
            P.dma("sp", iota[:], A["c_iota"][:, 0:CAP], w=["iota"])
            P.dma("sp", jcol[:], A["c_jcol"], w=["jcol"])
            with P.phase() as st:
                grow = self.bcast_row(st, "mo_g", A["norm_ffn"][l], D)
                sutri = P.sbuf("mo_sutri", [128, 128], F32, st)
                P.dma("sp", sutri[:], A["c_sutri"], w=["sutri"])
                rt = P.sbuf("mo_rt", [128, KC, NEXP], F32, st)
                P.dma("sp", rt[:], A["moe_router"][0].rearrange("(c p) e -> p c e", p=128), w=["rt"])
                hr = Ring(P, st, "mo_h", 2, [128, D], F32)
                h32r = Ring(P, st, "mo_h32", 2, [128, D], F32)
                h16r = Ring(P, st, "mo_h16", 2, [128, D], BF16)
                junk = P.sbuf("mo_junk", [128, D], BF16, st)
                ssr = Ring(P, st, "mo_ss", 2, [128, 2], F32)
                hT32r = Ring(P, st, "mo_hT32", 2, [128, KC, 128], F32)
                lgr = Ring(P, st, "mo_lg", 2, [128, 32], F32)
                sel = P.sbuf("mo_sel", [128, NT, NEXP], F32, st)
                run = P.sbuf("mo_run", [128, NEXP], F32, st)
                P.memset("dve", run[:], 0.0, ["run"])
                for t in range(NT):
                    ht, hk = hr.next()
                    h32, h32k = h32r.next()
                    h16, h16k = h16r.next()
                    ss, sk = ssr.next()
                    hT32, hT32k = hT32r.next()
                    lg, lgk = lgr.next()
                    P.dma("sp", ht[:], Sc["h"][t * 128:(t + 1) * 128, :], w=[hk])
                    P.act(junk[:], ht[:], AF.Square, [hk], ["mo_junk", sk], accum_out=ss[:, 0:1])
                    P.ts("dve", ss[:, 1:2], ss[:, 0:1], 1.0 / D, 1e-6, ALU.mult, ALU.add, [sk], [sk])
                    P.act(ss[:, 1:2], ss[:, 1:2], AF.Sqrt, [sk], [sk])
                    P.add("dve", lambda e, ss=ss: e.reciprocal(out=ss[:, 1:2], in_=ss[:, 1:2]), [sk], [sk])
                    P.stt("dve", h32[:], ht[:], ss[:, 1:2], grow[:], ALU.mult, ALU.mult, [hk, sk, "mo_g"], [h32k])
                    P.cp("act", h16[:], h32[:], [h32k], [h16k])
                    P.dma("sp", Sc["hn16"][t * 128:(t + 1) * 128, :], h16[:], r=[h16k])
                    for c4 in range(0, KC, 4):
                        pb, pbk = ps[(c4 // 4) % 2], f"ps{(c4 // 4) % 2}"
                        pv = pb[:, 0:512].rearrange("p (a b) -> p a b", b=128)
                        for j in range(4):
                            P.tr(pv[:, j, :], h32[:, (c4 + j) * 128:(c4 + j + 1) * 128], c["identf"][:], [h32k, "const"], [pbk])
                        P.cp("dve" if (c4 // 4) % 2 == 0 else "act", hT32[:, c4:c4 + 4, :], pv[:, 0:4, :], [pbk], [hT32k])
                    for k in range(KC):
                        P.mm(ps[2][:, 0:NEXP], hT32[:, k, :], rt[:, k, :], k == 0, k == KC - 1, [hT32k, "rt"], ["ps2"])
                    P.cp("dve", lg[:, 0:8], ps[2][:, 0:NEXP], ["ps2"], [lgk])
                    P.add("dve", lambda e, lg=lg: e.max(out=lg[:, 8:16], in_=lg[:, 0:8]), [lgk], [lgk])
                    P.ts("dve", lg[:, 16:24], lg[:, 0:8], lg[:, 8:9], None, ALU.subtract, None, [lgk], [lgk])
                    P.act(lg[:, 16:24], lg[:, 16:24], AF.Exp, [lgk], [lgk])
                    P.ts("dve", sel[:, t, :], lg[:, 0:8], lg[:, 9:10], None, ALU.is_ge, None, [lgk], [("sel", t)])
                    P.tt("dve", lg[:, 16:24], lg[:, 16:24], sel[:, t, :], ALU.mult, [lgk, ("sel", t)], [lgk])
                    P.add("dve", lambda e, lg=lg: e.reduce_sum(out=lg[:, 10:11], in_=lg[:, 16:24], axis=AX.X), [lgk], [lgk])
                    P.add("dve", lambda e, lg=lg: e.reciprocal(out=lg[:, 10:11], in_=lg[:, 10:11]), [lgk], [lgk])
                    P.ts("dve", gates[:, t, :], lg[:, 16:24], lg[:, 10:11], None, ALU.mult, None, [lgk], [("gates", t)])
                    P.mm(ps[3][:, 0:NEXP], sutri[:], sel[:, t, :], True, True, ["sutri", ("sel", t)], ["ps3"])
                    P.mm(ps[3][:, 8:8 + NEXP], c["ones"][:], sel[:, t, :], True, True, ["const", ("sel", t)], ["ps3"])
                    P.tt("dve", lg[:, 24:32], ps[3][:, 0:NEXP], run[:], ALU.add, ["ps3", "run"], [lgk])
                    P.stt("dve", posm[:, t, :], lg[:, 24:32], 1.0, sel[:, t, :], ALU.add, ALU.mult, [lgk, ("sel", t)], [("posm", t)])
                    P.ts("dve", posm[:, t, :], posm[:, t, :], -1.0, None, ALU.add, None, [("posm", t)], [("posm", t)])
                    P.tt("dve", run[:], run[:], ps[3][:, 8:8 + NEXP], ALU.add, ["ps3", "run"], ["run"])
            st = stg
            xgo = P.sbuf("mo_xgo", [128, KC * CAP], BF16, st)
            xgT = xgo[:, :].rearrange("p (k j) -> p k j", j=CAP)
            outg16 = xgo[:, :].rearrange("p (j d) -> p j d", d=D)
            actT = P.sbuf("mo_actT", [128, FQ, CAP], BF16, st)
            outg32 = P.sbuf("mo_outg32", [128, NJ, D], F32, st)
            posbc = P.sbuf("mo_posbc", [128, S], F32, st)
            w13r = Ring(P, st, "mo_w13", 2, [128, KC, 2, 256], BF16)
            w2r = Ring(P, st, "mo_w2", 2, [128, 8, 512], BF16)
            hnr = Ring(P, st, "mo_hn", 4, [128, D], BF16)
            xgr = Ring(P, st, "mo_xg", 1, [128, D], BF16)
            htr = Ring(P, st, "mo_ht", 2, [128, D], F32)
            sir = Ring(P, st, "mo_si", 2, [128, PW], F32)
            pmr = Ring(P, st, "mo_pm", 4, [128, 128], BF16)
            pmTr = Ring(P, st, "mo_pmT", 2, [128, NJ, 128], BF16)
            dgr = Ring(P, st, "mo_dg", 2, [128, 128], F32)
            pmall = [("posm", t) for t in range(NT)]
            for e_ in range(NEXP):
                W13, W2 = A["moe_w13"][0, e_], A["moe_w2"][0, e_]
                for t in range(NT):
                    dg, dgk = dgr.next()
                    P.ts("dve", dg[:], c["identf"][:], posm[:, t, e_:e_ + 1], None, ALU.mult, None, ["const", ("posm", t)], [dgk])
                    pb, pbk = ps[4 + t % 2], f"ps{4 + t % 2}"
                    P.mm(pb[:, 0:128], c["ones"][:], dg[:], True, True, ["const", dgk], [pbk])
                    P.cp("act", posbc[:, t * 128:(t + 1) * 128], pb[:, 0:128], [pbk], ["posbc"])
                for J in range(NJ):
                    for t in range(NT):
                        hn, hnk = hnr.next()
                        pm, pmk = pmr.next()
                        P.dma("sp", hn[:], Sc["hn16"][t * 128:(t + 1) * 128, :], w=[hnk])
                        P.ts("dve", pm[:], iota[:, J * 128:(J + 1) * 128], posm[:, t, e_:e_ + 1], None,
                             ALU.is_equal, None, ["iota", ("posm", t)], [pmk])
                        for q in range(4):
                            P.mm(ps[q][:, 0:512], pm[:], hn[:, q * 512:(q + 1) * 512], t == 0, t == NT - 1, [pmk, hnk], [f"ps{q}"])
                    xg, xgk = xgr.next()
                    for q in range(4):
                        P.cp("act" if q % 2 == 0 else "dve", xg[:, q * 512:(q + 1) * 512], ps[q][:, 0:512], [f"ps{q}"], [xgk])
                    for c4 in range(0, KC, 4):
                        pb, pbk = pt[(c4 // 4) % 2], f"pt{(c4 // 4) % 2}"
                        pv = pb[:, 0:512].rearrange("p (a b) -> p a b", b=128)
                        for j in range(4):
                            P.tr(pv[:, j, :], xg[:, (c4 + j) * 128:(c4 + j + 1) * 128], c["ident"][:], [xgk, "const"], [pbk])
                        P.cp("dve" if (c4 // 4) % 2 == 0 else "act", xgT[:, c4:c4 + 4, J * 128:(J + 1) * 128], pv[:, 0:4, :], [pbk], ["xgo"])
                for fq in range(NQ):
                    for fl in range(FQ):
                        f = fq * FQ + fl
                        f2 = fl % 2
                        if f2 == 0:
                            w13, w13k = w13r.next()
                            P.dma("pool", w13[:, :, 0, :], W13[:, f * 128:(f + 2) * 128].rearrange("(c p) n -> p c n", p=128), w=[w13k])
                            P.dma("pool", w13[:, :, 1, :], W13[:, FFN + f * 128:FFN + (f + 2) * 128].rearrange("(c p) n -> p c n", p=128), w=[w13k])
                        ws = slice(f2 * 128, (f2 + 1) * 128)
                        for pc in range(NP):
                            cs = slice(pc * PW, (pc + 1) * PW)
                            b1, b1k = ps[pc], f"ps{pc}"
                            b3, b3k = ps[2 + pc], f"ps{2 + pc}"
                            for k in range(KC):
                                P.mm(b1[:, 0:PW], w13[:, k, 0, ws], xgT[:, k, cs], k == 0, k == KC - 1, [w13k, "xgo"], [b1k])
                            for k in range(KC):
                                P.mm(b3[:, 0:PW], w13[:, k, 1, ws], xgT[:, k, cs], k == 0, k == KC - 1, [w13k, "xgo"], [b3k])
                            si, sik = sir.next()
                            P.act(si[:], b1[:, 0:PW], AF.Silu, [b1k], [sik])
                            P.tt("dve", actT[:, fl, cs], si[:], b3[:, 0:PW], ALU.mult, [sik, b3k], [("actT", fl)])
                    ar = [("actT", fl) for fl in range(FQ)]
                    for dcb in range(4):
                        for f8 in range(0, FQ, 8):
                            nf = min(8, FQ - f8)
                            w2, w2k = w2r.next()
                            r0 = (fq * FQ + f8) * 128
                            P.dma("pool", w2[:, 0:nf, :], W2[r0:r0 + nf * 128, dcb * 512:(dcb + 1) * 512].rearrange("(c p) n -> p c n", p=128), w=[w2k])
                            for f in range(nf):
                                for J in range(NJ):
                                    P.mm(ps[J][:, 0:512], actT[:, f8 + f, J * 128:(J + 1) * 128], w2[:, f, :],
                                         f8 + f == 0, f8 + f == FQ - 1, [w2k] + ar, [f"ps{J}"])
                        for J in range(NJ):
                            dst32 = outg32[:, J, dcb * 512:(dcb + 1) * 512]
                            if fq == 0:
                                P.cp("act" if J % 2 == 0 else "dve", dst32, ps[J][:, 0:512], [f"ps{J}"], [("og32", J, dcb)])
                            elif fq < NQ - 1:
                                P.tt("dve", dst32, dst32, ps[J][:, 0:512], ALU.add, [f"ps{J}", ("og32", J, dcb)], [("og32", J, dcb)])
                            else:
                                P.tt("dve", outg16[:, J, dcb * 512:(dcb + 1) * 512], dst32, ps[J][:, 0:512], ALU.add,
                                     [f"ps{J}", ("og32", J, dcb)], ["xgo"])
                for t in range(NT):
                    ht, hk = htr.next()
                    pmT, pmTk = pmTr.next()
                    P.dma("sp", ht[:], Sc["h"][t * 128:(t + 1) * 128, :], r=[("hD", t)], w=[hk])
                    for J in range(NJ):
                        P.ts("dve", pmT[:, J, :], posbc[:, t * 128:(t + 1) * 128], jcol[:, J:J + 1], None,
                             ALU.is_equal, None, ["posbc", "jcol"], [pmTk])
                    for dcb in range(4):
                        pb, pbk = ps[dcb], f"ps{dcb}"
                        for J in range(NJ):
                            P.mm(pb[:, 0:512], pmT[:, J, :], outg16[:, J, dcb * 512:(dcb + 1) * 512], J == 0, J == NJ - 1, [pmTk, "xgo"], [pbk])
                        hs = ht[:, dcb * 512:(dcb + 1) * 512]
                        P.stt("dve", hs, pb[:, 0:512], gates[:, t, e_:e_ + 1], hs, ALU.mult, ALU.add, [pbk, ("gates", t), hk], [hk])
                    P.dma("sp", Sc["h"][t * 128:(t + 1) * 128, :], ht[:], r=[hk], w=[("hD", t)])


def make_consts(S):
    bf = ml_dtypes.bfloat16
    i = np.arange(128)
    tri = (i[:, None] <= i[None, :]).astype(np.float32)
    inv = 1.0 / (10000.0 ** (np.arange(0, 64, 2, dtype=np.float32) / 64.0))
    ang = np.arange(S, dtype=np.float32)[:, None] * inv[None, :]
    cos, sin = np.cos(ang).astype(np.float32).T, np.sin(ang).astype(np.float32).T
    return dict(
        c_ident=np.eye(128, dtype=np.float32).astype(bf), c_identf=np.eye(128, dtype=np.float32),
        c_tri=tri, c_trib=tri.astype(bf), c_lowb=(1.0 - tri).astype(bf),
        c_mneg=((1.0 - tri) * NEG).astype(np.float32), c_ones=np.ones((128, 128), np.float32),
        c_sutri=(tri - np.eye(128, dtype=np.float32)).astype(np.float32),
        c_iota=np.ascontiguousarray(np.broadcast_to(np.arange(1024, dtype=np.float32)[None, :], (128, 1024))),
        c_jcol=np.ascontiguousarray((np.arange(8, dtype=np.float32)[None, :] * 128 + i[:, None]).astype(np.float32)),
        c_cos2=np.ascontiguousarray(np.concatenate([cos, cos], 0)),
        c_sin2=np.ascontiguousarray(np.concatenate([-sin, sin], 0)),
    )


_CACHE = {}


def kernel(**inputs):
    S = inputs["x"].shape[1]
    B = inputs["x"].shape[0]
    if S not in _CACHE:
        _CACHE[S] = Builder(S).build()
    nc = _CACHE[S]
    consts = make_consts(S)
    shared = {k: np.ascontiguousarray(v) for k, v in inputs.items() if k not in ("x", "mem")}
    in_maps = []
    for b in range(B):
        m = dict(shared)
        m.update(consts)
        m["x"] = np.ascontiguousarray(inputs["x"][b])
        m["mem"] = np.ascontiguousarray(inputs["mem"][b])
        in_maps.append(m)
    res = run_bass_kernel_spmd(nc, in_maps, core_ids=list(range(B)))
    return np.stack([r["out"] for r in res.results], axis=0).astype(np.float32)
```
